# Optimizing a Trainium2 kernel written in Bass

```python
import math
import jax
import jax.numpy as jnp
from jax import lax
import numpy as np

D_MODEL = 1024
BATCH = 4
SEQ = 4096
DEPTH = 1

GRID_W = 64
CTX_LEN = 256
SSD_EXPAND = 2
D_SSD = SSD_EXPAND * D_MODEL
SSD_HEADDIM = 64
SSD_HEADS = D_SSD // SSD_HEADDIM
SSD_GROUPS = 8
HEADS_PER_GROUP = SSD_HEADS // SSD_GROUPS
SSD_STATE = 128
SSD_CHUNK = 128
CONV_K = 5
D_XBC = D_SSD + 2 * SSD_GROUPS * SSD_STATE
NA_HEADDIM = 64
NA_HEADS = D_MODEL // NA_HEADDIM
D_NA = NA_HEADS * NA_HEADDIM
NA_KH = 8
NA_KW = 16
ROPE_THETA = 10000.0
N_GROUPS = 4
EXPERTS_PER_GROUP = 8
N_EXPERTS = N_GROUPS * EXPERTS_PER_GROUP
TOP_K = 2
D_EXPERT = D_MODEL // 2
MOE_BLOCK = 128
N_MOD = 6
EPS = 1e-6
COL_SPLITS = (D_SSD, D_SSD + D_XBC, D_SSD + D_XBC + 2 * SSD_HEADS, D_SSD + D_XBC + 2 * SSD_HEADS + 3 * D_NA)
D_IN = COL_SPLITS[-1] + 2 * D_MODEL

kernel_name = 'hybrid_ssd_natten_hmoe_dit_block'


def rmsnorm(x, w):
    xf = x.astype(jnp.float32)
    y = xf * lax.rsqrt(jnp.mean(xf * xf, axis=-1, keepdims=True) + EPS)
    return (y * w.astype(jnp.float32)).astype(x.dtype)


def dwconv_centred(x, w, b):
    ch = x.shape[-1]
    y = lax.conv_general_dilated(x, w[:, None, :], window_strides=(1,),
                                 padding=((CONV_K // 2, CONV_K // 2),),
                                 dimension_numbers=('NWC', 'WIO', 'NWC'), feature_group_count=ch)
    return y + b


def axial_rope_tables(length):
    t = jnp.arange(length, dtype=jnp.int32)
    row = (t // GRID_W).astype(jnp.float32)
    col = (t % GRID_W).astype(jnp.float32)
    half = SSD_STATE // 2
    inv = ROPE_THETA ** (-jnp.arange(0, half, 2, dtype=jnp.float32) / half)
    ar = row[:, None] * inv
    ac = col[:, None] * inv
    return (jnp.cos(ar), jnp.sin(ar), jnp.cos(ac), jnp.sin(ac))


def _rotate(x, cos, sin):
    x1, x2 = jnp.split(x, 2, axis=-1)
    cos = cos[None, :, None, :].astype(x.dtype)
    sin = sin[None, :, None, :].astype(x.dtype)
    return jnp.concatenate([x1 * cos - x2 * sin, x2 * cos + x1 * sin], axis=-1)


def apply_axial_rope(x, cos_r, sin_r, cos_c, sin_c):
    xr, xc = jnp.split(x, 2, axis=-1)
    return jnp.concatenate([_rotate(xr, cos_r, sin_r), _rotate(xc, cos_c, sin_c)], axis=-1)


def ssd_inputs(xbc, dt_raw, conv_w, conv_b, dt_bias_f, dt_bias_b, rope):
    b, l, _ = xbc.shape
    xbc = jax.nn.silu(dwconv_centred(xbc, conv_w, conv_b))
    xs, bm, cm = jnp.split(xbc, (D_SSD, D_SSD + SSD_GROUPS * SSD_STATE), axis=-1)
    xs = xs.reshape(b, l, SSD_HEADS, SSD_HEADDIM)
    bm = bm.reshape(b, l, SSD_GROUPS, SSD_STATE)
    cm = cm.reshape(b, l, SSD_GROUPS, SSD_STATE)
    if rope is not None:
        bm = apply_axial_rope(bm, *rope)
        cm = apply_axial_rope(cm, *rope)
    dtf = jax.nn.softplus((dt_raw[..., :SSD_HEADS] + dt_bias_f).astype(jnp.float32))
    dtb = jax.nn.softplus((dt_raw[..., SSD_HEADS:] + dt_bias_b).astype(jnp.float32))
    return xs, bm, cm, dtf, dtb


def _ssd_prepare(xs, dt, a, bm):
    b, l, h, p = xs.shape
    nc = l // SSD_CHUNK
    xd = (xs * dt[..., None].astype(xs.dtype)).reshape(b, nc, SSD_CHUNK, SSD_GROUPS, HEADS_PER_GROUP, p)
    a_cum = jnp.cumsum((dt * a).reshape(b, nc, SSD_CHUNK, SSD_GROUPS, HEADS_PER_GROUP), axis=2)
    bc = bm.reshape(b, nc, SSD_CHUNK, SSD_GROUPS, SSD_STATE)
    return xd, a_cum, bc


def _ssd_chunk_states(xd, a_cum, bc, h0):
    nc = xd.shape[1]
    decay_in = jnp.exp(a_cum[:, :, -1:] - a_cum).astype(xd.dtype)
    local = jnp.einsum('bcsgn,bcsgr,bcsgrp->bcgrpn', bc, decay_in, xd)
    states = jnp.concatenate([h0[:, None], local], axis=1)
    chunk_cum = jnp.cumsum(jnp.pad(a_cum[:, :, -1], ((0, 0), (1, 0), (0, 0), (0, 0))), axis=1)
    seg = chunk_cum[:, :, None] - chunk_cum[:, None]
    scan_order = jnp.tril(jnp.ones((nc + 1, nc + 1), dtype=bool))[None, :, :, None, None]
    decay_chunk = jnp.exp(jnp.where(scan_order, seg, -jnp.inf)).astype(xd.dtype)
    return jnp.einsum('bzcgr,bcgrpn->bzgrpn', decay_chunk, states)


def _ssd_outputs(xd, a_cum, bc, cc, start_states):
    b, nc = xd.shape[:2]
    seg = a_cum[:, :, :, None] - a_cum[:, :, None]
    scan_order = jnp.tril(jnp.ones((SSD_CHUNK, SSD_CHUNK), dtype=bool))[None, None, :, :, None, None]
    decay = jnp.exp(jnp.where(scan_order, seg, -jnp.inf)).astype(xd.dtype)
    cb = jnp.einsum('bclgn,bcsgn->bclsg', cc, bc)
    y_diag = jnp.einsum('bclsg,bclsgr,bcsgrp->bclgrp', cb, decay, xd)
    y_off = jnp.einsum('bclgn,bcgrpn,bclgr->bclgrp', cc, start_states, jnp.exp(a_cum).astype(xd.dtype))
    return (y_diag + y_off).reshape(b, nc * SSD_CHUNK, SSD_HEADS, SSD_HEADDIM)


def ssd_scan(xs, dt, a, bm, cm, h0):
    xd, a_cum, bc = _ssd_prepare(xs, dt, a, bm)
    states = _ssd_chunk_states(xd, a_cum, bc, h0)
    cc = cm.reshape(bc.shape)
    return _ssd_outputs(xd, a_cum, bc, cc, states[:, :-1])


def ssd_final_state(xs, dt, a, bm, h0):
    xd, a_cum, bc = _ssd_prepare(xs, dt, a, bm)
    return _ssd_chunk_states(xd, a_cum, bc, h0)[:, -1]


def ssd_bidirectional(xs, bm, cm, dtf, dtb, a_f, a_b, h0f, h0b, d_skip, z, norm_w):
    b, l = xs.shape[:2]
    flip = lambda t: jnp.flip(t, axis=1)
    yf = ssd_scan(xs, dtf, a_f, bm, cm, h0f)
    yb = flip(ssd_scan(flip(xs), flip(dtb), a_b, flip(bm), flip(cm), h0b))
    y = (yf + yb + xs * d_skip[:, None]).reshape(b, l, D_SSD)
    return rmsnorm(y * jax.nn.silu(z), norm_w)


def na_qkv(qkv, q_norm_w, k_norm_w):
    b, l, _ = qkv.shape
    q, k, v = [t.reshape(b, l, NA_HEADS, NA_HEADDIM) for t in jnp.split(qkv, 3, axis=-1)]
    return rmsnorm(q, q_norm_w), rmsnorm(k, k_norm_w), v


def neighbourhood_attention(q, k, v, kc, vc, rpb):
    b, s = q.shape[:2]
    rows = s // GRID_W
    kh = min(NA_KH, rows)
    scale = NA_HEADDIM ** -0.5
    qg = q.reshape(b, rows, GRID_W, NA_HEADS, NA_HEADDIM)
    kg = k.reshape(qg.shape)
    vg = v.reshape(qg.shape)
    col = jnp.arange(GRID_W, dtype=jnp.int32)
    cs = jnp.clip(col - NA_KW // 2, 0, GRID_W - NA_KW)
    col_idx = cs[:, None] + jnp.arange(NA_KW, dtype=jnp.int32)
    dc = col_idx - col[:, None] + (NA_KW - 1)

    def row_block(r):
        rs = jnp.clip(r - kh // 2, 0, rows - kh)
        qb = lax.dynamic_index_in_dim(qg, r, axis=1, keepdims=False)
        kband = lax.dynamic_slice_in_dim(kg, rs, kh, axis=1)
        vband = lax.dynamic_slice_in_dim(vg, rs, kh, axis=1)
        kw = kband[:, :, col_idx]
        vw = vband[:, :, col_idx]
        dr = rs + jnp.arange(kh, dtype=jnp.int32) - r + (NA_KH - 1)
        bias = rpb[:, dr][:, :, dc].transpose(0, 2, 1, 3)
        s_loc = jnp.einsum('bqhd,biqjhd->bhqij', qb, kw) * scale + bias[None]
        s_ctx = jnp.einsum('bqhd,bkhd->bhqk', qb, kc) * scale
        scores = jnp.concatenate([s_loc.reshape(b, NA_HEADS, GRID_W, kh * NA_KW), s_ctx], axis=-1)
        p = jax.nn.softmax(scores.astype(jnp.float32), axis=-1).astype(v.dtype)
        p_loc = p[..., :kh * NA_KW].reshape(b, NA_HEADS, GRID_W, kh, NA_KW)
        p_ctx = p[..., kh * NA_KW:]
        return jnp.einsum('bhqij,biqjhd->bqhd', p_loc, vw) + jnp.einsum('bhqk,bkhd->bqhd', p_ctx, vc)

    out = lax.map(row_block, jnp.arange(rows, dtype=jnp.int32))
    return out.transpose(1, 0, 2, 3, 4).reshape(b, s, D_NA)


def context_attention(qc, kc, vc):
    b, l = qc.shape[:2]
    scores = jnp.einsum('bqhd,bkhd->bhqk', qc, kc) * (NA_HEADDIM ** -0.5)
    p = jax.nn.softmax(scores.astype(jnp.float32), axis=-1).astype(vc.dtype)
    return jnp.einsum('bhqk,bkhd->bqhd', p, vc).reshape(b, l, D_NA)


def merge_branches(y_ssd, y_na, gates, w_br_ssd, w_br_na, w_out):
    g_ssd, g_na = jnp.split(gates, 2, axis=-1)
    merged = jax.nn.sigmoid(g_ssd) * (y_ssd @ w_br_ssd) + jax.nn.sigmoid(g_na) * (y_na @ w_br_na)
    return merged @ w_out


def hierarchical_moe(h, w_grp, b_grp, w_rt, b_rt, w1, w3, w2):
    t, d = h.shape
    grp_logits = (h @ w_grp + b_grp).astype(jnp.float32)
    grp = jnp.argmax(grp_logits, axis=-1)
    p_grp = jnp.take_along_axis(jax.nn.softmax(grp_logits, axis=-1), grp[:, None], axis=1)[:, 0]
    exp_logits = (h @ w_rt + b_rt).astype(jnp.float32).reshape(t, N_GROUPS, EXPERTS_PER_GROUP)
    in_grp = jnp.take_along_axis(exp_logits, grp[:, None, None], axis=1)[:, 0]
    top_v, top_i = lax.top_k(in_grp, TOP_K)
    gate = (jax.nn.softmax(top_v, axis=-1) * p_grp[:, None]).astype(h.dtype)
    expert = grp[:, None] * EXPERTS_PER_GROUP + top_i
    n_assign = t * TOP_K
    a_exp = expert.reshape(-1)
    a_tok = jnp.arange(n_assign, dtype=jnp.int32) // TOP_K
    a_gate = gate.reshape(-1)
    order = jnp.argsort(a_exp)
    se = a_exp[order]
    counts = jnp.bincount(a_exp, length=N_EXPERTS)
    padded = (counts + MOE_BLOCK - 1) // MOE_BLOCK * MOE_BLOCK
    start = jnp.cumsum(counts) - counts
    pend = jnp.cumsum(padded)
    pstart = pend - padded
    dest = pstart[se] + jnp.arange(n_assign, dtype=jnp.int32) - start[se]
    n_blk = (n_assign + N_EXPERTS * (MOE_BLOCK - 1) + MOE_BLOCK - 1) // MOE_BLOCK
    buf_tok = jnp.full((n_blk * MOE_BLOCK,), t, dtype=jnp.int32).at[dest].set(a_tok[order])
    buf_gate = jnp.zeros((n_blk * MOE_BLOCK,), h.dtype).at[dest].set(a_gate[order])
    blk_exp = jnp.minimum(jnp.searchsorted(pend, jnp.arange(n_blk, dtype=jnp.int32) * MOE_BLOCK, side='right'), N_EXPERTS - 1)
    h_pad = jnp.concatenate([h, jnp.zeros((1, d), h.dtype)], axis=0)
    xb = h_pad[buf_tok].reshape(n_blk, MOE_BLOCK, d)

    def expert_block(args):
        xblk, e = args
        return (jax.nn.silu(xblk @ w1[e]) * (xblk @ w3[e])) @ w2[e]

    yb = lax.map(expert_block, (xb, blk_exp)).reshape(-1, d)
    return jax.ops.segment_sum(yb * buf_gate[:, None], buf_tok, num_segments=t + 1)[:t]


def hybrid_layer(x, ctx, c, c_ctx, w_mod, b_mod, norm1_w, w_in, conv_w, conv_b, a_log_f, a_log_b,
                 dt_bias_f, dt_bias_b, d_skip, ssd_norm_w, q_norm_w, k_norm_w, rpb, w_br_ssd, w_br_na,
                 w_out, norm2_w, w_grp, b_grp, w_rt, b_rt, w1, w3, w2, update_ctx):
    b, s, d = x.shape
    mod = jax.nn.silu(c) @ w_mod + b_mod
    sh1, sc1, g1, sh2, sc2, g2 = [m[:, None] for m in jnp.split(mod, N_MOD, axis=-1)]
    modc = jax.nn.silu(c_ctx) @ w_mod + b_mod
    sh1c, sc1c, g1c, sh2c, sc2c, g2c = jnp.split(modc, N_MOD, axis=-1)

    h = rmsnorm(x, norm1_w) * (1.0 + sc1) + sh1
    hc = rmsnorm(ctx, norm1_w) * (1.0 + sc1c) + sh1c
    z, xbc, dtr, qkv, gates = jnp.split(h @ w_in, COL_SPLITS, axis=-1)
    zc, xbcc, dtrc, qkvc, gatesc = jnp.split(hc @ w_in, COL_SPLITS, axis=-1)

    a_f = -jnp.exp(a_log_f.astype(jnp.float32))
    a_b = -jnp.exp(a_log_b.astype(jnp.float32))
    xs, bm, cm, dtf, dtb = ssd_inputs(xbc, dtr, conv_w, conv_b, dt_bias_f, dt_bias_b, axial_rope_tables(s))
    xsc, bmc, cmc, dtfc, dtbc = ssd_inputs(xbcc, dtrc, conv_w, conv_b, dt_bias_f, dt_bias_b, None)
    h0 = jnp.zeros((b, SSD_GROUPS, HEADS_PER_GROUP, SSD_HEADDIM, SSD_STATE), x.dtype)
    flip = lambda t_: jnp.flip(t_, axis=1)
    h_ctx_f = ssd_final_state(xsc, dtfc, a_f, bmc, h0)
    h_ctx_b = ssd_final_state(flip(xsc), flip(dtbc), a_b, flip(bmc), h0)
    y_ssd = ssd_bidirectional(xs, bm, cm, dtf, dtb, a_f, a_b, h_ctx_f, h_ctx_b, d_skip, z, ssd_norm_w)

    q, k, v = na_qkv(qkv, q_norm_w, k_norm_w)
    qc, kc, vc = na_qkv(qkvc, q_norm_w, k_norm_w)
    y_na = neighbourhood_attention(q, k, v, kc, vc, rpb)

    x = x + g1 * merge_branches(y_ssd, y_na, gates, w_br_ssd, w_br_na, w_out)
    if update_ctx:
        yc_ssd = ssd_bidirectional(xsc, bmc, cmc, dtfc, dtbc, a_f, a_b, h0, h0, d_skip, zc, ssd_norm_w)
        yc_na = context_attention(qc, kc, vc)
        ctx = ctx + g1c * merge_branches(yc_ssd, yc_na, gatesc, w_br_ssd, w_br_na, w_out)

    h2 = rmsnorm(x, norm2_w) * (1.0 + sc2) + sh2
    x = x + g2 * hierarchical_moe(h2.reshape(-1, d), w_grp, b_grp, w_rt, b_rt, w1, w3, w2).reshape(x.shape)
    if update_ctx:
        h2c = rmsnorm(ctx, norm2_w) * (1.0 + sc2c) + sh2c
        ctx = ctx + g2c * hierarchical_moe(h2c.reshape(-1, d), w_grp, b_grp, w_rt, b_rt, w1, w3, w2).reshape(ctx.shape)
    return x, ctx


def setup_inputs(seed: int = 0) -> dict:
    key = jax.random.key(seed)
    ks = jax.random.split(key, 32)
    f32 = jnp.float32
    L = DEPTH

    def nrm(k, shape, scale):
        return jax.random.normal(k, shape, f32) * scale

    def gain(k, shape, scale=0.02):
        return 1.0 + scale * jax.random.normal(k, shape, f32)

    dt_f = jnp.exp(jax.random.uniform(ks[9], (L, SSD_HEADS), f32, math.log(1e-3), math.log(1e-1)))
    dt_b = jnp.exp(jax.random.uniform(ks[10], (L, SSD_HEADS), f32, math.log(1e-3), math.log(1e-1)))
    return {
        'x': nrm(ks[0], (BATCH, SEQ, D_MODEL), 1.0),
        'c': nrm(ks[1], (BATCH, D_MODEL), 1.0),
        'ctx': nrm(ks[2], (BATCH, CTX_LEN, D_MODEL), 1.0),
        'c_ctx': nrm(ks[3], (D_MODEL,), 1.0),
        'w_mod': nrm(ks[4], (L, D_MODEL, N_MOD * D_MODEL), 0.5 * D_MODEL ** -0.5),
        'b_mod': nrm(ks[5], (L, N_MOD * D_MODEL), 0.02),
        'norm1_w': gain(ks[6], (L, D_MODEL)),
        'w_in': nrm(ks[7], (L, D_MODEL, D_IN), D_MODEL ** -0.5),
        'conv_w': nrm(ks[8], (L, CONV_K, D_XBC), CONV_K ** -0.5),
        'conv_b': nrm(ks[11], (L, D_XBC), 0.02),
        'a_log_f': jnp.log(jax.random.uniform(ks[12], (L, SSD_HEADS), f32, 1.0, 16.0)),
        'a_log_b': jnp.log(jax.random.uniform(ks[13], (L, SSD_HEADS), f32, 1.0, 16.0)),
        'dt_bias_f': dt_f + jnp.log(-jnp.expm1(-dt_f)),
        'dt_bias_b': dt_b + jnp.log(-jnp.expm1(-dt_b)),
        'd_skip': gain(ks[14], (L, SSD_HEADS), 0.1),
        'ssd_norm_w': gain(ks[15], (L, D_SSD)),
        'q_norm_w': gain(ks[16], (L, NA_HEADDIM)),
        'k_norm_w': gain(ks[17], (L, NA_HEADDIM)),
        'rpb': nrm(ks[18], (L, NA_HEADS, 2 * NA_KH - 1, 2 * NA_KW - 1), 0.1),
        'w_br_ssd': nrm(ks[19], (L, D_SSD, D_MODEL), D_SSD ** -0.5),
        'w_br_na': nrm(ks[20], (L, D_NA, D_MODEL), D_NA ** -0.5),
        'w_out': nrm(ks[21], (L, D_MODEL, D_MODEL), D_MODEL ** -0.5),
        'norm2_w': gain(ks[22], (L, D_MODEL)),
        'w_grp': nrm(ks[23], (L, D_MODEL, N_GROUPS), D_MODEL ** -0.5),
        'b_grp': nrm(ks[24], (L, N_GROUPS), 0.01),
        'w_rt': nrm(ks[25], (L, D_MODEL, N_EXPERTS), D_MODEL ** -0.5),
        'b_rt': nrm(ks[26], (L, N_EXPERTS), 0.01),
        'w1': nrm(ks[27], (L, N_EXPERTS, D_MODEL, D_EXPERT), D_MODEL ** -0.5),
        'w3': nrm(ks[28], (L, N_EXPERTS, D_MODEL, D_EXPERT), D_MODEL ** -0.5),
        'w2': nrm(ks[29], (L, N_EXPERTS, D_EXPERT, D_MODEL), D_EXPERT ** -0.5),
    }


def reference(x, c, ctx, c_ctx, w_mod, b_mod, norm1_w, w_in, conv_w, conv_b, a_log_f, a_log_b,
              dt_bias_f, dt_bias_b, d_skip, ssd_norm_w, q_norm_w, k_norm_w, rpb, w_br_ssd, w_br_na,
              w_out, norm2_w, w_grp, b_grp, w_rt, b_rt, w1, w3, w2):
    for l in range(DEPTH):
        x, ctx = hybrid_layer(x, ctx, c, c_ctx, w_mod[l], b_mod[l], norm1_w[l], w_in[l], conv_w[l], conv_b[l],
                              a_log_f[l], a_log_b[l], dt_bias_f[l], dt_bias_b[l], d_skip[l], ssd_norm_w[l],
                              q_norm_w[l], k_norm_w[l], rpb[l], w_br_ssd[l], w_br_na[l], w_out[l], norm2_w[l],
                              w_grp[l], b_grp[l], w_rt[l], b_rt[l], w1[l], w3[l], w2[l],
                              update_ctx=(l < DEPTH - 1))
    return x
```

```python
import os
import contextlib
import numpy as np
import ml_dtypes
import concourse.bass as bass
import concourse.mybir as mybir
from concourse.bass_utils import run_bass_kernel_spmd

F32 = mybir.dt.float32
BF16 = mybir.dt.bfloat16
AF = mybir.ActivationFunctionType
ALU = mybir.AluOpType
AX = mybir.AxisListType

D = 1024
SEQ = 4096
OWN = 2048
CTX = 256
NTOK = CTX + SEQ
D_SSD = 2048
D_XBC = 4096
D_IN = 11328
C_Z, C_XBC, C_DT, C_QKV, C_G = 0, 2048, 6144, 6208, 9280
EPS = 1e-6
NEXP = 32
DEBUG = os.environ.get("KDEBUG", "")


class _Rec:
    def __getattr__(self, name):
        def f(*a, **kw):
            self.call = (name, a, kw)
            return self
        return f


class Sched:
    def __init__(self, nc, es, n_dma_sems=10, epoch=20000):
        self.nc = nc
        self.es = es
        self.eng = {"pe": nc.tensor, "act": nc.scalar, "dve": nc.vector, "pool": nc.gpsimd, "sp": nc.sync}
        self.ops = []
        self.last_w = {}
        self.readers = {}
        self.n_dma_sems = n_dma_sems
        self.epoch = epoch
        self._semc = 0
        self.bar = set()

    def barrier(self):
        last = {}
        dmas = {}
        for i, op in enumerate(self.ops):
            if op["dma"]:
                dmas.setdefault(op["eng"], []).append(i)
            else:
                last[op["eng"]] = i
        b = set(last.values())
        for q, l in dmas.items():
            b |= set(l[-self.n_dma_sems:])
        self.bar = b

    def _sem(self, name):
        self._semc += 1
        return self.es.enter_context(self.nc.semaphore(f"{name}_{self._semc}"))

    def add(self, eng, fn, r=(), w=(), dma=False):
        idx = len(self.ops)
        raw = set()
        oth = set()
        for k in r:
            lw = self.last_w.get(k)
            if lw is not None:
                raw.add(lw)
        for k in w:
            lw = self.last_w.get(k)
            if lw is not None:
                oth.add(lw)
            for rd in self.readers.get(k, ()):
                oth.add(rd)
        for k in r:
            self.readers.setdefault(k, []).append(idx)
        for k in w:
            self.last_w[k] = idx
            self.readers[k] = []
        raw |= self.bar
        raw.discard(idx)
        oth.discard(idx)
        rec = _Rec()
        fn(rec)
        self.ops.append(dict(eng=eng, call=rec.call, raw=raw, oth=oth - raw, dma=dma, signal=False))
        return idx

    def pe(self, fn, r=(), w=()):
        return self.add("pe", fn, r, w)

    def act(self, fn, r=(), w=()):
        return self.add("act", fn, r, w)

    def dve(self, fn, r=(), w=()):
        return self.add("dve", fn, r, w)

    def pool(self, fn, r=(), w=()):
        return self.add("pool", fn, r, w)

    def dma(self, q, fn, r=(), w=()):
        return self.add(q, fn, r, w, dma=True)

    def emit(self):
        ops = self.ops
        for i, op in enumerate(ops):
            deps = set()
            for p in op["raw"]:
                P = ops[p]
                if (not P["dma"]) and (not op["dma"]) and P["eng"] == op["eng"] and op["eng"] == "pe":
                    continue
                deps.add(p)
            for p in op["oth"]:
                P = ops[p]
                if (not P["dma"]) and (not op["dma"]) and P["eng"] == op["eng"] and op["eng"] == "pe":
                    continue
                deps.add(p)
            op["deps"] = deps
            for p in deps:
                ops[p]["signal"] = True
        dma_count, dma_sems, ring_prev = {}, {}, {}
        for i, op in enumerate(ops):
            if op["dma"]:
                q = op["eng"]
                k = dma_count.get(q, 0)
                dma_count[q] = k + 1
                if q not in dma_sems:
                    dma_sems[q] = [self._sem(f"dma_{q}") for _ in range(self.n_dma_sems)]
                slot = k % self.n_dma_sems
                op["sem"] = dma_sems[q][slot]
                op["val"] = 16 * (k // self.n_dma_sems + 1)
                prev = ring_prev.get((q, slot))
                if prev is not None:
                    op["deps"].add(prev)
                ring_prev[(q, slot)] = i
                op["signal"] = True
        cnt, eng_sems = {}, {}
        for i, op in enumerate(ops):
            if op["dma"] or not op["signal"]:
                continue
            e = op["eng"]
            c = cnt.get(e, 0)
            ep = c // self.epoch
            if (e, ep) not in eng_sems:
                eng_sems[(e, ep)] = self._sem(f"s_{e}{ep}")
            op["sem"] = eng_sems[(e, ep)]
            op["val"] = c % self.epoch + 1
            cnt[e] = c + 1
        waited = {}
        nwaits = 0
        for i, op in enumerate(ops):
            e = op["eng"]
            h = self.eng[e]
            need = {}
            for p in op["deps"]:
                P = ops[p]
                s = P["sem"]
                key = id(s)
                if key not in need or need[key][1] < P["val"]:
                    need[key] = (s, P["val"])
            for key, (s, v) in need.items():
                if waited.get((e, key), 0) >= v:
                    continue
                h.wait_ge(s, v)
                nwaits += 1
                waited[(e, key)] = v
            name, a, kw = op["call"]
            ins = getattr(h, name)(*a, **kw)
            if op["signal"]:
                ins.then_inc(op["sem"], 16 if op["dma"] else 1)
        h = self.eng["sp"]
        last = {}
        for op in ops:
            if op["dma"]:
                last[id(op["sem"])] = (op["sem"], op["val"])
        for s, v in last.values():
            h.wait_ge(s, v)
        counts = {}
        for op in ops:
            counts[op["eng"]] = counts.get(op["eng"], 0) + 1
        print("sched: ops", len(ops), counts, "waits", nwaits, "sems", self._semc, flush=True)


class K:
    def __init__(self):
        self.nc = bass.Bass("TRN2", target_bir_lowering=False)
        self.es = contextlib.ExitStack()
        self.S = Sched(self.nc, self.es)
        self.psum = []
        self.ps_rr = 0

    def din(self, name, shape, dt=F32):
        return self.nc.dram_tensor(name, list(shape), dt, kind="ExternalInput").ap()

    def dout(self, name, shape, dt=F32):
        return self.nc.dram_tensor(name, list(shape), dt, kind="ExternalOutput").ap()

    def dscr(self, name, shape, dt):
        return self.nc.dram_tensor(name, list(shape), dt, kind="Internal").ap()

    def sb(self, name, shape, dt=F32):
        return self.es.enter_context(self.nc.sbuf_tensor(name, list(shape), dt))

    def init_psum(self):
        for i in range(8):
            self.psum.append(self.es.enter_context(self.nc.psum_tensor(f"ps{i}", [128, 512], F32)))

    def ps(self):
        i = self.ps_rr
        self.ps_rr = (self.ps_rr + 1) % 8
        return i


def chunks(n, c):
    return [(i, min(c, n - i)) for i in range(0, n, c)]


def build(debug=""):
    k = K()
    nc, S = k.nc, k.S
    dbg = set(debug.split(",")) if debug else set()

    def scr(name, shape, dt):
        if name in dbg:
            return k.dout(name, shape, dt)
        return k.dscr(name, shape, dt)

    x_ext = k.din("x_ext", [SEQ, D])
    ctx_l = k.din("ctx_l", [CTX, D])
    cvT = k.din("cvT", [D, 2])
    w_mod = k.din("w_mod", [D, 6 * D])
    b_modT = k.din("b_modT", [128, 48])
    b_mod_g = k.din("b_mod_g", [1, 2048])
    n1wT = k.din("n1wT", [128, 8])
    n2wT = k.din("n2wT", [128, 8])
    ident_d = k.din("ident", [128, 128])
    y_out = k.dout("y", [OWN, D])
    hT_d = scr("hT_d", [128, 8, NTOK], BF16)

    k.init_psum()
    PS = k.psum

    def psb(i):
        return PS[i].bitcast(BF16)

    ident_f = k.sb("ident_f", [128, 128], F32)
    ident_b = k.sb("ident_b", [128, 128], BF16)
    S.dma("sp", lambda e: e.dma_start(out=ident_f[:], in_=ident_d[:, :]), w=["ident_f"])
    S.dma("pool", lambda e: e.dma_start(out=ident_b[:], in_=ident_d[:, :]), w=["ident_b"])

    ARENA_BYTES = 155 * 1024
    arena = k.sb("arena", [128, ARENA_BYTES // 2], BF16)

    class AR:
        off = 0

    def aalloc(shape, dt):
        n = int(np.prod(shape))
        nb = n * (4 if dt == F32 else 2)
        nb_al = (nb + 63) // 64 * 64
        assert AR.off + nb_al <= ARENA_BYTES, ("arena overflow", AR.off, nb_al)
        v = arena[:, AR.off // 2:(AR.off + nb) // 2]
        if dt == F32:
            v = v.bitcast(F32)
        AR.off += nb_al
        if len(shape) == 2:
            v = v.rearrange("p (a b) -> p a b", a=shape[0])
        elif len(shape) == 3:
            v = v.rearrange("p (a b c) -> p a b c", a=shape[0], b=shape[1])
        elif len(shape) == 4:
            v = v.rearrange("p (a b c d) -> p a b c d", a=shape[0], b=shape[1], c=shape[2])
        return v

    cv = k.sb("cv", [128, 8, 2], F32)
    siluc = k.sb("siluc", [128, 8, 2], F32)
    bmodT = k.sb("bmodT", [128, 48], F32)
    n1w = k.sb("n1w", [128, 8], F32)
    n2w = k.sb("n2w", [128, 8], F32)
    modT2 = k.sb("modT2", [128, 48, 2], F32)
    sel0 = k.sb("sel0", [2, 128], F32)
    A1 = k.sb("A1", [128, 8, 2], F32)
    A2 = k.sb("A2", [128, 8], F32)
    wst = [k.sb("wst0", [128, 8, 512], F32)]

    S.dve(lambda e: e.memset(modT2[:], 0.0), w=["modT2"])
    S.dma("sp", lambda e: e.dma_start(out=cv[:], in_=cvT.rearrange("(c p) r -> p c r", p=128)), w=["cv"])
    S.dma("sp", lambda e: e.dma_start(out=bmodT[:], in_=b_modT[:, :]), w=["bmodT"])
    S.dma("sp", lambda e: e.dma_start(out=n1w[:], in_=n1wT[:, :]), w=["n1w"])
    S.dma("sp", lambda e: e.dma_start(out=n2w[:], in_=n2wT[:, :]), w=["n2w"])
    S.act(lambda e: e.activation(out=siluc[:], in_=cv[:], func=AF.Silu), r=["cv"], w=["siluc"])
    epsb = k.sb("epsb", [128, 1], F32)
    S.dve(lambda e: e.memset(epsb[:], EPS), w=["epsb"])
    onesc = k.sb("onesc", [128, 1], F32)
    S.dve(lambda e: e.memset(onesc[:], -1.0), w=["onesc"])
    S.dve(lambda e: e.memset(sel0[:], 0.0), w=["sel0"])
    S.dve(lambda e: e.memset(sel0[0:1, :], 1.0), r=["sel0"], w=["sel0"])

    ROWBLK = {4: (0, 0), 5: (0, 512), 10: (1, 0), 11: (1, 512)}
    xin = [k.sb(f"xin{i}", [128, D], F32) for i in range(3)]
    gb_d = k.dscr("gb_d", [2, 2048], F32)
    wst1 = arena[:, 0:8192].bitcast(F32).rearrange("p (a b) -> p a b", a=8)
    grow = [arena[:, 8192 + i_ * 2048:8192 + (i_ + 1) * 2048].bitcast(F32) for i_ in range(2)]
    gbias = arena[:, 12288:14336].bitcast(F32)
    wsts = [wst[0], wst1]

    def mod_block(blk):
        wb = wsts[blk % 2]
        wk = f"wstage{blk % 2}"
        S.dma("sp", lambda e: e.dma_start(out=wb[:, :, :], in_=w_mod[:, blk * 512:(blk + 1) * 512].rearrange("(c p) n -> p c n", p=128)), w=[wk])
        if blk in ROWBLK:
            xi, off = ROWBLK[blk]
            pi = k.ps()
            for dc in range(8):
                S.pe(lambda e, dc=dc: e.matmul(PS[pi][0:2, :], lhsT=siluc[:, dc, :], rhs=wb[:, dc, :], start=(dc == 0), stop=(dc == 7)),
                     r=[wk, "siluc"], w=[("ps", pi)])
            S.act(lambda e: e.activation(out=grow[xi][0:2, off:off + 512], in_=PS[pi][0:2, :], func=AF.Copy), r=[("ps", pi)], w=[("grow", xi)])
        else:
            pi = k.ps()
            for jj in range(4):
                for dc in range(8):
                    S.pe(lambda e, dc=dc, jj=jj: e.matmul(PS[pi][:, jj * 2:jj * 2 + 2], lhsT=wb[:, dc, jj * 128:(jj + 1) * 128], rhs=siluc[:, dc, :],
                                                          start=(dc == 0), stop=(dc == 7)), r=[wk, "siluc"], w=[("ps", pi)])
            for jj in range(4):
                ch = blk * 4 + jj
                S.dve(lambda e, jj=jj, ch=ch: e.tensor_scalar(out=modT2[:, ch, :], in0=PS[pi][:, jj * 2:jj * 2 + 2], scalar1=bmodT[:, ch:ch + 1], scalar2=None,
                                                              op0=ALU.add), r=[("ps", pi), "bmodT"], w=["modT2"])

    def mod_rest():
        for blk in range(4, 12):
            mod_block(blk)
        for xi in range(2):
            S.dma("sp", lambda e, xi=xi: e.dma_start(out=gbias[0:2, :], in_=b_mod_g[0:1, xi * 1024:(xi + 1) * 1024].to_broadcast([2, 1024])), w=["gbias"])
            S.dve(lambda e, xi=xi: e.tensor_tensor(out=grow[xi][0:2, :], in0=grow[xi][0:2, :], in1=gbias[0:2, :], op=ALU.add),
                  r=[("grow", xi), "gbias"], w=[("grow", xi)])
            S.dma("sp", lambda e, xi=xi: e.dma_start(out=gb_d[0:2, xi * 1024:(xi + 1) * 1024], in_=grow[xi][0:2, :]), r=[("grow", xi)], w=[("gb_d", xi)])
        S.dve(lambda e: e.tensor_scalar(out=A2[:], in0=modT2[:, 32:40, 0], scalar1=1.0, scalar2=None, op0=ALU.add), r=["modT2"], w=["A2"])
        S.dve(lambda e: e.tensor_tensor(out=A2[:], in0=A2[:], in1=n2w[:], op=ALU.mult), r=["A2", "n2w"], w=["A2"])

    for blk in range(4):
        mod_block(blk)
    S.dve(lambda e: e.tensor_scalar(out=A1[:], in0=modT2[:, 8:16, :], scalar1=1.0, scalar2=None, op0=ALU.add),
          r=["modT2"], w=["A1"])
    S.dve(lambda e: e.tensor_tensor(out=A1[:], in0=A1[:], in1=n1w[:].unsqueeze(2).to_broadcast([128, 8, 2]), op=ALU.mult),
          r=["A1", "n1w"], w=["A1"])

    junk = k.sb("junk", [128, D], BF16)
    xn = [k.sb(f"xn{i}", [128, D], BF16) for i in range(2)]
    xn3 = [xn[0], xn[1], arena[:, 14336:15360]]
    ssq = k.sb("ssq", [128, 34], F32)
    rstd = k.sb("rstd", [128, 34], F32)
    hTt = [k.sb(f"hTt{i}", [128, 8, 512], BF16) for i in range(2)]
    groups = [(0, 2)] + [(2 + 4 * i, 4) for i in range(8)]
    P1L = float(os.environ.get("P1L", "9"))
    P1E = os.environ.get("P1E", "act")
    groups = groups[:int(os.environ.get("P1G", "99"))]
    tiles_ = []
    for gi, (t0, nt) in enumerate(groups):
        for tt in range(nt):
            tiles_.append((gi, t0, nt, tt))
    p1ps = {}

    def p1_stage1(n):
        gi, t0, nt, tt = tiles_[n]
        t = t0 + tt
        isctx = t < 2
        src = ctx_l[t * 128:(t + 1) * 128, :] if isctx else x_ext[(t - 2) * 128:(t - 1) * 128, :]
        xb, xk = xin[t % 3], f"xin{t % 3}"
        nb, nk = xn3[t % 3], f"xn{t % 3}"
        S.dma("sp", lambda e: e.dma_start(out=xb[:], in_=src), w=[xk])
        S.act(lambda e: e.activation(out=junk[:], in_=xb[:], func=AF.Square, accum_out=ssq[:, t:t + 1]), r=[xk], w=["junk", ("ssq", t)])
        S.act(lambda e: e.activation(out=rstd[:, t:t + 1], in_=ssq[:, t:t + 1], func=AF.Sqrt, scale=1.0 / D, bias=epsb[:, 0:1]),
              r=[("ssq", t), "epsb"], w=[("rstd", t)])
        S.pool(lambda e: e.tensor_tensor(out=rstd[:, t:t + 1], in0=rstd[:, t:t + 1], in1=onesc[:, 0:1], op=ALU.pow),
               r=[("rstd", t), "onesc"], w=[("rstd", t)])
        S.dve(lambda e: e.tensor_scalar(out=nb[:], in0=xb[:], scalar1=rstd[:, t:t + 1], scalar2=None, op0=ALU.mult), r=[xk, ("rstd", t)], w=[nk])
        pi = k.ps()
        p1ps[n] = pi
        for dc in range(8):
            S.pe(lambda e, dc=dc: e.transpose(out=psb(pi)[:, dc * 128:(dc + 1) * 128], in_=nb[:, dc * 128:(dc + 1) * 128], identity=ident_b[:]),
                 r=[nk, "ident_b"], w=[("ps", pi)])

    def p1_stage2(n):
        gi, t0, nt, tt = tiles_[n]
        t = t0 + tt
        ci = 1 if t < 2 else 0
        hb_ = hTt[gi % 2]
        hk = f"hTt{gi % 2}"
        pi = p1ps[n]
        for dc in range(8):
            S.act(lambda e, dc=dc: e.activation(out=hb_[:, dc, tt * 128:(tt + 1) * 128], in_=psb(pi)[:, dc * 128:(dc + 1) * 128], func=AF.Identity,
                                                scale=A1[:, dc, ci:ci + 1], bias=modT2[:, dc, ci:ci + 1]),
                  r=[("ps", pi), "A1", "modT2"], w=[(hk, tt, dc)])
        if tt == nt - 1:
            col0 = t0 * 128
            ncol = nt * 128
            allk = [(hk, tt_, dc) for tt_ in range(nt) for dc in range(8)]
            S.dma("sp", lambda e: e.dma_start(out=hT_d[:, :, col0:col0 + ncol], in_=hb_[:, :, 0:ncol]), r=allk, w=[("hT_d", gi)])

    P1LEAD = 2
    for n in range(len(tiles_) + P1LEAD):
        if n < len(tiles_):
            p1_stage1(n)
        if n >= P1LEAD:
            p1_stage2(n - P1LEAD)

    mod_rest()
    S.barrier()
    w_in = k.din("w_in", [D, D_IN])
    maskU_d = k.din("maskU", [128, 128])
    maskL_d = k.din("maskL", [128, 128])
    permT_d = k.din("permT", [128, 128])
    selc_d = k.din("selc", [128, 4096])
    cosT_d = k.din("cosT", [128, SEQ])
    sinT_d = k.din("sinT", [128, SEQ])
    dtb12_d = k.din("dtb12", [1, 64])
    alog12_d = k.din("alog12", [1, 64])
    dskip_d = k.din("dskip", [1, 32])
    convwT_d = k.din("conv_wT", [128, 32, 5])
    convbT_d = k.din("conv_bT", [128, 32])
    ssdnwT_d = k.din("ssd_nwT", [128, 16])
    ygT_d = scr("ygT_d", [128, 16, OWN], BF16)
    ssq2_d = scr("ssq2_d", [128, 16], F32) if "ssq2_d" in dbg else None

    hb = hTt
    blocks = [(0, 256)] + [(256 + 512 * i, 512) for i in range(8)]

    def load_hb(i, col0, ncol):
        S.dma("sp", lambda e: e.dma_start(out=hb[i][:, :, 0:ncol], in_=hT_d[:, :, col0:col0 + ncol]), w=[("hb", i)])

    rstd_ssd = aalloc([16], F32)
    mhalf = aalloc([1], F32)
    pmark = AR.off
    maskU = aalloc([128], F32)
    maskL = aalloc([128], F32)
    ones_f = aalloc([128], F32)
    permT = aalloc([128], BF16)
    selb = aalloc([32, 128], BF16)
    cosT = aalloc([SEQ], BF16)
    sinT = aalloc([SEQ], BF16)
    dtb_b = aalloc([64], F32)
    a_b = aalloc([64], F32)
    dsk_b = aalloc([32], F32)
    convw = aalloc([32, 5], F32)
    convb = aalloc([32], F32)
    ssdnw = aalloc([16], F32)
    one_b = aalloc([1], F32)
    S.dma("sp", lambda e: e.dma_start(out=maskU, in_=maskU_d[:, :]), w=["maskU"])
    S.dma("sp", lambda e: e.dma_start(out=maskL, in_=maskL_d[:, :]), w=["maskL"])
    S.dma("pool", lambda e: e.dma_start(out=permT, in_=permT_d[:, :]), w=["permT"])
    S.dma("pool", lambda e: e.dma_start(out=selb.rearrange("p a b -> p (a b)"), in_=selc_d[:, :]), w=["selb"])
    S.dma("pool", lambda e: e.dma_start(out=cosT, in_=cosT_d[:, :]), w=["cosT"])
    S.dma("pool", lambda e: e.dma_start(out=sinT, in_=sinT_d[:, :]), w=["sinT"])
    S.dma("sp", lambda e: e.dma_start(out=dtb_b, in_=dtb12_d[0:1, :].to_broadcast([128, 64])), w=["dtb_b"])
    S.dma("sp", lambda e: e.dma_start(out=a_b, in_=alog12_d[0:1, :].to_broadcast([128, 64])), w=["a_b"])
    S.dma("sp", lambda e: e.dma_start(out=dsk_b, in_=dskip_d[0:1, :].to_broadcast([128, 32])), w=["dsk_b"])
    S.dma("sp", lambda e: e.dma_start(out=convw, in_=convwT_d[:, :, :]), w=["convw"])
    S.dma("sp", lambda e: e.dma_start(out=convb, in_=convbT_d[:, :]), w=["convb"])
    S.dma("sp", lambda e: e.dma_start(out=ssdnw, in_=ssdnwT_d[:, :]), w=["ssdnw"])
    S.dve(lambda e: e.memset(ones_f, 1.0), w=["ones_f"])
    S.dve(lambda e: e.memset(one_b, 1.0), w=["one_b"])
    S.act(lambda e: e.activation(out=a_b, in_=a_b, func=AF.Exp), r=["a_b"], w=["a_b"])
    S.dve(lambda e: e.tensor_scalar(out=a_b, in0=a_b, scalar1=-1.0, scalar2=None, op0=ALU.mult), r=["a_b"], w=["a_b"])

    NT = 34

    def TK(name, lo=0, hi=NT):
        return [(name, t) for t in range(lo, hi)]

    wS = aalloc([NT, 64], F32)
    eat = aalloc([NT, 64], F32)
    acO = aalloc([16, 64], F32)
    bYo = aalloc([16, 64], F32)
    ea = aalloc([16, 64], F32)
    acT = aalloc([16, 128], BF16)
    p2mark = AR.off
    biasY = aalloc([NT, 64], F32)
    acat = aalloc([NT, 128], F32)
    dta = aalloc([NT, 64], F32)
    achl = aalloc([NT, 2, 2, 32], BF16)
    wdt = aalloc([8, 64], BF16)
    S.dma("pool", lambda e: e.dma_start(out=wdt, in_=w_in[:, C_DT:C_DT + 64].rearrange("(c p) n -> p c n", p=128)), w=["wdt"])
    for bi, (col0, ncol) in enumerate(blocks):
        load_hb(bi % 2, col0, ncol)
        for tt in range(ncol // 128):
            t = col0 // 128 + tt
            pi = k.ps()
            for dc in range(8):
                S.pe(lambda e, pi=pi, dc=dc, tt=tt, bi=bi: e.matmul(PS[pi][:, 0:64], lhsT=hb[bi % 2][:, dc, tt * 128:(tt + 1) * 128],
                                                                   rhs=wdt[:, dc, :], start=(dc == 0), stop=(dc == 7)),
                     r=[("hb", bi % 2), "wdt"], w=[("ps", pi)])
            S.dve(lambda e, pi=pi, t=t: e.tensor_tensor(out=wS[:, t, :], in0=PS[pi][:, 0:64], in1=dtb_b, op=ALU.add),
                  r=[("ps", pi), "dtb_b"], w=[("wS", t)])
    S.act(lambda e: e.activation(out=wS, in_=wS, func=AF.Exp), r=TK("wS"), w=TK("wS"))
    S.act(lambda e: e.activation(out=wS, in_=wS, func=AF.Ln, bias=one_b[:, 0:1]), r=TK("wS") + ["one_b"], w=TK("wS"))
    S.act(lambda e: e.activation(out=biasY, in_=wS, func=AF.Ln), r=TK("wS"), w=TK("biasY"))
    S.dve(lambda e: e.tensor_tensor(out=dta, in0=wS, in1=a_b.unsqueeze(1).to_broadcast([128, NT, 64]), op=ALU.mult),
          r=TK("wS") + ["a_b"], w=TK("dta"))
    for t in range(NT):
        pi = k.ps()
        S.pe(lambda e, pi=pi, t=t: e.matmul(PS[pi][:, 0:32], lhsT=maskU, rhs=dta[:, t, 0:32], start=True, stop=True),
             r=[("dta", t), "maskU"], w=[("ps", pi)])
        S.pe(lambda e, pi=pi, t=t: e.matmul(PS[pi][:, 32:64], lhsT=maskL, rhs=dta[:, t, 32:64], start=True, stop=True),
             r=[("dta", t), "maskL"], w=[("ps", pi)])
        S.pe(lambda e, pi=pi, t=t: e.matmul(PS[pi][:, 64:128], lhsT=ones_f, rhs=dta[:, t, 0:64], start=True, stop=True),
             r=[("dta", t), "ones_f"], w=[("ps", pi)])
        S.act(lambda e, pi=pi, t=t: e.activation(out=acat[:, t, :], in_=PS[pi][:, 0:128], func=AF.Copy),
              r=[("ps", pi)], w=[("acat", t)])
    acv = acat[:, :, 0:64].rearrange("p t (d h) -> p t d h", d=2)
    tmpv = dta.rearrange("p t (d h) -> p t d h", d=2)
    S.dve(lambda e: e.tensor_copy(out=achl[:, :, :, 0, :], in_=acv), r=TK("acat"), w=["achl"])
    S.dve(lambda e: e.tensor_copy(out=tmpv, in_=achl[:, :, :, 0, :]), r=["achl"] + TK("dta"), w=TK("dta"))
    S.dve(lambda e: e.tensor_tensor(out=achl[:, :, :, 1, :], in0=acv, in1=tmpv, op=ALU.subtract),
          r=TK("acat") + TK("dta") + ["achl"], w=["achl"])
    S.dve(lambda e: e.tensor_tensor(out=acv, in0=tmpv, in1=achl[:, :, :, 1, :], op=ALU.add),
          r=TK("dta") + ["achl"], w=TK("acat"))
    S.dve(lambda e: e.tensor_tensor(out=biasY, in0=acat[:, :, 0:64], in1=biasY, op=ALU.subtract),
          r=TK("acat") + TK("biasY"), w=TK("biasY"))
    S.dve(lambda e: e.tensor_tensor(out=wS, in0=acat[:, :, 64:128], in1=biasY, op=ALU.subtract),
          r=TK("acat") + TK("biasY") + TK("wS"), w=TK("wS"))
    S.act(lambda e: e.activation(out=wS, in_=wS, func=AF.Exp), r=TK("wS"), w=TK("wS"))
    S.act(lambda e: e.activation(out=eat, in_=acat[:, :, 64:128], func=AF.Exp), r=TK("acat"), w=["eat"])
    S.act(lambda e: e.activation(out=ea, in_=acat[:, 2:18, 0:64], func=AF.Exp), r=TK("acat"), w=["ea"])
    S.dve(lambda e: e.tensor_copy(out=acO, in_=acat[:, 2:18, 0:64]), r=TK("acat"), w=["acO"])
    S.dve(lambda e: e.tensor_copy(out=bYo, in_=biasY[:, 2:18, :]), r=TK("biasY"), w=["bYo"])
    for c in range(16):
        pi = k.ps()
        S.pe(lambda e, pi=pi, c=c: e.transpose(out=psb(pi)[:, 0:128], in_=achl[:, c + 2].rearrange("p d x h -> p (d x h)"),
                                               identity=ident_b[:]), r=["achl", "ident_b"], w=[("ps", pi)])
        S.act(lambda e, pi=pi, c=c: e.activation(out=acT[:, c, :], in_=psb(pi)[:, 0:128], func=AF.Copy),
              r=[("ps", pi)], w=[("acT", c)])
    if "stageP2" in dbg:
        d1 = k.dout("dbg_wS", [128, NT * 64], F32)
        d2 = k.dout("dbg_acat", [128, NT * 128], F32)
        d5 = k.dout("dbg_eat", [128, NT * 64], F32)
        S.dma("sp", lambda e: e.dma_start(out=d5[:, :], in_=eat.rearrange("p a b -> p (a b)")), r=["eat"])
        d3 = k.dout("dbg_biasY", [128, NT * 64], F32)
        d4 = k.dout("dbg_acT", [128, 16 * 128], BF16)
        S.dma("sp", lambda e: e.dma_start(out=d1[:, :], in_=wS.rearrange("p a b -> p (a b)")), r=TK("wS"))
        S.dma("sp", lambda e: e.dma_start(out=d2[:, :], in_=acat.rearrange("p a b -> p (a b)")), r=TK("acat"))
        S.dma("sp", lambda e: e.dma_start(out=d3[:, :], in_=biasY.rearrange("p a b -> p (a b)")), r=TK("biasY"))
        S.dma("sp", lambda e: e.dma_start(out=d4[:, :], in_=acT.rearrange("p a b -> p (a b)")), r=[("acT", c) for c in range(16)])
        S.emit()
        k.es.close()
        return nc
    S.barrier()
    AR.off = p2mark
    WT = 4360
    preA = aalloc([WT], BF16)
    preB = aalloc([WT], BF16)
    postB = aalloc([WT], BF16)
    postC = aalloc([2824], BF16)
    xs_tok = aalloc([18, 256], BF16)
    B_tok = aalloc([18, 128], BF16)
    y_acc = aalloc([16, 256], F32)
    wgrp = aalloc([8, 768], BF16)
    Sst = [aalloc([256], F32) for _ in range(2)]
    Sbf = [aalloc([256], BF16) for _ in range(2)]
    ssq2 = aalloc([16, 8], F32)
    negbY = aalloc([16, 64], F32)
    xsp = [aalloc([256], BF16) for _ in range(2)]
    Bp = [aalloc([128], BF16) for _ in range(2)]
    ygst = [aalloc([2, 512], BF16) for _ in range(2)]
    p3mark = AR.off
    accA = wst[0][:, :, :].rearrange("p a b -> p (a b)")
    accP = xin[2]
    T1 = [[xin[0][:, 0:512], xin[0][:, 512:1024]], [xin[1][:, 0:512], xin[1][:, 512:1024]]]
    T1K = [["x0a", "x0s1"], ["x1a", "x1s1"]]
    ROPE1 = xin[0][:, 0:512]
    ROPE2 = xin[1][:, 0:512]
    T2 = [aalloc([256], F32) for _ in range(2)]
    T2K = ["t2_0", "t2_1"]
    ZS = aalloc([256], F32)
    YG = aalloc([256], F32)
    DEC = [[xn[0][:, 0:512], aalloc([512], BF16)], [xn[1][:, 0:512], aalloc([512], BF16)]]
    DECK = [["n0a", "dec01"], ["n1a", "dec11"]]
    MTt = [[xn[0][:, 512:1024], aalloc([512], BF16)], [xn[1][:, 512:1024], aalloc([512], BF16)]]
    MTK = [["n0b", "mt01"], ["n1b", "mt11"]]
    XDD = [junk[:, 0:256], junk[:, 256:512]]
    XDDK = ["j0", "j1"]
    CBM = [[junk[:, 512:640], aalloc([128], BF16)], [junk[:, 640:768], aalloc([128], BF16)]]
    CBMK = [["j2", "cbm01"], ["j3", "cbm11"]]
    YGB = junk[:, 768:1024]
    for d_ in range(2):
        XdB = xin[d_][:].bitcast(BF16)
        DEC[d_].append(XdB[:, 1024:1536])
        DECK[d_].append(f"x{d_}s1a")
        MTt[d_].append(XdB[:, 1536:2048])
        MTK[d_].append(f"x{d_}s1b")
        CBM[d_].append(ygst[d_][:, 0, 0:128])
        CBMK[d_].append(("ygst", d_))
    S.pool(lambda e: e.memset(preA, 0.0), w=["preA"] + [("preA", r_) for r_ in range(10)])
    S.pool(lambda e: e.memset(preB, 0.0), w=["preB"] + [("preB", r_) for r_ in range(10)])
    S.dve(lambda e: e.memset(mhalf, -0.5), w=["mhalf"])
    NM = [maskU.bitcast(BF16)[:, 0:128], maskL.bitcast(BF16)[:, 0:128]]
    for d_, (mk_, mname) in enumerate(((maskU, "maskU"), (maskL, "maskL"))):
        S.dve(lambda e, mk_=mk_: e.tensor_scalar(out=T2[0][:, 0:128], in0=mk_, scalar1=-1.0, scalar2=30000.0, op0=ALU.add, op1=ALU.mult),
              r=[mname], w=["t2_0"])
        S.dve(lambda e, d_=d_: e.tensor_copy(out=NM[d_], in_=T2[0][:, 0:128]), r=["t2_0", mname], w=[mname, "negmask"])
    S.dve(lambda e: e.memset(ssq2, 0.0), w=[("ssq2", c, g) for c in range(16) for g in range(8)])
    S.dve(lambda e: e.tensor_scalar(out=negbY, in0=bYo, scalar1=-1.0, scalar2=None, op0=ALU.mult), r=["bYo"], w=["negbY"])

    def bcol(t):
        return 2 + t * 128 if t < 2 else 262 + (t - 2) * 128

    def RK(name):
        return [name] + [(name, r_) for r_ in range(10)]

    W0B = wst[0][:].bitcast(BF16).rearrange("p a b -> p (a b)")
    preX = W0B[:, 0:4360]
    preC = W0B[:, 4368:4368 + 2824]
    X2B = xin[2][:].bitcast(BF16)
    DGS = [W0B[:, 7232:7232 + 640].rearrange("p (a b) -> p a b", a=5)] + \
          [X2B[:, i_ * 640:(i_ + 1) * 640].rearrange("p (a b) -> p a b", a=5) for i_ in range(3)]
    DGN = [0]
    S.pool(lambda e: e.memset(preX, 0.0), w=RK("preX"))
    S.pool(lambda e: e.memset(preC, 0.0), w=RK("preC"))

    def conv_silu(src, srck, dst, dstk, chg, lo, hi):
        dgi = DGN[0] % 4
        DGN[0] += 1
        DG = DGS[dgi]
        dgk = ("diag", dgi)
        for kk in range(5):
            S.dve(lambda e, kk=kk: e.tensor_scalar(out=DG[:, kk, :], in0=ident_b[:], scalar1=convw[:, chg, kk:kk + 1], scalar2=None, op0=ALU.mult),
                  r=["ident_b", "convw"], w=[dgk])
        blks = chunks(hi - lo, 512)
        pis = {}

        def mm(bi_):
            o, m = blks[bi_]
            pi = k.ps()
            pis[bi_] = pi
            rk = [(srck, r_) for r_ in (bi_ - 1, bi_, bi_ + 1) if 0 <= r_ < len(blks)]
            for kk in range(5):
                S.pe(lambda e, pi=pi, kk=kk, o=o, m=m: e.matmul(PS[pi][:, 0:m], lhsT=DG[:, kk, :], rhs=src[:, lo + o - 2 + kk:lo + o - 2 + kk + m],
                                                                start=(kk == 0), stop=(kk == 4)), r=rk + [dgk], w=[("ps", pi)])

        def ev(bi_):
            o, m = blks[bi_]
            pi = pis[bi_]
            S.act(lambda e, pi=pi, o=o, m=m: e.activation(out=dst[:, lo + o:lo + o + m], in_=PS[pi][:, 0:m], func=AF.Silu, bias=convb[:, chg:chg + 1]),
                  r=[("ps", pi), "convb"], w=[(dstk, bi_)])

        mm(0)
        for bi_ in range(1, len(blks)):
            mm(bi_)
            ev(bi_ - 1)
        ev(len(blks) - 1)

    NG = int(os.environ.get("P3G", "8"))

    def load_w_xbc(g_):
        for (c0, n, o) in ((C_XBC + 256 * g_, 256, 0), (C_XBC + 2048 + 128 * g_, 128, 256), (C_XBC + 3072 + 128 * g_, 128, 384)):
            S.dma("pool", lambda e, c0=c0, n=n, o=o: e.dma_start(out=wgrp[:, :, o:o + n],
                                                               in_=w_in[:, c0:c0 + n].rearrange("(c p) n -> p c n", p=128)), w=["wgrp_x"])

    def load_w_z(g_):
        c0 = C_Z + 256 * g_
        S.dma("pool", lambda e: e.dma_start(out=wgrp[:, :, 512:768], in_=w_in[:, c0:c0 + 256].rearrange("(c p) n -> p c n", p=128)), w=["wgrp_z"])

    def gating_tile(gq, ob, tt, bi):
        c = ob * 4 + tt
        st = ygst[ob % 2]
        stk = ("ygst", ob % 2)
        pz = k.ps()
        for dc in range(8):
            S.pe(lambda e, pz=pz, dc=dc: e.matmul(PS[pz][:, 0:256], lhsT=hb[bi % 2][:, dc, tt * 128:(tt + 1) * 128],
                                                  rhs=wgrp[:, dc, 512:768], start=(dc == 0), stop=(dc == 7)),
                 r=[("hb", bi % 2), "wgrp_z"], w=[("ps", pz)])
        sl = c % 2
        zsb, zsk = (ZS, "zs") if sl == 0 else (YG, "yg")
        ygb, ygk = (YGB, "j4") if sl == 0 else (DEC[0][0][:, 0:256], "n0a")
        sqo, sqk = (MTt[0][0][:, 0:256], "n0b") if sl == 0 else (MTt[0][0][:, 256:512], "n0b2")
        S.act(lambda e: e.activation(out=zsb, in_=PS[pz][:, 0:256], func=AF.Silu), r=[("ps", pz)], w=[zsk])
        S.dve(lambda e: e.tensor_tensor(out=ygb, in0=y_acc[:, c, :], in1=zsb, op=ALU.mult), r=[("y_acc", c), zsk], w=[ygk])
        S.act(lambda e: e.activation(out=sqo, in_=ygb, func=AF.Square, accum_out=ssq2[:, c, gq:gq + 1]), r=[ygk], w=[sqk, ("ssq2", c, gq)])
        pt = k.ps()
        for j in range(2):
            S.pe(lambda e, j=j: e.transpose(out=psb(pt)[:, j * 128:(j + 1) * 128], in_=ygb[:, j * 128:(j + 1) * 128], identity=ident_b[:]),
                 r=[ygk, "ident_b"], w=[("ps", pt)])
        for j in range(2):
            S.act(lambda e, j=j: e.activation(out=st[:, j, tt * 128:(tt + 1) * 128], in_=psb(pt)[:, j * 128:(j + 1) * 128],
                                              func=AF.Copy, scale=ssdnw[:, 2 * gq + j:2 * gq + j + 1]),
                  r=[("ps", pt), "ssdnw"], w=[stk])
        if tt == 3:
            S.dma("sp", lambda e: e.dma_start(out=ygT_d[:, 2 * gq:2 * gq + 2, ob * 512:(ob + 1) * 512], in_=st), r=[stk], w=[("ygT_d", gq, ob)])

    load_w_xbc(0)
    for g in range(NG):
        hd0 = 4 * g
        PRE = ((preX, "preX", 256), (preC, "preC", 384), (preA, "preA", 0), (preB, "preB", 128))
        for bi, (col0, ncol) in enumerate(blocks):
            load_hb(bi % 2, col0, ncol)
            for j, (pre, prek, wo) in enumerate(PRE):
                if j == 1 and not (256 <= col0 < 256 + 2560):
                    continue
                pi = k.ps()
                for dc in range(8):
                    S.pe(lambda e, pi=pi, dc=dc, wo=wo, bi=bi, ncol=ncol: e.matmul(
                        PS[pi][:, 0:ncol], lhsT=wgrp[:, dc, wo:wo + 128], rhs=hb[bi % 2][:, dc, 0:ncol], start=(dc == 0), stop=(dc == 7)),
                        r=[("hb", bi % 2), "wgrp_x"], w=[("ps", pi)])
                dcol = col0 + 2 if col0 < 256 else col0 + 6
                if (bi + j) % 2 == 0:
                    S.act(lambda e, pi=pi, pre=pre, dcol=dcol, ncol=ncol: e.activation(out=pre[:, dcol:dcol + ncol], in_=PS[pi][:, 0:ncol], func=AF.Copy),
                          r=[("ps", pi)], w=RK(prek))
                else:
                    S.dve(lambda e, pi=pi, pre=pre, dcol=dcol, ncol=ncol: e.tensor_copy(out=pre[:, dcol:dcol + ncol], in_=PS[pi][:, 0:ncol]),
                          r=[("ps", pi)], w=RK(prek))
            if g > 0 and 1 <= bi <= 4:
                for tt in range(4):
                    gating_tile(g - 1, bi - 1, tt, bi)
        if g + 1 < NG:
            load_w_xbc(g + 1)
        load_w_z(g)
        conv_silu(preX, "preX", postB, "postB", 16 + g, 2, 4358)
        conv_silu(preC, "preC", postC, "postC", 24 + g, 262, 262 + 2050)
        conv_silu(preA, "preA", preA, "preA", 2 * g, 2, 4358)
        conv_silu(preB, "preB", preB, "preB", 2 * g + 1, 2, 4358)
        for (buf, bk, nblk) in ((postB, "postB", 8), (postC, "postC", 4)):
            for rb in range(nblk):
                c0 = 262 + rb * 512
                e0 = rb * 512
                pi = k.ps()
                S.pe(lambda e, pi=pi, buf=buf, c0=c0: e.matmul(PS[pi][:, :], lhsT=permT, rhs=buf[:, c0:c0 + 512], start=True, stop=True),
                     r=RK(bk) + ["permT"], w=[("ps", pi)])
                S.dve(lambda e, pi=pi, e0=e0: e.tensor_tensor(out=ROPE1, in0=PS[pi][:, :], in1=sinT[:, e0:e0 + 512], op=ALU.mult),
                      r=[("ps", pi), "sinT"], w=["x0a"])
                S.pool(lambda e, buf=buf, c0=c0, e0=e0: e.tensor_tensor(out=ROPE2, in0=buf[:, c0:c0 + 512], in1=cosT[:, e0:e0 + 512], op=ALU.mult),
                       r=RK(bk) + ["cosT"], w=["x1a"])
                S.dve(lambda e, buf=buf, c0=c0: e.tensor_tensor(out=buf[:, c0:c0 + 512], in0=ROPE1, in1=ROPE2, op=ALU.add),
                      r=["x0a", "x1a"] + RK(bk), w=RK(bk))
        for t in range(18):
            pi = k.ps()
            S.pe(lambda e, pi=pi, t=t: e.transpose(out=psb(pi)[:, 0:128], in_=postB[:, bcol(t):bcol(t) + 128], identity=ident_b[:]),
                 r=RK("postB") + ["ident_b"], w=[("ps", pi)])
            S.dve(lambda e, pi=pi, t=t: e.tensor_copy(out=B_tok[:, t, :], in_=psb(pi)[:, 0:128]), r=[("ps", pi)], w=[("B_tok", t)])
        for t in range(18):
            pi = k.ps()
            for j, (pre, prek) in enumerate(((preA, "preA"), (preB, "preB"))):
                S.pe(lambda e, pi=pi, j=j, pre=pre, t=t: e.transpose(out=psb(pi)[:, j * 128:(j + 1) * 128], in_=pre[:, bcol(t):bcol(t) + 128],
                                                                    identity=ident_b[:]), r=RK(prek) + ["ident_b"], w=[("ps", pi)])
            S.act(lambda e, pi=pi, t=t: e.activation(out=xs_tok[:, t, :], in_=psb(pi)[:, 0:256], func=AF.Copy),
                  r=[("ps", pi)], w=[("xs_tok", t)])

        for d in range(2):
            S.dve(lambda e, d=d: e.memset(Sst[d], 0.0), w=[("Sst", d)])
            S.dve(lambda e, d=d: e.memset(Sbf[d], 0.0), w=[("Sbf", d)])

        SB2 = [[Sbf[0], xsp[0]], [Sbf[1], xsp[1]]]
        SB2K = [[("Sbf", 0), ("xsp", 0)], [("Sbf", 1), ("xsp", 1)]]

        def state_update(d, xs_ap, xs_k, B_ap, B_k, tg, oslot=0, xslot=None, copy=True, split=None):
            hs = d * 32 + hd0
            xq = d if xslot is None else xslot
            if split != "back":
              S.dve(lambda e: e.tensor_tensor(out=XDD[xq].rearrange("p (h q) -> p h q", h=4), in0=xs_ap.rearrange("p (h q) -> p h q", h=4),
                                             in1=wS[:, tg, hs:hs + 4].unsqueeze(2).to_broadcast([128, 4, 64]), op=ALU.mult),
                   r=[xs_k, ("wS", tg)], w=[XDDK[xq]])
            if split == "front":
                return
            pi = k.ps()
            S.pe(lambda e, pi=pi: e.matmul(PS[pi][:, 0:256], lhsT=B_ap, rhs=XDD[xq], start=True, stop=True),
                 r=[B_k, XDDK[xq]], w=[("ps", pi)])
            S.dve(lambda e: e.tensor_tensor(out=Sst[d].rearrange("p (h q) -> p h q", h=4), in0=Sst[d].rearrange("p (h q) -> p h q", h=4),
                                            in1=eat[:, tg, hs:hs + 4].unsqueeze(2).to_broadcast([128, 4, 64]), op=ALU.mult),
                  r=[("Sst", d), "eat"], w=[("Sst", d)])
            S.dve(lambda e, pi=pi: e.tensor_tensor(out=Sst[d], in0=PS[pi][:, 0:256], in1=Sst[d], op=ALU.add),
                  r=[("ps", pi), ("Sst", d)], w=[("Sst", d)])
            if copy:
                S.act(lambda e: e.activation(out=SB2[d][oslot], in_=Sst[d], func=AF.Copy), r=[("Sst", d)], w=[SB2K[d][oslot]])

        def front(d, c, sl):
            tg = c + 2
            hs = d * 32 + hd0
            cc = bcol(tg)
            pi = k.ps()
            S.pe(lambda e, pi=pi: e.matmul(PS[pi][:, 0:128], lhsT=postB[:, cc:cc + 128], rhs=postC[:, cc:cc + 128], start=True, stop=True),
                 r=RK("postB") + RK("postC"), w=[("ps", pi)])
            S.act(lambda e, pi=pi: e.activation(out=CBM[d][sl], in_=PS[pi][:, 0:128], func=AF.Copy), r=[("ps", pi)], w=[CBMK[d][sl]])
            pb = k.ps()
            S.pe(lambda e, pb=pb: e.matmul(PS[pb][:, :].rearrange("p (h q) -> p h q", h=4), lhsT=ident_b[:],
                                           rhs=NM[d].unsqueeze(1).to_broadcast([128, 4, 128]), start=True, stop=False),
                 r=["ident_b", "negmask"], w=[("ps", pb)])
            for hh in range(4):
                S.pe(lambda e, pb=pb, hh=hh: e.matmul(PS[pb][:, hh * 128:(hh + 1) * 128], lhsT=selb[d * 64:(d + 1) * 64, hd0 + hh, :],
                                                      rhs=acT[d * 64:(d + 1) * 64, c, :], start=False, stop=(hh == 3)),
                     r=["selb", ("acT", c)], w=[("ps", pb)])
            for hh in range(4):
                S.act(lambda e, pb=pb, hh=hh: e.activation(out=DEC[d][sl][:, hh * 128:(hh + 1) * 128], in_=PS[pb][:, hh * 128:(hh + 1) * 128], func=AF.Exp,
                                                           bias=negbY[:, c, hs + hh:hs + hh + 1]), r=[("ps", pb), "negbY"], w=[DECK[d][sl]])
            S.dve(lambda e: e.tensor_tensor(out=MTt[d][sl].rearrange("p (h q) -> p h q", h=4), in0=DEC[d][sl].rearrange("p (h q) -> p h q", h=4),
                                             in1=CBM[d][sl].unsqueeze(1).to_broadcast([128, 4, 128]), op=ALU.mult),
                   r=[DECK[d][sl], CBMK[d][sl]], w=[MTK[d][sl]])

        def back(d, c, sl, islot=0):
            tg = c + 2
            hs = d * 32 + hd0
            cc = bcol(tg)
            py = k.ps()
            for hh in range(4):
                S.pe(lambda e, py=py, hh=hh: e.matmul(PS[py][:, hh * 64:(hh + 1) * 64], lhsT=MTt[d][sl][:, hh * 128:(hh + 1) * 128],
                                                      rhs=xs_tok[:, tg, hh * 64:(hh + 1) * 64], start=True, stop=True),
                     r=[MTK[d][sl], ("xs_tok", tg)], w=[("ps", py)])
            S.pe(lambda e, py=py: e.matmul(PS[py][:, 256:512], lhsT=postC[:, cc:cc + 128], rhs=SB2[d][islot], start=True, stop=True),
                 r=RK("postC") + [SB2K[d][islot]], w=[("ps", py)])
            S.dve(lambda e, py=py: e.tensor_tensor(out=T2[d].rearrange("p (h q) -> p h q", h=4), in0=PS[py][:, 256:512].rearrange("p (h q) -> p h q", h=4),
                                                   in1=ea[:, c, hs:hs + 4].unsqueeze(2).to_broadcast([128, 4, 64]), op=ALU.mult),
                  r=[("ps", py), "ea"], w=[T2K[d]])
            S.pool(lambda e: e.tensor_tensor(out=y_acc[:, c, :], in0=y_acc[:, c, :], in1=T2[d], op=ALU.add),
                   r=[T2K[d], ("y_acc", c)], w=[("y_acc", c)])
            S.dve(lambda e, py=py: e.tensor_tensor(out=y_acc[:, c, :], in0=PS[py][:, 0:256], in1=y_acc[:, c, :], op=ALU.add),
                  r=[("ps", py), ("y_acc", c)], w=[("y_acc", c)])

        for c in range(16):
            S.pool(lambda e, c=c: e.tensor_tensor(out=y_acc[:, c, :].rearrange("p (h q) -> p h q", h=4),
                                                  in0=xs_tok[:, c + 2, :].rearrange("p (h q) -> p h q", h=4),
                                                  in1=dsk_b[:, hd0:hd0 + 4].unsqueeze(2).to_broadcast([128, 4, 64]), op=ALU.mult),
                   r=[("xs_tok", c + 2), "dsk_b"], w=[("y_acc", c)])
        for t in (1, 0):
            state_update(1, xs_tok[:, t, :], ("xs_tok", t), B_tok[:, t, :], ("B_tok", t), t)
        plist = list(range(33, 17, -1))

        def pfront(t):
            q = t % 2
            pi = k.ps()
            for j, (pre, prek) in enumerate(((preA, "preA"), (preB, "preB"))):
                S.pe(lambda e, j=j, pre=pre: e.transpose(out=psb(pi)[:, j * 128:(j + 1) * 128], in_=pre[:, bcol(t):bcol(t) + 128],
                                                         identity=ident_b[:]), r=RK(prek) + ["ident_b"], w=[("ps", pi)])
            S.pe(lambda e: e.transpose(out=psb(pi)[:, 256:384], in_=postB[:, bcol(t):bcol(t) + 128], identity=ident_b[:]),
                 r=RK("postB") + ["ident_b"], w=[("ps", pi)])
            S.act(lambda e: e.activation(out=xsp[q], in_=psb(pi)[:, 0:256], func=AF.Copy), r=[("ps", pi)], w=[("xsp", q)])
            S.act(lambda e: e.activation(out=Bp[q], in_=psb(pi)[:, 256:384], func=AF.Copy), r=[("ps", pi)], w=[("Bp", q)])
            state_update(1, xsp[q], ("xsp", q), Bp[q], ("Bp", q), t, xslot=q, split="front")

        def pback(t, last):
            q = t % 2
            state_update(1, xsp[q], ("xsp", q), Bp[q], ("Bp", q), t, xslot=q, split="back", copy=last)

        pfront(plist[0])
        for n_ in range(len(plist)):
            if n_ + 1 < len(plist):
                pfront(plist[n_ + 1])
            pback(plist[n_], n_ == len(plist) - 1)
        for t in (0, 1):
            state_update(0, xs_tok[:, t, :], ("xs_tok", t), B_tok[:, t, :], ("B_tok", t), t)
        for i in range(18):
            if i < 16:
                front(0, i, i % 3)
                front(1, 15 - i, i % 3)
            if i >= 2:
                c1, c2 = i - 2, 17 - i
                j = i - 2
                if c1 < 15:
                    state_update(0, xs_tok[:, c1 + 2, :], ("xs_tok", c1 + 2), B_tok[:, c1 + 2, :], ("B_tok", c1 + 2), c1 + 2, oslot=(j + 1) % 2)
                if c2 > 0:
                    state_update(1, xs_tok[:, c2 + 2, :], ("xs_tok", c2 + 2), B_tok[:, c2 + 2, :], ("B_tok", c2 + 2), c2 + 2, oslot=(j + 1) % 2)
                back(0, c1, j % 3, islot=j % 2)
                back(1, c2, j % 3, islot=j % 2)

    for ob in range(4 if NG >= 1 else 0):
        bi = ob + 1
        col0, ncol = blocks[bi]
        load_hb(bi % 2, col0, ncol)
        for tt in range(4):
            gating_tile(NG - 1, ob, tt, bi)

    if NG == 8:
        S.dve(lambda e: e.tensor_reduce(out=rstd_ssd, in_=ssq2, axis=AX.X, op=ALU.add), r=[("ssq2", c, g) for c in range(16) for g in range(8)], w=["rstd_ssd"])
        S.dve(lambda e: e.tensor_scalar(out=rstd_ssd, in0=rstd_ssd, scalar1=1.0 / D_SSD, scalar2=EPS, op0=ALU.mult, op1=ALU.add), r=["rstd_ssd"], w=["rstd_ssd"])
        S.pool(lambda e: e.tensor_tensor(out=rstd_ssd, in0=rstd_ssd, in1=mhalf[:, 0:1].to_broadcast([128, 16]), op=ALU.pow), r=["rstd_ssd", "mhalf"], w=["rstd_ssd"])
    if "stageP3" in dbg:
        dssq = k.dout("dbg_ssq2", [128, 128], F32)
        S.dma("sp", lambda e: e.dma_start(out=dssq[:, :], in_=ssq2.rearrange("p a b -> p (a b)")), r=[("ssq2", c, g) for c in range(16) for g in range(NG)])
        S.emit()
        k.es.close()
        return nc
    S.barrier()
    AR.off = pmark
    NKT = 20
    nabias_d = k.din("nabias", [16, 128, 3 * 5 * 128])
    qkw_d = k.din("qkw", [1, 1024])
    ynaT_d = scr("ynaT_d", [128, 8, OWN], BF16)
    wq = aalloc([3, 8, 512], BF16)
    qT = aalloc([4, OWN], BF16)
    kT = aalloc([4, NKT * 128], BF16)
    Vaug = aalloc([NKT, 8, 65], BF16)
    qkw = aalloc([1024], F32)
    Ef = aalloc([15 * 128], F32)
    Eb = aalloc([15 * 128], BF16)
    PTs = [aalloc([7 * 128], BF16) for _ in range(3)]
    NSL = 5
    sqbs = [aalloc([512], F32) for _ in range(NSL)]
    nrms = [aalloc([512], F32) for _ in range(NSL)]
    qtks = [aalloc([512], BF16) for _ in range(NSL)]
    ss8s = [aalloc([8], F32) for _ in range(NSL)]
    denall = aalloc([16, 8], F32)
    ynatok = aalloc([16, 512], BF16)
    ynst = [aalloc([4, 512], BF16) for _ in range(2)]
    S.dma("sp", lambda e: e.dma_start(out=qkw, in_=qkw_d[0:1, :].to_broadcast([128, 1024])), w=["qkw"])
    S.dve(lambda e: e.memset(Vaug, 1.0), w=["Vaug"])

    def ktile_cols(kt):
        return kt * 128 if kt < 2 else 256 + (kt - 2) * 128

    for hp in range(2):
        for j3 in range(3 if hp == 0 else 0):
            c0 = C_QKV + j3 * 1024 + hp * 512
            S.dma("pool", lambda e, j3=j3, c0=c0: e.dma_start(out=wq[:, j3, :, :], in_=w_in[:, c0:c0 + 512].rearrange("(c p) n -> p c n", p=128)), w=["wq"])
        kblocks = [(0, 256)] + [(256 + 512 * i, 512) for i in range(5)]
        items = []
        for bi, (col0, ncol) in enumerate(kblocks):
            nt_ = 2 if bi == 5 else ncol // 128
            for tt in range(nt_):
                kt = col0 // 128 + tt
                own = 2 <= kt < 18
                for j3 in ((0, 1, 2) if own else (1, 2)):
                    items.append((bi, col0, ncol, tt, kt, j3, tt == 0 and j3 == (0 if own else 1)))
        stA = {}

        def stageA(n):
            bi, col0, ncol, tt, kt, j3, first = items[n]
            if first:
                load_hb(bi % 2, col0, ncol)
            sl = n % NSL
            pi = k.ps()
            for dc in range(8):
                S.pe(lambda e, dc=dc: e.matmul(PS[pi][:, :], lhsT=hb[bi % 2][:, dc, tt * 128:(tt + 1) * 128],
                                               rhs=wq[:, j3, dc, :], start=(dc == 0), stop=(dc == 7)),
                     r=[("hb", bi % 2), "wq"], w=[("ps", pi)])
            stA[n] = pi
            if j3 == 2:
                S.act(lambda e: e.activation(out=Vaug[:, kt, :, 0:64], in_=PS[pi][:, :].rearrange("p (h d) -> p h d", h=8), func=AF.Copy),
                      r=[("ps", pi)], w=["Vaug"])
                return
            S.act(lambda e: e.activation(out=sqbs[sl], in_=PS[pi][:, :], func=AF.Square), r=[("ps", pi)], w=[("sqb", sl)])
            S.dve(lambda e: e.tensor_reduce(out=ss8s[sl], in_=sqbs[sl].rearrange("p (h d) -> p h d", h=8), axis=AX.X, op=ALU.add), r=[("sqb", sl)], w=[("ss8", sl)])
            S.dve(lambda e: e.tensor_scalar(out=ss8s[sl], in0=ss8s[sl], scalar1=1.0 / 64, scalar2=EPS, op0=ALU.mult, op1=ALU.add), r=[("ss8", sl)], w=[("ss8", sl)])
            S.pool(lambda e: e.tensor_tensor(out=ss8s[sl], in0=ss8s[sl], in1=mhalf[:, 0:1].to_broadcast([128, 8]), op=ALU.pow), r=[("ss8", sl), "mhalf"], w=[("ss8", sl)])

        def stageB(n):
            bi, col0, ncol, tt, kt, j3, first = items[n]
            if j3 == 2:
                return
            sl = n % NSL
            pi = stA[n]
            S.dve(lambda e: e.tensor_tensor(out=nrms[sl].rearrange("p (h d) -> p h d", h=8), in0=PS[pi][:, :].rearrange("p (h d) -> p h d", h=8),
                                            in1=ss8s[sl].unsqueeze(2).to_broadcast([128, 8, 64]), op=ALU.mult), r=[("ps", pi), ("ss8", sl)], w=[("nrm", sl)])
            S.dve(lambda e: e.tensor_tensor(out=qtks[sl], in0=nrms[sl], in1=qkw[:, j3 * 512:(j3 + 1) * 512], op=ALU.mult), r=[("nrm", sl), "qkw"], w=[("qtk", sl)])
            pt = k.ps()
            for pr in range(4):
                S.pe(lambda e, pr=pr: e.transpose(out=psb(pt)[:, pr * 128:(pr + 1) * 128], in_=qtks[sl][:, pr * 128:(pr + 1) * 128], identity=ident_b[:]),
                     r=[("qtk", sl), "ident_b"], w=[("ps", pt)])
            if j3 == 0:
                c = kt - 2
                S.act(lambda e: e.activation(out=qT[:, :, c * 128:(c + 1) * 128], in_=psb(pt)[:, 0:512].rearrange("p (a b) -> p a b", a=4), func=AF.Copy),
                      r=[("ps", pt)], w=[("qT", c)])
            else:
                S.act(lambda e: e.activation(out=kT[:, :, kt * 128:(kt + 1) * 128], in_=psb(pt)[:, 0:512].rearrange("p (a b) -> p a b", a=4), func=AF.Copy),
                      r=[("ps", pt)], w=[("kT", kt)])

        LEAD = NSL - 1
        for n in range(len(items) + LEAD):
            if n < len(items):
                stageA(n)
            if n >= LEAD:
                stageB(n - LEAD)
        if hp == 0:
            for j3 in range(3):
                c0 = C_QKV + j3 * 1024 + 512
                S.dma("pool", lambda e, j3=j3, c0=c0: e.dma_start(out=wq[:, j3, :, :], in_=w_in[:, c0:c0 + 512].rearrange("(c p) n -> p c n", p=128)), w=["wq"])
        for hl in range(8):
            h = hp * 8 + hl
            pr, po = hl // 2, (hl % 2) * 64
            S.dma("sp", lambda e, h=h: e.dma_start(out=Ef, in_=nabias_d[h, :, :]), w=["Ef"])
            S.act(lambda e: e.activation(out=Eb, in_=Ef, func=AF.Exp), r=["Ef"], w=["Eb"])
            def na_front(i, sl):
                cls = min(i, 2)
                kts = [2 + x for x in ([0, 1, 2, 3, 4] if i < 2 else range(i - 2, i + 3))] + [0, 1]
                pa, pb_ = k.ps(), k.ps()
                for n_, kt in enumerate(kts):
                    dstp = PS[pa][:, n_ * 128:(n_ + 1) * 128] if n_ < 4 else PS[pb_][:, (n_ - 4) * 128:(n_ - 3) * 128]
                    S.pe(lambda e, dstp=dstp, kt=kt, i=i: e.matmul(dstp, lhsT=kT[po:po + 64, pr, kt * 128:(kt + 1) * 128],
                                                                   rhs=qT[po:po + 64, pr, i * 128:(i + 1) * 128], start=True, stop=True),
                         r=[("kT", kt), ("qT", i)], w=[("ps", pa if n_ < 4 else pb_)])
                S.act(lambda e, pa=pa: e.activation(out=PTs[sl][:, 0:512], in_=PS[pa][:, :], func=AF.Exp), r=[("ps", pa)], w=[("PTa", sl)])
                S.act(lambda e, pb_=pb_: e.activation(out=PTs[sl][:, 512:896], in_=PS[pb_][:, 0:384], func=AF.Exp), r=[("ps", pb_)], w=[("PTb", sl)])
                S.dve(lambda e: e.tensor_tensor(out=PTs[sl][:, 0:512], in0=PTs[sl][:, 0:512], in1=Eb[:, cls * 640:cls * 640 + 512], op=ALU.mult),
                      r=[("PTa", sl), "Eb"], w=[("PTa", sl)])
                S.dve(lambda e: e.tensor_tensor(out=PTs[sl][:, 512:640], in0=PTs[sl][:, 512:640], in1=Eb[:, cls * 640 + 512:cls * 640 + 640], op=ALU.mult),
                      r=[("PTb", sl), "Eb"], w=[("PTb", sl)])

            def na_back(i, sl):
                kts = [2 + x for x in ([0, 1, 2, 3, 4] if i < 2 else range(i - 2, i + 3))] + [0, 1]
                po_ = k.ps()
                for n_, kt in enumerate(kts):
                    S.pe(lambda e, po_=po_, n_=n_, kt=kt: e.matmul(PS[po_][:, 0:65], lhsT=PTs[sl][:, n_ * 128:(n_ + 1) * 128],
                                                                 rhs=Vaug[:, kt, hl, :], start=(n_ == 0), stop=(n_ == 6)),
                         r=[("PTa", sl), ("PTb", sl), "Vaug"], w=[("ps", po_)])
                S.dve(lambda e, po_=po_: e.tensor_copy(out=denall[:, i, hl:hl + 1], in_=PS[po_][:, 64:65]), r=[("ps", po_)], w=[("den", i, hl)])
                S.act(lambda e, po_=po_: e.activation(out=ynatok[:, i, hl * 64:(hl + 1) * 64], in_=PS[po_][:, 0:64], func=AF.Copy),
                      r=[("ps", po_)], w=[("ynatok", i, hl)])

            for it in range(18):
                if it < 16:
                    na_front(it, it % 3)
                if it >= 2:
                    na_back(it - 2, (it - 2) % 3)
        allden = [("den", i_, h_) for i_ in range(16) for h_ in range(8)]
        S.pool(lambda e: e.tensor_tensor(out=denall.rearrange("p a b -> p (a b)"), in0=denall.rearrange("p a b -> p (a b)"),
                                         in1=onesc[:, 0:1].to_broadcast([128, 128]), op=ALU.pow), r=allden + ["onesc"], w=["rden"])
        for i in range(16):
            S.dve(lambda e, i=i: e.tensor_tensor(out=ynatok[:, i, :].rearrange("p (h d) -> p h d", h=8), in0=ynatok[:, i, :].rearrange("p (h d) -> p h d", h=8),
                                                 in1=denall[:, i, :].unsqueeze(2).to_broadcast([128, 8, 64]), op=ALU.mult),
                  r=["rden"] + [("ynatok", i, h_) for h_ in range(8)], w=[("ynatok", i)])
        for i in range(16):
            pt = k.ps()
            for pr in range(4):
                S.pe(lambda e, pt=pt, pr=pr, i=i: e.transpose(out=psb(pt)[:, pr * 128:(pr + 1) * 128], in_=ynatok[:, i, pr * 128:(pr + 1) * 128], identity=ident_b[:]),
                     r=[("ynatok", i), "ident_b"], w=[("ps", pt)])
            sq_ = (i // 4) % 2
            st = ynst[sq_]
            S.act(lambda e, pt=pt, st=st, i=i: e.activation(out=st[:, :, (i % 4) * 128:(i % 4 + 1) * 128], in_=psb(pt)[:, 0:512].rearrange("p (a b) -> p a b", a=4), func=AF.Copy),
                  r=[("ps", pt)], w=[("ynst", sq_)])
            if i % 4 == 3:
                S.dma("sp", lambda e, st=st, i=i, hp=hp: e.dma_start(out=ynaT_d[:, hp * 4:hp * 4 + 4, (i // 4) * 512:(i // 4 + 1) * 512], in_=st),
                      r=[("ynst", sq_)], w=[("ynaT_d", hp, i // 4)])
    if "stageP4" in dbg:
        S.emit()
        k.es.close()
        return nc
    S.barrier()
    AR.off = pmark
    wbrs_d = k.din("w_br_ssd", [D_SSD, D])
    wbrn_d = k.din("w_br_na", [D, D])
    wout_d = k.din("w_out", [D, D])
    wrt_d = k.din("w_rt36", [D, 36])
    brt_d = k.din("b_rt36", [1, 36])
    w1_d = k.din("w1", [NEXP, D, 512])
    w3_d = k.din("w3", [NEXP, D, 512])
    w2_d = k.din("w2", [NEXP, 512, D])
    h2T = aalloc([8, OWN], BF16)
    gates = aalloc([16, 32], F32)
    gbb = aalloc([2048], F32)
    wrt = aalloc([8, 36], F32)
    brt = aalloc([36], F32)
    x1 = aalloc([16, D], F32)
    p5mark = AR.off
    AR.off = p5mark - 16 * D * 4
    wbrs = aalloc([16, D], BF16)
    wbrn = aalloc([8, D], BF16)
    wgt = aalloc([8, 2048], BF16)
    ygt = [aalloc([16, 128], BF16) for _ in range(2)]
    ynt_ = [aalloc([8, 128], BF16) for _ in range(2)]
    sg = aalloc([512], F32)
    m1 = aalloc([D], F32)
    mrg = aalloc([D], BF16)
    S.dma("sp", lambda e: e.dma_start(out=gbb, in_=gb_d[0:1, :].to_broadcast([128, 2048])), w=["gbb"])
    S.dma("sp", lambda e: e.dma_start(out=wrt, in_=wrt_d.rearrange("(c p) n -> p c n", p=128)), w=["wrt"])
    S.dma("sp", lambda e: e.dma_start(out=brt, in_=brt_d[0:1, :].to_broadcast([128, 36])), w=["brt"])
    S.dma("pool", lambda e: e.dma_start(out=wbrs, in_=wbrs_d.rearrange("(c p) n -> p c n", p=128)), w=["wbrs"])
    S.dma("pool", lambda e: e.dma_start(out=wbrn, in_=wbrn_d.rearrange("(c p) n -> p c n", p=128)), w=["wbrn"])
    S.dma("pool", lambda e: e.dma_start(out=wgt, in_=w_in[:, C_G:C_G + 2048].rearrange("(c p) n -> p c n", p=128)), w=["wgt"])
    ssq3 = ssq
    for ob in range(4):
        bi = ob + 1
        col0, ncol = blocks[bi]
        load_hb(bi % 2, col0, ncol)
        for tt in range(4):
            c = ob * 4 + tt
            q = c % 2
            S.dma("sp", lambda e, q=q, c=c: e.dma_start(out=ygt[q], in_=ygT_d[:, :, c * 128:(c + 1) * 128]), w=[("ygt", q)])
            S.dma("sp", lambda e, q=q, c=c: e.dma_start(out=ynt_[q], in_=ynaT_d[:, :, c * 128:(c + 1) * 128]), w=[("ynt", q)])
            for nb_ in range(2):
                cs_ = slice(nb_ * 512, (nb_ + 1) * 512)
                pa = k.ps()
                for ch in range(16):
                    S.pe(lambda e, pa=pa, ch=ch, q=q: e.matmul(PS[pa][:, :], lhsT=ygt[q][:, ch, :], rhs=wbrs[:, ch, cs_], start=(ch == 0), stop=(ch == 15)),
                         r=[("ygt", q), "wbrs"], w=[("ps", pa)])
                pg = k.ps()
                for dc in range(8):
                    S.pe(lambda e, pg=pg, dc=dc: e.matmul(PS[pg][:, :], lhsT=hb[bi % 2][:, dc, tt * 128:(tt + 1) * 128], rhs=wgt[:, dc, nb_ * 512:(nb_ + 1) * 512],
                                                          start=(dc == 0), stop=(dc == 7)), r=[("hb", bi % 2), "wgt"], w=[("ps", pg)])
                S.act(lambda e, pg=pg: e.activation(out=sg, in_=PS[pg][:, :], func=AF.Sigmoid), r=[("ps", pg)], w=["sg"])
                S.dve(lambda e, pa=pa, c=c: e.scalar_tensor_tensor(out=m1[:, cs_], in0=PS[pa][:, :], scalar=rstd_ssd[:, c:c + 1], in1=sg, op0=ALU.mult, op1=ALU.mult),
                      r=[("ps", pa), "rstd_ssd", "sg"], w=[("m1", nb_)])
                pn = k.ps()
                for ch in range(8):
                    S.pe(lambda e, pn=pn, ch=ch, q=q: e.matmul(PS[pn][:, :], lhsT=ynt_[q][:, ch, :], rhs=wbrn[:, ch, cs_], start=(ch == 0), stop=(ch == 7)),
                         r=[("ynt", q), "wbrn"], w=[("ps", pn)])
                pg2 = k.ps()
                for dc in range(8):
                    S.pe(lambda e, pg2=pg2, dc=dc: e.matmul(PS[pg2][:, :], lhsT=hb[bi % 2][:, dc, tt * 128:(tt + 1) * 128], rhs=wgt[:, dc, 1024 + nb_ * 512:1024 + (nb_ + 1) * 512],
                                                            start=(dc == 0), stop=(dc == 7)), r=[("hb", bi % 2), "wgt"], w=[("ps", pg2)])
                S.act(lambda e, pg2=pg2: e.activation(out=sg, in_=PS[pg2][:, :], func=AF.Sigmoid), r=[("ps", pg2)], w=["sg"])
                S.dve(lambda e, pn=pn: e.tensor_tensor(out=sg, in0=PS[pn][:, :], in1=sg, op=ALU.mult), r=[("ps", pn), "sg"], w=["sg"])
                S.pool(lambda e: e.tensor_tensor(out=mrg[:, cs_], in0=m1[:, cs_], in1=sg, op=ALU.add), r=[("m1", nb_), "sg"], w=[("mrg", nb_)])
            pt = k.ps()
            for ch in range(8):
                S.pe(lambda e, pt=pt, ch=ch: e.transpose(out=psb(pt)[:, ch * 128:(ch + 1) * 128], in_=mrg[:, ch * 128:(ch + 1) * 128], identity=ident_b[:]),
                     r=[("mrg", 0), ("mrg", 1), "ident_b"], w=[("ps", pt)])
            S.act(lambda e, pt=pt, c=c: e.activation(out=h2T[:, :, c * 128:(c + 1) * 128], in_=psb(pt)[:, :].rearrange("p (a b) -> p a b", a=8), func=AF.Copy),
                  r=[("ps", pt)], w=[("h2T", c)])
    S.barrier()
    AR.off = p5mark
    wout = aalloc([8, D], BF16)
    sg = aalloc([512], F32)
    m1 = aalloc([D], F32)
    h2f = aalloc([8, 128], F32)
    rla = aalloc([16, 36], F32)
    rv = aalloc([7, 16], F32)
    oh4 = aalloc([16, 4], F32)
    ge4 = aalloc([16, 4], F32)
    Em = aalloc([16, 32], F32)
    eq1 = aalloc([16, 32], F32)
    eq2 = aalloc([16, 32], F32)
    p6mark = AR.off
    S.dma("pool", lambda e: e.dma_start(out=wout, in_=wout_d.rearrange("(c p) n -> p c n", p=128)), w=["wout"])
    m1b = aalloc([D], F32)
    xsc2 = aalloc([D], F32)
    M1S = [m1, m1b]
    XLD = [xin[1], xin[2]]
    XSC = [xin[0], xsc2]

    def p5b_stage1(c):
        q = c % 2
        xl, xlk = XLD[q], f"xld{q}"
        mm_, mk_ = M1S[q], f"m1s{q}"
        xs_, xsk = XSC[q], f"xsc{q}"
        S.dma("sp", lambda e: e.dma_start(out=xl[:], in_=x_ext[c * 128:(c + 1) * 128, :]), w=[xlk])
        for nb_ in range(2):
            cs_ = slice(nb_ * 512, (nb_ + 1) * 512)
            po_ = k.ps()
            for ch in range(8):
                S.pe(lambda e, ch=ch: e.matmul(PS[po_][:, :], lhsT=h2T[:, ch, c * 128:(c + 1) * 128], rhs=wout[:, ch, cs_], start=(ch == 0), stop=(ch == 7)),
                     r=[("h2T", c), "wout"], w=[("ps", po_)])
            S.dve(lambda e: e.tensor_tensor(out=mm_[:, cs_], in0=PS[po_][:, :], in1=gbb[:, cs_], op=ALU.mult), r=[("ps", po_), "gbb"], w=[(mk_, nb_)])
            S.pool(lambda e: e.tensor_tensor(out=x1[:, c, cs_], in0=mm_[:, cs_], in1=xl[:, cs_], op=ALU.add), r=[(mk_, nb_), xlk], w=[("x1", c, nb_)])
        S.act(lambda e: e.activation(out=junk[:], in_=x1[:, c, :], func=AF.Square, accum_out=ssq3[:, c:c + 1]), r=[("x1", c, 0), ("x1", c, 1)], w=["junk", ("ssq3", c)])
        S.act(lambda e: e.activation(out=rstd[:, c:c + 1], in_=ssq3[:, c:c + 1], func=AF.Sqrt, scale=1.0 / D, bias=epsb[:, 0:1]), r=[("ssq3", c)], w=[("rstd3", c)])
        S.pool(lambda e: e.tensor_tensor(out=rstd[:, c:c + 1], in0=rstd[:, c:c + 1], in1=onesc[:, 0:1], op=ALU.pow), r=[("rstd3", c)], w=[("rstd3", c)])
        S.dve(lambda e: e.tensor_scalar(out=xs_[:] if q == 0 else xs_, in0=x1[:, c, :], scalar1=rstd[:, c:c + 1], scalar2=None, op0=ALU.mult),
              r=[("x1", c, 0), ("x1", c, 1), ("rstd3", c)], w=[xsk])

    def p5b_stage2(c):
        q = c % 2
        xs_, xsk = XSC[q], f"xsc{q}"
        pf1, pf2 = k.ps(), k.ps()
        for ch in range(8):
            pp = pf1 if ch < 4 else pf2
            S.pe(lambda e, pp=pp, ch=ch: e.transpose(out=PS[pp][:, (ch % 4) * 128:(ch % 4 + 1) * 128], in_=xs_[:, ch * 128:(ch + 1) * 128], identity=ident_f[:]),
                 r=[xsk, "ident_f"], w=[("ps", pp)])
        for ch in range(8):
            pp = pf1 if ch < 4 else pf2
            S.act(lambda e, pp=pp, ch=ch: e.activation(out=h2f[:, ch, :], in_=PS[pp][:, (ch % 4) * 128:(ch % 4 + 1) * 128], func=AF.Identity,
                                                       scale=A2[:, ch:ch + 1], bias=modT2[:, 24 + ch, 0:1]), r=[("ps", pp), "A2", "modT2"], w=[("h2f", ch)])
        S.pool(lambda e: e.tensor_copy(out=h2T[:, :, c * 128:(c + 1) * 128], in_=h2f), r=[("h2f", ch) for ch in range(8)], w=[("h2T", c)])
        pr_ = k.ps()
        for ch in range(8):
            S.pe(lambda e, ch=ch: e.matmul(PS[pr_][:, 0:36], lhsT=h2f[:, ch, :], rhs=wrt[:, ch, :], start=(ch == 0), stop=(ch == 7)),
                 r=[("h2f", ch), "wrt"], w=[("ps", pr_)])
        S.dve(lambda e: e.tensor_tensor(out=rla[:, c, :], in0=PS[pr_][:, 0:36], in1=brt, op=ALU.add), r=[("ps", pr_), "brt"], w=[("rla", c)])

    p5b_stage1(0)
    for c in range(1, 16):
        p5b_stage1(c)
        p5b_stage2(c - 1)
    p5b_stage2(15)
    RLA = [("rla", c) for c in range(16)]
    Gv = rla[:, :, 0:4]
    Ev = rla[:, :, 4:36]
    bc4 = lambda v: v.unsqueeze(2).to_broadcast([128, 16, 4])
    bc32 = lambda v: v.unsqueeze(2).to_broadcast([128, 16, 32])
    S.dve(lambda e: e.tensor_reduce(out=rv[:, 0, :], in_=Gv, axis=AX.X, op=ALU.max), r=RLA, w=["rv0"])
    S.dve(lambda e: e.tensor_tensor(out=oh4, in0=Gv, in1=bc4(rv[:, 0, :]), op=ALU.is_equal), r=RLA + ["rv0"], w=["oh4"])
    S.dve(lambda e: e.tensor_tensor(out=ge4, in0=Gv, in1=bc4(rv[:, 0, :]), op=ALU.subtract), r=RLA + ["rv0"], w=["ge4"])
    S.act(lambda e: e.activation(out=ge4, in_=ge4, func=AF.Exp), r=["ge4"], w=["ge4"])
    S.dve(lambda e: e.tensor_reduce(out=rv[:, 1, :], in_=ge4, axis=AX.X, op=ALU.add), r=["ge4"], w=["rv1"])
    S.dve(lambda e: e.tensor_scalar(out=oh4, in0=oh4, scalar1=-1.0, scalar2=1e30, op0=ALU.add, op1=ALU.mult), r=["oh4"], w=["oh4"])
    S.dve(lambda e: e.tensor_tensor(out=Em.rearrange("p t (g x) -> p t g x", g=4), in0=Ev.rearrange("p t (g x) -> p t g x", g=4),
                                    in1=oh4.unsqueeze(3).to_broadcast([128, 16, 4, 8]), op=ALU.add), r=RLA + ["oh4"], w=["Em"])
    S.dve(lambda e: e.tensor_reduce(out=rv[:, 2, :], in_=Em, axis=AX.X, op=ALU.max), r=["Em"], w=["rv2"])
    S.dve(lambda e: e.tensor_tensor(out=eq1, in0=Em, in1=bc32(rv[:, 2, :]), op=ALU.is_equal), r=["Em", "rv2"], w=["eq1"])
    S.dve(lambda e: e.scalar_tensor_tensor(out=Em, in0=eq1, scalar=-1e30, in1=Em, op0=ALU.mult, op1=ALU.add), r=["eq1", "Em"], w=["Em"])
    S.dve(lambda e: e.tensor_reduce(out=rv[:, 3, :], in_=Em, axis=AX.X, op=ALU.max), r=["Em"], w=["rv3"])
    S.dve(lambda e: e.tensor_tensor(out=eq2, in0=Em, in1=bc32(rv[:, 3, :]), op=ALU.is_equal), r=["Em", "rv3"], w=["eq2"])
    S.dve(lambda e: e.tensor_tensor(out=rv[:, 4, :], in0=rv[:, 3, :], in1=rv[:, 2, :], op=ALU.subtract), r=["rv2", "rv3"], w=["rv4"])
    S.act(lambda e: e.activation(out=rv[:, 4, :], in_=rv[:, 4, :], func=AF.Exp), r=["rv4"], w=["rv4"])
    S.dve(lambda e: e.tensor_scalar(out=rv[:, 5, :], in0=rv[:, 4, :], scalar1=1.0, scalar2=None, op0=ALU.add), r=["rv4"], w=["rv5"])
    S.dve(lambda e: e.tensor_tensor(out=rv[:, 5, :], in0=rv[:, 5, :], in1=rv[:, 1, :], op=ALU.mult), r=["rv5", "rv1"], w=["rv5"])
    S.pool(lambda e: e.tensor_tensor(out=rv[:, 5, :], in0=rv[:, 5, :], in1=onesc[:, 0:1].to_broadcast([128, 16]), op=ALU.pow), r=["rv5", "onesc"], w=["rv5"])
    S.dve(lambda e: e.tensor_tensor(out=rv[:, 6, :], in0=rv[:, 5, :], in1=rv[:, 4, :], op=ALU.mult), r=["rv5", "rv4"], w=["rv6"])
    S.dve(lambda e: e.tensor_tensor(out=eq1, in0=eq1, in1=bc32(rv[:, 5, :]), op=ALU.mult), r=["eq1", "rv5"], w=["eq1"])
    S.dve(lambda e: e.tensor_tensor(out=eq2, in0=eq2, in1=bc32(rv[:, 6, :]), op=ALU.mult), r=["eq2", "rv6"], w=["eq2"])
    S.dve(lambda e: e.tensor_tensor(out=gates, in0=eq1, in1=eq2, op=ALU.add), r=["eq1", "eq2"], w=[("gates", c) for c in range(16)])

    S.barrier()
    AR.off = p5mark
    w13a = aalloc([2, 8, 512], BF16)
    w13 = [w13a, wst[0][:].bitcast(BF16).rearrange("p a b -> p (a b)").rearrange("p (j c n) -> p j c n", j=2, c=8)]
    w2b1 = aalloc([4, D], BF16)
    w2b = [w2b1, hTt[0][:].rearrange("p a b -> p (a b)").rearrange("p (c n) -> p c n", c=4)]
    m1 = aalloc([D], F32)
    aT = aalloc([4, 512], BF16)
    hs1 = aalloc([512], F32)
    NE = int(os.environ.get("NEXPERTS", "32"))
    def load_expert(ex):
        q = ex % 2
        S.dma("pool", lambda e: e.dma_start(out=w13[q][:, 0, :, :], in_=w1_d[ex].rearrange("(c p) n -> p c n", p=128)), w=[("w13", q)])
        S.dma("pool", lambda e: e.dma_start(out=w13[q][:, 1, :, :], in_=w3_d[ex].rearrange("(c p) n -> p c n", p=128)), w=[("w13", q)])
        S.dma("pool", lambda e: e.dma_start(out=w2b[q], in_=w2_d[ex].rearrange("(c p) n -> p c n", p=128)), w=[("w2b", q)])

    load_expert(0)
    for ex in range(NE):
        q = ex % 2
        if ex + 1 < NE:
            load_expert(ex + 1)
        for tb in range(4):
            for fc in range(4):
                p1_, p3_ = k.ps(), k.ps()
                for dc in range(8):
                    S.pe(lambda e, p1_=p1_, dc=dc, q=q: e.matmul(PS[p1_][:, :], lhsT=w13[q][:, 0, dc, fc * 128:(fc + 1) * 128], rhs=h2T[:, dc, tb * 512:(tb + 1) * 512],
                                                                start=(dc == 0), stop=(dc == 7)), r=[("w13", q)] + [("h2T", tb * 4 + u) for u in range(4)], w=[("ps", p1_)])
                for dc in range(8):
                    S.pe(lambda e, p3_=p3_, dc=dc, q=q: e.matmul(PS[p3_][:, :], lhsT=w13[q][:, 1, dc, fc * 128:(fc + 1) * 128], rhs=h2T[:, dc, tb * 512:(tb + 1) * 512],
                                                                start=(dc == 0), stop=(dc == 7)), r=[("w13", q)] + [("h2T", tb * 4 + u) for u in range(4)], w=[("ps", p3_)])
                S.act(lambda e, p1_=p1_: e.activation(out=hs1, in_=PS[p1_][:, :], func=AF.Silu), r=[("ps", p1_)], w=["hs1"])
                S.dve(lambda e, p3_=p3_, fc=fc: e.tensor_tensor(out=aT[:, fc, :], in0=PS[p3_][:, :], in1=hs1, op=ALU.mult), r=[("ps", p3_), "hs1"], w=[("aT", fc)])
            for tt in range(4):
                c = tb * 4 + tt
                for nb_ in range(2):
                    cs_ = slice(nb_ * 512, (nb_ + 1) * 512)
                    po_ = k.ps()
                    for fc in range(4):
                        S.pe(lambda e, po_=po_, fc=fc, q=q: e.matmul(PS[po_][:, :], lhsT=aT[:, fc, tt * 128:(tt + 1) * 128], rhs=w2b[q][:, fc, cs_],
                                                                    start=(fc == 0), stop=(fc == 3)), r=[("aT", fc) for fc in range(4)] + [("w2b", q)], w=[("ps", po_)])
                    S.dve(lambda e, po_=po_, c=c, ex=ex: e.scalar_tensor_tensor(out=m1[:, cs_], in0=PS[po_][:, :], scalar=gates[:, c, ex:ex + 1], in1=gbb[:, 1024 + nb_ * 512:1024 + (nb_ + 1) * 512],
                                                                               op0=ALU.mult, op1=ALU.mult), r=[("ps", po_), ("gates", c), "gbb"], w=[("m1", nb_)])
                    S.pool(lambda e, c=c: e.tensor_tensor(out=x1[:, c, cs_], in0=x1[:, c, cs_], in1=m1[:, cs_], op=ALU.add), r=[("m1", nb_), ("x1", c, nb_)], w=[("x1", c, nb_)])
    for c in range(16):
        S.dma("sp", lambda e, c=c: e.dma_start(out=y_out[c * 128:(c + 1) * 128, :], in_=x1[:, c, :]), r=[("x1", c, 0), ("x1", c, 1)], w=[("y", c)])
    S.emit()
    k.es.close()
    return nc


def _consts():
    c = {}
    f32 = np.float32
    c["ident"] = np.eye(128, dtype=f32)
    kk = np.arange(128)
    c["maskU"] = (kk[:, None] <= kk[None, :]).astype(f32)
    c["maskL"] = (kk[:, None] >= kk[None, :]).astype(f32)
    P = np.zeros((128, 128), f32)
    for n in range(128):
        if (n % 64) < 32:
            P[n, n + 32] = -1.0
        else:
            P[n, n - 32] = 1.0
    c["permT"] = np.ascontiguousarray(P.T)
    sel = np.zeros((128, 32, 128), f32)
    for kq in range(128):
        sel[kq, kq % 32, :] = 1.0
    c["selc"] = sel.reshape(128, 4096)
    return c


def _na_bias(rpb, hf):
    out = np.full((16, 128, 3, 5, 128), -30000.0, np.float32)
    kidx = np.arange(128)
    for cls in range(3):
        i = cls
        kts = [0, 1, 2, 3, 4] if i < 2 else list(range(i - 2, i + 3))
        qr = 2 * i + kidx // 64
        qc = kidx % 64
        gqr, gqc = (qr, qc) if hf == 0 else (63 - qr, 63 - qc)
        rs = np.clip(gqr - 4, 0, 56)
        cs = np.clip(gqc - 8, 0, 48)
        for rel, kt in enumerate(kts):
            kr = 2 * kt + kidx // 64
            kc = kidx % 64
            gkr, gkc = (kr, kc) if hf == 0 else (63 - kr, 63 - kc)
            ok = ((gkr[:, None] >= rs[None, :]) & (gkr[:, None] <= rs[None, :] + 7) &
                  (gkc[:, None] >= cs[None, :]) & (gkc[:, None] <= cs[None, :] + 15))
            dr = np.clip(gkr[:, None] - gqr[None, :] + 7, 0, 14)
            dc = np.clip(gkc[:, None] - gqc[None, :] + 15, 0, 30)
            vals = rpb[:, dr, dc]
            out[:, :, cls, rel, :] = np.where(ok[None], vals, np.float32(-30000.0))
    return np.ascontiguousarray(out.reshape(16, 128, 3 * 5 * 128))


def _rope_tables(hf):
    i = np.arange(SEQ)
    t = i if hf == 0 else (SEQ - 1 - i)
    row = (t // 64).astype(np.float32)
    col = (t % 64).astype(np.float32)
    inv = (10000.0 ** (-np.arange(0, 64, 2, dtype=np.float32) / 64.0)).astype(np.float32)
    ar = row[None, :] * inv[:, None]
    ac = col[None, :] * inv[:, None]
    ang = np.concatenate([ar, ar, ac, ac], axis=0)
    return np.cos(ang).astype(np.float32), np.sin(ang).astype(np.float32)


def prep_core_inputs(inp, b, hf, consts):
    f32 = np.float32
    m = {}
    x = inp["x"][b]
    ctx = inp["ctx"][b]
    if hf == 1:
        x = x[::-1]
        ctx = ctx[::-1]
    m["x_ext"] = np.ascontiguousarray(x, dtype=f32)
    m["ctx_l"] = np.ascontiguousarray(ctx, dtype=f32)
    m["cvT"] = np.ascontiguousarray(np.stack([inp["c"][b], inp["c_ctx"]], axis=1), dtype=f32)
    m["w_mod"] = np.ascontiguousarray(inp["w_mod"][0], dtype=f32)
    bm = inp["b_mod"][0]
    m["b_modT"] = np.ascontiguousarray(bm.reshape(48, 128).T, dtype=f32)
    m["b_mod_g"] = np.ascontiguousarray(np.concatenate([bm[2048:3072], bm[5120:6144]])[None, :], dtype=f32)
    m["n1wT"] = np.ascontiguousarray(inp["norm1_w"][0].reshape(8, 128).T, dtype=f32)
    m["n2wT"] = np.ascontiguousarray(inp["norm2_w"][0].reshape(8, 128).T, dtype=f32)
    L = 0
    w_in = np.array(inp["w_in"][L], dtype=f32)
    if hf == 1:
        w_in[:, C_DT:C_DT + 64] = np.concatenate([w_in[:, C_DT + 32:C_DT + 64], w_in[:, C_DT:C_DT + 32]], axis=1)
    m["w_in"] = w_in
    p1, p2 = ("f", "b") if hf == 0 else ("b", "f")
    m["dtb12"] = np.concatenate([inp["dt_bias_" + p1][L], inp["dt_bias_" + p2][L]])[None, :].astype(f32)
    m["alog12"] = np.concatenate([inp["a_log_" + p1][L], inp["a_log_" + p2][L]])[None, :].astype(f32)
    m["dskip"] = np.ascontiguousarray(inp["d_skip"][L][None, :], dtype=f32)
    cw = inp["conv_w"][L]
    if hf == 1:
        cw = cw[::-1]
    m["conv_wT"] = np.ascontiguousarray(cw.T.reshape(32, 128, 5).transpose(1, 0, 2), dtype=f32)
    m["conv_bT"] = np.ascontiguousarray(inp["conv_b"][L].reshape(32, 128).T, dtype=f32)
    m["ssd_nwT"] = np.ascontiguousarray(inp["ssd_norm_w"][L].reshape(16, 128).T, dtype=f32)
    m["w_br_ssd"] = np.ascontiguousarray(inp["w_br_ssd"][L], dtype=f32)
    m["w_br_na"] = np.ascontiguousarray(inp["w_br_na"][L], dtype=f32)
    m["w_out"] = np.ascontiguousarray(inp["w_out"][L], dtype=f32)
    m["w_rt36"] = np.ascontiguousarray(np.concatenate([inp["w_grp"][L], inp["w_rt"][L]], axis=1), dtype=f32)
    m["b_rt36"] = np.concatenate([inp["b_grp"][L], inp["b_rt"][L]])[None, :].astype(f32)
    m["w1"] = np.ascontiguousarray(inp["w1"][L], dtype=f32)
    m["w3"] = np.ascontiguousarray(inp["w3"][L], dtype=f32)
    m["w2"] = np.ascontiguousarray(inp["w2"][L], dtype=f32)
    m["qkw"] = np.concatenate([np.tile(inp["q_norm_w"][L], 8) * 0.125, np.tile(inp["k_norm_w"][L], 8)])[None, :].astype(f32)
    m["nabias"] = _na_bias(inp["rpb"][L], hf)
    cs, sn = _rope_tables(hf)
    m["cosT"], m["sinT"] = cs, sn
    m.update(consts)
    return m


def kernel(**inputs):
    inp = {k_: np.asarray(v) for k_, v in inputs.items()}
    nc = build("")
    consts = _consts()
    shared = {}
    maps = []
    for c in range(8):
        m = prep_core_inputs(inp, c // 2, c % 2, consts)
        maps.append(m)
    res = run_bass_kernel_spmd(nc, maps, core_ids=list(range(8)))
    out = np.zeros((4, SEQ, D), dtype=np.float32)
    for c in range(8):
        b, hf = c // 2, c % 2
        y = np.asarray(res.results[c]["y"], dtype=np.float32)
        if hf == 0:
            out[b, :OWN] = y
        else:
            out[b, OWN:] = y[::-1]
    return out
```

```python
import os
import contextlib
import numpy as np
import ml_dtypes
import concourse.bass as bass
import concourse.mybir as mybir
from concourse.bass_utils import run_bass_kernel_spmd

F32 = mybir.dt.float32
BF16 = mybir.dt.bfloat16
AF = mybir.ActivationFunctionType
ALU = mybir.AluOpType
AX = mybir.AxisListType

D = 1024
SEQ = 4096
OWN = 2048
CTX = 256
NTOK = CTX + SEQ
D_SSD = 2048
D_XBC = 4096
D_IN = 11328
C_Z, C_XBC, C_DT, C_QKV, C_G = 0, 2048, 6144, 6208, 9280
EPS = 1e-6
NEXP = 32
DEBUG = os.environ.get("KDEBUG", "")


class _Rec:
    def __getattr__(self, name):
        def f(*a, **kw):
            self.call = (name, a, kw)
            return self
        return f


class Sched:
    def __init__(self, nc, es, n_dma_sems=10, epoch=20000):
        self.nc = nc
        self.es = es
        self.eng = {"pe": nc.tensor, "act": nc.scalar, "dve": nc.vector, "pool": nc.gpsimd, "sp": nc.sync}
        self.ops = []
        self.last_w = {}
        self.readers = {}
        self.n_dma_sems = n_dma_sems
        self.epoch = epoch
        self._semc = 0
        self.bar = set()

    def barrier(self):
        last = {}
        dmas = {}
        for i, op in enumerate(self.ops):
            if op["dma"]:
                dmas.setdefault(op["eng"], []).append(i)
            else:
                last[op["eng"]] = i
        b = set(last.values())
        for q, l in dmas.items():
            b |= set(l[-self.n_dma_sems:])
        self.bar = b

    def _sem(self, name):
        self._semc += 1
        return self.es.enter_context(self.nc.semaphore(f"{name}_{self._semc}"))

    def add(self, eng, fn, r=(), w=(), dma=False):
        idx = len(self.ops)
        raw = set()
        oth = set()
        for k in r:
            lw = self.last_w.get(k)
            if lw is not None:
                raw.add(lw)
        for k in w:
            lw = self.last_w.get(k)
            if lw is not None:
                oth.add(lw)
            for rd in self.readers.get(k, ()):
                oth.add(rd)
        for k in r:
            self.readers.setdefault(k, []).append(idx)
        for k in w:
            self.last_w[k] = idx
            self.readers[k] = []
        raw |= self.bar
        raw.discard(idx)
        oth.discard(idx)
        rec = _Rec()
        fn(rec)
        self.ops.append(dict(eng=eng, call=rec.call, raw=raw, oth=oth - raw, dma=dma, signal=False))
        return idx

    def pe(self, fn, r=(), w=()):
        return self.add("pe", fn, r, w)

    def act(self, fn, r=(), w=()):
        return self.add("act", fn, r, w)

    def dve(self, fn, r=(), w=()):
        return self.add("dve", fn, r, w)

    def pool(self, fn, r=(), w=()):
        return self.add("pool", fn, r, w)

    def dma(self, q, fn, r=(), w=()):
        return self.add(q, fn, r, w, dma=True)

    def emit(self):
        ops = self.ops
        for i, op in enumerate(ops):
            deps = set()
            for p in op["raw"]:
                P = ops[p]
                if (not P["dma"]) and (not op["dma"]) and P["eng"] == op["eng"] and op["eng"] == "pe":
                    continue
                deps.add(p)
            for p in op["oth"]:
                P = ops[p]
                if (not P["dma"]) and (not op["dma"]) and P["eng"] == op["eng"] and op["eng"] == "pe":
                    continue
                deps.add(p)
            op["deps"] = deps
            for p in deps:
                ops[p]["signal"] = True
        dma_count, dma_sems, ring_prev = {}, {}, {}
        for i, op in enumerate(ops):
            if op["dma"]:
                q = op["eng"]
                k = dma_count.get(q, 0)
                dma_count[q] = k + 1
                if q not in dma_sems:
                    dma_sems[q] = [self._sem(f"dma_{q}") for _ in range(self.n_dma_sems)]
                slot = k % self.n_dma_sems
                op["sem"] = dma_sems[q][slot]
                op["val"] = 16 * (k // self.n_dma_sems + 1)
                prev = ring_prev.get((q, slot))
                if prev is not None:
                    op["deps"].add(prev)
                ring_prev[(q, slot)] = i
                op["signal"] = True
        cnt, eng_sems = {}, {}
        for i, op in enumerate(ops):
            if op["dma"] or not op["signal"]:
                continue
            e = op["eng"]
            c = cnt.get(e, 0)
            ep = c // self.epoch
            if (e, ep) not in eng_sems:
                eng_sems[(e, ep)] = self._sem(f"s_{e}{ep}")
            op["sem"] = eng_sems[(e, ep)]
            op["val"] = c % self.epoch + 1
            cnt[e] = c + 1
        waited = {}
        nwaits = 0
        for i, op in enumerate(ops):
            e = op["eng"]
            h = self.eng[e]
            need = {}
            for p in op["deps"]:
                P = ops[p]
                s = P["sem"]
                key = id(s)
                if key not in need or need[key][1] < P["val"]:
                    need[key] = (s, P["val"])
            for key, (s, v) in need.items():
                if waited.get((e, key), 0) >= v:
                    continue
                h.wait_ge(s, v)
                nwaits += 1
                waited[(e, key)] = v
            name, a, kw = op["call"]
            ins = getattr(h, name)(*a, **kw)
            if op["signal"]:
                ins.then_inc(op["sem"], 16 if op["dma"] else 1)
        h = self.eng["sp"]
        last = {}
        for op in ops:
            if op["dma"]:
                last[id(op["sem"])] = (op["sem"], op["val"])
        for s, v in last.values():
            h.wait_ge(s, v)
        counts = {}
        for op in ops:
            counts[op["eng"]] = counts.get(op["eng"], 0) + 1
        print("sched: ops", len(ops), counts, "waits", nwaits, "sems", self._semc, flush=True)


class K:
    def __init__(self):
        self.nc = bass.Bass("TRN2", target_bir_lowering=False)
        self.es = contextlib.ExitStack()
        self.S = Sched(self.nc, self.es)
        self.psum = []
        self.ps_rr = 0

    def din(self, name, shape, dt=F32):
        return self.nc.dram_tensor(name, list(shape), dt, kind="ExternalInput").ap()

    def dout(self, name, shape, dt=F32):
        return self.nc.dram_tensor(name, list(shape), dt, kind="ExternalOutput").ap()

    def dscr(self, name, shape, dt):
        return self.nc.dram_tensor(name, list(shape), dt, kind="Internal").ap()

    def sb(self, name, shape, dt=F32):
        return self.es.enter_context(self.nc.sbuf_tensor(name, list(shape), dt))

    def init_psum(self):
        for i in range(8):
            self.psum.append(self.es.enter_context(self.nc.psum_tensor(f"ps{i}", [128, 512], F32)))

    def ps(self):
        i = self.ps_rr
        self.ps_rr = (self.ps_rr + 1) % 8
        return i


def chunks(n, c):
    return [(i, min(c, n - i)) for i in range(0, n, c)]


def build(debug=""):
    k = K()
    nc, S = k.nc, k.S
    dbg = set(debug.split(",")) if debug else set()

    def scr(name, shape, dt):
        if name in dbg:
            return k.dout(name, shape, dt)
        return k.dscr(name, shape, dt)

    x_ext = k.din("x_ext", [SEQ, D])
    ctx_l = k.din("ctx_l", [CTX, D])
    cvT = k.din("cvT", [D, 2])
    w_mod = k.din("w_mod", [D, 6 * D])
    b_modT = k.din("b_modT", [128, 48])
    b_mod_g = k.din("b_mod_g", [1, 2048])
    n1wT = k.din("n1wT", [128, 8])
    n2wT = k.din("n2wT", [128, 8])
    ident_d = k.din("ident", [128, 128])
    y_out = k.dout("y", [OWN, D])
    hT_d = scr("hT_d", [128, 8, NTOK], BF16)

    k.init_psum()
    PS = k.psum

    def psb(i):
        return PS[i].bitcast(BF16)

    ident_f = k.sb("ident_f", [128, 128], F32)
    ident_b = k.sb("ident_b", [128, 128], BF16)
    S.dma("sp", lambda e: e.dma_start(out=ident_f[:], in_=ident_d[:, :]), w=["ident_f"])
    S.dma("pool", lambda e: e.dma_start(out=ident_b[:], in_=ident_d[:, :]), w=["ident_b"])

    ARENA_BYTES = 155 * 1024
    arena = k.sb("arena", [128, ARENA_BYTES // 2], BF16)

    class AR:
        off = 0

    def aalloc(shape, dt):
        n = int(np.prod(shape))
        nb = n * (4 if dt == F32 else 2)
        nb_al = (nb + 63) // 64 * 64
        assert AR.off + nb_al <= ARENA_BYTES, ("arena overflow", AR.off, nb_al)
        v = arena[:, AR.off // 2:(AR.off + nb) // 2]
        if dt == F32:
            v = v.bitcast(F32)
        AR.off += nb_al
        if len(shape) == 2:
            v = v.rearrange("p (a b) -> p a b", a=shape[0])
        elif len(shape) == 3:
            v = v.rearrange("p (a b c) -> p a b c", a=shape[0], b=shape[1])
        elif len(shape) == 4:
            v = v.rearrange("p (a b c d) -> p a b c d", a=shape[0], b=shape[1], c=shape[2])
        return v

    cv = k.sb("cv", [128, 8, 2], F32)
    siluc = k.sb("siluc", [128, 8, 2], F32)
    bmodT = k.sb("bmodT", [128, 48], F32)
    n1w = k.sb("n1w", [128, 8], F32)
    n2w = k.sb("n2w", [128, 8], F32)
    modT2 = k.sb("modT2", [128, 48, 2], F32)
    sel0 = k.sb("sel0", [2, 128], F32)
    A1 = k.sb("A1", [128, 8, 2], F32)
    A2 = k.sb("A2", [128, 8], F32)
    wst = [k.sb("wst0", [128, 8, 512], F32)]

    S.dve(lambda e: e.memset(modT2[:], 0.0), w=["modT2"])
    S.dma("sp", lambda e: e.dma_start(out=cv[:], in_=cvT.rearrange("(c p) r -> p c r", p=128)), w=["cv"])
    S.dma("sp", lambda e: e.dma_start(out=bmodT[:], in_=b_modT[:, :]), w=["bmodT"])
    S.dma("sp", lambda e: e.dma_start(out=n1w[:], in_=n1wT[:, :]), w=["n1w"])
    S.dma("sp", lambda e: e.dma_start(out=n2w[:], in_=n2wT[:, :]), w=["n2w"])
    S.act(lambda e: e.activation(out=siluc[:], in_=cv[:], func=AF.Silu), r=["cv"], w=["siluc"])
    epsb = k.sb("epsb", [128, 1], F32)
    S.dve(lambda e: e.memset(epsb[:], EPS), w=["epsb"])
    onesc = k.sb("onesc", [128, 1], F32)
    S.dve(lambda e: e.memset(onesc[:], -1.0), w=["onesc"])
    S.dve(lambda e: e.memset(sel0[:], 0.0), w=["sel0"])
    S.dve(lambda e: e.memset(sel0[0:1, :], 1.0), r=["sel0"], w=["sel0"])

    ROWBLK = {4: (0, 0), 5: (0, 512), 10: (1, 0), 11: (1, 512)}
    xin = [k.sb(f"xin{i}", [128, D], F32) for i in range(3)]
    gb_d = k.dscr("gb_d", [2, 2048], F32)
    wst1 = arena[:, 0:8192].bitcast(F32).rearrange("p (a b) -> p a b", a=8)
    grow = [arena[:, 8192 + i_ * 2048:8192 + (i_ + 1) * 2048].bitcast(F32) for i_ in range(2)]
    gbias = arena[:, 12288:14336].bitcast(F32)
    wsts = [wst[0], wst1]

    def mod_block(blk):
        wb = wsts[blk % 2]
        wk = f"wstage{blk % 2}"
        S.dma("sp", lambda e: e.dma_start(out=wb[:, :, :], in_=w_mod[:, blk * 512:(blk + 1) * 512].rearrange("(c p) n -> p c n", p=128)), w=[wk])
        if blk in ROWBLK:
            xi, off = ROWBLK[blk]
            pi = k.ps()
            for dc in range(8):
                S.pe(lambda e, dc=dc: e.matmul(PS[pi][0:2, :], lhsT=siluc[:, dc, :], rhs=wb[:, dc, :], start=(dc == 0), stop=(dc == 7)),
                     r=[wk, "siluc"], w=[("ps", pi)])
            S.act(lambda e: e.activation(out=grow[xi][0:2, off:off + 512], in_=PS[pi][0:2, :], func=AF.Copy), r=[("ps", pi)], w=[("grow", xi)])
        else:
            pi = k.ps()
            for jj in range(4):
                for dc in range(8):
                    S.pe(lambda e, dc=dc, jj=jj: e.matmul(PS[pi][:, jj * 2:jj * 2 + 2], lhsT=wb[:, dc, jj * 128:(jj + 1) * 128], rhs=siluc[:, dc, :],
                                                          start=(dc == 0), stop=(dc == 7)), r=[wk, "siluc"], w=[("ps", pi)])
            for jj in range(4):
                ch = blk * 4 + jj
                S.dve(lambda e, jj=jj, ch=ch: e.tensor_scalar(out=modT2[:, ch, :], in0=PS[pi][:, jj * 2:jj * 2 + 2], scalar1=bmodT[:, ch:ch + 1], scalar2=None,
                                                              op0=ALU.add), r=[("ps", pi), "bmodT"], w=["modT2"])

    def mod_rest():
        for blk in range(4, 12):
            mod_block(blk)
        for xi in range(2):
            S.dma("sp", lambda e, xi=xi: e.dma_start(out=gbias[0:2, :], in_=b_mod_g[0:1, xi * 1024:(xi + 1) * 1024].to_broadcast([2, 1024])), w=["gbias"])
            S.dve(lambda e, xi=xi: e.tensor_tensor(out=grow[xi][0:2, :], in0=grow[xi][0:2, :], in1=gbias[0:2, :], op=ALU.add),
                  r=[("grow", xi), "gbias"], w=[("grow", xi)])
            S.dma("sp", lambda e, xi=xi: e.dma_start(out=gb_d[0:2, xi * 1024:(xi + 1) * 1024], in_=grow[xi][0:2, :]), r=[("grow", xi)], w=[("gb_d", xi)])
        S.dve(lambda e: e.tensor_scalar(out=A2[:], in0=modT2[:, 32:40, 0], scalar1=1.0, scalar2=None, op0=ALU.add), r=["modT2"], w=["A2"])
        S.dve(lambda e: e.tensor_tensor(out=A2[:], in0=A2[:], in1=n2w[:], op=ALU.mult), r=["A2", "n2w"], w=["A2"])

    for blk in range(4):
        mod_block(blk)
    S.dve(lambda e: e.tensor_scalar(out=A1[:], in0=modT2[:, 8:16, :], scalar1=1.0, scalar2=None, op0=ALU.add),
          r=["modT2"], w=["A1"])
    S.dve(lambda e: e.tensor_tensor(out=A1[:], in0=A1[:], in1=n1w[:].unsqueeze(2).to_broadcast([128, 8, 2]), op=ALU.mult),
          r=["A1", "n1w"], w=["A1"])

    junk = k.sb("junk", [128, D], BF16)
    xn = [k.sb(f"xn{i}", [128, D], BF16) for i in range(2)]
    xn3 = [xn[0], xn[1], arena[:, 14336:15360]]
    ssq = k.sb("ssq", [128, 34], F32)
    rstd = k.sb("rstd", [128, 34], F32)
    hTt = [k.sb(f"hTt{i}", [128, 8, 512], BF16) for i in range(2)]
    groups = [(0, 2)] + [(2 + 4 * i, 4) for i in range(8)]
    P1L = float(os.environ.get("P1L", "9"))
    P1E = os.environ.get("P1E", "act")
    groups = groups[:int(os.environ.get("P1G", "99"))]
    tiles_ = []
    for gi, (t0, nt) in enumerate(groups):
        for tt in range(nt):
            tiles_.append((gi, t0, nt, tt))
    p1ps = {}

    def p1_stage1(n):
        gi, t0, nt, tt = tiles_[n]
        t = t0 + tt
        isctx = t < 2
        src = ctx_l[t * 128:(t + 1) * 128, :] if isctx else x_ext[(t - 2) * 128:(t - 1) * 128, :]
        xb, xk = xin[t % 3], f"xin{t % 3}"
        nb, nk = xn3[t % 3], f"xn{t % 3}"
        S.dma("sp", lambda e: e.dma_start(out=xb[:], in_=src), w=[xk])
        S.act(lambda e: e.activation(out=junk[:], in_=xb[:], func=AF.Square, accum_out=ssq[:, t:t + 1]), r=[xk], w=["junk", ("ssq", t)])
        S.act(lambda e: e.activation(out=rstd[:, t:t + 1], in_=ssq[:, t:t + 1], func=AF.Sqrt, scale=1.0 / D, bias=epsb[:, 0:1]),
              r=[("ssq", t), "epsb"], w=[("rstd", t)])
        S.pool(lambda e: e.tensor_tensor(out=rstd[:, t:t + 1], in0=rstd[:, t:t + 1], in1=onesc[:, 0:1], op=ALU.pow),
               r=[("rstd", t), "onesc"], w=[("rstd", t)])
        S.dve(lambda e: e.tensor_scalar(out=nb[:], in0=xb[:], scalar1=rstd[:, t:t + 1], scalar2=None, op0=ALU.mult), r=[xk, ("rstd", t)], w=[nk])
        pi = k.ps()
        p1ps[n] = pi
        for dc in range(8):
            S.pe(lambda e, dc=dc: e.transpose(out=psb(pi)[:, dc * 128:(dc + 1) * 128], in_=nb[:, dc * 128:(dc + 1) * 128], identity=ident_b[:]),
                 r=[nk, "ident_b"], w=[("ps", pi)])

    def p1_stage2(n):
        gi, t0, nt, tt = tiles_[n]
        t = t0 + tt
        ci = 1 if t < 2 else 0
        hb_ = hTt[gi % 2]
        hk = f"hTt{gi % 2}"
        pi = p1ps[n]
        for dc in range(8):
            S.act(lambda e, dc=dc: e.activation(out=hb_[:, dc, tt * 128:(tt + 1) * 128], in_=psb(pi)[:, dc * 128:(dc + 1) * 128], func=AF.Identity,
                                                scale=A1[:, dc, ci:ci + 1], bias=modT2[:, dc, ci:ci + 1]),
                  r=[("ps", pi), "A1", "modT2"], w=[(hk, tt, dc)])
        if tt == nt - 1:
            col0 = t0 * 128
            ncol = nt * 128
            allk = [(hk, tt_, dc) for tt_ in range(nt) for dc in range(8)]
            S.dma("sp", lambda e: e.dma_start(out=hT_d[:, :, col0:col0 + ncol], in_=hb_[:, :, 0:ncol]), r=allk, w=[("hT_d", gi)])

    P1LEAD = 2
    for n in range(len(tiles_) + P1LEAD):
        if n < len(tiles_):
            p1_stage1(n)
        if n >= P1LEAD:
            p1_stage2(n - P1LEAD)

    mod_rest()
    S.barrier()
    w_in = k.din("w_in", [D, D_IN])
    maskU_d = k.din("maskU", [128, 128])
    maskL_d = k.din("maskL", [128, 128])
    permT_d = k.din("permT", [128, 128])
    selc_d = k.din("selc", [128, 4096])
    cosT_d = k.din("cosT", [128, SEQ])
    sinT_d = k.din("sinT", [128, SEQ])
    dtb12_d = k.din("dtb12", [1, 64])
    alog12_d = k.din("alog12", [1, 64])
    dskip_d = k.din("dskip", [1, 32])
    convwT_d = k.din("conv_wT", [128, 32, 5])
    convbT_d = k.din("conv_bT", [128, 32])
    ssdnwT_d = k.din("ssd_nwT", [128, 16])
    ygT_d = scr("ygT_d", [128, 16, OWN], BF16)
    ssq2_d = scr("ssq2_d", [128, 16], F32) if "ssq2_d" in dbg else None

    hb = hTt
    blocks = [(0, 256)] + [(256 + 512 * i, 512) for i in range(8)]

    def load_hb(i, col0, ncol):
        S.dma("sp", lambda e: e.dma_start(out=hb[i][:, :, 0:ncol], in_=hT_d[:, :, col0:col0 + ncol]), w=[("hb", i)])

    rstd_ssd = aalloc([16], F32)
    mhalf = aalloc([1], F32)
    pmark = AR.off
    maskU = aalloc([128], F32)
    maskL = aalloc([128], F32)
    ones_f = aalloc([128], F32)
    permT = aalloc([128], BF16)
    selb = aalloc([32, 128], BF16)
    cosT = aalloc([SEQ], BF16)
    sinT = aalloc([SEQ], BF16)
    dtb_b = aalloc([64], F32)
    a_b = aalloc([64], F32)
    dsk_b = aalloc([32], F32)
    convw = aalloc([32, 5], F32)
    convb = aalloc([32], F32)
    ssdnw = aalloc([16], F32)
    one_b = aalloc([1], F32)
    S.dma("sp", lambda e: e.dma_start(out=maskU, in_=maskU_d[:, :]), w=["maskU"])
    S.dma("sp", lambda e: e.dma_start(out=maskL, in_=maskL_d[:, :]), w=["maskL"])
    S.dma("pool", lambda e: e.dma_start(out=permT, in_=permT_d[:, :]), w=["permT"])
    S.dma("pool", lambda e: e.dma_start(out=selb.rearrange("p a b -> p (a b)"), in_=selc_d[:, :]), w=["selb"])
    S.dma("pool", lambda e: e.dma_start(out=cosT, in_=cosT_d[:, :]), w=["cosT"])
    S.dma("pool", lambda e: e.dma_start(out=sinT, in_=sinT_d[:, :]), w=["sinT"])
    S.dma("sp", lambda e: e.dma_start(out=dtb_b, in_=dtb12_d[0:1, :].to_broadcast([128, 64])), w=["dtb_b"])
    S.dma("sp", lambda e: e.dma_start(out=a_b, in_=alog12_d[0:1, :].to_broadcast([128, 64])), w=["a_b"])
    S.dma("sp", lambda e: e.dma_start(out=dsk_b, in_=dskip_d[0:1, :].to_broadcast([128, 32])), w=["dsk_b"])
    S.dma("sp", lambda e: e.dma_start(out=convw, in_=convwT_d[:, :, :]), w=["convw"])
    S.dma("sp", lambda e: e.dma_start(out=convb, in_=convbT_d[:, :]), w=["convb"])
    S.dma("sp", lambda e: e.dma_start(out=ssdnw, in_=ssdnwT_d[:, :]), w=["ssdnw"])
    S.dve(lambda e: e.memset(ones_f, 1.0), w=["ones_f"])
    S.dve(lambda e: e.memset(one_b, 1.0), w=["one_b"])
    S.act(lambda e: e.activation(out=a_b, in_=a_b, func=AF.Exp), r=["a_b"], w=["a_b"])
    S.dve(lambda e: e.tensor_scalar(out=a_b, in0=a_b, scalar1=-1.0, scalar2=None, op0=ALU.mult), r=["a_b"], w=["a_b"])

    NT = 34

    def TK(name, lo=0, hi=NT):
        return [(name, t) for t in range(lo, hi)]

    wS = aalloc([NT, 64], F32)
    eat = aalloc([NT, 64], F32)
    acO = aalloc([16, 64], F32)
    bYo = aalloc([16, 64], F32)
    ea = aalloc([16, 64], F32)
    acT = aalloc([16, 128], BF16)
    p2mark = AR.off
    biasY = aalloc([NT, 64], F32)
    acat = aalloc([NT, 128], F32)
    dta = aalloc([NT, 64], F32)
    achl = aalloc([NT, 2, 2, 32], BF16)
    wdt = aalloc([8, 64], BF16)
    S.dma("pool", lambda e: e.dma_start(out=wdt, in_=w_in[:, C_DT:C_DT + 64].rearrange("(c p) n -> p c n", p=128)), w=["wdt"])
    for bi, (col0, ncol) in enumerate(blocks):
        load_hb(bi % 2, col0, ncol)
        for tt in range(ncol // 128):
            t = col0 // 128 + tt
            pi = k.ps()
            for dc in range(8):
                S.pe(lambda e, pi=pi, dc=dc, tt=tt, bi=bi: e.matmul(PS[pi][:, 0:64], lhsT=hb[bi % 2][:, dc, tt * 128:(tt + 1) * 128],
                                                                   rhs=wdt[:, dc, :], start=(dc == 0), stop=(dc == 7)),
                     r=[("hb", bi % 2), "wdt"], w=[("ps", pi)])
            S.dve(lambda e, pi=pi, t=t: e.tensor_tensor(out=wS[:, t, :], in0=PS[pi][:, 0:64], in1=dtb_b, op=ALU.add),
                  r=[("ps", pi), "dtb_b"], w=[("wS", t)])
    S.act(lambda e: e.activation(out=wS, in_=wS, func=AF.Exp), r=TK("wS"), w=TK("wS"))
    S.act(lambda e: e.activation(out=wS, in_=wS, func=AF.Ln, bias=one_b[:, 0:1]), r=TK("wS") + ["one_b"], w=TK("wS"))
    S.act(lambda e: e.activation(out=biasY, in_=wS, func=AF.Ln), r=TK("wS"), w=TK("biasY"))
    S.dve(lambda e: e.tensor_tensor(out=dta, in0=wS, in1=a_b.unsqueeze(1).to_broadcast([128, NT, 64]), op=ALU.mult),
          r=TK("wS") + ["a_b"], w=TK("dta"))
    for t in range(NT):
        pi = k.ps()
        S.pe(lambda e, pi=pi, t=t: e.matmul(PS[pi][:, 0:32], lhsT=maskU, rhs=dta[:, t, 0:32], start=True, stop=True),
             r=[("dta", t), "maskU"], w=[("ps", pi)])
        S.pe(lambda e, pi=pi, t=t: e.matmul(PS[pi][:, 32:64], lhsT=maskL, rhs=dta[:, t, 32:64], start=True, stop=True),
             r=[("dta", t), "maskL"], w=[("ps", pi)])
        S.pe(lambda e, pi=pi, t=t: e.matmul(PS[pi][:, 64:128], lhsT=ones_f, rhs=dta[:, t, 0:64], start=True, stop=True),
             r=[("dta", t), "ones_f"], w=[("ps", pi)])
        S.act(lambda e, pi=pi, t=t: e.activation(out=acat[:, t, :], in_=PS[pi][:, 0:128], func=AF.Copy),
              r=[("ps", pi)], w=[("acat", t)])
    acv = acat[:, :, 0:64].rearrange("p t (d h) -> p t d h", d=2)
    tmpv = dta.rearrange("p t (d h) -> p t d h", d=2)
    S.dve(lambda e: e.tensor_copy(out=achl[:, :, :, 0, :], in_=acv), r=TK("acat"), w=["achl"])
    S.dve(lambda e: e.tensor_copy(out=tmpv, in_=achl[:, :, :, 0, :]), r=["achl"] + TK("dta"), w=TK("dta"))
    S.dve(lambda e: e.tensor_tensor(out=achl[:, :, :, 1, :], in0=acv, in1=tmpv, op=ALU.subtract),
          r=TK("acat") + TK("dta") + ["achl"], w=["achl"])
    S.dve(lambda e: e.tensor_tensor(out=acv, in0=tmpv, in1=achl[:, :, :, 1, :], op=ALU.add),
          r=TK("dta") + ["achl"], w=TK("acat"))
    S.dve(lambda e: e.tensor_tensor(out=biasY, in0=acat[:, :, 0:64], in1=biasY, op=ALU.subtract),
          r=TK("acat") + TK("biasY"), w=TK("biasY"))
    S.dve(lambda e: e.tensor_tensor(out=wS, in0=acat[:, :, 64:128], in1=biasY, op=ALU.subtract),
          r=TK("acat") + TK("biasY") + TK("wS"), w=TK("wS"))
    S.act(lambda e: e.activation(out=wS, in_=wS, func=AF.Exp), r=TK("wS"), w=TK("wS"))
    S.act(lambda e: e.activation(out=eat, in_=acat[:, :, 64:128], func=AF.Exp), r=TK("acat"), w=["eat"])
    S.act(lambda e: e.activation(out=ea, in_=acat[:, 2:18, 0:64], func=AF.Exp), r=TK("acat"), w=["ea"])
    S.dve(lambda e: e.tensor_copy(out=acO, in_=acat[:, 2:18, 0:64]), r=TK("acat"), w=["acO"])
    S.dve(lambda e: e.tensor_copy(out=bYo, in_=biasY[:, 2:18, :]), r=TK("biasY"), w=["bYo"])
    for c in range(16):
        pi = k.ps()
        S.pe(lambda e, pi=pi, c=c: e.transpose(out=psb(pi)[:, 0:128], in_=achl[:, c + 2].rearrange("p d x h -> p (d x h)"),
                                               identity=ident_b[:]), r=["achl", "ident_b"], w=[("ps", pi)])
        S.act(lambda e, pi=pi, c=c: e.activation(out=acT[:, c, :], in_=psb(pi)[:, 0:128], func=AF.Copy),
              r=[("ps", pi)], w=[("acT", c)])
    if "stageP2" in dbg:
        d1 = k.dout("dbg_wS", [128, NT * 64], F32)
        d2 = k.dout("dbg_acat", [128, NT * 128], F32)
        d5 = k.dout("dbg_eat", [128, NT * 64], F32)
        S.dma("sp", lambda e: e.dma_start(out=d5[:, :], in_=eat.rearrange("p a b -> p (a b)")), r=["eat"])
        d3 = k.dout("dbg_biasY", [128, NT * 64], F32)
        d4 = k.dout("dbg_acT", [128, 16 * 128], BF16)
        S.dma("sp", lambda e: e.dma_start(out=d1[:, :], in_=wS.rearrange("p a b -> p (a b)")), r=TK("wS"))
        S.dma("sp", lambda e: e.dma_start(out=d2[:, :], in_=acat.rearrange("p a b -> p (a b)")), r=TK("acat"))
        S.dma("sp", lambda e: e.dma_start(out=d3[:, :], in_=biasY.rearrange("p a b -> p (a b)")), r=TK("biasY"))
        S.dma("sp", lambda e: e.dma_start(out=d4[:, :], in_=acT.rearrange("p a b -> p (a b)")), r=[("acT", c) for c in range(16)])
        S.emit()
        k.es.close()
        return nc
    S.barrier()
    AR.off = p2mark
    WT = 4360
    preA = aalloc([WT], BF16)
    preB = aalloc([WT], BF16)
    postB = aalloc([WT], BF16)
    postC = aalloc([2824], BF16)
    xs_tok = aalloc([18, 256], BF16)
    B_tok = aalloc([18, 128], BF16)
    y_acc = aalloc([16, 256], F32)
    wgrp = aalloc([8, 768], BF16)
    Sst = [aalloc([256], F32) for _ in range(2)]
    Sbf = [aalloc([256], BF16) for _ in range(2)]
    ssq2 = aalloc([16, 8], F32)
    negbY = aalloc([16, 64], F32)
    xsp = [aalloc([256], BF16) for _ in range(2)]
    Bp = [aalloc([128], BF16) for _ in range(2)]
    ygst = [aalloc([2, 512], BF16) for _ in range(2)]
    p3mark = AR.off
    accA = wst[0][:, :, :].rearrange("p a b -> p (a b)")
    accP = xin[2]
    T1 = [[xin[0][:, 0:512], xin[0][:, 512:1024]], [xin[1][:, 0:512], xin[1][:, 512:1024]]]
    T1K = [["x0a", "x0s1"], ["x1a", "x1s1"]]
    ROPE1 = xin[0][:, 0:512]
    ROPE2 = xin[1][:, 0:512]
    T2 = [aalloc([256], F32) for _ in range(2)]
    T2K = ["t2_0", "t2_1"]
    ZS = aalloc([256], F32)
    YG = aalloc([256], F32)
    DEC = [[xn[0][:, 0:512], aalloc([512], BF16)], [xn[1][:, 0:512], aalloc([512], BF16)]]
    DECK = [["n0a", "dec01"], ["n1a", "dec11"]]
    MTt = [[xn[0][:, 512:1024], aalloc([512], BF16)], [xn[1][:, 512:1024], aalloc([512], BF16)]]
    MTK = [["n0b", "mt01"], ["n1b", "mt11"]]
    XDD = [junk[:, 0:256], junk[:, 256:512]]
    XDDK = ["j0", "j1"]
    CBM = [[junk[:, 512:640], aalloc([128], BF16)], [junk[:, 640:768], aalloc([128], BF16)]]
    CBMK = [["j2", "cbm01"], ["j3", "cbm11"]]
    YGB = junk[:, 768:1024]
    S.pool(lambda e: e.memset(preA, 0.0), w=["preA"] + [("preA", r_) for r_ in range(10)])
    S.pool(lambda e: e.memset(preB, 0.0), w=["preB"] + [("preB", r_) for r_ in range(10)])
    S.dve(lambda e: e.memset(mhalf, -0.5), w=["mhalf"])
    NM = [maskU.bitcast(BF16)[:, 0:128], maskL.bitcast(BF16)[:, 0:128]]
    for d_, (mk_, mname) in enumerate(((maskU, "maskU"), (maskL, "maskL"))):
        S.dve(lambda e, mk_=mk_: e.tensor_scalar(out=T2[0][:, 0:128], in0=mk_, scalar1=-1.0, scalar2=30000.0, op0=ALU.add, op1=ALU.mult),
              r=[mname], w=["t2_0"])
        S.dve(lambda e, d_=d_: e.tensor_copy(out=NM[d_], in_=T2[0][:, 0:128]), r=["t2_0", mname], w=[mname, "negmask"])
    S.dve(lambda e: e.memset(ssq2, 0.0), w=[("ssq2", c, g) for c in range(16) for g in range(8)])
    NBH = negbY.bitcast(BF16)
    XT = xin[0][:].rearrange("p (a b) -> p a b", a=16)
    XT2 = xin[1][:].rearrange("p (a b) -> p a b", a=16)
    S.dve(lambda e: e.tensor_scalar(out=XT, in0=bYo, scalar1=-1.0, scalar2=None, op0=ALU.mult), r=["bYo"], w=["x0a", "x0s1"])
    S.dve(lambda e: e.tensor_copy(out=NBH[:, :, 0:64], in_=XT), r=["x0a", "x0s1"], w=["negbY"])
    S.dve(lambda e: e.tensor_copy(out=XT2, in_=NBH[:, :, 0:64]), r=["negbY"], w=["x1a", "x1s1"])
    S.dve(lambda e: e.tensor_tensor(out=NBH[:, :, 64:128], in0=XT, in1=XT2, op=ALU.subtract), r=["x0a", "x0s1", "x1a", "x1s1", "negbY"], w=["negbY"])

    def bcol(t):
        return 2 + t * 128 if t < 2 else 262 + (t - 2) * 128

    def RK(name):
        return [name] + [(name, r_) for r_ in range(10)]

    W0B = wst[0][:].bitcast(BF16).rearrange("p a b -> p (a b)")
    preX = W0B[:, 0:4360]
    preC = W0B[:, 4368:4368 + 2824]
    X2B = xin[2][:].bitcast(BF16)
    DGS = [W0B[:, 7232:7232 + 640].rearrange("p (a b) -> p a b", a=5)] + \
          [X2B[:, i_ * 640:(i_ + 1) * 640].rearrange("p (a b) -> p a b", a=5) for i_ in range(3)]
    DGN = [0]
    S.pool(lambda e: e.memset(preX, 0.0), w=RK("preX"))
    S.pool(lambda e: e.memset(preC, 0.0), w=RK("preC"))

    def conv_silu(src, srck, dst, dstk, chg, lo, hi):
        dgi = DGN[0] % 4
        DGN[0] += 1
        DG = DGS[dgi]
        dgk = ("diag", dgi)
        for kk in range(5):
            S.dve(lambda e, kk=kk: e.tensor_scalar(out=DG[:, kk, :], in0=ident_b[:], scalar1=convw[:, chg, kk:kk + 1], scalar2=None, op0=ALU.mult),
                  r=["ident_b", "convw"], w=[dgk])
        blks = chunks(hi - lo, 512)
        pis = {}

        def mm(bi_):
            o, m = blks[bi_]
            pi = k.ps()
            pis[bi_] = pi
            rk = [(srck, r_) for r_ in (bi_ - 1, bi_, bi_ + 1) if 0 <= r_ < len(blks)]
            for kk in range(5):
                S.pe(lambda e, pi=pi, kk=kk, o=o, m=m: e.matmul(PS[pi][:, 0:m], lhsT=DG[:, kk, :], rhs=src[:, lo + o - 2 + kk:lo + o - 2 + kk + m],
                                                                start=(kk == 0), stop=(kk == 4)), r=rk + [dgk], w=[("ps", pi)])

        def ev(bi_):
            o, m = blks[bi_]
            pi = pis[bi_]
            S.act(lambda e, pi=pi, o=o, m=m: e.activation(out=dst[:, lo + o:lo + o + m], in_=PS[pi][:, 0:m], func=AF.Silu, bias=convb[:, chg:chg + 1]),
                  r=[("ps", pi), "convb"], w=[(dstk, bi_)])

        mm(0)
        for bi_ in range(1, len(blks)):
            mm(bi_)
            ev(bi_ - 1)
        ev(len(blks) - 1)

    NG = int(os.environ.get("P3G", "8"))

    def load_w_xbc(g_):
        for (c0, n, o) in ((C_XBC + 256 * g_, 256, 0), (C_XBC + 2048 + 128 * g_, 128, 256), (C_XBC + 3072 + 128 * g_, 128, 384)):
            S.dma("pool", lambda e, c0=c0, n=n, o=o: e.dma_start(out=wgrp[:, :, o:o + n],
                                                               in_=w_in[:, c0:c0 + n].rearrange("(c p) n -> p c n", p=128)), w=["wgrp_x"])

    def load_w_z(g_):
        c0 = C_Z + 256 * g_
        S.dma("pool", lambda e: e.dma_start(out=wgrp[:, :, 512:768], in_=w_in[:, c0:c0 + 256].rearrange("(c p) n -> p c n", p=128)), w=["wgrp_z"])

    def gating_tile(gq, ob, tt, bi):
        c = ob * 4 + tt
        st = ygst[ob % 2]
        stk = ("ygst", ob % 2)
        pz = k.ps()
        for dc in range(8):
            S.pe(lambda e, pz=pz, dc=dc: e.matmul(PS[pz][:, 0:256], lhsT=hb[bi % 2][:, dc, tt * 128:(tt + 1) * 128],
                                                  rhs=wgrp[:, dc, 512:768], start=(dc == 0), stop=(dc == 7)),
                 r=[("hb", bi % 2), "wgrp_z"], w=[("ps", pz)])
        sl = c % 2
        zsb, zsk = (ZS, "zs") if sl == 0 else (YG, "yg")
        ygb, ygk = (YGB, "j4") if sl == 0 else (DEC[0][0][:, 0:256], "n0a")
        sqo, sqk = (MTt[0][0][:, 0:256], "n0b") if sl == 0 else (MTt[0][0][:, 256:512], "n0b2")
        S.act(lambda e: e.activation(out=zsb, in_=PS[pz][:, 0:256], func=AF.Silu), r=[("ps", pz)], w=[zsk])
        S.dve(lambda e: e.tensor_tensor(out=ygb, in0=y_acc[:, c, :], in1=zsb, op=ALU.mult), r=[("y_acc", c), zsk], w=[ygk])
        S.act(lambda e: e.activation(out=sqo, in_=ygb, func=AF.Square, accum_out=ssq2[:, c, gq:gq + 1]), r=[ygk], w=[sqk, ("ssq2", c, gq)])
        pt = k.ps()
        for j in range(2):
            S.pe(lambda e, j=j: e.transpose(out=psb(pt)[:, j * 128:(j + 1) * 128], in_=ygb[:, j * 128:(j + 1) * 128], identity=ident_b[:]),
                 r=[ygk, "ident_b"], w=[("ps", pt)])
        for j in range(2):
            S.act(lambda e, j=j: e.activation(out=st[:, j, tt * 128:(tt + 1) * 128], in_=psb(pt)[:, j * 128:(j + 1) * 128],
                                              func=AF.Copy, scale=ssdnw[:, 2 * gq + j:2 * gq + j + 1]),
                  r=[("ps", pt), "ssdnw"], w=[stk])
        if tt == 3:
            S.dma("sp", lambda e: e.dma_start(out=ygT_d[:, 2 * gq:2 * gq + 2, ob * 512:(ob + 1) * 512], in_=st), r=[stk], w=[("ygT_d", gq, ob)])

    load_w_xbc(0)
    for g in range(NG):
        hd0 = 4 * g
        PRE = ((preX, "preX", 256), (preC, "preC", 384), (preA, "preA", 0), (preB, "preB", 128))
        for bi, (col0, ncol) in enumerate(blocks):
            load_hb(bi % 2, col0, ncol)
            for j, (pre, prek, wo) in enumerate(PRE):
                if j == 1 and not (256 <= col0 < 256 + 2560):
                    continue
                pi = k.ps()
                for dc in range(8):
                    S.pe(lambda e, pi=pi, dc=dc, wo=wo, bi=bi, ncol=ncol: e.matmul(
                        PS[pi][:, 0:ncol], lhsT=wgrp[:, dc, wo:wo + 128], rhs=hb[bi % 2][:, dc, 0:ncol], start=(dc == 0), stop=(dc == 7)),
                        r=[("hb", bi % 2), "wgrp_x"], w=[("ps", pi)])
                dcol = col0 + 2 if col0 < 256 else col0 + 6
                if (bi + j) % 2 == 0:
                    S.act(lambda e, pi=pi, pre=pre, dcol=dcol, ncol=ncol: e.activation(out=pre[:, dcol:dcol + ncol], in_=PS[pi][:, 0:ncol], func=AF.Copy),
                          r=[("ps", pi)], w=RK(prek))
                else:
                    S.dve(lambda e, pi=pi, pre=pre, dcol=dcol, ncol=ncol: e.tensor_copy(out=pre[:, dcol:dcol + ncol], in_=PS[pi][:, 0:ncol]),
                          r=[("ps", pi)], w=RK(prek))
            if g > 0 and 1 <= bi <= 4:
                for tt in range(4):
                    gating_tile(g - 1, bi - 1, tt, bi)
        if g + 1 < NG:
            load_w_xbc(g + 1)
        load_w_z(g)
        conv_silu(preX, "preX", postB, "postB", 16 + g, 2, 4358)
        conv_silu(preC, "preC", postC, "postC", 24 + g, 262, 262 + 2050)
        conv_silu(preA, "preA", preA, "preA", 2 * g, 2, 4358)
        conv_silu(preB, "preB", preB, "preB", 2 * g + 1, 2, 4358)
        for (buf, bk, nblk) in ((postB, "postB", 8), (postC, "postC", 4)):
            for rb in range(nblk):
                c0 = 262 + rb * 512
                e0 = rb * 512
                pi = k.ps()
                S.pe(lambda e, pi=pi, buf=buf, c0=c0: e.matmul(PS[pi][:, :], lhsT=permT, rhs=buf[:, c0:c0 + 512], start=True, stop=True),
                     r=RK(bk) + ["permT"], w=[("ps", pi)])
                S.dve(lambda e, pi=pi, e0=e0: e.tensor_tensor(out=ROPE1, in0=PS[pi][:, :], in1=sinT[:, e0:e0 + 512], op=ALU.mult),
                      r=[("ps", pi), "sinT"], w=["x0a"])
                S.pool(lambda e, buf=buf, c0=c0, e0=e0: e.tensor_tensor(out=ROPE2, in0=buf[:, c0:c0 + 512], in1=cosT[:, e0:e0 + 512], op=ALU.mult),
                       r=RK(bk) + ["cosT"], w=["x1a"])
                S.dve(lambda e, buf=buf, c0=c0: e.tensor_tensor(out=buf[:, c0:c0 + 512], in0=ROPE1, in1=ROPE2, op=ALU.add),
                      r=["x0a", "x1a"] + RK(bk), w=RK(bk))
        for t in range(18):
            pi = k.ps()
            S.pe(lambda e, pi=pi, t=t: e.transpose(out=psb(pi)[:, 0:128], in_=postB[:, bcol(t):bcol(t) + 128], identity=ident_b[:]),
                 r=RK("postB") + ["ident_b"], w=[("ps", pi)])
            S.dve(lambda e, pi=pi, t=t: e.tensor_copy(out=B_tok[:, t, :], in_=psb(pi)[:, 0:128]), r=[("ps", pi)], w=[("B_tok", t)])
        for t in range(18):
            pi = k.ps()
            for j, (pre, prek) in enumerate(((preA, "preA"), (preB, "preB"))):
                S.pe(lambda e, pi=pi, j=j, pre=pre, t=t: e.transpose(out=psb(pi)[:, j * 128:(j + 1) * 128], in_=pre[:, bcol(t):bcol(t) + 128],
                                                                    identity=ident_b[:]), r=RK(prek) + ["ident_b"], w=[("ps", pi)])
            S.act(lambda e, pi=pi, t=t: e.activation(out=xs_tok[:, t, :], in_=psb(pi)[:, 0:256], func=AF.Copy),
                  r=[("ps", pi)], w=[("xs_tok", t)])

        for d in range(2):
            S.dve(lambda e, d=d: e.memset(Sst[d], 0.0), w=[("Sst", d)])
            S.dve(lambda e, d=d: e.memset(Sbf[d], 0.0), w=[("Sbf", d)])

        SB2 = [[Sbf[0], xsp[0]], [Sbf[1], xsp[1]]]
        SB2K = [[("Sbf", 0), ("xsp", 0)], [("Sbf", 1), ("xsp", 1)]]

        def state_update(d, xs_ap, xs_k, B_ap, B_k, tg, oslot=0, xslot=None, copy=True, split=None):
            hs = d * 32 + hd0
            xq = d if xslot is None else xslot
            if split != "back":
              S.dve(lambda e: e.tensor_tensor(out=XDD[xq].rearrange("p (h q) -> p h q", h=4), in0=xs_ap.rearrange("p (h q) -> p h q", h=4),
                                             in1=wS[:, tg, hs:hs + 4].unsqueeze(2).to_broadcast([128, 4, 64]), op=ALU.mult),
                   r=[xs_k, ("wS", tg)], w=[XDDK[xq]])
            if split == "front":
                return
            pi = k.ps()
            S.pe(lambda e, pi=pi: e.matmul(PS[pi][:, 0:256], lhsT=B_ap, rhs=XDD[xq], start=True, stop=True),
                 r=[B_k, XDDK[xq]], w=[("ps", pi)])
            S.dve(lambda e: e.tensor_tensor(out=Sst[d].rearrange("p (h q) -> p h q", h=4), in0=Sst[d].rearrange("p (h q) -> p h q", h=4),
                                            in1=eat[:, tg, hs:hs + 4].unsqueeze(2).to_broadcast([128, 4, 64]), op=ALU.mult),
                  r=[("Sst", d), "eat"], w=[("Sst", d)])
            S.dve(lambda e, pi=pi: e.tensor_tensor(out=Sst[d], in0=PS[pi][:, 0:256], in1=Sst[d], op=ALU.add),
                  r=[("ps", pi), ("Sst", d)], w=[("Sst", d)])
            if copy:
                S.act(lambda e: e.activation(out=SB2[d][oslot], in_=Sst[d], func=AF.Copy), r=[("Sst", d)], w=[SB2K[d][oslot]])

        def front(d, c, sl):
            tg = c + 2
            hs = d * 32 + hd0
            cc = bcol(tg)
            pi = k.ps()
            S.pe(lambda e, pi=pi: e.matmul(PS[pi][:, 0:128], lhsT=postB[:, cc:cc + 128], rhs=postC[:, cc:cc + 128], start=True, stop=True),
                 r=RK("postB") + RK("postC"), w=[("ps", pi)])
            S.act(lambda e, pi=pi: e.activation(out=CBM[d][sl], in_=PS[pi][:, 0:128], func=AF.Copy), r=[("ps", pi)], w=[CBMK[d][sl]])
            pb = k.ps()
            S.pe(lambda e, pb=pb: e.matmul(PS[pb][:, :].rearrange("p (h q) -> p h q", h=4), lhsT=ident_b[:],
                                           rhs=NM[d].unsqueeze(1).to_broadcast([128, 4, 128]), start=True, stop=False),
                 r=["ident_b", "negmask"], w=[("ps", pb)])
            for part in range(2):
                S.pe(lambda e, pb=pb, part=part: e.matmul(PS[pb][:, :].rearrange("p (h q) -> p h q", h=4), lhsT=ident_b[:],
                                                          rhs=NBH[:, c, part * 64 + hs:part * 64 + hs + 4].unsqueeze(2).to_broadcast([128, 4, 128]),
                                                          start=False, stop=False), r=["ident_b", "negbY"], w=[("ps", pb)])
            for hh in range(4):
                S.pe(lambda e, pb=pb, hh=hh: e.matmul(PS[pb][:, hh * 128:(hh + 1) * 128], lhsT=selb[d * 64:(d + 1) * 64, hd0 + hh, :],
                                                      rhs=acT[d * 64:(d + 1) * 64, c, :], start=False, stop=(hh == 3)),
                     r=["selb", ("acT", c)], w=[("ps", pb)])
            S.act(lambda e, pb=pb: e.activation(out=DEC[d][sl], in_=PS[pb][:, :], func=AF.Exp), r=[("ps", pb)], w=[DECK[d][sl]])
            S.dve(lambda e: e.tensor_tensor(out=MTt[d][sl].rearrange("p (h q) -> p h q", h=4), in0=DEC[d][sl].rearrange("p (h q) -> p h q", h=4),
                                             in1=CBM[d][sl].unsqueeze(1).to_broadcast([128, 4, 128]), op=ALU.mult),
                   r=[DECK[d][sl], CBMK[d][sl]], w=[MTK[d][sl]])

        def back(d, c, sl, islot=0):
            tg = c + 2
            hs = d * 32 + hd0
            cc = bcol(tg)
            py = k.ps()
            for hh in range(4):
                S.pe(lambda e, py=py, hh=hh: e.matmul(PS[py][:, hh * 64:(hh + 1) * 64], lhsT=MTt[d][sl][:, hh * 128:(hh + 1) * 128],
                                                      rhs=xs_tok[:, tg, hh * 64:(hh + 1) * 64], start=True, stop=True),
                     r=[MTK[d][sl], ("xs_tok", tg)], w=[("ps", py)])
            S.pe(lambda e, py=py: e.matmul(PS[py][:, 256:512], lhsT=postC[:, cc:cc + 128], rhs=SB2[d][islot], start=True, stop=True),
                 r=RK("postC") + [SB2K[d][islot]], w=[("ps", py)])
            S.dve(lambda e, py=py: e.tensor_tensor(out=T2[d].rearrange("p (h q) -> p h q", h=4), in0=PS[py][:, 256:512].rearrange("p (h q) -> p h q", h=4),
                                                   in1=ea[:, c, hs:hs + 4].unsqueeze(2).to_broadcast([128, 4, 64]), op=ALU.mult),
                  r=[("ps", py), "ea"], w=[T2K[d]])
            S.pool(lambda e: e.tensor_tensor(out=y_acc[:, c, :], in0=y_acc[:, c, :], in1=T2[d], op=ALU.add),
                   r=[T2K[d], ("y_acc", c)], w=[("y_acc", c)])
            S.dve(lambda e, py=py: e.tensor_tensor(out=y_acc[:, c, :], in0=PS[py][:, 0:256], in1=y_acc[:, c, :], op=ALU.add),
                  r=[("ps", py), ("y_acc", c)], w=[("y_acc", c)])

        for c in range(16):
            S.pool(lambda e, c=c: e.tensor_tensor(out=y_acc[:, c, :].rearrange("p (h q) -> p h q", h=4),
                                                  in0=xs_tok[:, c + 2, :].rearrange("p (h q) -> p h q", h=4),
                                                  in1=dsk_b[:, hd0:hd0 + 4].unsqueeze(2).to_broadcast([128, 4, 64]), op=ALU.mult),
                   r=[("xs_tok", c + 2), "dsk_b"], w=[("y_acc", c)])
        for t in (1, 0):
            state_update(1, xs_tok[:, t, :], ("xs_tok", t), B_tok[:, t, :], ("B_tok", t), t)
        plist = list(range(33, 17, -1))

        def pfront(t):
            q = t % 2
            pi = k.ps()
            for j, (pre, prek) in enumerate(((preA, "preA"), (preB, "preB"))):
                S.pe(lambda e, j=j, pre=pre: e.transpose(out=psb(pi)[:, j * 128:(j + 1) * 128], in_=pre[:, bcol(t):bcol(t) + 128],
                                                         identity=ident_b[:]), r=RK(prek) + ["ident_b"], w=[("ps", pi)])
            S.pe(lambda e: e.transpose(out=psb(pi)[:, 256:384], in_=postB[:, bcol(t):bcol(t) + 128], identity=ident_b[:]),
                 r=RK("postB") + ["ident_b"], w=[("ps", pi)])
            S.act(lambda e: e.activation(out=xsp[q], in_=psb(pi)[:, 0:256], func=AF.Copy), r=[("ps", pi)], w=[("xsp", q)])
            S.act(lambda e: e.activation(out=Bp[q], in_=psb(pi)[:, 256:384], func=AF.Copy), r=[("ps", pi)], w=[("Bp", q)])
            state_update(1, xsp[q], ("xsp", q), Bp[q], ("Bp", q), t, xslot=q, split="front")

        def pback(t, last):
            q = t % 2
            state_update(1, xsp[q], ("xsp", q), Bp[q], ("Bp", q), t, xslot=q, split="back", copy=last)

        pfront(plist[0])
        for n_ in range(len(plist)):
            if n_ + 1 < len(plist):
                pfront(plist[n_ + 1])
            pback(plist[n_], n_ == len(plist) - 1)
        for t in (0, 1):
            state_update(0, xs_tok[:, t, :], ("xs_tok", t), B_tok[:, t, :], ("B_tok", t), t)
        for i in range(17):
            if i >= 1:
                c1, c2 = i - 1, 16 - i
                j = i - 1
                if c1 < 15:
                    state_update(0, xs_tok[:, c1 + 2, :], ("xs_tok", c1 + 2), B_tok[:, c1 + 2, :], ("B_tok", c1 + 2), c1 + 2, oslot=(j + 1) % 2, split="front")
                if c2 > 0:
                    state_update(1, xs_tok[:, c2 + 2, :], ("xs_tok", c2 + 2), B_tok[:, c2 + 2, :], ("B_tok", c2 + 2), c2 + 2, oslot=(j + 1) % 2, split="front")
            if i < 16:
                front(0, i, i % 2)
                front(1, 15 - i, i % 2)
            if i >= 1:
                if c1 < 15:
                    state_update(0, xs_tok[:, c1 + 2, :], ("xs_tok", c1 + 2), B_tok[:, c1 + 2, :], ("B_tok", c1 + 2), c1 + 2, oslot=(j + 1) % 2, split="back")
                if c2 > 0:
                    state_update(1, xs_tok[:, c2 + 2, :], ("xs_tok", c2 + 2), B_tok[:, c2 + 2, :], ("B_tok", c2 + 2), c2 + 2, oslot=(j + 1) % 2, split="back")
                back(0, c1, (i - 1) % 2, islot=j % 2)
                back(1, c2, (i - 1) % 2, islot=j % 2)

    for ob in range(4 if NG >= 1 else 0):
        bi = ob + 1
        col0, ncol = blocks[bi]
        load_hb(bi % 2, col0, ncol)
        for tt in range(4):
            gating_tile(NG - 1, ob, tt, bi)

    if NG == 8:
        S.dve(lambda e: e.tensor_reduce(out=rstd_ssd, in_=ssq2, axis=AX.X, op=ALU.add), r=[("ssq2", c, g) for c in range(16) for g in range(8)], w=["rstd_ssd"])
        S.dve(lambda e: e.tensor_scalar(out=rstd_ssd, in0=rstd_ssd, scalar1=1.0 / D_SSD, scalar2=EPS, op0=ALU.mult, op1=ALU.add), r=["rstd_ssd"], w=["rstd_ssd"])
        S.pool(lambda e: e.tensor_tensor(out=rstd_ssd, in0=rstd_ssd, in1=mhalf[:, 0:1].to_broadcast([128, 16]), op=ALU.pow), r=["rstd_ssd", "mhalf"], w=["rstd_ssd"])
    if "stageP3" in dbg:
        dssq = k.dout("dbg_ssq2", [128, 128], F32)
        S.dma("sp", lambda e: e.dma_start(out=dssq[:, :], in_=ssq2.rearrange("p a b -> p (a b)")), r=[("ssq2", c, g) for c in range(16) for g in range(NG)])
        S.emit()
        k.es.close()
        return nc
    S.barrier()
    AR.off = pmark
    NKT = 20
    nabias_d = k.din("nabias", [16, 128, 3 * 5 * 128])
    qkw_d = k.din("qkw", [1, 1024])
    ynaT_d = scr("ynaT_d", [128, 8, OWN], BF16)
    wq = aalloc([3, 8, 512], BF16)
    qT = aalloc([4, OWN], BF16)
    kT = aalloc([4, NKT * 128], BF16)
    Vaug = aalloc([NKT, 8, 65], BF16)
    qkw = aalloc([1024], F32)
    Ef = aalloc([15 * 128], F32)
    Eb = aalloc([15 * 128], BF16)
    PTs = [aalloc([7 * 128], BF16) for _ in range(3)]
    NSL = 5
    sqbs = [aalloc([512], F32) for _ in range(NSL)]
    nrms = [aalloc([512], F32) for _ in range(NSL)]
    qtks = [aalloc([512], BF16) for _ in range(NSL)]
    ss8s = [aalloc([8], F32) for _ in range(NSL)]
    denall = aalloc([16, 8], F32)
    ynatok = aalloc([16, 512], BF16)
    ynst = [aalloc([4, 512], BF16) for _ in range(2)]
    S.dma("sp", lambda e: e.dma_start(out=qkw, in_=qkw_d[0:1, :].to_broadcast([128, 1024])), w=["qkw"])
    S.dve(lambda e: e.memset(Vaug, 1.0), w=["Vaug"])

    def ktile_cols(kt):
        return kt * 128 if kt < 2 else 256 + (kt - 2) * 128

    for hp in range(2):
        for j3 in range(3 if hp == 0 else 0):
            c0 = C_QKV + j3 * 1024 + hp * 512
            S.dma("pool", lambda e, j3=j3, c0=c0: e.dma_start(out=wq[:, j3, :, :], in_=w_in[:, c0:c0 + 512].rearrange("(c p) n -> p c n", p=128)), w=["wq"])
        kblocks = [(0, 256)] + [(256 + 512 * i, 512) for i in range(5)]
        items = []
        for bi, (col0, ncol) in enumerate(kblocks):
            nt_ = 2 if bi == 5 else ncol // 128
            for tt in range(nt_):
                kt = col0 // 128 + tt
                own = 2 <= kt < 18
                for j3 in ((0, 1, 2) if own else (1, 2)):
                    items.append((bi, col0, ncol, tt, kt, j3, tt == 0 and j3 == (0 if own else 1)))
        stA = {}

        def stageA(n):
            bi, col0, ncol, tt, kt, j3, first = items[n]
            if first:
                load_hb(bi % 2, col0, ncol)
            sl = n % NSL
            pi = k.ps()
            for dc in range(8):
                S.pe(lambda e, dc=dc: e.matmul(PS[pi][:, :], lhsT=hb[bi % 2][:, dc, tt * 128:(tt + 1) * 128],
                                               rhs=wq[:, j3, dc, :], start=(dc == 0), stop=(dc == 7)),
                     r=[("hb", bi % 2), "wq"], w=[("ps", pi)])
            stA[n] = pi
            if j3 == 2:
                S.act(lambda e: e.activation(out=Vaug[:, kt, :, 0:64], in_=PS[pi][:, :].rearrange("p (h d) -> p h d", h=8), func=AF.Copy),
                      r=[("ps", pi)], w=["Vaug"])
                return
            S.act(lambda e: e.activation(out=sqbs[sl], in_=PS[pi][:, :], func=AF.Square), r=[("ps", pi)], w=[("sqb", sl)])
            S.dve(lambda e: e.tensor_reduce(out=ss8s[sl], in_=sqbs[sl].rearrange("p (h d) -> p h d", h=8), axis=AX.X, op=ALU.add), r=[("sqb", sl)], w=[("ss8", sl)])
            S.dve(lambda e: e.tensor_scalar(out=ss8s[sl], in0=ss8s[sl], scalar1=1.0 / 64, scalar2=EPS, op0=ALU.mult, op1=ALU.add), r=[("ss8", sl)], w=[("ss8", sl)])
            S.pool(lambda e: e.tensor_tensor(out=ss8s[sl], in0=ss8s[sl], in1=mhalf[:, 0:1].to_broadcast([128, 8]), op=ALU.pow), r=[("ss8", sl), "mhalf"], w=[("ss8", sl)])

        def stageB(n):
            bi, col0, ncol, tt, kt, j3, first = items[n]
            if j3 == 2:
                return
            sl = n % NSL
            pi = stA[n]
            S.dve(lambda e: e.tensor_tensor(out=nrms[sl].rearrange("p (h d) -> p h d", h=8), in0=PS[pi][:, :].rearrange("p (h d) -> p h d", h=8),
                                            in1=ss8s[sl].unsqueeze(2).to_broadcast([128, 8, 64]), op=ALU.mult), r=[("ps", pi), ("ss8", sl)], w=[("nrm", sl)])
            S.dve(lambda e: e.tensor_tensor(out=qtks[sl], in0=nrms[sl], in1=qkw[:, j3 * 512:(j3 + 1) * 512], op=ALU.mult), r=[("nrm", sl), "qkw"], w=[("qtk", sl)])
            pt = k.ps()
            for pr in range(4):
                S.pe(lambda e, pr=pr: e.transpose(out=psb(pt)[:, pr * 128:(pr + 1) * 128], in_=qtks[sl][:, pr * 128:(pr + 1) * 128], identity=ident_b[:]),
                     r=[("qtk", sl), "ident_b"], w=[("ps", pt)])
            if j3 == 0:
                c = kt - 2
                S.act(lambda e: e.activation(out=qT[:, :, c * 128:(c + 1) * 128], in_=psb(pt)[:, 0:512].rearrange("p (a b) -> p a b", a=4), func=AF.Copy),
                      r=[("ps", pt)], w=[("qT", c)])
            else:
                S.act(lambda e: e.activation(out=kT[:, :, kt * 128:(kt + 1) * 128], in_=psb(pt)[:, 0:512].rearrange("p (a b) -> p a b", a=4), func=AF.Copy),
                      r=[("ps", pt)], w=[("kT", kt)])

        LEAD = NSL - 1
        for n in range(len(items) + LEAD):
            if n < len(items):
                stageA(n)
            if n >= LEAD:
                stageB(n - LEAD)
        if hp == 0:
            for j3 in range(3):
                c0 = C_QKV + j3 * 1024 + 512
                S.dma("pool", lambda e, j3=j3, c0=c0: e.dma_start(out=wq[:, j3, :, :], in_=w_in[:, c0:c0 + 512].rearrange("(c p) n -> p c n", p=128)), w=["wq"])
        for hl in range(8):
            h = hp * 8 + hl
            pr, po = hl // 2, (hl % 2) * 64
            S.dma("sp", lambda e, h=h: e.dma_start(out=Ef, in_=nabias_d[h, :, :]), w=["Ef"])
            S.act(lambda e: e.activation(out=Eb, in_=Ef, func=AF.Exp), r=["Ef"], w=["Eb"])
            def na_front(i, sl):
                cls = min(i, 2)
                kts = [2 + x for x in ([0, 1, 2, 3, 4] if i < 2 else range(i - 2, i + 3))] + [0, 1]
                pa, pb_ = k.ps(), k.ps()
                for n_, kt in enumerate(kts):
                    dstp = PS[pa][:, n_ * 128:(n_ + 1) * 128] if n_ < 4 else PS[pb_][:, (n_ - 4) * 128:(n_ - 3) * 128]
                    S.pe(lambda e, dstp=dstp, kt=kt, i=i: e.matmul(dstp, lhsT=kT[po:po + 64, pr, kt * 128:(kt + 1) * 128],
                                                                   rhs=qT[po:po + 64, pr, i * 128:(i + 1) * 128], start=True, stop=True),
                         r=[("kT", kt), ("qT", i)], w=[("ps", pa if n_ < 4 else pb_)])
                S.act(lambda e, pa=pa: e.activation(out=PTs[sl][:, 0:512], in_=PS[pa][:, :], func=AF.Exp), r=[("ps", pa)], w=[("PTa", sl)])
                S.act(lambda e, pb_=pb_: e.activation(out=PTs[sl][:, 512:896], in_=PS[pb_][:, 0:384], func=AF.Exp), r=[("ps", pb_)], w=[("PTb", sl)])
                S.dve(lambda e: e.tensor_tensor(out=PTs[sl][:, 0:512], in0=PTs[sl][:, 0:512], in1=Eb[:, cls * 640:cls * 640 + 512], op=ALU.mult),
                      r=[("PTa", sl), "Eb"], w=[("PTa", sl)])
                S.dve(lambda e: e.tensor_tensor(out=PTs[sl][:, 512:640], in0=PTs[sl][:, 512:640], in1=Eb[:, cls * 640 + 512:cls * 640 + 640], op=ALU.mult),
                      r=[("PTb", sl), "Eb"], w=[("PTb", sl)])

            def na_back(i, sl):
                kts = [2 + x for x in ([0, 1, 2, 3, 4] if i < 2 else range(i - 2, i + 3))] + [0, 1]
                po_ = k.ps()
                for n_, kt in enumerate(kts):
                    S.pe(lambda e, po_=po_, n_=n_, kt=kt: e.matmul(PS[po_][:, 0:65], lhsT=PTs[sl][:, n_ * 128:(n_ + 1) * 128],
                                                                 rhs=Vaug[:, kt, hl, :], start=(n_ == 0), stop=(n_ == 6)),
                         r=[("PTa", sl), ("PTb", sl), "Vaug"], w=[("ps", po_)])
                S.dve(lambda e, po_=po_: e.tensor_copy(out=denall[:, i, hl:hl + 1], in_=PS[po_][:, 64:65]), r=[("ps", po_)], w=[("den", i, hl)])
                S.act(lambda e, po_=po_: e.activation(out=ynatok[:, i, hl * 64:(hl + 1) * 64], in_=PS[po_][:, 0:64], func=AF.Copy),
                      r=[("ps", po_)], w=[("ynatok", i, hl)])

            for it in range(18):
                if it < 16:
                    na_front(it, it % 3)
                if it >= 2:
                    na_back(it - 2, (it - 2) % 3)
        allden = [("den", i_, h_) for i_ in range(16) for h_ in range(8)]
        S.pool(lambda e: e.tensor_tensor(out=denall.rearrange("p a b -> p (a b)"), in0=denall.rearrange("p a b -> p (a b)"),
                                         in1=onesc[:, 0:1].to_broadcast([128, 128]), op=ALU.pow), r=allden + ["onesc"], w=["rden"])
        for i in range(16):
            S.dve(lambda e, i=i: e.tensor_tensor(out=ynatok[:, i, :].rearrange("p (h d) -> p h d", h=8), in0=ynatok[:, i, :].rearrange("p (h d) -> p h d", h=8),
                                                 in1=denall[:, i, :].unsqueeze(2).to_broadcast([128, 8, 64]), op=ALU.mult),
                  r=["rden"] + [("ynatok", i, h_) for h_ in range(8)], w=[("ynatok", i)])
        for i in range(16):
            pt = k.ps()
            for pr in range(4):
                S.pe(lambda e, pt=pt, pr=pr, i=i: e.transpose(out=psb(pt)[:, pr * 128:(pr + 1) * 128], in_=ynatok[:, i, pr * 128:(pr + 1) * 128], identity=ident_b[:]),
                     r=[("ynatok", i), "ident_b"], w=[("ps", pt)])
            sq_ = (i // 4) % 2
            st = ynst[sq_]
            S.act(lambda e, pt=pt, st=st, i=i: e.activation(out=st[:, :, (i % 4) * 128:(i % 4 + 1) * 128], in_=psb(pt)[:, 0:512].rearrange("p (a b) -> p a b", a=4), func=AF.Copy),
                  r=[("ps", pt)], w=[("ynst", sq_)])
            if i % 4 == 3:
                S.dma("sp", lambda e, st=st, i=i, hp=hp: e.dma_start(out=ynaT_d[:, hp * 4:hp * 4 + 4, (i // 4) * 512:(i // 4 + 1) * 512], in_=st),
                      r=[("ynst", sq_)], w=[("ynaT_d", hp, i // 4)])
    if "stageP4" in dbg:
        S.emit()
        k.es.close()
        return nc
    S.barrier()
    AR.off = pmark
    wbrs_d = k.din("w_br_ssd", [D_SSD, D])
    wbrn_d = k.din("w_br_na", [D, D])
    wout_d = k.din("w_out", [D, D])
    wrt_d = k.din("w_rt36", [D, 36])
    brt_d = k.din("b_rt36", [1, 36])
    w1_d = k.din("w1", [NEXP, D, 512])
    w3_d = k.din("w3", [NEXP, D, 512])
    w2_d = k.din("w2", [NEXP, 512, D])
    h2T = aalloc([8, OWN], BF16)
    gates = aalloc([16, 32], F32)
    gbb = aalloc([2048], F32)
    wrt = aalloc([8, 36], F32)
    brt = aalloc([36], F32)
    x1 = aalloc([16, D], F32)
    p5mark = AR.off
    AR.off = p5mark - 16 * D * 4
    wbrs = aalloc([16, D], BF16)
    wbrn = aalloc([8, D], BF16)
    wgt = aalloc([8, 2048], BF16)
    ygt = [aalloc([16, 128], BF16) for _ in range(2)]
    ynt_ = [aalloc([8, 128], BF16) for _ in range(2)]
    sg = aalloc([512], F32)
    m1 = aalloc([D], F32)
    mrg = aalloc([D], BF16)
    S.dma("sp", lambda e: e.dma_start(out=gbb, in_=gb_d[0:1, :].to_broadcast([128, 2048])), w=["gbb"])
    S.dma("sp", lambda e: e.dma_start(out=wrt, in_=wrt_d.rearrange("(c p) n -> p c n", p=128)), w=["wrt"])
    S.dma("sp", lambda e: e.dma_start(out=brt, in_=brt_d[0:1, :].to_broadcast([128, 36])), w=["brt"])
    S.dma("pool", lambda e: e.dma_start(out=wbrs, in_=wbrs_d.rearrange("(c p) n -> p c n", p=128)), w=["wbrs"])
    S.dma("pool", lambda e: e.dma_start(out=wbrn, in_=wbrn_d.rearrange("(c p) n -> p c n", p=128)), w=["wbrn"])
    S.dma("pool", lambda e: e.dma_start(out=wgt, in_=w_in[:, C_G:C_G + 2048].rearrange("(c p) n -> p c n", p=128)), w=["wgt"])
    ssq3 = ssq
    for ob in range(4):
        bi = ob + 1
        col0, ncol = blocks[bi]
        load_hb(bi % 2, col0, ncol)
        for tt in range(4):
            c = ob * 4 + tt
            q = c % 2
            S.dma("sp", lambda e, q=q, c=c: e.dma_start(out=ygt[q], in_=ygT_d[:, :, c * 128:(c + 1) * 128]), w=[("ygt", q)])
            S.dma("sp", lambda e, q=q, c=c: e.dma_start(out=ynt_[q], in_=ynaT_d[:, :, c * 128:(c + 1) * 128]), w=[("ynt", q)])
            for nb_ in range(2):
                cs_ = slice(nb_ * 512, (nb_ + 1) * 512)
                pa = k.ps()
                for ch in range(16):
                    S.pe(lambda e, pa=pa, ch=ch, q=q: e.matmul(PS[pa][:, :], lhsT=ygt[q][:, ch, :], rhs=wbrs[:, ch, cs_], start=(ch == 0), stop=(ch == 15)),
                         r=[("ygt", q), "wbrs"], w=[("ps", pa)])
                pg = k.ps()
                for dc in range(8):
                    S.pe(lambda e, pg=pg, dc=dc: e.matmul(PS[pg][:, :], lhsT=hb[bi % 2][:, dc, tt * 128:(tt + 1) * 128], rhs=wgt[:, dc, nb_ * 512:(nb_ + 1) * 512],
                                                          start=(dc == 0), stop=(dc == 7)), r=[("hb", bi % 2), "wgt"], w=[("ps", pg)])
                S.act(lambda e, pg=pg: e.activation(out=sg, in_=PS[pg][:, :], func=AF.Sigmoid), r=[("ps", pg)], w=["sg"])
                S.dve(lambda e, pa=pa, c=c: e.scalar_tensor_tensor(out=m1[:, cs_], in0=PS[pa][:, :], scalar=rstd_ssd[:, c:c + 1], in1=sg, op0=ALU.mult, op1=ALU.mult),
                      r=[("ps", pa), "rstd_ssd", "sg"], w=[("m1", nb_)])
                pn = k.ps()
                for ch in range(8):
                    S.pe(lambda e, pn=pn, ch=ch, q=q: e.matmul(PS[pn][:, :], lhsT=ynt_[q][:, ch, :], rhs=wbrn[:, ch, cs_], start=(ch == 0), stop=(ch == 7)),
                         r=[("ynt", q), "wbrn"], w=[("ps", pn)])
                pg2 = k.ps()
                for dc in range(8):
                    S.pe(lambda e, pg2=pg2, dc=dc: e.matmul(PS[pg2][:, :], lhsT=hb[bi % 2][:, dc, tt * 128:(tt + 1) * 128], rhs=wgt[:, dc, 1024 + nb_ * 512:1024 + (nb_ + 1) * 512],
                                                            start=(dc == 0), stop=(dc == 7)), r=[("hb", bi % 2), "wgt"], w=[("ps", pg2)])
                S.act(lambda e, pg2=pg2: e.activation(out=sg, in_=PS[pg2][:, :], func=AF.Sigmoid), r=[("ps", pg2)], w=["sg"])
                S.dve(lambda e, pn=pn: e.tensor_tensor(out=sg, in0=PS[pn][:, :], in1=sg, op=ALU.mult), r=[("ps", pn), "sg"], w=["sg"])
                S.pool(lambda e: e.tensor_tensor(out=mrg[:, cs_], in0=m1[:, cs_], in1=sg, op=ALU.add), r=[("m1", nb_), "sg"], w=[("mrg", nb_)])
            pt = k.ps()
            for ch in range(8):
                S.pe(lambda e, pt=pt, ch=ch: e.transpose(out=psb(pt)[:, ch * 128:(ch + 1) * 128], in_=mrg[:, ch * 128:(ch + 1) * 128], identity=ident_b[:]),
                     r=[("mrg", 0), ("mrg", 1), "ident_b"], w=[("ps", pt)])
            S.act(lambda e, pt=pt, c=c: e.activation(out=h2T[:, :, c * 128:(c + 1) * 128], in_=psb(pt)[:, :].rearrange("p (a b) -> p a b", a=8), func=AF.Copy),
                  r=[("ps", pt)], w=[("h2T", c)])
    S.barrier()
    AR.off = p5mark
    wout = aalloc([8, D], BF16)
    sg = aalloc([512], F32)
    m1 = aalloc([D], F32)
    h2f = aalloc([8, 128], F32)
    rla = aalloc([16, 36], F32)
    rv = aalloc([7, 16], F32)
    oh4 = aalloc([16, 4], F32)
    ge4 = aalloc([16, 4], F32)
    Em = aalloc([16, 32], F32)
    eq1 = aalloc([16, 32], F32)
    eq2 = aalloc([16, 32], F32)
    p6mark = AR.off
    S.dma("pool", lambda e: e.dma_start(out=wout, in_=wout_d.rearrange("(c p) n -> p c n", p=128)), w=["wout"])
    m1b = aalloc([D], F32)
    xsc2 = aalloc([D], F32)
    M1S = [m1, m1b]
    XLD = [xin[1], xin[2]]
    XSC = [xin[0], xsc2]

    def p5b_stage1(c):
        q = c % 2
        xl, xlk = XLD[q], f"xld{q}"
        mm_, mk_ = M1S[q], f"m1s{q}"
        xs_, xsk = XSC[q], f"xsc{q}"
        S.dma("sp", lambda e: e.dma_start(out=xl[:], in_=x_ext[c * 128:(c + 1) * 128, :]), w=[xlk])
        for nb_ in range(2):
            cs_ = slice(nb_ * 512, (nb_ + 1) * 512)
            po_ = k.ps()
            for ch in range(8):
                S.pe(lambda e, ch=ch: e.matmul(PS[po_][:, :], lhsT=h2T[:, ch, c * 128:(c + 1) * 128], rhs=wout[:, ch, cs_], start=(ch == 0), stop=(ch == 7)),
                     r=[("h2T", c), "wout"], w=[("ps", po_)])
            S.dve(lambda e: e.tensor_tensor(out=mm_[:, cs_], in0=PS[po_][:, :], in1=gbb[:, cs_], op=ALU.mult), r=[("ps", po_), "gbb"], w=[(mk_, nb_)])
            S.pool(lambda e: e.tensor_tensor(out=x1[:, c, cs_], in0=mm_[:, cs_], in1=xl[:, cs_], op=ALU.add), r=[(mk_, nb_), xlk], w=[("x1", c, nb_)])
        S.act(lambda e: e.activation(out=junk[:], in_=x1[:, c, :], func=AF.Square, accum_out=ssq3[:, c:c + 1]), r=[("x1", c, 0), ("x1", c, 1)], w=["junk", ("ssq3", c)])
        S.act(lambda e: e.activation(out=rstd[:, c:c + 1], in_=ssq3[:, c:c + 1], func=AF.Sqrt, scale=1.0 / D, bias=epsb[:, 0:1]), r=[("ssq3", c)], w=[("rstd3", c)])
        S.pool(lambda e: e.tensor_tensor(out=rstd[:, c:c + 1], in0=rstd[:, c:c + 1], in1=onesc[:, 0:1], op=ALU.pow), r=[("rstd3", c)], w=[("rstd3", c)])
        S.dve(lambda e: e.tensor_scalar(out=xs_[:] if q == 0 else xs_, in0=x1[:, c, :], scalar1=rstd[:, c:c + 1], scalar2=None, op0=ALU.mult),
              r=[("x1", c, 0), ("x1", c, 1), ("rstd3", c)], w=[xsk])

    def p5b_stage2(c):
        q = c % 2
        xs_, xsk = XSC[q], f"xsc{q}"
        pf1, pf2 = k.ps(), k.ps()
        for ch in range(8):
            pp = pf1 if ch < 4 else pf2
            S.pe(lambda e, pp=pp, ch=ch: e.transpose(out=PS[pp][:, (ch % 4) * 128:(ch % 4 + 1) * 128], in_=xs_[:, ch * 128:(ch + 1) * 128], identity=ident_f[:]),
                 r=[xsk, "ident_f"], w=[("ps", pp)])
        for ch in range(8):
            pp = pf1 if ch < 4 else pf2
            S.act(lambda e, pp=pp, ch=ch: e.activation(out=h2f[:, ch, :], in_=PS[pp][:, (ch % 4) * 128:(ch % 4 + 1) * 128], func=AF.Identity,
                                                       scale=A2[:, ch:ch + 1], bias=modT2[:, 24 + ch, 0:1]), r=[("ps", pp), "A2", "modT2"], w=[("h2f", ch)])
        S.pool(lambda e: e.tensor_copy(out=h2T[:, :, c * 128:(c + 1) * 128], in_=h2f), r=[("h2f", ch) for ch in range(8)], w=[("h2T", c)])
        pr_ = k.ps()
        for ch in range(8):
            S.pe(lambda e, ch=ch: e.matmul(PS[pr_][:, 0:36], lhsT=h2f[:, ch, :], rhs=wrt[:, ch, :], start=(ch == 0), stop=(ch == 7)),
                 r=[("h2f", ch), "wrt"], w=[("ps", pr_)])
        S.dve(lambda e: e.tensor_tensor(out=rla[:, c, :], in0=PS[pr_][:, 0:36], in1=brt, op=ALU.add), r=[("ps", pr_), "brt"], w=[("rla", c)])

    p5b_stage1(0)
    for c in range(1, 16):
        p5b_stage1(c)
        p5b_stage2(c - 1)
    p5b_stage2(15)
    RLA = [("rla", c) for c in range(16)]
    Gv = rla[:, :, 0:4]
    Ev = rla[:, :, 4:36]
    bc4 = lambda v: v.unsqueeze(2).to_broadcast([128, 16, 4])
    bc32 = lambda v: v.unsqueeze(2).to_broadcast([128, 16, 32])
    S.dve(lambda e: e.tensor_reduce(out=rv[:, 0, :], in_=Gv, axis=AX.X, op=ALU.max), r=RLA, w=["rv0"])
    S.dve(lambda e: e.tensor_tensor(out=oh4, in0=Gv, in1=bc4(rv[:, 0, :]), op=ALU.is_equal), r=RLA + ["rv0"], w=["oh4"])
    S.dve(lambda e: e.tensor_tensor(out=ge4, in0=Gv, in1=bc4(rv[:, 0, :]), op=ALU.subtract), r=RLA + ["rv0"], w=["ge4"])
    S.act(lambda e: e.activation(out=ge4, in_=ge4, func=AF.Exp), r=["ge4"], w=["ge4"])
    S.dve(lambda e: e.tensor_reduce(out=rv[:, 1, :], in_=ge4, axis=AX.X, op=ALU.add), r=["ge4"], w=["rv1"])
    S.dve(lambda e: e.tensor_scalar(out=oh4, in0=oh4, scalar1=-1.0, scalar2=1e30, op0=ALU.add, op1=ALU.mult), r=["oh4"], w=["oh4"])
    S.dve(lambda e: e.tensor_tensor(out=Em.rearrange("p t (g x) -> p t g x", g=4), in0=Ev.rearrange("p t (g x) -> p t g x", g=4),
                                    in1=oh4.unsqueeze(3).to_broadcast([128, 16, 4, 8]), op=ALU.add), r=RLA + ["oh4"], w=["Em"])
    S.dve(lambda e: e.tensor_reduce(out=rv[:, 2, :], in_=Em, axis=AX.X, op=ALU.max), r=["Em"], w=["rv2"])
    S.dve(lambda e: e.tensor_tensor(out=eq1, in0=Em, in1=bc32(rv[:, 2, :]), op=ALU.is_equal), r=["Em", "rv2"], w=["eq1"])
    S.dve(lambda e: e.scalar_tensor_tensor(out=Em, in0=eq1, scalar=-1e30, in1=Em, op0=ALU.mult, op1=ALU.add), r=["eq1", "Em"], w=["Em"])
    S.dve(lambda e: e.tensor_reduce(out=rv[:, 3, :], in_=Em, axis=AX.X, op=ALU.max), r=["Em"], w=["rv3"])
    S.dve(lambda e: e.tensor_tensor(out=eq2, in0=Em, in1=bc32(rv[:, 3, :]), op=ALU.is_equal), r=["Em", "rv3"], w=["eq2"])
    S.dve(lambda e: e.tensor_tensor(out=rv[:, 4, :], in0=rv[:, 3, :], in1=rv[:, 2, :], op=ALU.subtract), r=["rv2", "rv3"], w=["rv4"])
    S.act(lambda e: e.activation(out=rv[:, 4, :], in_=rv[:, 4, :], func=AF.Exp), r=["rv4"], w=["rv4"])
    S.dve(lambda e: e.tensor_scalar(out=rv[:, 5, :], in0=rv[:, 4, :], scalar1=1.0, scalar2=None, op0=ALU.add), r=["rv4"], w=["rv5"])
    S.dve(lambda e: e.tensor_tensor(out=rv[:, 5, :], in0=rv[:, 5, :], in1=rv[:, 1, :], op=ALU.mult), r=["rv5", "rv1"], w=["rv5"])
    S.pool(lambda e: e.tensor_tensor(out=rv[:, 5, :], in0=rv[:, 5, :], in1=onesc[:, 0:1].to_broadcast([128, 16]), op=ALU.pow), r=["rv5", "onesc"], w=["rv5"])
    S.dve(lambda e: e.tensor_tensor(out=rv[:, 6, :], in0=rv[:, 5, :], in1=rv[:, 4, :], op=ALU.mult), r=["rv5", "rv4"], w=["rv6"])
    S.dve(lambda e: e.tensor_tensor(out=eq1, in0=eq1, in1=bc32(rv[:, 5, :]), op=ALU.mult), r=["eq1", "rv5"], w=["eq1"])
    S.dve(lambda e: e.tensor_tensor(out=eq2, in0=eq2, in1=bc32(rv[:, 6, :]), op=ALU.mult), r=["eq2", "rv6"], w=["eq2"])
    S.dve(lambda e: e.tensor_tensor(out=gates, in0=eq1, in1=eq2, op=ALU.add), r=["eq1", "eq2"], w=[("gates", c) for c in range(16)])

    S.barrier()
    AR.off = p5mark
    w13a = aalloc([2, 8, 512], BF16)
    w13 = [w13a, wst[0][:].bitcast(BF16).rearrange("p a b -> p (a b)").rearrange("p (j c n) -> p j c n", j=2, c=8)]
    w2b1 = aalloc([4, D], BF16)
    w2b = [w2b1, hTt[0][:].rearrange("p a b -> p (a b)").rearrange("p (c n) -> p c n", c=4)]
    m1 = aalloc([D], F32)
    aT = aalloc([4, 512], BF16)
    hs1 = aalloc([512], F32)
    NE = int(os.environ.get("NEXPERTS", "32"))
    def load_expert(ex):
        q = ex % 2
        S.dma("pool", lambda e: e.dma_start(out=w13[q][:, 0, :, :], in_=w1_d[ex].rearrange("(c p) n -> p c n", p=128)), w=[("w13", q)])
        S.dma("pool", lambda e: e.dma_start(out=w13[q][:, 1, :, :], in_=w3_d[ex].rearrange("(c p) n -> p c n", p=128)), w=[("w13", q)])
        S.dma("pool", lambda e: e.dma_start(out=w2b[q], in_=w2_d[ex].rearrange("(c p) n -> p c n", p=128)), w=[("w2b", q)])

    load_expert(0)
    for ex in range(NE):
        q = ex % 2
        if ex + 1 < NE:
            load_expert(ex + 1)
        for tb in range(4):
            for fc in range(4):
                p1_, p3_ = k.ps(), k.ps()
                for dc in range(8):
                    S.pe(lambda e, p1_=p1_, dc=dc, q=q: e.matmul(PS[p1_][:, :], lhsT=w13[q][:, 0, dc, fc * 128:(fc + 1) * 128], rhs=h2T[:, dc, tb * 512:(tb + 1) * 512],
                                                                start=(dc == 0), stop=(dc == 7)), r=[("w13", q)] + [("h2T", tb * 4 + u) for u in range(4)], w=[("ps", p1_)])
                for dc in range(8):
                    S.pe(lambda e, p3_=p3_, dc=dc, q=q: e.matmul(PS[p3_][:, :], lhsT=w13[q][:, 1, dc, fc * 128:(fc + 1) * 128], rhs=h2T[:, dc, tb * 512:(tb + 1) * 512],
                                                                start=(dc == 0), stop=(dc == 7)), r=[("w13", q)] + [("h2T", tb * 4 + u) for u in range(4)], w=[("ps", p3_)])
                S.act(lambda e, p1_=p1_: e.activation(out=hs1, in_=PS[p1_][:, :], func=AF.Silu), r=[("ps", p1_)], w=["hs1"])
                S.dve(lambda e, p3_=p3_, fc=fc: e.tensor_tensor(out=aT[:, fc, :], in0=PS[p3_][:, :], in1=hs1, op=ALU.mult), r=[("ps", p3_), "hs1"], w=[("aT", fc)])
            for tt in range(4):
                c = tb * 4 + tt
                for nb_ in range(2):
                    cs_ = slice(nb_ * 512, (nb_ + 1) * 512)
                    po_ = k.ps()
                    for fc in range(4):
                        S.pe(lambda e, po_=po_, fc=fc, q=q: e.matmul(PS[po_][:, :], lhsT=aT[:, fc, tt * 128:(tt + 1) * 128], rhs=w2b[q][:, fc, cs_],
                                                                    start=(fc == 0), stop=(fc == 3)), r=[("aT", fc) for fc in range(4)] + [("w2b", q)], w=[("ps", po_)])
                    S.dve(lambda e, po_=po_, c=c, ex=ex: e.scalar_tensor_tensor(out=m1[:, cs_], in0=PS[po_][:, :], scalar=gates[:, c, ex:ex + 1], in1=gbb[:, 1024 + nb_ * 512:1024 + (nb_ + 1) * 512],
                                                                               op0=ALU.mult, op1=ALU.mult), r=[("ps", po_), ("gates", c), "gbb"], w=[("m1", nb_)])
                    S.pool(lambda e, c=c: e.tensor_tensor(out=x1[:, c, cs_], in0=x1[:, c, cs_], in1=m1[:, cs_], op=ALU.add), r=[("m1", nb_), ("x1", c, nb_)], w=[("x1", c, nb_)])
    for c in range(16):
        S.dma("sp", lambda e, c=c: e.dma_start(out=y_out[c * 128:(c + 1) * 128, :], in_=x1[:, c, :]), r=[("x1", c, 0), ("x1", c, 1)], w=[("y", c)])
    S.emit()
    k.es.close()
    return nc


def _consts():
    c = {}
    f32 = np.float32
    c["ident"] = np.eye(128, dtype=f32)
    kk = np.arange(128)
    c["maskU"] = (kk[:, None] <= kk[None, :]).astype(f32)
    c["maskL"] = (kk[:, None] >= kk[None, :]).astype(f32)
    P = np.zeros((128, 128), f32)
    for n in range(128):
        if (n % 64) < 32:
            P[n, n + 32] = -1.0
        else:
            P[n, n - 32] = 1.0
    c["permT"] = np.ascontiguousarray(P.T)
    sel = np.zeros((128, 32, 128), f32)
    for kq in range(128):
        sel[kq, kq % 32, :] = 1.0
    c["selc"] = sel.reshape(128, 4096)
    return c


def _na_bias(rpb, hf):
    out = np.full((16, 128, 3, 5, 128), -30000.0, np.float32)
    kidx = np.arange(128)
    for cls in range(3):
        i = cls
        kts = [0, 1, 2, 3, 4] if i < 2 else list(range(i - 2, i + 3))
        qr = 2 * i + kidx // 64
        qc = kidx % 64
        gqr, gqc = (qr, qc) if hf == 0 else (63 - qr, 63 - qc)
        rs = np.clip(gqr - 4, 0, 56)
        cs = np.clip(gqc - 8, 0, 48)
        for rel, kt in enumerate(kts):
            kr = 2 * kt + kidx // 64
            kc = kidx % 64
            gkr, gkc = (kr, kc) if hf == 0 else (63 - kr, 63 - kc)
            ok = ((gkr[:, None] >= rs[None, :]) & (gkr[:, None] <= rs[None, :] + 7) &
                  (gkc[:, None] >= cs[None, :]) & (gkc[:, None] <= cs[None, :] + 15))
            dr = np.clip(gkr[:, None] - gqr[None, :] + 7, 0, 14)
            dc = np.clip(gkc[:, None] - gqc[None, :] + 15, 0, 30)
            vals = rpb[:, dr, dc]
            out[:, :, cls, rel, :] = np.where(ok[None], vals, np.float32(-30000.0))
    return np.ascontiguousarray(out.reshape(16, 128, 3 * 5 * 128))


def _rope_tables(hf):
    i = np.arange(SEQ)
    t = i if hf == 0 else (SEQ - 1 - i)
    row = (t // 64).astype(np.float32)
    col = (t % 64).astype(np.float32)
    inv = (10000.0 ** (-np.arange(0, 64, 2, dtype=np.float32) / 64.0)).astype(np.float32)
    ar = row[None, :] * inv[:, None]
    ac = col[None, :] * inv[:, None]
    ang = np.concatenate([ar, ar, ac, ac], axis=0)
    return np.cos(ang).astype(np.float32), np.sin(ang).astype(np.float32)


def prep_core_inputs(inp, b, hf, consts):
    f32 = np.float32
    m = {}
    x = inp["x"][b]
    ctx = inp["ctx"][b]
    if hf == 1:
        x = x[::-1]
        ctx = ctx[::-1]
    m["x_ext"] = np.ascontiguousarray(x, dtype=f32)
    m["ctx_l"] = np.ascontiguousarray(ctx, dtype=f32)
    m["cvT"] = np.ascontiguousarray(np.stack([inp["c"][b], inp["c_ctx"]], axis=1), dtype=f32)
    m["w_mod"] = np.ascontiguousarray(inp["w_mod"][0], dtype=f32)
    bm = inp["b_mod"][0]
    m["b_modT"] = np.ascontiguousarray(bm.reshape(48, 128).T, dtype=f32)
    m["b_mod_g"] = np.ascontiguousarray(np.concatenate([bm[2048:3072], bm[5120:6144]])[None, :], dtype=f32)
    m["n1wT"] = np.ascontiguousarray(inp["norm1_w"][0].reshape(8, 128).T, dtype=f32)
    m["n2wT"] = np.ascontiguousarray(inp["norm2_w"][0].reshape(8, 128).T, dtype=f32)
    L = 0
    w_in = np.array(inp["w_in"][L], dtype=f32)
    if hf == 1:
        w_in[:, C_DT:C_DT + 64] = np.concatenate([w_in[:, C_DT + 32:C_DT + 64], w_in[:, C_DT:C_DT + 32]], axis=1)
    m["w_in"] = w_in
    p1, p2 = ("f", "b") if hf == 0 else ("b", "f")
    m["dtb12"] = np.concatenate([inp["dt_bias_" + p1][L], inp["dt_bias_" + p2][L]])[None, :].astype(f32)
    m["alog12"] = np.concatenate([inp["a_log_" + p1][L], inp["a_log_" + p2][L]])[None, :].astype(f32)
    m["dskip"] = np.ascontiguousarray(inp["d_skip"][L][None, :], dtype=f32)
    cw = inp["conv_w"][L]
    if hf == 1:
        cw = cw[::-1]
    m["conv_wT"] = np.ascontiguousarray(cw.T.reshape(32, 128, 5).transpose(1, 0, 2), dtype=f32)
    m["conv_bT"] = np.ascontiguousarray(inp["conv_b"][L].reshape(32, 128).T, dtype=f32)
    m["ssd_nwT"] = np.ascontiguousarray(inp["ssd_norm_w"][L].reshape(16, 128).T, dtype=f32)
    m["w_br_ssd"] = np.ascontiguousarray(inp["w_br_ssd"][L], dtype=f32)
    m["w_br_na"] = np.ascontiguousarray(inp["w_br_na"][L], dtype=f32)
    m["w_out"] = np.ascontiguousarray(inp["w_out"][L], dtype=f32)
    m["w_rt36"] = np.ascontiguousarray(np.concatenate([inp["w_grp"][L], inp["w_rt"][L]], axis=1), dtype=f32)
    m["b_rt36"] = np.concatenate([inp["b_grp"][L], inp["b_rt"][L]])[None, :].astype(f32)
    m["w1"] = np.ascontiguousarray(inp["w1"][L], dtype=f32)
    m["w3"] = np.ascontiguousarray(inp["w3"][L], dtype=f32)
    m["w2"] = np.ascontiguousarray(inp["w2"][L], dtype=f32)
    m["qkw"] = np.concatenate([np.tile(inp["q_norm_w"][L], 8) * 0.125, np.tile(inp["k_norm_w"][L], 8)])[None, :].astype(f32)
    m["nabias"] = _na_bias(inp["rpb"][L], hf)
    cs, sn = _rope_tables(hf)
    m["cosT"], m["sinT"] = cs, sn
    m.update(consts)
    return m


def kernel(**inputs):
    inp = {k_: np.asarray(v) for k_, v in inputs.items()}
    nc = build("")
    consts = _consts()
    shared = {}
    maps = []
    for c in range(8):
        m = prep_core_inputs(inp, c // 2, c % 2, consts)
        maps.append(m)
    res = run_bass_kernel_spmd(nc, maps, core_ids=list(range(8)))
    out = np.zeros((4, SEQ, D), dtype=np.float32)
    for c in range(8):
        b, hf = c // 2, c % 2
        y = np.asarray(res.results[c]["y"], dtype=np.float32)
        if hf == 0:
            out[b, :OWN] = y
        else:
            out[b, OWN:] = y[::-1]
    return out
```

```python
import os
import contextlib
import numpy as np
import ml_dtypes
import concourse.bass as bass
import concourse.mybir as mybir
from concourse.bass_utils import run_bass_kernel_spmd

F32 = mybir.dt.float32
BF16 = mybir.dt.bfloat16
AF = mybir.ActivationFunctionType
ALU = mybir.AluOpType
AX = mybir.AxisListType

D = 1024
SEQ = 4096
OWN = 2048
CTX = 256
NTOK = CTX + SEQ
D_SSD = 2048
D_XBC = 4096
D_IN = 11328
C_Z, C_XBC, C_DT, C_QKV, C_G = 0, 2048, 6144, 6208, 9280
EPS = 1e-6
NEXP = 32
DEBUG = os.environ.get("KDEBUG", "")


class _Rec:
    def __getattr__(self, name):
        def f(*a, **kw):
            self.call = (name, a, kw)
            return self
        return f


class Sched:
    def __init__(self, nc, es, n_dma_sems=10, epoch=20000):
        self.nc = nc
        self.es = es
        self.eng = {"pe": nc.tensor, "act": nc.scalar, "dve": nc.vector, "pool": nc.gpsimd, "sp": nc.sync}
        self.ops = []
        self.last_w = {}
        self.readers = {}
        self.n_dma_sems = n_dma_sems
        self.epoch = epoch
        self._semc = 0
        self.bar = set()

    def barrier(self):
        last = {}
        dmas = {}
        for i, op in enumerate(self.ops):
            if op["dma"]:
                dmas.setdefault(op["eng"], []).append(i)
            else:
                last[op["eng"]] = i
        b = set(last.values())
        for q, l in dmas.items():
            b |= set(l[-self.n_dma_sems:])
        self.bar = b

    def _sem(self, name):
        self._semc += 1
        return self.es.enter_context(self.nc.semaphore(f"{name}_{self._semc}"))

    def add(self, eng, fn, r=(), w=(), dma=False):
        idx = len(self.ops)
        raw = set()
        oth = set()
        for k in r:
            lw = self.last_w.get(k)
            if lw is not None:
                raw.add(lw)
        for k in w:
            lw = self.last_w.get(k)
            if lw is not None:
                oth.add(lw)
            for rd in self.readers.get(k, ()):
                oth.add(rd)
        for k in r:
            self.readers.setdefault(k, []).append(idx)
        for k in w:
            self.last_w[k] = idx
            self.readers[k] = []
        raw |= self.bar
        raw.discard(idx)
        oth.discard(idx)
        rec = _Rec()
        fn(rec)
        self.ops.append(dict(eng=eng, call=rec.call, raw=raw, oth=oth - raw, dma=dma, signal=False))
        return idx

    def pe(self, fn, r=(), w=()):
        return self.add("pe", fn, r, w)

    def act(self, fn, r=(), w=()):
        return self.add("act", fn, r, w)

    def dve(self, fn, r=(), w=()):
        return self.add("dve", fn, r, w)

    def pool(self, fn, r=(), w=()):
        return self.add("pool", fn, r, w)

    def dma(self, q, fn, r=(), w=()):
        return self.add(q, fn, r, w, dma=True)

    def emit(self):
        ops = self.ops
        for i, op in enumerate(ops):
            deps = set()
            for p in op["raw"]:
                P = ops[p]
                if (not P["dma"]) and (not op["dma"]) and P["eng"] == op["eng"] and op["eng"] == "pe":
                    continue
                deps.add(p)
            for p in op["oth"]:
                P = ops[p]
                if (not P["dma"]) and (not op["dma"]) and P["eng"] == op["eng"] and op["eng"] == "pe":
                    continue
                deps.add(p)
            op["deps"] = deps
            for p in deps:
                ops[p]["signal"] = True
        dma_count, dma_sems, ring_prev = {}, {}, {}
        for i, op in enumerate(ops):
            if op["dma"]:
                q = op["eng"]
                k = dma_count.get(q, 0)
                dma_count[q] = k + 1
                if q not in dma_sems:
                    dma_sems[q] = [self._sem(f"dma_{q}") for _ in range(self.n_dma_sems)]
                slot = k % self.n_dma_sems
                op["sem"] = dma_sems[q][slot]
                op["val"] = 16 * (k // self.n_dma_sems + 1)
                prev = ring_prev.get((q, slot))
                if prev is not None:
                    op["deps"].add(prev)
                ring_prev[(q, slot)] = i
                op["signal"] = True
        cnt, eng_sems = {}, {}
        for i, op in enumerate(ops):
            if op["dma"] or not op["signal"]:
                continue
            e = op["eng"]
            c = cnt.get(e, 0)
            ep = c // self.epoch
            if (e, ep) not in eng_sems:
                eng_sems[(e, ep)] = self._sem(f"s_{e}{ep}")
            op["sem"] = eng_sems[(e, ep)]
            op["val"] = c % self.epoch + 1
            cnt[e] = c + 1
        waited = {}
        nwaits = 0
        for i, op in enumerate(ops):
            e = op["eng"]
            h = self.eng[e]
            need = {}
            for p in op["deps"]:
                P = ops[p]
                s = P["sem"]
                key = id(s)
                if key not in need or need[key][1] < P["val"]:
                    need[key] = (s, P["val"])
            for key, (s, v) in need.items():
                if waited.get((e, key), 0) >= v:
                    continue
                h.wait_ge(s, v)
                nwaits += 1
                waited[(e, key)] = v
            name, a, kw = op["call"]
            ins = getattr(h, name)(*a, **kw)
            if op["signal"]:
                ins.then_inc(op["sem"], 16 if op["dma"] else 1)
        h = self.eng["sp"]
        last = {}
        for op in ops:
            if op["dma"]:
                last[id(op["sem"])] = (op["sem"], op["val"])
        for s, v in last.values():
            h.wait_ge(s, v)
        counts = {}
        for op in ops:
            counts[op["eng"]] = counts.get(op["eng"], 0) + 1
        print("sched: ops", len(ops), counts, "waits", nwaits, "sems", self._semc, flush=True)


class K:
    def __init__(self):
        self.nc = bass.Bass("TRN2", target_bir_lowering=False)
        self.es = contextlib.ExitStack()
        self.S = Sched(self.nc, self.es)
        self.psum = []
        self.ps_rr = 0

    def din(self, name, shape, dt=F32):
        return self.nc.dram_tensor(name, list(shape), dt, kind="ExternalInput").ap()

    def dout(self, name, shape, dt=F32):
        return self.nc.dram_tensor(name, list(shape), dt, kind="ExternalOutput").ap()

    def dscr(self, name, shape, dt):
        return self.nc.dram_tensor(name, list(shape), dt, kind="Internal").ap()

    def sb(self, name, shape, dt=F32):
        return self.es.enter_context(self.nc.sbuf_tensor(name, list(shape), dt))

    def init_psum(self):
        for i in range(8):
            self.psum.append(self.es.enter_context(self.nc.psum_tensor(f"ps{i}", [128, 512], F32)))

    def ps(self):
        i = self.ps_rr
        self.ps_rr = (self.ps_rr + 1) % 8
        return i


def chunks(n, c):
    return [(i, min(c, n - i)) for i in range(0, n, c)]


def build(debug=""):
    k = K()
    nc, S = k.nc, k.S
    dbg = set(debug.split(",")) if debug else set()

    def scr(name, shape, dt):
        if name in dbg:
            return k.dout(name, shape, dt)
        return k.dscr(name, shape, dt)

    x_ext = k.din("x_ext", [SEQ, D])
    ctx_l = k.din("ctx_l", [CTX, D])
    cvT = k.din("cvT", [D, 2])
    w_mod = k.din("w_mod", [D, 6 * D])
    b_modT = k.din("b_modT", [128, 48])
    b_mod_g = k.din("b_mod_g", [1, 2048])
    n1wT = k.din("n1wT", [128, 8])
    n2wT = k.din("n2wT", [128, 8])
    ident_d = k.din("ident", [128, 128])
    y_out = k.dout("y", [OWN, D])
    hT_d = scr("hT_d", [128, 8, NTOK], BF16)

    k.init_psum()
    PS = k.psum

    def psb(i):
        return PS[i].bitcast(BF16)

    ident_f = k.sb("ident_f", [128, 128], F32)
    ident_b = k.sb("ident_b", [128, 128], BF16)
    S.dma("sp", lambda e: e.dma_start(out=ident_f[:], in_=ident_d[:, :]), w=["ident_f"])
    S.dma("pool", lambda e: e.dma_start(out=ident_b[:], in_=ident_d[:, :]), w=["ident_b"])

    ARENA_BYTES = 155 * 1024
    arena = k.sb("arena", [128, ARENA_BYTES // 2], BF16)

    class AR:
        off = 0

    def aalloc(shape, dt):
        n = int(np.prod(shape))
        nb = n * (4 if dt == F32 else 2)
        nb_al = (nb + 63) // 64 * 64
        assert AR.off + nb_al <= ARENA_BYTES, ("arena overflow", AR.off, nb_al)
        v = arena[:, AR.off // 2:(AR.off + nb) // 2]
        if dt == F32:
            v = v.bitcast(F32)
        AR.off += nb_al
        if len(shape) == 2:
            v = v.rearrange("p (a b) -> p a b", a=shape[0])
        elif len(shape) == 3:
            v = v.rearrange("p (a b c) -> p a b c", a=shape[0], b=shape[1])
        elif len(shape) == 4:
            v = v.rearrange("p (a b c d) -> p a b c d", a=shape[0], b=shape[1], c=shape[2])
        return v

    cv = k.sb("cv", [128, 8, 2], F32)
    siluc = k.sb("siluc", [128, 8, 2], F32)
    bmodT = k.sb("bmodT", [128, 48], F32)
    n1w = k.sb("n1w", [128, 8], F32)
    n2w = k.sb("n2w", [128, 8], F32)
    modT2 = k.sb("modT2", [128, 48, 2], F32)
    sel0 = k.sb("sel0", [2, 128], F32)
    A1 = k.sb("A1", [128, 8, 2], F32)
    A2 = k.sb("A2", [128, 8], F32)
    wst = [k.sb("wst0", [128, 8, 512], F32)]

    S.dve(lambda e: e.memset(modT2[:], 0.0), w=["modT2"])
    S.dma("sp", lambda e: e.dma_start(out=cv[:], in_=cvT.rearrange("(c p) r -> p c r", p=128)), w=["cv"])
    S.dma("sp", lambda e: e.dma_start(out=bmodT[:], in_=b_modT[:, :]), w=["bmodT"])
    S.dma("sp", lambda e: e.dma_start(out=n1w[:], in_=n1wT[:, :]), w=["n1w"])
    S.dma("sp", lambda e: e.dma_start(out=n2w[:], in_=n2wT[:, :]), w=["n2w"])
    S.act(lambda e: e.activation(out=siluc[:], in_=cv[:], func=AF.Silu), r=["cv"], w=["siluc"])
    epsb = k.sb("epsb", [128, 1], F32)
    S.dve(lambda e: e.memset(epsb[:], EPS), w=["epsb"])
    onesc = k.sb("onesc", [128, 1], F32)
    S.dve(lambda e: e.memset(onesc[:], -1.0), w=["onesc"])
    S.dve(lambda e: e.memset(sel0[:], 0.0), w=["sel0"])
    S.dve(lambda e: e.memset(sel0[0:1, :], 1.0), r=["sel0"], w=["sel0"])

    ROWBLK = {4: (0, 0), 5: (0, 512), 10: (1, 0), 11: (1, 512)}
    xin = [k.sb(f"xin{i}", [128, D], F32) for i in range(3)]
    gb_d = k.dscr("gb_d", [2, 2048], F32)
    wst1 = arena[:, 0:8192].bitcast(F32).rearrange("p (a b) -> p a b", a=8)
    grow = [arena[:, 8192 + i_ * 2048:8192 + (i_ + 1) * 2048].bitcast(F32) for i_ in range(2)]
    gbias = arena[:, 12288:14336].bitcast(F32)
    wsts = [wst[0], wst1]

    def mod_block(blk):
        wb = wsts[blk % 2]
        wk = f"wstage{blk % 2}"
        S.dma("sp", lambda e: e.dma_start(out=wb[:, :, :], in_=w_mod[:, blk * 512:(blk + 1) * 512].rearrange("(c p) n -> p c n", p=128)), w=[wk])
        if blk in ROWBLK:
            xi, off = ROWBLK[blk]
            pi = k.ps()
            for dc in range(8):
                S.pe(lambda e, dc=dc: e.matmul(PS[pi][0:2, :], lhsT=siluc[:, dc, :], rhs=wb[:, dc, :], start=(dc == 0), stop=(dc == 7)),
                     r=[wk, "siluc"], w=[("ps", pi)])
            S.act(lambda e: e.activation(out=grow[xi][0:2, off:off + 512], in_=PS[pi][0:2, :], func=AF.Copy), r=[("ps", pi)], w=[("grow", xi)])
        else:
            pi = k.ps()
            for jj in range(4):
                for dc in range(8):
                    S.pe(lambda e, dc=dc, jj=jj: e.matmul(PS[pi][:, jj * 2:jj * 2 + 2], lhsT=wb[:, dc, jj * 128:(jj + 1) * 128], rhs=siluc[:, dc, :],
                                                          start=(dc == 0), stop=(dc == 7)), r=[wk, "siluc"], w=[("ps", pi)])
            for jj in range(4):
                ch = blk * 4 + jj
                S.dve(lambda e, jj=jj, ch=ch: e.tensor_scalar(out=modT2[:, ch, :], in0=PS[pi][:, jj * 2:jj * 2 + 2], scalar1=bmodT[:, ch:ch + 1], scalar2=None,
                                                              op0=ALU.add), r=[("ps", pi), "bmodT"], w=["modT2"])

    def mod_rest():
        for blk in range(4, 12):
            mod_block(blk)
        for xi in range(2):
            S.dma("sp", lambda e, xi=xi: e.dma_start(out=gbias[0:2, :], in_=b_mod_g[0:1, xi * 1024:(xi + 1) * 1024].to_broadcast([2, 1024])), w=["gbias"])
            S.dve(lambda e, xi=xi: e.tensor_tensor(out=grow[xi][0:2, :], in0=grow[xi][0:2, :], in1=gbias[0:2, :], op=ALU.add),
                  r=[("grow", xi), "gbias"], w=[("grow", xi)])
            S.dma("sp", lambda e, xi=xi: e.dma_start(out=gb_d[0:2, xi * 1024:(xi + 1) * 1024], in_=grow[xi][0:2, :]), r=[("grow", xi)], w=[("gb_d", xi)])
        S.dve(lambda e: e.tensor_scalar(out=A2[:], in0=modT2[:, 32:40, 0], scalar1=1.0, scalar2=None, op0=ALU.add), r=["modT2"], w=["A2"])
        S.dve(lambda e: e.tensor_tensor(out=A2[:], in0=A2[:], in1=n2w[:], op=ALU.mult), r=["A2", "n2w"], w=["A2"])

    for blk in range(4):
        mod_block(blk)
    S.dve(lambda e: e.tensor_scalar(out=A1[:], in0=modT2[:, 8:16, :], scalar1=1.0, scalar2=None, op0=ALU.add),
          r=["modT2"], w=["A1"])
    S.dve(lambda e: e.tensor_tensor(out=A1[:], in0=A1[:], in1=n1w[:].unsqueeze(2).to_broadcast([128, 8, 2]), op=ALU.mult),
          r=["A1", "n1w"], w=["A1"])

    junk = k.sb("junk", [128, D], BF16)
    xn = [k.sb(f"xn{i}", [128, D], BF16) for i in range(2)]
    xn3 = [xn[0], xn[1], arena[:, 14336:15360]]
    ssq = k.sb("ssq", [128, 34], F32)
    rstd = k.sb("rstd", [128, 34], F32)
    hTt = [k.sb(f"hTt{i}", [128, 8, 512], BF16) for i in range(2)]
    groups = [(0, 2)] + [(2 + 4 * i, 4) for i in range(8)]
    P1L = float(os.environ.get("P1L", "9"))
    P1E = os.environ.get("P1E", "act")
    groups = groups[:int(os.environ.get("P1G", "99"))]
    tiles_ = []
    for gi, (t0, nt) in enumerate(groups):
        for tt in range(nt):
            tiles_.append((gi, t0, nt, tt))
    p1ps = {}

    def p1_stage1(n):
        gi, t0, nt, tt = tiles_[n]
        t = t0 + tt
        isctx = t < 2
        src = ctx_l[t * 128:(t + 1) * 128, :] if isctx else x_ext[(t - 2) * 128:(t - 1) * 128, :]
        xb, xk = xin[t % 3], f"xin{t % 3}"
        nb, nk = xn3[t % 3], f"xn{t % 3}"
        S.dma("sp", lambda e: e.dma_start(out=xb[:], in_=src), w=[xk])
        S.act(lambda e: e.activation(out=junk[:], in_=xb[:], func=AF.Square, accum_out=ssq[:, t:t + 1]), r=[xk], w=["junk", ("ssq", t)])
        S.act(lambda e: e.activation(out=rstd[:, t:t + 1], in_=ssq[:, t:t + 1], func=AF.Sqrt, scale=1.0 / D, bias=epsb[:, 0:1]),
              r=[("ssq", t), "epsb"], w=[("rstd", t)])
        S.pool(lambda e: e.tensor_tensor(out=rstd[:, t:t + 1], in0=rstd[:, t:t + 1], in1=onesc[:, 0:1], op=ALU.pow),
               r=[("rstd", t), "onesc"], w=[("rstd", t)])
        S.dve(lambda e: e.tensor_scalar(out=nb[:], in0=xb[:], scalar1=rstd[:, t:t + 1], scalar2=None, op0=ALU.mult), r=[xk, ("rstd", t)], w=[nk])
        pi = k.ps()
        p1ps[n] = pi
        for dc in range(8):
            S.pe(lambda e, dc=dc: e.transpose(out=psb(pi)[:, dc * 128:(dc + 1) * 128], in_=nb[:, dc * 128:(dc + 1) * 128], identity=ident_b[:]),
                 r=[nk, "ident_b"], w=[("ps", pi)])

    def p1_stage2(n):
        gi, t0, nt, tt = tiles_[n]
        t = t0 + tt
        ci = 1 if t < 2 else 0
        hb_ = hTt[gi % 2]
        hk = f"hTt{gi % 2}"
        pi = p1ps[n]
        for dc in range(8):
            S.act(lambda e, dc=dc: e.activation(out=hb_[:, dc, tt * 128:(tt + 1) * 128], in_=psb(pi)[:, dc * 128:(dc + 1) * 128], func=AF.Identity,
                                                scale=A1[:, dc, ci:ci + 1], bias=modT2[:, dc, ci:ci + 1]),
                  r=[("ps", pi), "A1", "modT2"], w=[(hk, tt, dc)])
        if tt == nt - 1:
            col0 = t0 * 128
            ncol = nt * 128
            allk = [(hk, tt_, dc) for tt_ in range(nt) for dc in range(8)]
            S.dma("sp", lambda e: e.dma_start(out=hT_d[:, :, col0:col0 + ncol], in_=hb_[:, :, 0:ncol]), r=allk, w=[("hT_d", gi)])

    P1LEAD = 2
    for n in range(len(tiles_) + P1LEAD):
        if n < len(tiles_):
            p1_stage1(n)
        if n >= P1LEAD:
            p1_stage2(n - P1LEAD)

    mod_rest()
    S.barrier()
    w_in = k.din("w_in", [D, D_IN])
    maskU_d = k.din("maskU", [128, 128])
    maskL_d = k.din("maskL", [128, 128])
    permT_d = k.din("permT", [128, 128])
    selc_d = k.din("selc", [128, 4096])
    cosT_d = k.din("cosT", [128, SEQ])
    sinT_d = k.din("sinT", [128, SEQ])
    dtb12_d = k.din("dtb12", [1, 64])
    alog12_d = k.din("alog12", [1, 64])
    dskip_d = k.din("dskip", [1, 32])
    convwT_d = k.din("conv_wT", [128, 32, 5])
    convbT_d = k.din("conv_bT", [128, 32])
    ssdnwT_d = k.din("ssd_nwT", [128, 16])
    ygT_d = scr("ygT_d", [128, 16, OWN], BF16)
    ssq2_d = scr("ssq2_d", [128, 16], F32) if "ssq2_d" in dbg else None

    hb = hTt
    blocks = [(0, 256)] + [(256 + 512 * i, 512) for i in range(8)]

    def load_hb(i, col0, ncol):
        S.dma("sp", lambda e: e.dma_start(out=hb[i][:, :, 0:ncol], in_=hT_d[:, :, col0:col0 + ncol]), w=[("hb", i)])

    rstd_ssd = aalloc([16], F32)
    mhalf = aalloc([1], F32)
    pmark = AR.off
    maskU = aalloc([128], F32)
    maskL = aalloc([128], F32)
    ones_f = aalloc([128], F32)
    permT = aalloc([128], BF16)
    selb = aalloc([32, 128], BF16)
    cosT = aalloc([SEQ], BF16)
    sinT = aalloc([SEQ], BF16)
    dtb_b = aalloc([64], F32)
    a_b = aalloc([64], F32)
    dsk_b = aalloc([32], F32)
    convw = aalloc([32, 5], F32)
    convb = aalloc([32], F32)
    ssdnw = aalloc([16], F32)
    one_b = aalloc([1], F32)
    S.dma("sp", lambda e: e.dma_start(out=maskU, in_=maskU_d[:, :]), w=["maskU"])
    S.dma("sp", lambda e: e.dma_start(out=maskL, in_=maskL_d[:, :]), w=["maskL"])
    S.dma("pool", lambda e: e.dma_start(out=permT, in_=permT_d[:, :]), w=["permT"])
    S.dma("pool", lambda e: e.dma_start(out=selb.rearrange("p a b -> p (a b)"), in_=selc_d[:, :]), w=["selb"])
    S.dma("pool", lambda e: e.dma_start(out=cosT, in_=cosT_d[:, :]), w=["cosT"])
    S.dma("pool", lambda e: e.dma_start(out=sinT, in_=sinT_d[:, :]), w=["sinT"])
    S.dma("sp", lambda e: e.dma_start(out=dtb_b, in_=dtb12_d[0:1, :].to_broadcast([128, 64])), w=["dtb_b"])
    S.dma("sp", lambda e: e.dma_start(out=a_b, in_=alog12_d[0:1, :].to_broadcast([128, 64])), w=["a_b"])
    S.dma("sp", lambda e: e.dma_start(out=dsk_b, in_=dskip_d[0:1, :].to_broadcast([128, 32])), w=["dsk_b"])
    S.dma("sp", lambda e: e.dma_start(out=convw, in_=convwT_d[:, :, :]), w=["convw"])
    S.dma("sp", lambda e: e.dma_start(out=convb, in_=convbT_d[:, :]), w=["convb"])
    S.dma("sp", lambda e: e.dma_start(out=ssdnw, in_=ssdnwT_d[:, :]), w=["ssdnw"])
    S.dve(lambda e: e.memset(ones_f, 1.0), w=["ones_f"])
    S.dve(lambda e: e.memset(one_b, 1.0), w=["one_b"])
    S.act(lambda e: e.activation(out=a_b, in_=a_b, func=AF.Exp), r=["a_b"], w=["a_b"])
    S.dve(lambda e: e.tensor_scalar(out=a_b, in0=a_b, scalar1=-1.0, scalar2=None, op0=ALU.mult), r=["a_b"], w=["a_b"])

    NT = 34

    def TK(name, lo=0, hi=NT):
        return [(name, t) for t in range(lo, hi)]

    wS = aalloc([NT, 64], F32)
    eat = aalloc([NT, 64], F32)
    acO = aalloc([16, 64], F32)
    bYo = aalloc([16, 64], F32)
    ea = aalloc([16, 64], F32)
    acT = aalloc([16, 128], BF16)
    p2mark = AR.off
    biasY = aalloc([NT, 64], F32)
    acat = aalloc([NT, 128], F32)
    dta = aalloc([NT, 64], F32)
    achl = aalloc([NT, 2, 2, 32], BF16)
    wdt = aalloc([8, 64], BF16)
    S.dma("pool", lambda e: e.dma_start(out=wdt, in_=w_in[:, C_DT:C_DT + 64].rearrange("(c p) n -> p c n", p=128)), w=["wdt"])
    for bi, (col0, ncol) in enumerate(blocks):
        load_hb(bi % 2, col0, ncol)
        for tt in range(ncol // 128):
            t = col0 // 128 + tt
            pi = k.ps()
            for dc in range(8):
                S.pe(lambda e, pi=pi, dc=dc, tt=tt, bi=bi: e.matmul(PS[pi][:, 0:64], lhsT=hb[bi % 2][:, dc, tt * 128:(tt + 1) * 128],
                                                                   rhs=wdt[:, dc, :], start=(dc == 0), stop=(dc == 7)),
                     r=[("hb", bi % 2), "wdt"], w=[("ps", pi)])
            S.dve(lambda e, pi=pi, t=t: e.tensor_tensor(out=wS[:, t, :], in0=PS[pi][:, 0:64], in1=dtb_b, op=ALU.add),
                  r=[("ps", pi), "dtb_b"], w=[("wS", t)])
    S.act(lambda e: e.activation(out=wS, in_=wS, func=AF.Exp), r=TK("wS"), w=TK("wS"))
    S.act(lambda e: e.activation(out=wS, in_=wS, func=AF.Ln, bias=one_b[:, 0:1]), r=TK("wS") + ["one_b"], w=TK("wS"))
    S.act(lambda e: e.activation(out=biasY, in_=wS, func=AF.Ln), r=TK("wS"), w=TK("biasY"))
    S.dve(lambda e: e.tensor_tensor(out=dta, in0=wS, in1=a_b.unsqueeze(1).to_broadcast([128, NT, 64]), op=ALU.mult),
          r=TK("wS") + ["a_b"], w=TK("dta"))
    for t in range(NT):
        pi = k.ps()
        S.pe(lambda e, pi=pi, t=t: e.matmul(PS[pi][:, 0:32], lhsT=maskU, rhs=dta[:, t, 0:32], start=True, stop=True),
             r=[("dta", t), "maskU"], w=[("ps", pi)])
        S.pe(lambda e, pi=pi, t=t: e.matmul(PS[pi][:, 32:64], lhsT=maskL, rhs=dta[:, t, 32:64], start=True, stop=True),
             r=[("dta", t), "maskL"], w=[("ps", pi)])
        S.pe(lambda e, pi=pi, t=t: e.matmul(PS[pi][:, 64:128], lhsT=ones_f, rhs=dta[:, t, 0:64], start=True, stop=True),
             r=[("dta", t), "ones_f"], w=[("ps", pi)])
        S.act(lambda e, pi=pi, t=t: e.activation(out=acat[:, t, :], in_=PS[pi][:, 0:128], func=AF.Copy),
              r=[("ps", pi)], w=[("acat", t)])
    acv = acat[:, :, 0:64].rearrange("p t (d h) -> p t d h", d=2)
    tmpv = dta.rearrange("p t (d h) -> p t d h", d=2)
    S.dve(lambda e: e.tensor_copy(out=achl[:, :, :, 0, :], in_=acv), r=TK("acat"), w=["achl"])
    S.dve(lambda e: e.tensor_copy(out=tmpv, in_=achl[:, :, :, 0, :]), r=["achl"] + TK("dta"), w=TK("dta"))
    S.dve(lambda e: e.tensor_tensor(out=achl[:, :, :, 1, :], in0=acv, in1=tmpv, op=ALU.subtract),
          r=TK("acat") + TK("dta") + ["achl"], w=["achl"])
    S.dve(lambda e: e.tensor_tensor(out=acv, in0=tmpv, in1=achl[:, :, :, 1, :], op=ALU.add),
          r=TK("dta") + ["achl"], w=TK("acat"))
    S.dve(lambda e: e.tensor_tensor(out=biasY, in0=acat[:, :, 0:64], in1=biasY, op=ALU.subtract),
          r=TK("acat") + TK("biasY"), w=TK("biasY"))
    S.dve(lambda e: e.tensor_tensor(out=wS, in0=acat[:, :, 64:128], in1=biasY, op=ALU.subtract),
          r=TK("acat") + TK("biasY") + TK("wS"), w=TK("wS"))
    S.act(lambda e: e.activation(out=wS, in_=wS, func=AF.Exp), r=TK("wS"), w=TK("wS"))
    S.act(lambda e: e.activation(out=eat, in_=acat[:, :, 64:128], func=AF.Exp), r=TK("acat"), w=["eat"])
    S.act(lambda e: e.activation(out=ea, in_=acat[:, 2:18, 0:64], func=AF.Exp), r=TK("acat"), w=["ea"])
    S.dve(lambda e: e.tensor_copy(out=acO, in_=acat[:, 2:18, 0:64]), r=TK("acat"), w=["acO"])
    S.dve(lambda e: e.tensor_copy(out=bYo, in_=biasY[:, 2:18, :]), r=TK("biasY"), w=["bYo"])
    for c in range(16):
        pi = k.ps()
        S.pe(lambda e, pi=pi, c=c: e.transpose(out=psb(pi)[:, 0:128], in_=achl[:, c + 2].rearrange("p d x h -> p (d x h)"),
                                               identity=ident_b[:]), r=["achl", "ident_b"], w=[("ps", pi)])
        S.act(lambda e, pi=pi, c=c: e.activation(out=acT[:, c, :], in_=psb(pi)[:, 0:128], func=AF.Copy),
              r=[("ps", pi)], w=[("acT", c)])
    if "stageP2" in dbg:
        d1 = k.dout("dbg_wS", [128, NT * 64], F32)
        d2 = k.dout("dbg_acat", [128, NT * 128], F32)
        d5 = k.dout("dbg_eat", [128, NT * 64], F32)
        S.dma("sp", lambda e: e.dma_start(out=d5[:, :], in_=eat.rearrange("p a b -> p (a b)")), r=["eat"])
        d3 = k.dout("dbg_biasY", [128, NT * 64], F32)
        d4 = k.dout("dbg_acT", [128, 16 * 128], BF16)
        S.dma("sp", lambda e: e.dma_start(out=d1[:, :], in_=wS.rearrange("p a b -> p (a b)")), r=TK("wS"))
        S.dma("sp", lambda e: e.dma_start(out=d2[:, :], in_=acat.rearrange("p a b -> p (a b)")), r=TK("acat"))
        S.dma("sp", lambda e: e.dma_start(out=d3[:, :], in_=biasY.rearrange("p a b -> p (a b)")), r=TK("biasY"))
        S.dma("sp", lambda e: e.dma_start(out=d4[:, :], in_=acT.rearrange("p a b -> p (a b)")), r=[("acT", c) for c in range(16)])
        S.emit()
        k.es.close()
        return nc
    S.barrier()
    AR.off = p2mark
    WT = 4360
    preA = aalloc([WT], BF16)
    preB = aalloc([WT], BF16)
    postB = aalloc([WT], BF16)
    postC = aalloc([2824], BF16)
    xs_tok = aalloc([18, 256], BF16)
    B_tok = aalloc([18, 128], BF16)
    y_acc = aalloc([16, 256], F32)
    wgrp = aalloc([8, 768], BF16)
    Sst = [aalloc([256], F32) for _ in range(2)]
    Sbf = [aalloc([256], BF16) for _ in range(2)]
    ssq2 = aalloc([16, 8], F32)
    negbY = aalloc([16, 64], F32)
    xsp = [aalloc([256], BF16) for _ in range(2)]
    Bp = [aalloc([128], BF16) for _ in range(2)]
    ygst = [aalloc([2, 512], BF16) for _ in range(2)]
    p3mark = AR.off
    accA = wst[0][:, :, :].rearrange("p a b -> p (a b)")
    accP = xin[2]
    T1 = [[xin[0][:, 0:512], xin[0][:, 512:1024]], [xin[1][:, 0:512], xin[1][:, 512:1024]]]
    T1K = [["x0a", "x0s1"], ["x1a", "x1s1"]]
    ROPE1 = xin[0][:, 0:512]
    ROPE2 = xin[1][:, 0:512]
    T2 = [aalloc([256], F32) for _ in range(2)]
    T2K = ["t2_0", "t2_1"]
    ZS = aalloc([256], F32)
    YG = aalloc([256], F32)
    DEC = [[xn[0][:, 0:512], aalloc([512], BF16)], [xn[1][:, 0:512], aalloc([512], BF16)]]
    DECK = [["n0a", "dec01"], ["n1a", "dec11"]]
    MTt = [[xn[0][:, 512:1024], aalloc([512], BF16)], [xn[1][:, 512:1024], aalloc([512], BF16)]]
    MTK = [["n0b", "mt01"], ["n1b", "mt11"]]
    XDD = [junk[:, 0:256], junk[:, 256:512]]
    XDDK = ["j0", "j1"]
    CBM = [[junk[:, 512:640], aalloc([128], BF16)], [junk[:, 640:768], aalloc([128], BF16)]]
    CBMK = [["j2", "cbm01"], ["j3", "cbm11"]]
    YGB = junk[:, 768:1024]
    S.pool(lambda e: e.memset(preA, 0.0), w=["preA"] + [("preA", r_) for r_ in range(10)])
    S.pool(lambda e: e.memset(preB, 0.0), w=["preB"] + [("preB", r_) for r_ in range(10)])
    S.dve(lambda e: e.memset(mhalf, -0.5), w=["mhalf"])
    NM = [maskU.bitcast(BF16)[:, 0:128], maskL.bitcast(BF16)[:, 0:128]]
    for d_, (mk_, mname) in enumerate(((maskU, "maskU"), (maskL, "maskL"))):
        S.dve(lambda e, mk_=mk_: e.tensor_scalar(out=T2[0][:, 0:128], in0=mk_, scalar1=-1.0, scalar2=30000.0, op0=ALU.add, op1=ALU.mult),
              r=[mname], w=["t2_0"])
        S.dve(lambda e, d_=d_: e.tensor_copy(out=NM[d_], in_=T2[0][:, 0:128]), r=["t2_0", mname], w=[mname, "negmask"])
    S.dve(lambda e: e.memset(ssq2, 0.0), w=[("ssq2", c, g) for c in range(16) for g in range(8)])
    NBH = negbY.bitcast(BF16)
    XT = xin[0][:].rearrange("p (a b) -> p a b", a=16)
    XT2 = xin[1][:].rearrange("p (a b) -> p a b", a=16)
    S.dve(lambda e: e.tensor_scalar(out=XT, in0=bYo, scalar1=-1.0, scalar2=None, op0=ALU.mult), r=["bYo"], w=["x0a", "x0s1"])
    S.dve(lambda e: e.tensor_copy(out=NBH[:, :, 0:64], in_=XT), r=["x0a", "x0s1"], w=["negbY"])
    S.dve(lambda e: e.tensor_copy(out=XT2, in_=NBH[:, :, 0:64]), r=["negbY"], w=["x1a", "x1s1"])
    S.dve(lambda e: e.tensor_tensor(out=NBH[:, :, 64:128], in0=XT, in1=XT2, op=ALU.subtract), r=["x0a", "x0s1", "x1a", "x1s1", "negbY"], w=["negbY"])

    def bcol(t):
        return 2 + t * 128 if t < 2 else 262 + (t - 2) * 128

    def RK(name):
        return [name] + [(name, r_) for r_ in range(10)]

    W0B = wst[0][:].bitcast(BF16).rearrange("p a b -> p (a b)")
    preX = W0B[:, 0:4360]
    preC = W0B[:, 4368:4368 + 2824]
    X2B = xin[2][:].bitcast(BF16)
    DGS = [W0B[:, 7232:7232 + 640].rearrange("p (a b) -> p a b", a=5)] + \
          [X2B[:, i_ * 640:(i_ + 1) * 640].rearrange("p (a b) -> p a b", a=5) for i_ in range(3)]
    DGN = [0]
    S.pool(lambda e: e.memset(preX, 0.0), w=RK("preX"))
    S.pool(lambda e: e.memset(preC, 0.0), w=RK("preC"))

    def conv_silu(src, srck, dst, dstk, chg, lo, hi):
        dgi = DGN[0] % 4
        DGN[0] += 1
        DG = DGS[dgi]
        dgk = ("diag", dgi)
        for kk in range(5):
            S.dve(lambda e, kk=kk: e.tensor_scalar(out=DG[:, kk, :], in0=ident_b[:], scalar1=convw[:, chg, kk:kk + 1], scalar2=None, op0=ALU.mult),
                  r=["ident_b", "convw"], w=[dgk])
        blks = chunks(hi - lo, 512)
        pis = {}

        def mm(bi_):
            o, m = blks[bi_]
            pi = k.ps()
            pis[bi_] = pi
            rk = [(srck, r_) for r_ in (bi_ - 1, bi_, bi_ + 1) if 0 <= r_ < len(blks)]
            for kk in range(5):
                S.pe(lambda e, pi=pi, kk=kk, o=o, m=m: e.matmul(PS[pi][:, 0:m], lhsT=DG[:, kk, :], rhs=src[:, lo + o - 2 + kk:lo + o - 2 + kk + m],
                                                                start=(kk == 0), stop=(kk == 4)), r=rk + [dgk], w=[("ps", pi)])

        def ev(bi_):
            o, m = blks[bi_]
            pi = pis[bi_]
            S.act(lambda e, pi=pi, o=o, m=m: e.activation(out=dst[:, lo + o:lo + o + m], in_=PS[pi][:, 0:m], func=AF.Silu, bias=convb[:, chg:chg + 1]),
                  r=[("ps", pi), "convb"], w=[(dstk, bi_)])

        mm(0)
        for bi_ in range(1, len(blks)):
            mm(bi_)
            ev(bi_ - 1)
        ev(len(blks) - 1)

    NG = int(os.environ.get("P3G", "8"))

    def load_w_xbc(g_):
        for (c0, n, o) in ((C_XBC + 256 * g_, 256, 0), (C_XBC + 2048 + 128 * g_, 128, 256), (C_XBC + 3072 + 128 * g_, 128, 384)):
            S.dma("pool", lambda e, c0=c0, n=n, o=o: e.dma_start(out=wgrp[:, :, o:o + n],
                                                               in_=w_in[:, c0:c0 + n].rearrange("(c p) n -> p c n", p=128)), w=["wgrp_x"])

    def load_w_z(g_):
        c0 = C_Z + 256 * g_
        S.dma("pool", lambda e: e.dma_start(out=wgrp[:, :, 512:768], in_=w_in[:, c0:c0 + 256].rearrange("(c p) n -> p c n", p=128)), w=["wgrp_z"])

    def gating_tile(gq, ob, tt, bi):
        c = ob * 4 + tt
        st = ygst[ob % 2]
        stk = ("ygst", ob % 2)
        pz = k.ps()
        for dc in range(8):
            S.pe(lambda e, pz=pz, dc=dc: e.matmul(PS[pz][:, 0:256], lhsT=hb[bi % 2][:, dc, tt * 128:(tt + 1) * 128],
                                                  rhs=wgrp[:, dc, 512:768], start=(dc == 0), stop=(dc == 7)),
                 r=[("hb", bi % 2), "wgrp_z"], w=[("ps", pz)])
        sl = c % 2
        zsb, zsk = (ZS, "zs") if sl == 0 else (YG, "yg")
        ygb, ygk = (YGB, "j4") if sl == 0 else (DEC[0][0][:, 0:256], "n0a")
        sqo, sqk = (MTt[0][0][:, 0:256], "n0b") if sl == 0 else (MTt[0][0][:, 256:512], "n0b2")
        S.act(lambda e: e.activation(out=zsb, in_=PS[pz][:, 0:256], func=AF.Silu), r=[("ps", pz)], w=[zsk])
        S.dve(lambda e: e.tensor_tensor(out=ygb, in0=y_acc[:, c, :], in1=zsb, op=ALU.mult), r=[("y_acc", c), zsk], w=[ygk])
        S.act(lambda e: e.activation(out=sqo, in_=ygb, func=AF.Square, accum_out=ssq2[:, c, gq:gq + 1]), r=[ygk], w=[sqk, ("ssq2", c, gq)])
        pt = k.ps()
        for j in range(2):
            S.pe(lambda e, j=j: e.transpose(out=psb(pt)[:, j * 128:(j + 1) * 128], in_=ygb[:, j * 128:(j + 1) * 128], identity=ident_b[:]),
                 r=[ygk, "ident_b"], w=[("ps", pt)])
        for j in range(2):
            S.act(lambda e, j=j: e.activation(out=st[:, j, tt * 128:(tt + 1) * 128], in_=psb(pt)[:, j * 128:(j + 1) * 128],
                                              func=AF.Copy, scale=ssdnw[:, 2 * gq + j:2 * gq + j + 1]),
                  r=[("ps", pt), "ssdnw"], w=[stk])
        if tt == 3:
            S.dma("sp", lambda e: e.dma_start(out=ygT_d[:, 2 * gq:2 * gq + 2, ob * 512:(ob + 1) * 512], in_=st), r=[stk], w=[("ygT_d", gq, ob)])

    load_w_xbc(0)
    for g in range(NG):
        hd0 = 4 * g
        PRE = ((preX, "preX", 256), (preC, "preC", 384), (preA, "preA", 0), (preB, "preB", 128))
        for bi, (col0, ncol) in enumerate(blocks):
            load_hb(bi % 2, col0, ncol)
            for j, (pre, prek, wo) in enumerate(PRE):
                if j == 1 and not (256 <= col0 < 256 + 2560):
                    continue
                pi = k.ps()
                for dc in range(8):
                    S.pe(lambda e, pi=pi, dc=dc, wo=wo, bi=bi, ncol=ncol: e.matmul(
                        PS[pi][:, 0:ncol], lhsT=wgrp[:, dc, wo:wo + 128], rhs=hb[bi % 2][:, dc, 0:ncol], start=(dc == 0), stop=(dc == 7)),
                        r=[("hb", bi % 2), "wgrp_x"], w=[("ps", pi)])
                dcol = col0 + 2 if col0 < 256 else col0 + 6
                if (bi + j) % 2 == 0:
                    S.act(lambda e, pi=pi, pre=pre, dcol=dcol, ncol=ncol: e.activation(out=pre[:, dcol:dcol + ncol], in_=PS[pi][:, 0:ncol], func=AF.Copy),
                          r=[("ps", pi)], w=RK(prek))
                else:
                    S.dve(lambda e, pi=pi, pre=pre, dcol=dcol, ncol=ncol: e.tensor_copy(out=pre[:, dcol:dcol + ncol], in_=PS[pi][:, 0:ncol]),
                          r=[("ps", pi)], w=RK(prek))
            if g > 0 and 1 <= bi <= 4:
                for tt in range(4):
                    gating_tile(g - 1, bi - 1, tt, bi)
        if g + 1 < NG:
            load_w_xbc(g + 1)
        load_w_z(g)
        conv_silu(preX, "preX", postB, "postB", 16 + g, 2, 4358)
        conv_silu(preC, "preC", postC, "postC", 24 + g, 262, 262 + 2050)
        conv_silu(preA, "preA", preA, "preA", 2 * g, 2, 4358)
        conv_silu(preB, "preB", preB, "preB", 2 * g + 1, 2, 4358)
        for (buf, bk, nblk) in ((postB, "postB", 8), (postC, "postC", 4)):
            for rb in range(nblk):
                c0 = 262 + rb * 512
                e0 = rb * 512
                pi = k.ps()
                S.pe(lambda e, pi=pi, buf=buf, c0=c0: e.matmul(PS[pi][:, :], lhsT=permT, rhs=buf[:, c0:c0 + 512], start=True, stop=True),
                     r=RK(bk) + ["permT"], w=[("ps", pi)])
                S.dve(lambda e, pi=pi, e0=e0: e.tensor_tensor(out=ROPE1, in0=PS[pi][:, :], in1=sinT[:, e0:e0 + 512], op=ALU.mult),
                      r=[("ps", pi), "sinT"], w=["x0a"])
                S.pool(lambda e, buf=buf, c0=c0, e0=e0: e.tensor_tensor(out=ROPE2, in0=buf[:, c0:c0 + 512], in1=cosT[:, e0:e0 + 512], op=ALU.mult),
                       r=RK(bk) + ["cosT"], w=["x1a"])
                S.dve(lambda e, buf=buf, c0=c0: e.tensor_tensor(out=buf[:, c0:c0 + 512], in0=ROPE1, in1=ROPE2, op=ALU.add),
                      r=["x0a", "x1a"] + RK(bk), w=RK(bk))
        for t in range(18):
            pi = k.ps()
            S.pe(lambda e, pi=pi, t=t: e.transpose(out=psb(pi)[:, 0:128], in_=postB[:, bcol(t):bcol(t) + 128], identity=ident_b[:]),
                 r=RK("postB") + ["ident_b"], w=[("ps", pi)])
            S.dve(lambda e, pi=pi, t=t: e.tensor_copy(out=B_tok[:, t, :], in_=psb(pi)[:, 0:128]), r=[("ps", pi)], w=[("B_tok", t)])
        for t in range(18):
            pi = k.ps()
            for j, (pre, prek) in enumerate(((preA, "preA"), (preB, "preB"))):
                S.pe(lambda e, pi=pi, j=j, pre=pre, t=t: e.transpose(out=psb(pi)[:, j * 128:(j + 1) * 128], in_=pre[:, bcol(t):bcol(t) + 128],
                                                                    identity=ident_b[:]), r=RK(prek) + ["ident_b"], w=[("ps", pi)])
            S.act(lambda e, pi=pi, t=t: e.activation(out=xs_tok[:, t, :], in_=psb(pi)[:, 0:256], func=AF.Copy),
                  r=[("ps", pi)], w=[("xs_tok", t)])

        for d in range(2):
            S.dve(lambda e, d=d: e.memset(Sst[d], 0.0), w=[("Sst", d)])
            S.dve(lambda e, d=d: e.memset(Sbf[d], 0.0), w=[("Sbf", d)])

        SB2 = [[Sbf[0], xsp[0]], [Sbf[1], xsp[1]]]
        SB2K = [[("Sbf", 0), ("xsp", 0)], [("Sbf", 1), ("xsp", 1)]]

        def state_update(d, xs_ap, xs_k, B_ap, B_k, tg, oslot=0, xslot=None, copy=True, split=None):
            hs = d * 32 + hd0
            xq = d if xslot is None else xslot
            if split != "back":
              S.dve(lambda e: e.tensor_tensor(out=XDD[xq].rearrange("p (h q) -> p h q", h=4), in0=xs_ap.rearrange("p (h q) -> p h q", h=4),
                                             in1=wS[:, tg, hs:hs + 4].unsqueeze(2).to_broadcast([128, 4, 64]), op=ALU.mult),
                   r=[xs_k, ("wS", tg)], w=[XDDK[xq]])
            if split == "front":
                return
            pi = k.ps()
            S.pe(lambda e, pi=pi: e.matmul(PS[pi][:, 0:256], lhsT=B_ap, rhs=XDD[xq], start=True, stop=True),
                 r=[B_k, XDDK[xq]], w=[("ps", pi)])
            S.dve(lambda e: e.tensor_tensor(out=Sst[d].rearrange("p (h q) -> p h q", h=4), in0=Sst[d].rearrange("p (h q) -> p h q", h=4),
                                            in1=eat[:, tg, hs:hs + 4].unsqueeze(2).to_broadcast([128, 4, 64]), op=ALU.mult),
                  r=[("Sst", d), "eat"], w=[("Sst", d)])
            S.dve(lambda e, pi=pi: e.tensor_tensor(out=Sst[d], in0=PS[pi][:, 0:256], in1=Sst[d], op=ALU.add),
                  r=[("ps", pi), ("Sst", d)], w=[("Sst", d)])
            if copy:
                S.act(lambda e: e.activation(out=SB2[d][oslot], in_=Sst[d], func=AF.Copy), r=[("Sst", d)], w=[SB2K[d][oslot]])

        def front(d, c, sl):
            tg = c + 2
            hs = d * 32 + hd0
            cc = bcol(tg)
            pi = k.ps()
            S.pe(lambda e, pi=pi: e.matmul(PS[pi][:, 0:128], lhsT=postB[:, cc:cc + 128], rhs=postC[:, cc:cc + 128], start=True, stop=True),
                 r=RK("postB") + RK("postC"), w=[("ps", pi)])
            S.act(lambda e, pi=pi: e.activation(out=CBM[d][sl], in_=PS[pi][:, 0:128], func=AF.Copy), r=[("ps", pi)], w=[CBMK[d][sl]])
            pb = k.ps()
            S.pe(lambda e, pb=pb: e.matmul(PS[pb][:, :].rearrange("p (h q) -> p h q", h=4), lhsT=ident_b[:],
                                           rhs=NM[d].unsqueeze(1).to_broadcast([128, 4, 128]), start=True, stop=False),
                 r=["ident_b", "negmask"], w=[("ps", pb)])
            for part in range(2):
                S.pe(lambda e, pb=pb, part=part: e.matmul(PS[pb][:, :].rearrange("p (h q) -> p h q", h=4), lhsT=ident_b[:],
                                                          rhs=NBH[:, c, part * 64 + hs:part * 64 + hs + 4].unsqueeze(2).to_broadcast([128, 4, 128]),
                                                          start=False, stop=False), r=["ident_b", "negbY"], w=[("ps", pb)])
            for hh in range(4):
                S.pe(lambda e, pb=pb, hh=hh: e.matmul(PS[pb][:, hh * 128:(hh + 1) * 128], lhsT=selb[d * 64:(d + 1) * 64, hd0 + hh, :],
                                                      rhs=acT[d * 64:(d + 1) * 64, c, :], start=False, stop=(hh == 3)),
                     r=["selb", ("acT", c)], w=[("ps", pb)])
            S.act(lambda e, pb=pb: e.activation(out=DEC[d][sl], in_=PS[pb][:, :], func=AF.Exp), r=[("ps", pb)], w=[DECK[d][sl]])
            S.dve(lambda e: e.tensor_tensor(out=MTt[d][sl].rearrange("p (h q) -> p h q", h=4), in0=DEC[d][sl].rearrange("p (h q) -> p h q", h=4),
                                             in1=CBM[d][sl].unsqueeze(1).to_broadcast([128, 4, 128]), op=ALU.mult),
                   r=[DECK[d][sl], CBMK[d][sl]], w=[MTK[d][sl]])

        def back(d, c, sl, islot=0):
            tg = c + 2
            hs = d * 32 + hd0
            cc = bcol(tg)
            py = k.ps()
            for hh in range(4):
                S.pe(lambda e, py=py, hh=hh: e.matmul(PS[py][:, hh * 64:(hh + 1) * 64], lhsT=MTt[d][sl][:, hh * 128:(hh + 1) * 128],
                                                      rhs=xs_tok[:, tg, hh * 64:(hh + 1) * 64], start=True, stop=True),
                     r=[MTK[d][sl], ("xs_tok", tg)], w=[("ps", py)])
            S.pe(lambda e, py=py: e.matmul(PS[py][:, 256:512], lhsT=postC[:, cc:cc + 128], rhs=SB2[d][islot], start=True, stop=True),
                 r=RK("postC") + [SB2K[d][islot]], w=[("ps", py)])
            S.dve(lambda e, py=py: e.tensor_tensor(out=T2[d].rearrange("p (h q) -> p h q", h=4), in0=PS[py][:, 256:512].rearrange("p (h q) -> p h q", h=4),
                                                   in1=ea[:, c, hs:hs + 4].unsqueeze(2).to_broadcast([128, 4, 64]), op=ALU.mult),
                  r=[("ps", py), "ea"], w=[T2K[d]])
            S.pool(lambda e: e.tensor_tensor(out=y_acc[:, c, :], in0=y_acc[:, c, :], in1=T2[d], op=ALU.add),
                   r=[T2K[d], ("y_acc", c)], w=[("y_acc", c)])
            S.dve(lambda e, py=py: e.tensor_tensor(out=y_acc[:, c, :], in0=PS[py][:, 0:256], in1=y_acc[:, c, :], op=ALU.add),
                  r=[("ps", py), ("y_acc", c)], w=[("y_acc", c)])

        for c in range(16):
            S.pool(lambda e, c=c: e.tensor_tensor(out=y_acc[:, c, :].rearrange("p (h q) -> p h q", h=4),
                                                  in0=xs_tok[:, c + 2, :].rearrange("p (h q) -> p h q", h=4),
                                                  in1=dsk_b[:, hd0:hd0 + 4].unsqueeze(2).to_broadcast([128, 4, 64]), op=ALU.mult),
                   r=[("xs_tok", c + 2), "dsk_b"], w=[("y_acc", c)])
        for t in (1, 0):
            state_update(1, xs_tok[:, t, :], ("xs_tok", t), B_tok[:, t, :], ("B_tok", t), t)
        plist = list(range(33, 17, -1))

        def pfront(t):
            q = t % 2
            pi = k.ps()
            for j, (pre, prek) in enumerate(((preA, "preA"), (preB, "preB"))):
                S.pe(lambda e, j=j, pre=pre: e.transpose(out=psb(pi)[:, j * 128:(j + 1) * 128], in_=pre[:, bcol(t):bcol(t) + 128],
                                                         identity=ident_b[:]), r=RK(prek) + ["ident_b"], w=[("ps", pi)])
            S.pe(lambda e: e.transpose(out=psb(pi)[:, 256:384], in_=postB[:, bcol(t):bcol(t) + 128], identity=ident_b[:]),
                 r=RK("postB") + ["ident_b"], w=[("ps", pi)])
            S.act(lambda e: e.activation(out=xsp[q], in_=psb(pi)[:, 0:256], func=AF.Copy), r=[("ps", pi)], w=[("xsp", q)])
            S.act(lambda e: e.activation(out=Bp[q], in_=psb(pi)[:, 256:384], func=AF.Copy), r=[("ps", pi)], w=[("Bp", q)])
            state_update(1, xsp[q], ("xsp", q), Bp[q], ("Bp", q), t, xslot=q, split="front")

        def pback(t, last):
            q = t % 2
            state_update(1, xsp[q], ("xsp", q), Bp[q], ("Bp", q), t, xslot=q, split="back", copy=last)

        pfront(plist[0])
        for n_ in range(len(plist)):
            if n_ + 1 < len(plist):
                pfront(plist[n_ + 1])
            pback(plist[n_], n_ == len(plist) - 1)
        for t in (0, 1):
            state_update(0, xs_tok[:, t, :], ("xs_tok", t), B_tok[:, t, :], ("B_tok", t), t)
        NWARM = int(os.environ.get("NWARM", "24"))
        if NWARM:
            pw = k.ps()
            for _w in range(NWARM):
                S.pe(lambda e: e.matmul(PS[pw][:, :], lhsT=ident_b[:], rhs=postB[:, 262:262 + 512], start=True, stop=True),
                     r=RK("postB") + ["ident_b"], w=[("ps", pw)])
        for i in range(17):
            if i >= 1:
                c1, c2 = i - 1, 16 - i
                j = i - 1
                if c1 < 15:
                    state_update(0, xs_tok[:, c1 + 2, :], ("xs_tok", c1 + 2), B_tok[:, c1 + 2, :], ("B_tok", c1 + 2), c1 + 2, oslot=(j + 1) % 2, split="front")
                if c2 > 0:
                    state_update(1, xs_tok[:, c2 + 2, :], ("xs_tok", c2 + 2), B_tok[:, c2 + 2, :], ("B_tok", c2 + 2), c2 + 2, oslot=(j + 1) % 2, split="front")
            if i < 16:
                front(0, i, i % 2)
                front(1, 15 - i, i % 2)
            if i >= 1:
                if c1 < 15:
                    state_update(0, xs_tok[:, c1 + 2, :], ("xs_tok", c1 + 2), B_tok[:, c1 + 2, :], ("B_tok", c1 + 2), c1 + 2, oslot=(j + 1) % 2, split="back")
                if c2 > 0:
                    state_update(1, xs_tok[:, c2 + 2, :], ("xs_tok", c2 + 2), B_tok[:, c2 + 2, :], ("B_tok", c2 + 2), c2 + 2, oslot=(j + 1) % 2, split="back")
                back(0, c1, (i - 1) % 2, islot=j % 2)
                back(1, c2, (i - 1) % 2, islot=j % 2)

    for ob in range(4 if NG >= 1 else 0):
        bi = ob + 1
        col0, ncol = blocks[bi]
        load_hb(bi % 2, col0, ncol)
        for tt in range(4):
            gating_tile(NG - 1, ob, tt, bi)

    if NG == 8:
        S.dve(lambda e: e.tensor_reduce(out=rstd_ssd, in_=ssq2, axis=AX.X, op=ALU.add), r=[("ssq2", c, g) for c in range(16) for g in range(8)], w=["rstd_ssd"])
        S.dve(lambda e: e.tensor_scalar(out=rstd_ssd, in0=rstd_ssd, scalar1=1.0 / D_SSD, scalar2=EPS, op0=ALU.mult, op1=ALU.add), r=["rstd_ssd"], w=["rstd_ssd"])
        S.pool(lambda e: e.tensor_tensor(out=rstd_ssd, in0=rstd_ssd, in1=mhalf[:, 0:1].to_broadcast([128, 16]), op=ALU.pow), r=["rstd_ssd", "mhalf"], w=["rstd_ssd"])
    if "stageP3" in dbg:
        dssq = k.dout("dbg_ssq2", [128, 128], F32)
        S.dma("sp", lambda e: e.dma_start(out=dssq[:, :], in_=ssq2.rearrange("p a b -> p (a b)")), r=[("ssq2", c, g) for c in range(16) for g in range(NG)])
        S.emit()
        k.es.close()
        return nc
    S.barrier()
    AR.off = pmark
    NKT = 20
    nabias_d = k.din("nabias", [16, 128, 3 * 5 * 128])
    qkw_d = k.din("qkw", [1, 1024])
    ynaT_d = scr("ynaT_d", [128, 8, OWN], BF16)
    wq = aalloc([3, 8, 512], BF16)
    qT = aalloc([4, OWN], BF16)
    kT = aalloc([4, NKT * 128], BF16)
    Vaug = aalloc([NKT, 8, 65], BF16)
    qkw = aalloc([1024], F32)
    Ef = aalloc([15 * 128], F32)
    Eb = aalloc([15 * 128], BF16)
    PTs = [aalloc([7 * 128], BF16) for _ in range(3)]
    NSL = 5
    sqbs = [aalloc([512], F32) for _ in range(NSL)]
    nrms = [aalloc([512], F32) for _ in range(NSL)]
    qtks = [aalloc([512], BF16) for _ in range(NSL)]
    ss8s = [aalloc([8], F32) for _ in range(NSL)]
    denall = aalloc([16, 8], F32)
    ynatok = aalloc([16, 512], BF16)
    ynst = [aalloc([4, 512], BF16) for _ in range(2)]
    S.dma("sp", lambda e: e.dma_start(out=qkw, in_=qkw_d[0:1, :].to_broadcast([128, 1024])), w=["qkw"])
    S.dve(lambda e: e.memset(Vaug, 1.0), w=["Vaug"])

    def ktile_cols(kt):
        return kt * 128 if kt < 2 else 256 + (kt - 2) * 128

    for hp in range(2):
        for j3 in range(3 if hp == 0 else 0):
            c0 = C_QKV + j3 * 1024 + hp * 512
            S.dma("pool", lambda e, j3=j3, c0=c0: e.dma_start(out=wq[:, j3, :, :], in_=w_in[:, c0:c0 + 512].rearrange("(c p) n -> p c n", p=128)), w=["wq"])
        kblocks = [(0, 256)] + [(256 + 512 * i, 512) for i in range(5)]
        items = []
        for bi, (col0, ncol) in enumerate(kblocks):
            nt_ = 2 if bi == 5 else ncol // 128
            for tt in range(nt_):
                kt = col0 // 128 + tt
                own = 2 <= kt < 18
                for j3 in ((0, 1, 2) if own else (1, 2)):
                    items.append((bi, col0, ncol, tt, kt, j3, tt == 0 and j3 == (0 if own else 1)))
        stA = {}

        def stageA(n):
            bi, col0, ncol, tt, kt, j3, first = items[n]
            if first:
                load_hb(bi % 2, col0, ncol)
            sl = n % NSL
            pi = k.ps()
            for dc in range(8):
                S.pe(lambda e, dc=dc: e.matmul(PS[pi][:, :], lhsT=hb[bi % 2][:, dc, tt * 128:(tt + 1) * 128],
                                               rhs=wq[:, j3, dc, :], start=(dc == 0), stop=(dc == 7)),
                     r=[("hb", bi % 2), "wq"], w=[("ps", pi)])
            stA[n] = pi
            if j3 == 2:
                S.act(lambda e: e.activation(out=Vaug[:, kt, :, 0:64], in_=PS[pi][:, :].rearrange("p (h d) -> p h d", h=8), func=AF.Copy),
                      r=[("ps", pi)], w=["Vaug"])
                return
            S.act(lambda e: e.activation(out=sqbs[sl], in_=PS[pi][:, :], func=AF.Square), r=[("ps", pi)], w=[("sqb", sl)])
            S.dve(lambda e: e.tensor_reduce(out=ss8s[sl], in_=sqbs[sl].rearrange("p (h d) -> p h d", h=8), axis=AX.X, op=ALU.add), r=[("sqb", sl)], w=[("ss8", sl)])
            S.dve(lambda e: e.tensor_scalar(out=ss8s[sl], in0=ss8s[sl], scalar1=1.0 / 64, scalar2=EPS, op0=ALU.mult, op1=ALU.add), r=[("ss8", sl)], w=[("ss8", sl)])
            S.pool(lambda e: e.tensor_tensor(out=ss8s[sl], in0=ss8s[sl], in1=mhalf[:, 0:1].to_broadcast([128, 8]), op=ALU.pow), r=[("ss8", sl), "mhalf"], w=[("ss8", sl)])

        def stageB(n):
            bi, col0, ncol, tt, kt, j3, first = items[n]
            if j3 == 2:
                return
            sl = n % NSL
            pi = stA[n]
            S.dve(lambda e: e.tensor_tensor(out=nrms[sl].rearrange("p (h d) -> p h d", h=8), in0=PS[pi][:, :].rearrange("p (h d) -> p h d", h=8),
                                            in1=ss8s[sl].unsqueeze(2).to_broadcast([128, 8, 64]), op=ALU.mult), r=[("ps", pi), ("ss8", sl)], w=[("nrm", sl)])
            S.dve(lambda e: e.tensor_tensor(out=qtks[sl], in0=nrms[sl], in1=qkw[:, j3 * 512:(j3 + 1) * 512], op=ALU.mult), r=[("nrm", sl), "qkw"], w=[("qtk", sl)])
            pt = k.ps()
            for pr in range(4):
                S.pe(lambda e, pr=pr: e.transpose(out=psb(pt)[:, pr * 128:(pr + 1) * 128], in_=qtks[sl][:, pr * 128:(pr + 1) * 128], identity=ident_b[:]),
                     r=[("qtk", sl), "ident_b"], w=[("ps", pt)])
            if j3 == 0:
                c = kt - 2
                S.act(lambda e: e.activation(out=qT[:, :, c * 128:(c + 1) * 128], in_=psb(pt)[:, 0:512].rearrange("p (a b) -> p a b", a=4), func=AF.Copy),
                      r=[("ps", pt)], w=[("qT", c)])
            else:
                S.act(lambda e: e.activation(out=kT[:, :, kt * 128:(kt + 1) * 128], in_=psb(pt)[:, 0:512].rearrange("p (a b) -> p a b", a=4), func=AF.Copy),
                      r=[("ps", pt)], w=[("kT", kt)])

        LEAD = NSL - 1
        for n in range(len(items) + LEAD):
            if n < len(items):
                stageA(n)
            if n >= LEAD:
                stageB(n - LEAD)
        if hp == 0:
            for j3 in range(3):
                c0 = C_QKV + j3 * 1024 + 512
                S.dma("pool", lambda e, j3=j3, c0=c0: e.dma_start(out=wq[:, j3, :, :], in_=w_in[:, c0:c0 + 512].rearrange("(c p) n -> p c n", p=128)), w=["wq"])
        for hl in range(8):
            h = hp * 8 + hl
            pr, po = hl // 2, (hl % 2) * 64
            S.dma("sp", lambda e, h=h: e.dma_start(out=Ef, in_=nabias_d[h, :, :]), w=["Ef"])
            S.act(lambda e: e.activation(out=Eb, in_=Ef, func=AF.Exp), r=["Ef"], w=["Eb"])
            def na_front(i, sl):
                cls = min(i, 2)
                kts = [2 + x for x in ([0, 1, 2, 3, 4] if i < 2 else range(i - 2, i + 3))] + [0, 1]
                pa, pb_ = k.ps(), k.ps()
                for n_, kt in enumerate(kts):
                    dstp = PS[pa][:, n_ * 128:(n_ + 1) * 128] if n_ < 4 else PS[pb_][:, (n_ - 4) * 128:(n_ - 3) * 128]
                    S.pe(lambda e, dstp=dstp, kt=kt, i=i: e.matmul(dstp, lhsT=kT[po:po + 64, pr, kt * 128:(kt + 1) * 128],
                                                                   rhs=qT[po:po + 64, pr, i * 128:(i + 1) * 128], start=True, stop=True),
                         r=[("kT", kt), ("qT", i)], w=[("ps", pa if n_ < 4 else pb_)])
                S.act(lambda e, pa=pa: e.activation(out=PTs[sl][:, 0:512], in_=PS[pa][:, :], func=AF.Exp), r=[("ps", pa)], w=[("PTa", sl)])
                S.act(lambda e, pb_=pb_: e.activation(out=PTs[sl][:, 512:896], in_=PS[pb_][:, 0:384], func=AF.Exp), r=[("ps", pb_)], w=[("PTb", sl)])
                S.dve(lambda e: e.tensor_tensor(out=PTs[sl][:, 0:512], in0=PTs[sl][:, 0:512], in1=Eb[:, cls * 640:cls * 640 + 512], op=ALU.mult),
                      r=[("PTa", sl), "Eb"], w=[("PTa", sl)])
                S.dve(lambda e: e.tensor_tensor(out=PTs[sl][:, 512:640], in0=PTs[sl][:, 512:640], in1=Eb[:, cls * 640 + 512:cls * 640 + 640], op=ALU.mult),
                      r=[("PTb", sl), "Eb"], w=[("PTb", sl)])

            def na_back(i, sl):
                kts = [2 + x for x in ([0, 1, 2, 3, 4] if i < 2 else range(i - 2, i + 3))] + [0, 1]
                po_ = k.ps()
                for n_, kt in enumerate(kts):
                    S.pe(lambda e, po_=po_, n_=n_, kt=kt: e.matmul(PS[po_][:, 0:65], lhsT=PTs[sl][:, n_ * 128:(n_ + 1) * 128],
                                                                 rhs=Vaug[:, kt, hl, :], start=(n_ == 0), stop=(n_ == 6)),
                         r=[("PTa", sl), ("PTb", sl), "Vaug"], w=[("ps", po_)])
                S.dve(lambda e, po_=po_: e.tensor_copy(out=denall[:, i, hl:hl + 1], in_=PS[po_][:, 64:65]), r=[("ps", po_)], w=[("den", i, hl)])
                S.act(lambda e, po_=po_: e.activation(out=ynatok[:, i, hl * 64:(hl + 1) * 64], in_=PS[po_][:, 0:64], func=AF.Copy),
                      r=[("ps", po_)], w=[("ynatok", i, hl)])

            for it in range(18):
                if it < 16:
                    na_front(it, it % 3)
                if it >= 2:
                    na_back(it - 2, (it - 2) % 3)
        allden = [("den", i_, h_) for i_ in range(16) for h_ in range(8)]
        S.pool(lambda e: e.tensor_tensor(out=denall.rearrange("p a b -> p (a b)"), in0=denall.rearrange("p a b -> p (a b)"),
                                         in1=onesc[:, 0:1].to_broadcast([128, 128]), op=ALU.pow), r=allden + ["onesc"], w=["rden"])
        for i in range(16):
            S.dve(lambda e, i=i: e.tensor_tensor(out=ynatok[:, i, :].rearrange("p (h d) -> p h d", h=8), in0=ynatok[:, i, :].rearrange("p (h d) -> p h d", h=8),
                                                 in1=denall[:, i, :].unsqueeze(2).to_broadcast([128, 8, 64]), op=ALU.mult),
                  r=["rden"] + [("ynatok", i, h_) for h_ in range(8)], w=[("ynatok", i)])
        for i in range(16):
            pt = k.ps()
            for pr in range(4):
                S.pe(lambda e, pt=pt, pr=pr, i=i: e.transpose(out=psb(pt)[:, pr * 128:(pr + 1) * 128], in_=ynatok[:, i, pr * 128:(pr + 1) * 128], identity=ident_b[:]),
                     r=[("ynatok", i), "ident_b"], w=[("ps", pt)])
            sq_ = (i // 4) % 2
            st = ynst[sq_]
            S.act(lambda e, pt=pt, st=st, i=i: e.activation(out=st[:, :, (i % 4) * 128:(i % 4 + 1) * 128], in_=psb(pt)[:, 0:512].rearrange("p (a b) -> p a b", a=4), func=AF.Copy),
                  r=[("ps", pt)], w=[("ynst", sq_)])
            if i % 4 == 3:
                S.dma("sp", lambda e, st=st, i=i, hp=hp: e.dma_start(out=ynaT_d[:, hp * 4:hp * 4 + 4, (i // 4) * 512:(i // 4 + 1) * 512], in_=st),
                      r=[("ynst", sq_)], w=[("ynaT_d", hp, i // 4)])
    if "stageP4" in dbg:
        S.emit()
        k.es.close()
        return nc
    S.barrier()
    AR.off = pmark
    wbrs_d = k.din("w_br_ssd", [D_SSD, D])
    wbrn_d = k.din("w_br_na", [D, D])
    wout_d = k.din("w_out", [D, D])
    wrt_d = k.din("w_rt36", [D, 36])
    brt_d = k.din("b_rt36", [1, 36])
    w1_d = k.din("w1", [NEXP, D, 512])
    w3_d = k.din("w3", [NEXP, D, 512])
    w2_d = k.din("w2", [NEXP, 512, D])
    h2T = aalloc([8, OWN], BF16)
    gates = aalloc([16, 32], F32)
    gbb = aalloc([2048], F32)
    wrt = aalloc([8, 36], F32)
    brt = aalloc([36], F32)
    x1 = aalloc([16, D], F32)
    p5mark = AR.off
    AR.off = p5mark - 16 * D * 4
    wbrs = aalloc([16, D], BF16)
    wbrn = aalloc([8, D], BF16)
    wgt = aalloc([8, 2048], BF16)
    ygt = [aalloc([16, 128], BF16) for _ in range(2)]
    ynt_ = [aalloc([8, 128], BF16) for _ in range(2)]
    sg = aalloc([512], F32)
    m1 = aalloc([D], F32)
    mrg = aalloc([D], BF16)
    S.dma("sp", lambda e: e.dma_start(out=gbb, in_=gb_d[0:1, :].to_broadcast([128, 2048])), w=["gbb"])
    S.dma("sp", lambda e: e.dma_start(out=wrt, in_=wrt_d.rearrange("(c p) n -> p c n", p=128)), w=["wrt"])
    S.dma("sp", lambda e: e.dma_start(out=brt, in_=brt_d[0:1, :].to_broadcast([128, 36])), w=["brt"])
    S.dma("pool", lambda e: e.dma_start(out=wbrs, in_=wbrs_d.rearrange("(c p) n -> p c n", p=128)), w=["wbrs"])
    S.dma("pool", lambda e: e.dma_start(out=wbrn, in_=wbrn_d.rearrange("(c p) n -> p c n", p=128)), w=["wbrn"])
    S.dma("pool", lambda e: e.dma_start(out=wgt, in_=w_in[:, C_G:C_G + 2048].rearrange("(c p) n -> p c n", p=128)), w=["wgt"])
    ssq3 = ssq
    for ob in range(4):
        bi = ob + 1
        col0, ncol = blocks[bi]
        load_hb(bi % 2, col0, ncol)
        for tt in range(4):
            c = ob * 4 + tt
            q = c % 2
            S.dma("sp", lambda e, q=q, c=c: e.dma_start(out=ygt[q], in_=ygT_d[:, :, c * 128:(c + 1) * 128]), w=[("ygt", q)])
            S.dma("sp", lambda e, q=q, c=c: e.dma_start(out=ynt_[q], in_=ynaT_d[:, :, c * 128:(c + 1) * 128]), w=[("ynt", q)])
            for nb_ in range(2):
                cs_ = slice(nb_ * 512, (nb_ + 1) * 512)
                pa = k.ps()
                for ch in range(16):
                    S.pe(lambda e, pa=pa, ch=ch, q=q: e.matmul(PS[pa][:, :], lhsT=ygt[q][:, ch, :], rhs=wbrs[:, ch, cs_], start=(ch == 0), stop=(ch == 15)),
                         r=[("ygt", q), "wbrs"], w=[("ps", pa)])
                pg = k.ps()
                for dc in range(8):
                    S.pe(lambda e, pg=pg, dc=dc: e.matmul(PS[pg][:, :], lhsT=hb[bi % 2][:, dc, tt * 128:(tt + 1) * 128], rhs=wgt[:, dc, nb_ * 512:(nb_ + 1) * 512],
                                                          start=(dc == 0), stop=(dc == 7)), r=[("hb", bi % 2), "wgt"], w=[("ps", pg)])
                S.act(lambda e, pg=pg: e.activation(out=sg, in_=PS[pg][:, :], func=AF.Sigmoid), r=[("ps", pg)], w=["sg"])
                S.dve(lambda e, pa=pa, c=c: e.scalar_tensor_tensor(out=m1[:, cs_], in0=PS[pa][:, :], scalar=rstd_ssd[:, c:c + 1], in1=sg, op0=ALU.mult, op1=ALU.mult),
                      r=[("ps", pa), "rstd_ssd", "sg"], w=[("m1", nb_)])
                pn = k.ps()
                for ch in range(8):
                    S.pe(lambda e, pn=pn, ch=ch, q=q: e.matmul(PS[pn][:, :], lhsT=ynt_[q][:, ch, :], rhs=wbrn[:, ch, cs_], start=(ch == 0), stop=(ch == 7)),
                         r=[("ynt", q), "wbrn"], w=[("ps", pn)])
                pg2 = k.ps()
                for dc in range(8):
                    S.pe(lambda e, pg2=pg2, dc=dc: e.matmul(PS[pg2][:, :], lhsT=hb[bi % 2][:, dc, tt * 128:(tt + 1) * 128], rhs=wgt[:, dc, 1024 + nb_ * 512:1024 + (nb_ + 1) * 512],
                                                            start=(dc == 0), stop=(dc == 7)), r=[("hb", bi % 2), "wgt"], w=[("ps", pg2)])
                S.act(lambda e, pg2=pg2: e.activation(out=sg, in_=PS[pg2][:, :], func=AF.Sigmoid), r=[("ps", pg2)], w=["sg"])
                S.dve(lambda e, pn=pn: e.tensor_tensor(out=sg, in0=PS[pn][:, :], in1=sg, op=ALU.mult), r=[("ps", pn), "sg"], w=["sg"])
                S.pool(lambda e: e.tensor_tensor(out=mrg[:, cs_], in0=m1[:, cs_], in1=sg, op=ALU.add), r=[("m1", nb_), "sg"], w=[("mrg", nb_)])
            pt = k.ps()
            for ch in range(8):
                S.pe(lambda e, pt=pt, ch=ch: e.transpose(out=psb(pt)[:, ch * 128:(ch + 1) * 128], in_=mrg[:, ch * 128:(ch + 1) * 128], identity=ident_b[:]),
                     r=[("mrg", 0), ("mrg", 1), "ident_b"], w=[("ps", pt)])
            S.act(lambda e, pt=pt, c=c: e.activation(out=h2T[:, :, c * 128:(c + 1) * 128], in_=psb(pt)[:, :].rearrange("p (a b) -> p a b", a=8), func=AF.Copy),
                  r=[("ps", pt)], w=[("h2T", c)])
    S.barrier()
    AR.off = p5mark
    wout = aalloc([8, D], BF16)
    sg = aalloc([512], F32)
    m1 = aalloc([D], F32)
    h2f = aalloc([8, 128], F32)
    rla = aalloc([16, 36], F32)
    rv = aalloc([7, 16], F32)
    oh4 = aalloc([16, 4], F32)
    ge4 = aalloc([16, 4], F32)
    Em = aalloc([16, 32], F32)
    eq1 = aalloc([16, 32], F32)
    eq2 = aalloc([16, 32], F32)
    p6mark = AR.off
    S.dma("pool", lambda e: e.dma_start(out=wout, in_=wout_d.rearrange("(c p) n -> p c n", p=128)), w=["wout"])
    m1b = aalloc([D], F32)
    xsc2 = aalloc([D], F32)
    M1S = [m1, m1b]
    XLD = [xin[1], xin[2]]
    XSC = [xin[0], xsc2]

    def p5b_stage1(c):
        q = c % 2
        xl, xlk = XLD[q], f"xld{q}"
        mm_, mk_ = M1S[q], f"m1s{q}"
        xs_, xsk = XSC[q], f"xsc{q}"
        S.dma("sp", lambda e: e.dma_start(out=xl[:], in_=x_ext[c * 128:(c + 1) * 128, :]), w=[xlk])
        for nb_ in range(2):
            cs_ = slice(nb_ * 512, (nb_ + 1) * 512)
            po_ = k.ps()
            for ch in range(8):
                S.pe(lambda e, ch=ch: e.matmul(PS[po_][:, :], lhsT=h2T[:, ch, c * 128:(c + 1) * 128], rhs=wout[:, ch, cs_], start=(ch == 0), stop=(ch == 7)),
                     r=[("h2T", c), "wout"], w=[("ps", po_)])
            S.dve(lambda e: e.tensor_tensor(out=mm_[:, cs_], in0=PS[po_][:, :], in1=gbb[:, cs_], op=ALU.mult), r=[("ps", po_), "gbb"], w=[(mk_, nb_)])
            S.pool(lambda e: e.tensor_tensor(out=x1[:, c, cs_], in0=mm_[:, cs_], in1=xl[:, cs_], op=ALU.add), r=[(mk_, nb_), xlk], w=[("x1", c, nb_)])
        S.act(lambda e: e.activation(out=junk[:], in_=x1[:, c, :], func=AF.Square, accum_out=ssq3[:, c:c + 1]), r=[("x1", c, 0), ("x1", c, 1)], w=["junk", ("ssq3", c)])
        S.act(lambda e: e.activation(out=rstd[:, c:c + 1], in_=ssq3[:, c:c + 1], func=AF.Sqrt, scale=1.0 / D, bias=epsb[:, 0:1]), r=[("ssq3", c)], w=[("rstd3", c)])
        S.pool(lambda e: e.tensor_tensor(out=rstd[:, c:c + 1], in0=rstd[:, c:c + 1], in1=onesc[:, 0:1], op=ALU.pow), r=[("rstd3", c)], w=[("rstd3", c)])
        S.dve(lambda e: e.tensor_scalar(out=xs_[:] if q == 0 else xs_, in0=x1[:, c, :], scalar1=rstd[:, c:c + 1], scalar2=None, op0=ALU.mult),
              r=[("x1", c, 0), ("x1", c, 1), ("rstd3", c)], w=[xsk])

    def p5b_stage2(c):
        q = c % 2
        xs_, xsk = XSC[q], f"xsc{q}"
        pf1, pf2 = k.ps(), k.ps()
        for ch in range(8):
            pp = pf1 if ch < 4 else pf2
            S.pe(lambda e, pp=pp, ch=ch: e.transpose(out=PS[pp][:, (ch % 4) * 128:(ch % 4 + 1) * 128], in_=xs_[:, ch * 128:(ch + 1) * 128], identity=ident_f[:]),
                 r=[xsk, "ident_f"], w=[("ps", pp)])
        for ch in range(8):
            pp = pf1 if ch < 4 else pf2
            S.act(lambda e, pp=pp, ch=ch: e.activation(out=h2f[:, ch, :], in_=PS[pp][:, (ch % 4) * 128:(ch % 4 + 1) * 128], func=AF.Identity,
                                                       scale=A2[:, ch:ch + 1], bias=modT2[:, 24 + ch, 0:1]), r=[("ps", pp), "A2", "modT2"], w=[("h2f", ch)])
        S.pool(lambda e: e.tensor_copy(out=h2T[:, :, c * 128:(c + 1) * 128], in_=h2f), r=[("h2f", ch) for ch in range(8)], w=[("h2T", c)])
        pr_ = k.ps()
        for ch in range(8):
            S.pe(lambda e, ch=ch: e.matmul(PS[pr_][:, 0:36], lhsT=h2f[:, ch, :], rhs=wrt[:, ch, :], start=(ch == 0), stop=(ch == 7)),
                 r=[("h2f", ch), "wrt"], w=[("ps", pr_)])
        S.dve(lambda e: e.tensor_tensor(out=rla[:, c, :], in0=PS[pr_][:, 0:36], in1=brt, op=ALU.add), r=[("ps", pr_), "brt"], w=[("rla", c)])

    p5b_stage1(0)
    for c in range(1, 16):
        p5b_stage1(c)
        p5b_stage2(c - 1)
    p5b_stage2(15)
    RLA = [("rla", c) for c in range(16)]
    Gv = rla[:, :, 0:4]
    Ev = rla[:, :, 4:36]
    bc4 = lambda v: v.unsqueeze(2).to_broadcast([128, 16, 4])
    bc32 = lambda v: v.unsqueeze(2).to_broadcast([128, 16, 32])
    S.dve(lambda e: e.tensor_reduce(out=rv[:, 0, :], in_=Gv, axis=AX.X, op=ALU.max), r=RLA, w=["rv0"])
    S.dve(lambda e: e.tensor_tensor(out=oh4, in0=Gv, in1=bc4(rv[:, 0, :]), op=ALU.is_equal), r=RLA + ["rv0"], w=["oh4"])
    S.dve(lambda e: e.tensor_tensor(out=ge4, in0=Gv, in1=bc4(rv[:, 0, :]), op=ALU.subtract), r=RLA + ["rv0"], w=["ge4"])
    S.act(lambda e: e.activation(out=ge4, in_=ge4, func=AF.Exp), r=["ge4"], w=["ge4"])
    S.dve(lambda e: e.tensor_reduce(out=rv[:, 1, :], in_=ge4, axis=AX.X, op=ALU.add), r=["ge4"], w=["rv1"])
    S.dve(lambda e: e.tensor_scalar(out=oh4, in0=oh4, scalar1=-1.0, scalar2=1e30, op0=ALU.add, op1=ALU.mult), r=["oh4"], w=["oh4"])
    S.dve(lambda e: e.tensor_tensor(out=Em.rearrange("p t (g x) -> p t g x", g=4), in0=Ev.rearrange("p t (g x) -> p t g x", g=4),
                                    in1=oh4.unsqueeze(3).to_broadcast([128, 16, 4, 8]), op=ALU.add), r=RLA + ["oh4"], w=["Em"])
    S.dve(lambda e: e.tensor_reduce(out=rv[:, 2, :], in_=Em, axis=AX.X, op=ALU.max), r=["Em"], w=["rv2"])
    S.dve(lambda e: e.tensor_tensor(out=eq1, in0=Em, in1=bc32(rv[:, 2, :]), op=ALU.is_equal), r=["Em", "rv2"], w=["eq1"])
    S.dve(lambda e: e.scalar_tensor_tensor(out=Em, in0=eq1, scalar=-1e30, in1=Em, op0=ALU.mult, op1=ALU.add), r=["eq1", "Em"], w=["Em"])
    S.dve(lambda e: e.tensor_reduce(out=rv[:, 3, :], in_=Em, axis=AX.X, op=ALU.max), r=["Em"], w=["rv3"])
    S.dve(lambda e: e.tensor_tensor(out=eq2, in0=Em, in1=bc32(rv[:, 3, :]), op=ALU.is_equal), r=["Em", "rv3"], w=["eq2"])
    S.dve(lambda e: e.tensor_tensor(out=rv[:, 4, :], in0=rv[:, 3, :], in1=rv[:, 2, :], op=ALU.subtract), r=["rv2", "rv3"], w=["rv4"])
    S.act(lambda e: e.activation(out=rv[:, 4, :], in_=rv[:, 4, :], func=AF.Exp), r=["rv4"], w=["rv4"])
    S.dve(lambda e: e.tensor_scalar(out=rv[:, 5, :], in0=rv[:, 4, :], scalar1=1.0, scalar2=None, op0=ALU.add), r=["rv4"], w=["rv5"])
    S.dve(lambda e: e.tensor_tensor(out=rv[:, 5, :], in0=rv[:, 5, :], in1=rv[:, 1, :], op=ALU.mult), r=["rv5", "rv1"], w=["rv5"])
    S.pool(lambda e: e.tensor_tensor(out=rv[:, 5, :], in0=rv[:, 5, :], in1=onesc[:, 0:1].to_broadcast([128, 16]), op=ALU.pow), r=["rv5", "onesc"], w=["rv5"])
    S.dve(lambda e: e.tensor_tensor(out=rv[:, 6, :], in0=rv[:, 5, :], in1=rv[:, 4, :], op=ALU.mult), r=["rv5", "rv4"], w=["rv6"])
    S.dve(lambda e: e.tensor_tensor(out=eq1, in0=eq1, in1=bc32(rv[:, 5, :]), op=ALU.mult), r=["eq1", "rv5"], w=["eq1"])
    S.dve(lambda e: e.tensor_tensor(out=eq2, in0=eq2, in1=bc32(rv[:, 6, :]), op=ALU.mult), r=["eq2", "rv6"], w=["eq2"])
    S.dve(lambda e: e.tensor_tensor(out=gates, in0=eq1, in1=eq2, op=ALU.add), r=["eq1", "eq2"], w=[("gates", c) for c in range(16)])

    S.barrier()
    AR.off = p5mark
    w13a = aalloc([2, 8, 512], BF16)
    w13 = [w13a, wst[0][:].bitcast(BF16).rearrange("p a b -> p (a b)").rearrange("p (j c n) -> p j c n", j=2, c=8)]
    w2b1 = aalloc([4, D], BF16)
    w2b = [w2b1, hTt[0][:].rearrange("p a b -> p (a b)").rearrange("p (c n) -> p c n", c=4)]
    m1 = aalloc([D], F32)
    aT = aalloc([4, 512], BF16)
    hs1 = aalloc([512], F32)
    NE = int(os.environ.get("NEXPERTS", "32"))
    def load_expert(ex):
        q = ex % 2
        S.dma("pool", lambda e: e.dma_start(out=w13[q][:, 0, :, :], in_=w1_d[ex].rearrange("(c p) n -> p c n", p=128)), w=[("w13", q)])
        S.dma("pool", lambda e: e.dma_start(out=w13[q][:, 1, :, :], in_=w3_d[ex].rearrange("(c p) n -> p c n", p=128)), w=[("w13", q)])
        S.dma("pool", lambda e: e.dma_start(out=w2b[q], in_=w2_d[ex].rearrange("(c p) n -> p c n", p=128)), w=[("w2b", q)])

    load_expert(0)
    for ex in range(NE):
        q = ex % 2
        if ex + 1 < NE:
            load_expert(ex + 1)
        for tb in range(4):
            for fc in range(4):
                p1_, p3_ = k.ps(), k.ps()
                for dc in range(8):
                    S.pe(lambda e, p1_=p1_, dc=dc, q=q: e.matmul(PS[p1_][:, :], lhsT=w13[q][:, 0, dc, fc * 128:(fc + 1) * 128], rhs=h2T[:, dc, tb * 512:(tb + 1) * 512],
                                                                start=(dc == 0), stop=(dc == 7)), r=[("w13", q)] + [("h2T", tb * 4 + u) for u in range(4)], w=[("ps", p1_)])
                for dc in range(8):
                    S.pe(lambda e, p3_=p3_, dc=dc, q=q: e.matmul(PS[p3_][:, :], lhsT=w13[q][:, 1, dc, fc * 128:(fc + 1) * 128], rhs=h2T[:, dc, tb * 512:(tb + 1) * 512],
                                                                start=(dc == 0), stop=(dc == 7)), r=[("w13", q)] + [("h2T", tb * 4 + u) for u in range(4)], w=[("ps", p3_)])
                S.act(lambda e, p1_=p1_: e.activation(out=hs1, in_=PS[p1_][:, :], func=AF.Silu), r=[("ps", p1_)], w=["hs1"])
                S.dve(lambda e, p3_=p3_, fc=fc: e.tensor_tensor(out=aT[:, fc, :], in0=PS[p3_][:, :], in1=hs1, op=ALU.mult), r=[("ps", p3_), "hs1"], w=[("aT", fc)])
            for tt in range(4):
                c = tb * 4 + tt
                for nb_ in range(2):
                    cs_ = slice(nb_ * 512, (nb_ + 1) * 512)
                    po_ = k.ps()
                    for fc in range(4):
                        S.pe(lambda e, po_=po_, fc=fc, q=q: e.matmul(PS[po_][:, :], lhsT=aT[:, fc, tt * 128:(tt + 1) * 128], rhs=w2b[q][:, fc, cs_],
                                                                    start=(fc == 0), stop=(fc == 3)), r=[("aT", fc) for fc in range(4)] + [("w2b", q)], w=[("ps", po_)])
                    S.dve(lambda e, po_=po_, c=c, ex=ex: e.scalar_tensor_tensor(out=m1[:, cs_], in0=PS[po_][:, :], scalar=gates[:, c, ex:ex + 1], in1=gbb[:, 1024 + nb_ * 512:1024 + (nb_ + 1) * 512],
                                                                               op0=ALU.mult, op1=ALU.mult), r=[("ps", po_), ("gates", c), "gbb"], w=[("m1", nb_)])
                    S.pool(lambda e, c=c: e.tensor_tensor(out=x1[:, c, cs_], in0=x1[:, c, cs_], in1=m1[:, cs_], op=ALU.add), r=[("m1", nb_), ("x1", c, nb_)], w=[("x1", c, nb_)])
    for c in range(16):
        S.dma("sp", lambda e, c=c: e.dma_start(out=y_out[c * 128:(c + 1) * 128, :], in_=x1[:, c, :]), r=[("x1", c, 0), ("x1", c, 1)], w=[("y", c)])
    S.emit()
    k.es.close()
    return nc


def _consts():
    c = {}
    f32 = np.float32
    c["ident"] = np.eye(128, dtype=f32)
    kk = np.arange(128)
    c["maskU"] = (kk[:, None] <= kk[None, :]).astype(f32)
    c["maskL"] = (kk[:, None] >= kk[None, :]).astype(f32)
    P = np.zeros((128, 128), f32)
    for n in range(128):
        if (n % 64) < 32:
            P[n, n + 32] = -1.0
        else:
            P[n, n - 32] = 1.0
    c["permT"] = np.ascontiguousarray(P.T)
    sel = np.zeros((128, 32, 128), f32)
    for kq in range(128):
        sel[kq, kq % 32, :] = 1.0
    c["selc"] = sel.reshape(128, 4096)
    return c


def _na_bias(rpb, hf):
    out = np.full((16, 128, 3, 5, 128), -30000.0, np.float32)
    kidx = np.arange(128)
    for cls in range(3):
        i = cls
        kts = [0, 1, 2, 3, 4] if i < 2 else list(range(i - 2, i + 3))
        qr = 2 * i + kidx // 64
        qc = kidx % 64
        gqr, gqc = (qr, qc) if hf == 0 else (63 - qr, 63 - qc)
        rs = np.clip(gqr - 4, 0, 56)
        cs = np.clip(gqc - 8, 0, 48)
        for rel, kt in enumerate(kts):
            kr = 2 * kt + kidx // 64
            kc = kidx % 64
            gkr, gkc = (kr, kc) if hf == 0 else (63 - kr, 63 - kc)
            ok = ((gkr[:, None] >= rs[None, :]) & (gkr[:, None] <= rs[None, :] + 7) &
                  (gkc[:, None] >= cs[None, :]) & (gkc[:, None] <= cs[None, :] + 15))
            dr = np.clip(gkr[:, None] - gqr[None, :] + 7, 0, 14)
            dc = np.clip(gkc[:, None] - gqc[None, :] + 15, 0, 30)
            vals = rpb[:, dr, dc]
            out[:, :, cls, rel, :] = np.where(ok[None], vals, np.float32(-30000.0))
    return np.ascontiguousarray(out.reshape(16, 128, 3 * 5 * 128))


def _rope_tables(hf):
    i = np.arange(SEQ)
    t = i if hf == 0 else (SEQ - 1 - i)
    row = (t // 64).astype(np.float32)
    col = (t % 64).astype(np.float32)
    inv = (10000.0 ** (-np.arange(0, 64, 2, dtype=np.float32) / 64.0)).astype(np.float32)
    ar = row[None, :] * inv[:, None]
    ac = col[None, :] * inv[:, None]
    ang = np.concatenate([ar, ar, ac, ac], axis=0)
    return np.cos(ang).astype(np.float32), np.sin(ang).astype(np.float32)


def prep_core_inputs(inp, b, hf, consts):
    f32 = np.float32
    m = {}
    x = inp["x"][b]
    ctx = inp["ctx"][b]
    if hf == 1:
        x = x[::-1]
        ctx = ctx[::-1]
    m["x_ext"] = np.ascontiguousarray(x, dtype=f32)
    m["ctx_l"] = np.ascontiguousarray(ctx, dtype=f32)
    m["cvT"] = np.ascontiguousarray(np.stack([inp["c"][b], inp["c_ctx"]], axis=1), dtype=f32)
    m["w_mod"] = np.ascontiguousarray(inp["w_mod"][0], dtype=f32)
    bm = inp["b_mod"][0]
    m["b_modT"] = np.ascontiguousarray(bm.reshape(48, 128).T, dtype=f32)
    m["b_mod_g"] = np.ascontiguousarray(np.concatenate([bm[2048:3072], bm[5120:6144]])[None, :], dtype=f32)
    m["n1wT"] = np.ascontiguousarray(inp["norm1_w"][0].reshape(8, 128).T, dtype=f32)
    m["n2wT"] = np.ascontiguousarray(inp["norm2_w"][0].reshape(8, 128).T, dtype=f32)
    L = 0
    w_in = np.array(inp["w_in"][L], dtype=f32)
    if hf == 1:
        w_in[:, C_DT:C_DT + 64] = np.concatenate([w_in[:, C_DT + 32:C_DT + 64], w_in[:, C_DT:C_DT + 32]], axis=1)
    m["w_in"] = w_in
    p1, p2 = ("f", "b") if hf == 0 else ("b", "f")
    m["dtb12"] = np.concatenate([inp["dt_bias_" + p1][L], inp["dt_bias_" + p2][L]])[None, :].astype(f32)
    m["alog12"] = np.concatenate([inp["a_log_" + p1][L], inp["a_log_" + p2][L]])[None, :].astype(f32)
    m["dskip"] = np.ascontiguousarray(inp["d_skip"][L][None, :], dtype=f32)
    cw = inp["conv_w"][L]
    if hf == 1:
        cw = cw[::-1]
    m["conv_wT"] = np.ascontiguousarray(cw.T.reshape(32, 128, 5).transpose(1, 0, 2), dtype=f32)
    m["conv_bT"] = np.ascontiguousarray(inp["conv_b"][L].reshape(32, 128).T, dtype=f32)
    m["ssd_nwT"] = np.ascontiguousarray(inp["ssd_norm_w"][L].reshape(16, 128).T, dtype=f32)
    m["w_br_ssd"] = np.ascontiguousarray(inp["w_br_ssd"][L], dtype=f32)
    m["w_br_na"] = np.ascontiguousarray(inp["w_br_na"][L], dtype=f32)
    m["w_out"] = np.ascontiguousarray(inp["w_out"][L], dtype=f32)
    m["w_rt36"] = np.ascontiguousarray(np.concatenate([inp["w_grp"][L], inp["w_rt"][L]], axis=1), dtype=f32)
    m["b_rt36"] = np.concatenate([inp["b_grp"][L], inp["b_rt"][L]])[None, :].astype(f32)
    m["w1"] = np.ascontiguousarray(inp["w1"][L], dtype=f32)
    m["w3"] = np.ascontiguousarray(inp["w3"][L], dtype=f32)
    m["w2"] = np.ascontiguousarray(inp["w2"][L], dtype=f32)
    m["qkw"] = np.concatenate([np.tile(inp["q_norm_w"][L], 8) * 0.125, np.tile(inp["k_norm_w"][L], 8)])[None, :].astype(f32)
    m["nabias"] = _na_bias(inp["rpb"][L], hf)
    cs, sn = _rope_tables(hf)
    m["cosT"], m["sinT"] = cs, sn
    m.update(consts)
    return m


def kernel(**inputs):
    inp = {k_: np.asarray(v) for k_, v in inputs.items()}
    nc = build("")
    consts = _consts()
    shared = {}
    maps = []
    for c in range(8):
        m = prep_core_inputs(inp, c // 2, c % 2, consts)
        maps.append(m)
    res = run_bass_kernel_spmd(nc, maps, core_ids=list(range(8)))
    out = np.zeros((4, SEQ, D), dtype=np.float32)
    for c in range(8):
        b, hf = c // 2, c % 2
        y = np.asarray(res.results[c]["y"], dtype=np.float32)
        if hf == 0:
            out[b, :OWN] = y
        else:
            out[b, OWN:] = y[::-1]
    return out
```

```python
import os
import contextlib
import numpy as np
import ml_dtypes
import concourse.bass as bass
import concourse.mybir as mybir
from concourse.bass_utils import run_bass_kernel_spmd

F32 = mybir.dt.float32
BF16 = mybir.dt.bfloat16
AF = mybir.ActivationFunctionType
ALU = mybir.AluOpType
AX = mybir.AxisListType

D = 1024
SEQ = 4096
OWN = 2048
CTX = 256
NTOK = CTX + SEQ
D_SSD = 2048
D_XBC = 4096
D_IN = 11328
C_Z, C_XBC, C_DT, C_QKV, C_G = 0, 2048, 6144, 6208, 9280
EPS = 1e-6
NEXP = 32
DEBUG = os.environ.get("KDEBUG", "")


class _Rec:
    def __getattr__(self, name):
        def f(*a, **kw):
            self.call = (name, a, kw)
            return self
        return f


class Sched:
    def __init__(self, nc, es, n_dma_sems=10, epoch=20000):
        self.nc = nc
        self.es = es
        self.eng = {"pe": nc.tensor, "act": nc.scalar, "dve": nc.vector, "pool": nc.gpsimd, "sp": nc.sync}
        self.ops = []
        self.last_w = {}
        self.readers = {}
        self.n_dma_sems = n_dma_sems
        self.epoch = epoch
        self._semc = 0
        self.bar = set()

    def barrier(self):
        last = {}
        dmas = {}
        for i, op in enumerate(self.ops):
            if op["dma"]:
                dmas.setdefault(op["eng"], []).append(i)
            else:
                last[op["eng"]] = i
        b = set(last.values())
        for q, l in dmas.items():
            b |= set(l[-self.n_dma_sems:])
        self.bar = b

    def _sem(self, name):
        self._semc += 1
        return self.es.enter_context(self.nc.semaphore(f"{name}_{self._semc}"))

    def add(self, eng, fn, r=(), w=(), dma=False):
        idx = len(self.ops)
        raw = set()
        oth = set()
        for k in r:
            lw = self.last_w.get(k)
            if lw is not None:
                raw.add(lw)
        for k in w:
            lw = self.last_w.get(k)
            if lw is not None:
                oth.add(lw)
            for rd in self.readers.get(k, ()):
                oth.add(rd)
        for k in r:
            self.readers.setdefault(k, []).append(idx)
        for k in w:
            self.last_w[k] = idx
            self.readers[k] = []
        raw |= self.bar
        raw.discard(idx)
        oth.discard(idx)
        rec = _Rec()
        fn(rec)
        self.ops.append(dict(eng=eng, call=rec.call, raw=raw, oth=oth - raw, dma=dma, signal=False))
        return idx

    def pe(self, fn, r=(), w=()):
        return self.add("pe", fn, r, w)

    def act(self, fn, r=(), w=()):
        return self.add("act", fn, r, w)

    def dve(self, fn, r=(), w=()):
        return self.add("dve", fn, r, w)

    def pool(self, fn, r=(), w=()):
        return self.add("pool", fn, r, w)

    def dma(self, q, fn, r=(), w=()):
        return self.add(q, fn, r, w, dma=True)

    def emit(self):
        ops = self.ops
        for i, op in enumerate(ops):
            deps = set()
            for p in op["raw"]:
                P = ops[p]
                if (not P["dma"]) and (not op["dma"]) and P["eng"] == op["eng"] and op["eng"] == "pe":
                    continue
                deps.add(p)
            for p in op["oth"]:
                P = ops[p]
                if (not P["dma"]) and (not op["dma"]) and P["eng"] == op["eng"] and op["eng"] == "pe":
                    continue
                deps.add(p)
            op["deps"] = deps
            for p in deps:
                ops[p]["signal"] = True
        dma_count, dma_sems, ring_prev = {}, {}, {}
        for i, op in enumerate(ops):
            if op["dma"]:
                q = op["eng"]
                k = dma_count.get(q, 0)
                dma_count[q] = k + 1
                if q not in dma_sems:
                    dma_sems[q] = [self._sem(f"dma_{q}") for _ in range(self.n_dma_sems)]
                slot = k % self.n_dma_sems
                op["sem"] = dma_sems[q][slot]
                op["val"] = 16 * (k // self.n_dma_sems + 1)
                prev = ring_prev.get((q, slot))
                if prev is not None:
                    op["deps"].add(prev)
                ring_prev[(q, slot)] = i
                op["signal"] = True
        cnt, eng_sems = {}, {}
        for i, op in enumerate(ops):
            if op["dma"] or not op["signal"]:
                continue
            e = op["eng"]
            c = cnt.get(e, 0)
            ep = c // self.epoch
            if (e, ep) not in eng_sems:
                eng_sems[(e, ep)] = self._sem(f"s_{e}{ep}")
            op["sem"] = eng_sems[(e, ep)]
            op["val"] = c % self.epoch + 1
            cnt[e] = c + 1
        waited = {}
        nwaits = 0
        for i, op in enumerate(ops):
            e = op["eng"]
            h = self.eng[e]
            need = {}
            for p in op["deps"]:
                P = ops[p]
                s = P["sem"]
                key = id(s)
                if key not in need or need[key][1] < P["val"]:
                    need[key] = (s, P["val"])
            for key, (s, v) in need.items():
                if waited.get((e, key), 0) >= v:
                    continue
                h.wait_ge(s, v)
                nwaits += 1
                waited[(e, key)] = v
            name, a, kw = op["call"]
            ins = getattr(h, name)(*a, **kw)
            if op["signal"]:
                ins.then_inc(op["sem"], 16 if op["dma"] else 1)
        h = self.eng["sp"]
        last = {}
        for op in ops:
            if op["dma"]:
                last[id(op["sem"])] = (op["sem"], op["val"])
        for s, v in last.values():
            h.wait_ge(s, v)
        counts = {}
        for op in ops:
            counts[op["eng"]] = counts.get(op["eng"], 0) + 1
        print("sched: ops", len(ops), counts, "waits", nwaits, "sems", self._semc, flush=True)


class K:
    def __init__(self):
        self.nc = bass.Bass("TRN2", target_bir_lowering=False)
        self.es = contextlib.ExitStack()
        self.S = Sched(self.nc, self.es)
        self.psum = []
        self.ps_rr = 0

    def din(self, name, shape, dt=F32):
        return self.nc.dram_tensor(name, list(shape), dt, kind="ExternalInput").ap()

    def dout(self, name, shape, dt=F32):
        return self.nc.dram_tensor(name, list(shape), dt, kind="ExternalOutput").ap()

    def dscr(self, name, shape, dt):
        return self.nc.dram_tensor(name, list(shape), dt, kind="Internal").ap()

    def sb(self, name, shape, dt=F32):
        return self.es.enter_context(self.nc.sbuf_tensor(name, list(shape), dt))

    def init_psum(self):
        for i in range(8):
            self.psum.append(self.es.enter_context(self.nc.psum_tensor(f"ps{i}", [128, 512], F32)))

    def ps(self):
        i = self.ps_rr
        self.ps_rr = (self.ps_rr + 1) % 8
        return i


def chunks(n, c):
    return [(i, min(c, n - i)) for i in range(0, n, c)]


def build(debug=""):
    k = K()
    nc, S = k.nc, k.S
    dbg = set(debug.split(",")) if debug else set()

    def scr(name, shape, dt):
        if name in dbg:
            return k.dout(name, shape, dt)
        return k.dscr(name, shape, dt)

    x_ext = k.din("x_ext", [SEQ, D])
    ctx_l = k.din("ctx_l", [CTX, D])
    cvT = k.din("cvT", [D, 2])
    w_mod = k.din("w_mod", [D, 6 * D])
    b_modT = k.din("b_modT", [128, 48])
    b_mod_g = k.din("b_mod_g", [1, 2048])
    n1wT = k.din("n1wT", [128, 8])
    n2wT = k.din("n2wT", [128, 8])
    ident_d = k.din("ident", [128, 128])
    y_out = k.dout("y", [OWN, D])
    hT_d = scr("hT_d", [128, 8, NTOK], BF16)

    k.init_psum()
    PS = k.psum

    def psb(i):
        return PS[i].bitcast(BF16)

    ident_f = k.sb("ident_f", [128, 128], F32)
    ident_b = k.sb("ident_b", [128, 128], BF16)
    S.dma("sp", lambda e: e.dma_start(out=ident_f[:], in_=ident_d[:, :]), w=["ident_f"])
    S.dma("pool", lambda e: e.dma_start(out=ident_b[:], in_=ident_d[:, :]), w=["ident_b"])

    ARENA_BYTES = 155 * 1024
    arena = k.sb("arena", [128, ARENA_BYTES // 2], BF16)

    class AR:
        off = 0

    def aalloc(shape, dt):
        n = int(np.prod(shape))
        nb = n * (4 if dt == F32 else 2)
        nb_al = (nb + 63) // 64 * 64
        assert AR.off + nb_al <= ARENA_BYTES, ("arena overflow", AR.off, nb_al)
        v = arena[:, AR.off // 2:(AR.off + nb) // 2]
        if dt == F32:
            v = v.bitcast(F32)
        AR.off += nb_al
        if len(shape) == 2:
            v = v.rearrange("p (a b) -> p a b", a=shape[0])
        elif len(shape) == 3:
            v = v.rearrange("p (a b c) -> p a b c", a=shape[0], b=shape[1])
        elif len(shape) == 4:
            v = v.rearrange("p (a b c d) -> p a b c d", a=shape[0], b=shape[1], c=shape[2])
        return v

    cv = k.sb("cv", [128, 8, 2], F32)
    siluc = k.sb("siluc", [128, 8, 2], F32)
    bmodT = k.sb("bmodT", [128, 48], F32)
    n1w = k.sb("n1w", [128, 8], F32)
    n2w = k.sb("n2w", [128, 8], F32)
    modT2 = k.sb("modT2", [128, 48, 2], F32)
    sel0 = k.sb("sel0", [2, 128], F32)
    A1 = k.sb("A1", [128, 8, 2], F32)
    A2 = k.sb("A2", [128, 8], F32)
    wst = [k.sb("wst0", [128, 8, 512], F32)]

    S.dve(lambda e: e.memset(modT2[:], 0.0), w=["modT2"])
    S.dma("sp", lambda e: e.dma_start(out=cv[:], in_=cvT.rearrange("(c p) r -> p c r", p=128)), w=["cv"])
    S.dma("sp", lambda e: e.dma_start(out=bmodT[:], in_=b_modT[:, :]), w=["bmodT"])
    S.dma("sp", lambda e: e.dma_start(out=n1w[:], in_=n1wT[:, :]), w=["n1w"])
    S.dma("sp", lambda e: e.dma_start(out=n2w[:], in_=n2wT[:, :]), w=["n2w"])
    S.act(lambda e: e.activation(out=siluc[:], in_=cv[:], func=AF.Silu), r=["cv"], w=["siluc"])
    epsb = k.sb("epsb", [128, 1], F32)
    S.dve(lambda e: e.memset(epsb[:], EPS), w=["epsb"])
    onesc = k.sb("onesc", [128, 1], F32)
    S.dve(lambda e: e.memset(onesc[:], -1.0), w=["onesc"])
    S.dve(lambda e: e.memset(sel0[:], 0.0), w=["sel0"])
    S.dve(lambda e: e.memset(sel0[0:1, :], 1.0), r=["sel0"], w=["sel0"])

    ROWBLK = {4: (0, 0), 5: (0, 512), 10: (1, 0), 11: (1, 512)}
    xin = [k.sb(f"xin{i}", [128, D], F32) for i in range(3)]
    gb_d = k.dscr("gb_d", [2, 2048], F32)
    wst1 = arena[:, 0:8192].bitcast(F32).rearrange("p (a b) -> p a b", a=8)
    grow = [arena[:, 8192 + i_ * 2048:8192 + (i_ + 1) * 2048].bitcast(F32) for i_ in range(2)]
    gbias = arena[:, 12288:14336].bitcast(F32)
    wsts = [wst[0], wst1]

    def mod_block(blk):
        wb = wsts[blk % 2]
        wk = f"wstage{blk % 2}"
        S.dma("sp", lambda e: e.dma_start(out=wb[:, :, :], in_=w_mod[:, blk * 512:(blk + 1) * 512].rearrange("(c p) n -> p c n", p=128)), w=[wk])
        if blk in ROWBLK:
            xi, off = ROWBLK[blk]
            pi = k.ps()
            for dc in range(8):
                S.pe(lambda e, dc=dc: e.matmul(PS[pi][0:2, :], lhsT=siluc[:, dc, :], rhs=wb[:, dc, :], start=(dc == 0), stop=(dc == 7)),
                     r=[wk, "siluc"], w=[("ps", pi)])
            S.act(lambda e: e.activation(out=grow[xi][0:2, off:off + 512], in_=PS[pi][0:2, :], func=AF.Copy), r=[("ps", pi)], w=[("grow", xi)])
        else:
            pi = k.ps()
            for jj in range(4):
                for dc in range(8):
                    S.pe(lambda e, dc=dc, jj=jj: e.matmul(PS[pi][:, jj * 2:jj * 2 + 2], lhsT=wb[:, dc, jj * 128:(jj + 1) * 128], rhs=siluc[:, dc, :],
                                                          start=(dc == 0), stop=(dc == 7)), r=[wk, "siluc"], w=[("ps", pi)])
            for jj in range(4):
                ch = blk * 4 + jj
                S.dve(lambda e, jj=jj, ch=ch: e.tensor_scalar(out=modT2[:, ch, :], in0=PS[pi][:, jj * 2:jj * 2 + 2], scalar1=bmodT[:, ch:ch + 1], scalar2=None,
                                                              op0=ALU.add), r=[("ps", pi), "bmodT"], w=["modT2"])

    def mod_rest():
        for blk in range(4, 12):
            mod_block(blk)
        for xi in range(2):
            S.dma("sp", lambda e, xi=xi: e.dma_start(out=gbias[0:2, :], in_=b_mod_g[0:1, xi * 1024:(xi + 1) * 1024].to_broadcast([2, 1024])), w=["gbias"])
            S.dve(lambda e, xi=xi: e.tensor_tensor(out=grow[xi][0:2, :], in0=grow[xi][0:2, :], in1=gbias[0:2, :], op=ALU.add),
                  r=[("grow", xi), "gbias"], w=[("grow", xi)])
            S.dma("sp", lambda e, xi=xi: e.dma_start(out=gb_d[0:2, xi * 1024:(xi + 1) * 1024], in_=grow[xi][0:2, :]), r=[("grow", xi)], w=[("gb_d", xi)])
        S.dve(lambda e: e.tensor_scalar(out=A2[:], in0=modT2[:, 32:40, 0], scalar1=1.0, scalar2=None, op0=ALU.add), r=["modT2"], w=["A2"])
        S.dve(lambda e: e.tensor_tensor(out=A2[:], in0=A2[:], in1=n2w[:], op=ALU.mult), r=["A2", "n2w"], w=["A2"])

    for blk in range(4):
        mod_block(blk)
    S.dve(lambda e: e.tensor_scalar(out=A1[:], in0=modT2[:, 8:16, :], scalar1=1.0, scalar2=None, op0=ALU.add),
          r=["modT2"], w=["A1"])
    S.dve(lambda e: e.tensor_tensor(out=A1[:], in0=A1[:], in1=n1w[:].unsqueeze(2).to_broadcast([128, 8, 2]), op=ALU.mult),
          r=["A1", "n1w"], w=["A1"])

    junk = k.sb("junk", [128, D], BF16)
    xn = [k.sb(f"xn{i}", [128, D], BF16) for i in range(2)]
    xn3 = [xn[0], xn[1], arena[:, 14336:15360]]
    ssq = k.sb("ssq", [128, 34], F32)
    rstd = k.sb("rstd", [128, 34], F32)
    hTt = [k.sb(f"hTt{i}", [128, 8, 512], BF16) for i in range(2)]
    groups = [(0, 2)] + [(2 + 4 * i, 4) for i in range(8)]
    P1L = float(os.environ.get("P1L", "9"))
    P1E = os.environ.get("P1E", "act")
    groups = groups[:int(os.environ.get("P1G", "99"))]
    tiles_ = []
    for gi, (t0, nt) in enumerate(groups):
        for tt in range(nt):
            tiles_.append((gi, t0, nt, tt))
    p1ps = {}

    def p1_stage1(n):
        gi, t0, nt, tt = tiles_[n]
        t = t0 + tt
        isctx = t < 2
        src = ctx_l[t * 128:(t + 1) * 128, :] if isctx else x_ext[(t - 2) * 128:(t - 1) * 128, :]
        xb, xk = xin[t % 3], f"xin{t % 3}"
        nb, nk = xn3[t % 3], f"xn{t % 3}"
        S.dma("sp", lambda e: e.dma_start(out=xb[:], in_=src), w=[xk])
        S.act(lambda e: e.activation(out=junk[:], in_=xb[:], func=AF.Square, accum_out=ssq[:, t:t + 1]), r=[xk], w=["junk", ("ssq", t)])
        S.act(lambda e: e.activation(out=rstd[:, t:t + 1], in_=ssq[:, t:t + 1], func=AF.Sqrt, scale=1.0 / D, bias=epsb[:, 0:1]),
              r=[("ssq", t), "epsb"], w=[("rstd", t)])
        S.pool(lambda e: e.tensor_tensor(out=rstd[:, t:t + 1], in0=rstd[:, t:t + 1], in1=onesc[:, 0:1], op=ALU.pow),
               r=[("rstd", t), "onesc"], w=[("rstd", t)])
        S.dve(lambda e: e.tensor_scalar(out=nb[:], in0=xb[:], scalar1=rstd[:, t:t + 1], scalar2=None, op0=ALU.mult), r=[xk, ("rstd", t)], w=[nk])
        pi = k.ps()
        p1ps[n] = pi
        for dc in range(8):
            S.pe(lambda e, dc=dc: e.transpose(out=psb(pi)[:, dc * 128:(dc + 1) * 128], in_=nb[:, dc * 128:(dc + 1) * 128], identity=ident_b[:]),
                 r=[nk, "ident_b"], w=[("ps", pi)])

    def p1_stage2(n):
        gi, t0, nt, tt = tiles_[n]
        t = t0 + tt
        ci = 1 if t < 2 else 0
        hb_ = hTt[gi % 2]
        hk = f"hTt{gi % 2}"
        pi = p1ps[n]
        for dc in range(8):
            S.act(lambda e, dc=dc: e.activation(out=hb_[:, dc, tt * 128:(tt + 1) * 128], in_=psb(pi)[:, dc * 128:(dc + 1) * 128], func=AF.Identity,
                                                scale=A1[:, dc, ci:ci + 1], bias=modT2[:, dc, ci:ci + 1]),
                  r=[("ps", pi), "A1", "modT2"], w=[(hk, tt, dc)])
        if tt == nt - 1:
            col0 = t0 * 128
            ncol = nt * 128
            allk = [(hk, tt_, dc) for tt_ in range(nt) for dc in range(8)]
            S.dma("sp", lambda e: e.dma_start(out=hT_d[:, :, col0:col0 + ncol], in_=hb_[:, :, 0:ncol]), r=allk, w=[("hT_d", gi)])

    P1LEAD = 2
    for n in range(len(tiles_) + P1LEAD):
        if n < len(tiles_):
            p1_stage1(n)
        if n >= P1LEAD:
            p1_stage2(n - P1LEAD)

    mod_rest()
    S.barrier()
    w_in = k.din("w_in", [D, D_IN])
    maskU_d = k.din("maskU", [128, 128])
    maskL_d = k.din("maskL", [128, 128])
    permT_d = k.din("permT", [128, 128])
    selc_d = k.din("selc", [128, 4096])
    cosT_d = k.din("cosT", [128, SEQ])
    sinT_d = k.din("sinT", [128, SEQ])
    dtb12_d = k.din("dtb12", [1, 64])
    alog12_d = k.din("alog12", [1, 64])
    dskip_d = k.din("dskip", [1, 32])
    convwT_d = k.din("conv_wT", [128, 32, 5])
    convbT_d = k.din("conv_bT", [128, 32])
    ssdnwT_d = k.din("ssd_nwT", [128, 16])
    ygT_d = scr("ygT_d", [128, 16, OWN], BF16)
    ssq2_d = scr("ssq2_d", [128, 16], F32) if "ssq2_d" in dbg else None

    hb = hTt
    blocks = [(0, 256)] + [(256 + 512 * i, 512) for i in range(8)]

    def load_hb(i, col0, ncol):
        S.dma("sp", lambda e: e.dma_start(out=hb[i][:, :, 0:ncol], in_=hT_d[:, :, col0:col0 + ncol]), w=[("hb", i)])

    rstd_ssd = aalloc([16], F32)
    mhalf = aalloc([1], F32)
    pmark = AR.off
    maskU = aalloc([128], F32)
    maskL = aalloc([128], F32)
    ones_f = aalloc([128], F32)
    permT = aalloc([128], BF16)
    selb = aalloc([32, 128], BF16)
    cosT = aalloc([SEQ], BF16)
    sinT = aalloc([SEQ], BF16)
    dtb_b = aalloc([64], F32)
    a_b = aalloc([64], F32)
    dsk_b = aalloc([32], F32)
    convw = aalloc([32, 5], F32)
    convb = aalloc([32], F32)
    ssdnw = aalloc([16], F32)
    one_b = aalloc([1], F32)
    S.dma("sp", lambda e: e.dma_start(out=maskU, in_=maskU_d[:, :]), w=["maskU"])
    S.dma("sp", lambda e: e.dma_start(out=maskL, in_=maskL_d[:, :]), w=["maskL"])
    S.dma("pool", lambda e: e.dma_start(out=permT, in_=permT_d[:, :]), w=["permT"])
    S.dma("pool", lambda e: e.dma_start(out=selb.rearrange("p a b -> p (a b)"), in_=selc_d[:, :]), w=["selb"])
    S.dma("pool", lambda e: e.dma_start(out=cosT, in_=cosT_d[:, :]), w=["cosT"])
    S.dma("pool", lambda e: e.dma_start(out=sinT, in_=sinT_d[:, :]), w=["sinT"])
    S.dma("sp", lambda e: e.dma_start(out=dtb_b, in_=dtb12_d[0:1, :].to_broadcast([128, 64])), w=["dtb_b"])
    S.dma("sp", lambda e: e.dma_start(out=a_b, in_=alog12_d[0:1, :].to_broadcast([128, 64])), w=["a_b"])
    S.dma("sp", lambda e: e.dma_start(out=dsk_b, in_=dskip_d[0:1, :].to_broadcast([128, 32])), w=["dsk_b"])
    S.dma("sp", lambda e: e.dma_start(out=convw, in_=convwT_d[:, :, :]), w=["convw"])
    S.dma("sp", lambda e: e.dma_start(out=convb, in_=convbT_d[:, :]), w=["convb"])
    S.dma("sp", lambda e: e.dma_start(out=ssdnw, in_=ssdnwT_d[:, :]), w=["ssdnw"])
    S.dve(lambda e: e.memset(ones_f, 1.0), w=["ones_f"])
    S.dve(lambda e: e.memset(one_b, 1.0), w=["one_b"])
    S.act(lambda e: e.activation(out=a_b, in_=a_b, func=AF.Exp), r=["a_b"], w=["a_b"])
    S.dve(lambda e: e.tensor_scalar(out=a_b, in0=a_b, scalar1=-1.0, scalar2=None, op0=ALU.mult), r=["a_b"], w=["a_b"])

    NT = 34

    def TK(name, lo=0, hi=NT):
        return [(name, t) for t in range(lo, hi)]

    wS = aalloc([NT, 64], F32)
    eat = aalloc([NT, 64], F32)
    acO = aalloc([16, 64], F32)
    bYo = aalloc([16, 64], F32)
    ea = aalloc([16, 64], F32)
    acT = aalloc([16, 128], BF16)
    p2mark = AR.off
    biasY = aalloc([NT, 64], F32)
    acat = aalloc([NT, 128], F32)
    dta = aalloc([NT, 64], F32)
    achl = aalloc([NT, 2, 2, 32], BF16)
    wdt = aalloc([8, 64], BF16)
    S.dma("pool", lambda e: e.dma_start(out=wdt, in_=w_in[:, C_DT:C_DT + 64].rearrange("(c p) n -> p c n", p=128)), w=["wdt"])
    for bi, (col0, ncol) in enumerate(blocks):
        load_hb(bi % 2, col0, ncol)
        for tt in range(ncol // 128):
            t = col0 // 128 + tt
            pi = k.ps()
            for dc in range(8):
                S.pe(lambda e, pi=pi, dc=dc, tt=tt, bi=bi: e.matmul(PS[pi][:, 0:64], lhsT=hb[bi % 2][:, dc, tt * 128:(tt + 1) * 128],
                                                                   rhs=wdt[:, dc, :], start=(dc == 0), stop=(dc == 7)),
                     r=[("hb", bi % 2), "wdt"], w=[("ps", pi)])
            S.dve(lambda e, pi=pi, t=t: e.tensor_tensor(out=wS[:, t, :], in0=PS[pi][:, 0:64], in1=dtb_b, op=ALU.add),
                  r=[("ps", pi), "dtb_b"], w=[("wS", t)])
    S.act(lambda e: e.activation(out=wS, in_=wS, func=AF.Exp), r=TK("wS"), w=TK("wS"))
    S.act(lambda e: e.activation(out=wS, in_=wS, func=AF.Ln, bias=one_b[:, 0:1]), r=TK("wS") + ["one_b"], w=TK("wS"))
    S.act(lambda e: e.activation(out=biasY, in_=wS, func=AF.Ln), r=TK("wS"), w=TK("biasY"))
    S.dve(lambda e: e.tensor_tensor(out=dta, in0=wS, in1=a_b.unsqueeze(1).to_broadcast([128, NT, 64]), op=ALU.mult),
          r=TK("wS") + ["a_b"], w=TK("dta"))
    for t in range(NT):
        pi = k.ps()
        S.pe(lambda e, pi=pi, t=t: e.matmul(PS[pi][:, 0:32], lhsT=maskU, rhs=dta[:, t, 0:32], start=True, stop=True),
             r=[("dta", t), "maskU"], w=[("ps", pi)])
        S.pe(lambda e, pi=pi, t=t: e.matmul(PS[pi][:, 32:64], lhsT=maskL, rhs=dta[:, t, 32:64], start=True, stop=True),
             r=[("dta", t), "maskL"], w=[("ps", pi)])
        S.pe(lambda e, pi=pi, t=t: e.matmul(PS[pi][:, 64:128], lhsT=ones_f, rhs=dta[:, t, 0:64], start=True, stop=True),
             r=[("dta", t), "ones_f"], w=[("ps", pi)])
        S.act(lambda e, pi=pi, t=t: e.activation(out=acat[:, t, :], in_=PS[pi][:, 0:128], func=AF.Copy),
              r=[("ps", pi)], w=[("acat", t)])
    acv = acat[:, :, 0:64].rearrange("p t (d h) -> p t d h", d=2)
    tmpv = dta.rearrange("p t (d h) -> p t d h", d=2)
    S.dve(lambda e: e.tensor_copy(out=achl[:, :, :, 0, :], in_=acv), r=TK("acat"), w=["achl"])
    S.dve(lambda e: e.tensor_copy(out=tmpv, in_=achl[:, :, :, 0, :]), r=["achl"] + TK("dta"), w=TK("dta"))
    S.dve(lambda e: e.tensor_tensor(out=achl[:, :, :, 1, :], in0=acv, in1=tmpv, op=ALU.subtract),
          r=TK("acat") + TK("dta") + ["achl"], w=["achl"])
    S.dve(lambda e: e.tensor_tensor(out=acv, in0=tmpv, in1=achl[:, :, :, 1, :], op=ALU.add),
          r=TK("dta") + ["achl"], w=TK("acat"))
    S.dve(lambda e: e.tensor_tensor(out=biasY, in0=acat[:, :, 0:64], in1=biasY, op=ALU.subtract),
          r=TK("acat") + TK("biasY"), w=TK("biasY"))
    S.dve(lambda e: e.tensor_tensor(out=wS, in0=acat[:, :, 64:128], in1=biasY, op=ALU.subtract),
          r=TK("acat") + TK("biasY") + TK("wS"), w=TK("wS"))
    S.act(lambda e: e.activation(out=wS, in_=wS, func=AF.Exp), r=TK("wS"), w=TK("wS"))
    S.act(lambda e: e.activation(out=eat, in_=acat[:, :, 64:128], func=AF.Exp), r=TK("acat"), w=["eat"])
    S.act(lambda e: e.activation(out=ea, in_=acat[:, 2:18, 0:64], func=AF.Exp), r=TK("acat"), w=["ea"])
    S.dve(lambda e: e.tensor_copy(out=acO, in_=acat[:, 2:18, 0:64]), r=TK("acat"), w=["acO"])
    S.dve(lambda e: e.tensor_copy(out=bYo, in_=biasY[:, 2:18, :]), r=TK("biasY"), w=["bYo"])
    for c in range(16):
        pi = k.ps()
        S.pe(lambda e, pi=pi, c=c: e.transpose(out=psb(pi)[:, 0:128], in_=achl[:, c + 2].rearrange("p d x h -> p (d x h)"),
                                               identity=ident_b[:]), r=["achl", "ident_b"], w=[("ps", pi)])
        S.act(lambda e, pi=pi, c=c: e.activation(out=acT[:, c, :], in_=psb(pi)[:, 0:128], func=AF.Copy),
              r=[("ps", pi)], w=[("acT", c)])
    if "stageP2" in dbg:
        d1 = k.dout("dbg_wS", [128, NT * 64], F32)
        d2 = k.dout("dbg_acat", [128, NT * 128], F32)
        d5 = k.dout("dbg_eat", [128, NT * 64], F32)
        S.dma("sp", lambda e: e.dma_start(out=d5[:, :], in_=eat.rearrange("p a b -> p (a b)")), r=["eat"])
        d3 = k.dout("dbg_biasY", [128, NT * 64], F32)
        d4 = k.dout("dbg_acT", [128, 16 * 128], BF16)
        S.dma("sp", lambda e: e.dma_start(out=d1[:, :], in_=wS.rearrange("p a b -> p (a b)")), r=TK("wS"))
        S.dma("sp", lambda e: e.dma_start(out=d2[:, :], in_=acat.rearrange("p a b -> p (a b)")), r=TK("acat"))
        S.dma("sp", lambda e: e.dma_start(out=d3[:, :], in_=biasY.rearrange("p a b -> p (a b)")), r=TK("biasY"))
        S.dma("sp", lambda e: e.dma_start(out=d4[:, :], in_=acT.rearrange("p a b -> p (a b)")), r=[("acT", c) for c in range(16)])
        S.emit()
        k.es.close()
        return nc
    S.barrier()
    AR.off = p2mark
    WT = 4360
    preA = aalloc([WT], BF16)
    preB = aalloc([WT], BF16)
    postB = aalloc([WT], BF16)
    postC = aalloc([2824], BF16)
    xs_tok = aalloc([18, 256], BF16)
    B_tok = aalloc([18, 128], BF16)
    y_acc = aalloc([16, 256], F32)
    wgrp = aalloc([8, 768], BF16)
    Sst = [aalloc([256], F32) for _ in range(2)]
    Sbf = [aalloc([256], BF16) for _ in range(2)]
    ssq2 = aalloc([16, 8], F32)
    negbY = aalloc([16, 64], F32)
    xsp = [aalloc([256], BF16) for _ in range(2)]
    Bp = [aalloc([128], BF16) for _ in range(2)]
    ygst = [aalloc([2, 512], BF16) for _ in range(2)]
    p3mark = AR.off
    accA = wst[0][:, :, :].rearrange("p a b -> p (a b)")
    accP = xin[2]
    T1 = [[xin[0][:, 0:512], xin[0][:, 512:1024]], [xin[1][:, 0:512], xin[1][:, 512:1024]]]
    T1K = [["x0a", "x0s1"], ["x1a", "x1s1"]]
    ROPE1 = xin[0][:, 0:512]
    ROPE2 = xin[1][:, 0:512]
    T2 = [aalloc([256], F32) for _ in range(2)]
    T2K = ["t2_0", "t2_1"]
    ZS = aalloc([256], F32)
    YG = aalloc([256], F32)
    DEC = [[xn[0][:, 0:512], aalloc([512], BF16)], [xn[1][:, 0:512], aalloc([512], BF16)]]
    DECK = [["n0a", "dec01"], ["n1a", "dec11"]]
    MTt = [[xn[0][:, 512:1024], aalloc([512], BF16)], [xn[1][:, 512:1024], aalloc([512], BF16)]]
    MTK = [["n0b", "mt01"], ["n1b", "mt11"]]
    XDD = [junk[:, 0:256], junk[:, 256:512]]
    XDDK = ["j0", "j1"]
    CBM = [[junk[:, 512:640], aalloc([128], BF16)], [junk[:, 640:768], aalloc([128], BF16)]]
    CBMK = [["j2", "cbm01"], ["j3", "cbm11"]]
    YGB = junk[:, 768:1024]
    S.pool(lambda e: e.memset(preA, 0.0), w=["preA"] + [("preA", r_) for r_ in range(10)])
    S.pool(lambda e: e.memset(preB, 0.0), w=["preB"] + [("preB", r_) for r_ in range(10)])
    S.dve(lambda e: e.memset(mhalf, -0.5), w=["mhalf"])
    NM = [maskU.bitcast(BF16)[:, 0:128], maskL.bitcast(BF16)[:, 0:128]]
    for d_, (mk_, mname) in enumerate(((maskU, "maskU"), (maskL, "maskL"))):
        S.dve(lambda e, mk_=mk_: e.tensor_scalar(out=T2[0][:, 0:128], in0=mk_, scalar1=-1.0, scalar2=30000.0, op0=ALU.add, op1=ALU.mult),
              r=[mname], w=["t2_0"])
        S.dve(lambda e, d_=d_: e.tensor_copy(out=NM[d_], in_=T2[0][:, 0:128]), r=["t2_0", mname], w=[mname, "negmask"])
    S.dve(lambda e: e.memset(ssq2, 0.0), w=[("ssq2", c, g) for c in range(16) for g in range(8)])
    NBH = negbY.bitcast(BF16)
    XT = xin[0][:].rearrange("p (a b) -> p a b", a=16)
    XT2 = xin[1][:].rearrange("p (a b) -> p a b", a=16)
    S.dve(lambda e: e.tensor_scalar(out=XT, in0=bYo, scalar1=-1.0, scalar2=None, op0=ALU.mult), r=["bYo"], w=["x0a", "x0s1"])
    S.dve(lambda e: e.tensor_copy(out=NBH[:, :, 0:64], in_=XT), r=["x0a", "x0s1"], w=["negbY"])
    S.dve(lambda e: e.tensor_copy(out=XT2, in_=NBH[:, :, 0:64]), r=["negbY"], w=["x1a", "x1s1"])
    S.dve(lambda e: e.tensor_tensor(out=NBH[:, :, 64:128], in0=XT, in1=XT2, op=ALU.subtract), r=["x0a", "x0s1", "x1a", "x1s1", "negbY"], w=["negbY"])

    def bcol(t):
        return 2 + t * 128 if t < 2 else 262 + (t - 2) * 128

    def RK(name):
        return [name] + [(name, r_) for r_ in range(10)]

    W0B = wst[0][:].bitcast(BF16).rearrange("p a b -> p (a b)")
    preX = W0B[:, 0:4360]
    preC = W0B[:, 4368:4368 + 2824]
    X2B = xin[2][:].bitcast(BF16)
    DGS = [W0B[:, 7232:7232 + 640].rearrange("p (a b) -> p a b", a=5)] + \
          [X2B[:, i_ * 640:(i_ + 1) * 640].rearrange("p (a b) -> p a b", a=5) for i_ in range(3)]
    DGN = [0]
    S.pool(lambda e: e.memset(preX, 0.0), w=RK("preX"))
    S.pool(lambda e: e.memset(preC, 0.0), w=RK("preC"))

    def conv_silu(src, srck, dst, dstk, chg, lo, hi):
        dgi = DGN[0] % 4
        DGN[0] += 1
        DG = DGS[dgi]
        dgk = ("diag", dgi)
        for kk in range(5):
            S.dve(lambda e, kk=kk: e.tensor_scalar(out=DG[:, kk, :], in0=ident_b[:], scalar1=convw[:, chg, kk:kk + 1], scalar2=None, op0=ALU.mult),
                  r=["ident_b", "convw"], w=[dgk])
        blks = chunks(hi - lo, 512)
        pis = {}

        def mm(bi_):
            o, m = blks[bi_]
            pi = k.ps()
            pis[bi_] = pi
            rk = [(srck, r_) for r_ in (bi_ - 1, bi_, bi_ + 1) if 0 <= r_ < len(blks)]
            for kk in range(5):
                S.pe(lambda e, pi=pi, kk=kk, o=o, m=m: e.matmul(PS[pi][:, 0:m], lhsT=DG[:, kk, :], rhs=src[:, lo + o - 2 + kk:lo + o - 2 + kk + m],
                                                                start=(kk == 0), stop=(kk == 4)), r=rk + [dgk], w=[("ps", pi)])

        def ev(bi_):
            o, m = blks[bi_]
            pi = pis[bi_]
            S.act(lambda e, pi=pi, o=o, m=m: e.activation(out=dst[:, lo + o:lo + o + m], in_=PS[pi][:, 0:m], func=AF.Silu, bias=convb[:, chg:chg + 1]),
                  r=[("ps", pi), "convb"], w=[(dstk, bi_)])

        mm(0)
        for bi_ in range(1, len(blks)):
            mm(bi_)
            ev(bi_ - 1)
        ev(len(blks) - 1)

    NG = int(os.environ.get("P3G", "8"))

    def load_w_xbc(g_):
        for (c0, n, o) in ((C_XBC + 256 * g_, 256, 0), (C_XBC + 2048 + 128 * g_, 128, 256), (C_XBC + 3072 + 128 * g_, 128, 384)):
            S.dma("pool", lambda e, c0=c0, n=n, o=o: e.dma_start(out=wgrp[:, :, o:o + n],
                                                               in_=w_in[:, c0:c0 + n].rearrange("(c p) n -> p c n", p=128)), w=["wgrp_x"])

    def load_w_z(g_):
        c0 = C_Z + 256 * g_
        S.dma("pool", lambda e: e.dma_start(out=wgrp[:, :, 512:768], in_=w_in[:, c0:c0 + 256].rearrange("(c p) n -> p c n", p=128)), w=["wgrp_z"])

    def gating_tile(gq, ob, tt, bi):
        c = ob * 4 + tt
        st = ygst[ob % 2]
        stk = ("ygst", ob % 2)
        pz = k.ps()
        for dc in range(8):
            S.pe(lambda e, pz=pz, dc=dc: e.matmul(PS[pz][:, 0:256], lhsT=hb[bi % 2][:, dc, tt * 128:(tt + 1) * 128],
                                                  rhs=wgrp[:, dc, 512:768], start=(dc == 0), stop=(dc == 7)),
                 r=[("hb", bi % 2), "wgrp_z"], w=[("ps", pz)])
        sl = c % 2
        zsb, zsk = (ZS, "zs") if sl == 0 else (YG, "yg")
        ygb, ygk = (YGB, "j4") if sl == 0 else (DEC[0][0][:, 0:256], "n0a")
        sqo, sqk = (MTt[0][0][:, 0:256], "n0b") if sl == 0 else (MTt[0][0][:, 256:512], "n0b2")
        S.act(lambda e: e.activation(out=zsb, in_=PS[pz][:, 0:256], func=AF.Silu), r=[("ps", pz)], w=[zsk])
        S.dve(lambda e: e.tensor_tensor(out=ygb, in0=y_acc[:, c, :], in1=zsb, op=ALU.mult), r=[("y_acc", c), zsk], w=[ygk])
        S.act(lambda e: e.activation(out=sqo, in_=ygb, func=AF.Square, accum_out=ssq2[:, c, gq:gq + 1]), r=[ygk], w=[sqk, ("ssq2", c, gq)])
        pt = k.ps()
        for j in range(2):
            S.pe(lambda e, j=j: e.transpose(out=psb(pt)[:, j * 128:(j + 1) * 128], in_=ygb[:, j * 128:(j + 1) * 128], identity=ident_b[:]),
                 r=[ygk, "ident_b"], w=[("ps", pt)])
        for j in range(2):
            S.act(lambda e, j=j: e.activation(out=st[:, j, tt * 128:(tt + 1) * 128], in_=psb(pt)[:, j * 128:(j + 1) * 128],
                                              func=AF.Copy, scale=ssdnw[:, 2 * gq + j:2 * gq + j + 1]),
                  r=[("ps", pt), "ssdnw"], w=[stk])
        if tt == 3:
            S.dma("sp", lambda e: e.dma_start(out=ygT_d[:, 2 * gq:2 * gq + 2, ob * 512:(ob + 1) * 512], in_=st), r=[stk], w=[("ygT_d", gq, ob)])

    load_w_xbc(0)
    for g in range(NG):
        hd0 = 4 * g
        PRE = ((preX, "preX", 256), (preC, "preC", 384), (preA, "preA", 0), (preB, "preB", 128))
        for bi, (col0, ncol) in enumerate(blocks):
            load_hb(bi % 2, col0, ncol)
            for j, (pre, prek, wo) in enumerate(PRE):
                if j == 1 and not (256 <= col0 < 256 + 2560):
                    continue
                pi = k.ps()
                for dc in range(8):
                    S.pe(lambda e, pi=pi, dc=dc, wo=wo, bi=bi, ncol=ncol: e.matmul(
                        PS[pi][:, 0:ncol], lhsT=wgrp[:, dc, wo:wo + 128], rhs=hb[bi % 2][:, dc, 0:ncol], start=(dc == 0), stop=(dc == 7)),
                        r=[("hb", bi % 2), "wgrp_x"], w=[("ps", pi)])
                dcol = col0 + 2 if col0 < 256 else col0 + 6
                if (bi + j) % 2 == 0:
                    S.act(lambda e, pi=pi, pre=pre, dcol=dcol, ncol=ncol: e.activation(out=pre[:, dcol:dcol + ncol], in_=PS[pi][:, 0:ncol], func=AF.Copy),
                          r=[("ps", pi)], w=RK(prek))
                else:
                    S.dve(lambda e, pi=pi, pre=pre, dcol=dcol, ncol=ncol: e.tensor_copy(out=pre[:, dcol:dcol + ncol], in_=PS[pi][:, 0:ncol]),
                          r=[("ps", pi)], w=RK(prek))
            if g > 0 and 1 <= bi <= 4:
                for tt in range(4):
                    gating_tile(g - 1, bi - 1, tt, bi)
        if g + 1 < NG:
            load_w_xbc(g + 1)
        load_w_z(g)
        conv_silu(preX, "preX", postB, "postB", 16 + g, 2, 4358)
        conv_silu(preC, "preC", postC, "postC", 24 + g, 262, 262 + 2050)
        conv_silu(preA, "preA", preA, "preA", 2 * g, 2, 4358)
        conv_silu(preB, "preB", preB, "preB", 2 * g + 1, 2, 4358)
        for (buf, bk, nblk) in ((postB, "postB", 8), (postC, "postC", 4)):
            for rb in range(nblk):
                c0 = 262 + rb * 512
                e0 = rb * 512
                pi = k.ps()
                S.pe(lambda e, pi=pi, buf=buf, c0=c0: e.matmul(PS[pi][:, :], lhsT=permT, rhs=buf[:, c0:c0 + 512], start=True, stop=True),
                     r=RK(bk) + ["permT"], w=[("ps", pi)])
                S.dve(lambda e, pi=pi, e0=e0: e.tensor_tensor(out=ROPE1, in0=PS[pi][:, :], in1=sinT[:, e0:e0 + 512], op=ALU.mult),
                      r=[("ps", pi), "sinT"], w=["x0a"])
                S.pool(lambda e, buf=buf, c0=c0, e0=e0: e.tensor_tensor(out=ROPE2, in0=buf[:, c0:c0 + 512], in1=cosT[:, e0:e0 + 512], op=ALU.mult),
                       r=RK(bk) + ["cosT"], w=["x1a"])
                S.dve(lambda e, buf=buf, c0=c0: e.tensor_tensor(out=buf[:, c0:c0 + 512], in0=ROPE1, in1=ROPE2, op=ALU.add),
                      r=["x0a", "x1a"] + RK(bk), w=RK(bk))
        for t in range(18):
            pi = k.ps()
            S.pe(lambda e, pi=pi, t=t: e.transpose(out=psb(pi)[:, 0:128], in_=postB[:, bcol(t):bcol(t) + 128], identity=ident_b[:]),
                 r=RK("postB") + ["ident_b"], w=[("ps", pi)])
            S.dve(lambda e, pi=pi, t=t: e.tensor_copy(out=B_tok[:, t, :], in_=psb(pi)[:, 0:128]), r=[("ps", pi)], w=[("B_tok", t)])
        for t in range(18):
            pi = k.ps()
            for j, (pre, prek) in enumerate(((preA, "preA"), (preB, "preB"))):
                S.pe(lambda e, pi=pi, j=j, pre=pre, t=t: e.transpose(out=psb(pi)[:, j * 128:(j + 1) * 128], in_=pre[:, bcol(t):bcol(t) + 128],
                                                                    identity=ident_b[:]), r=RK(prek) + ["ident_b"], w=[("ps", pi)])
            S.act(lambda e, pi=pi, t=t: e.activation(out=xs_tok[:, t, :], in_=psb(pi)[:, 0:256], func=AF.Copy),
                  r=[("ps", pi)], w=[("xs_tok", t)])

        for d in range(2):
            S.dve(lambda e, d=d: e.memset(Sst[d], 0.0), w=[("Sst", d)])
            S.dve(lambda e, d=d: e.memset(Sbf[d], 0.0), w=[("Sbf", d)])

        SB2 = [[Sbf[0], xsp[0]], [Sbf[1], xsp[1]]]
        SB2K = [[("Sbf", 0), ("xsp", 0)], [("Sbf", 1), ("xsp", 1)]]

        def state_update(d, xs_ap, xs_k, B_ap, B_k, tg, oslot=0, xslot=None, copy=True, split=None):
            hs = d * 32 + hd0
            xq = d if xslot is None else xslot
            if split != "back":
              S.pool(lambda e: e.tensor_tensor(out=XDD[xq].rearrange("p (h q) -> p h q", h=4), in0=xs_ap.rearrange("p (h q) -> p h q", h=4),
                                             in1=wS[:, tg, hs:hs + 4].unsqueeze(2).to_broadcast([128, 4, 64]), op=ALU.mult),
                   r=[xs_k, ("wS", tg)], w=[XDDK[xq]])
            if split == "front":
                return
            pi = k.ps()
            S.pe(lambda e, pi=pi: e.matmul(PS[pi][:, 0:256], lhsT=B_ap, rhs=XDD[xq], start=True, stop=True),
                 r=[B_k, XDDK[xq]], w=[("ps", pi)])
            S.dve(lambda e: e.tensor_tensor(out=Sst[d].rearrange("p (h q) -> p h q", h=4), in0=Sst[d].rearrange("p (h q) -> p h q", h=4),
                                            in1=eat[:, tg, hs:hs + 4].unsqueeze(2).to_broadcast([128, 4, 64]), op=ALU.mult),
                  r=[("Sst", d), "eat"], w=[("Sst", d)])
            S.dve(lambda e, pi=pi: e.tensor_tensor(out=Sst[d], in0=PS[pi][:, 0:256], in1=Sst[d], op=ALU.add),
                  r=[("ps", pi), ("Sst", d)], w=[("Sst", d)])
            if copy:
                S.act(lambda e: e.activation(out=SB2[d][oslot], in_=Sst[d], func=AF.Copy), r=[("Sst", d)], w=[SB2K[d][oslot]])

        def front(d, c, sl):
            tg = c + 2
            hs = d * 32 + hd0
            cc = bcol(tg)
            pi = k.ps()
            S.pe(lambda e, pi=pi: e.matmul(PS[pi][:, 0:128], lhsT=postB[:, cc:cc + 128], rhs=postC[:, cc:cc + 128], start=True, stop=True),
                 r=RK("postB") + RK("postC"), w=[("ps", pi)])
            S.act(lambda e, pi=pi: e.activation(out=CBM[d][sl], in_=PS[pi][:, 0:128], func=AF.Copy), r=[("ps", pi)], w=[CBMK[d][sl]])
            pb = k.ps()
            S.pe(lambda e, pb=pb: e.matmul(PS[pb][:, :].rearrange("p (h q) -> p h q", h=4), lhsT=ident_b[:],
                                           rhs=NM[d].unsqueeze(1).to_broadcast([128, 4, 128]), start=True, stop=False),
                 r=["ident_b", "negmask"], w=[("ps", pb)])
            for part in range(2):
                S.pe(lambda e, pb=pb, part=part: e.matmul(PS[pb][:, :].rearrange("p (h q) -> p h q", h=4), lhsT=ident_b[:],
                                                          rhs=NBH[:, c, part * 64 + hs:part * 64 + hs + 4].unsqueeze(2).to_broadcast([128, 4, 128]),
                                                          start=False, stop=False), r=["ident_b", "negbY"], w=[("ps", pb)])
            for hh in range(4):
                S.pe(lambda e, pb=pb, hh=hh: e.matmul(PS[pb][:, hh * 128:(hh + 1) * 128], lhsT=selb[d * 64:(d + 1) * 64, hd0 + hh, :],
                                                      rhs=acT[d * 64:(d + 1) * 64, c, :], start=False, stop=(hh == 3)),
                     r=["selb", ("acT", c)], w=[("ps", pb)])
            S.act(lambda e, pb=pb: e.activation(out=DEC[d][sl], in_=PS[pb][:, :], func=AF.Exp), r=[("ps", pb)], w=[DECK[d][sl]])
            S.dve(lambda e: e.tensor_tensor(out=MTt[d][sl].rearrange("p (h q) -> p h q", h=4), in0=DEC[d][sl].rearrange("p (h q) -> p h q", h=4),
                                             in1=CBM[d][sl].unsqueeze(1).to_broadcast([128, 4, 128]), op=ALU.mult),
                   r=[DECK[d][sl], CBMK[d][sl]], w=[MTK[d][sl]])

        def back(d, c, sl, islot=0):
            tg = c + 2
            hs = d * 32 + hd0
            cc = bcol(tg)
            py = k.ps()
            for hh in range(4):
                S.pe(lambda e, py=py, hh=hh: e.matmul(PS[py][:, hh * 64:(hh + 1) * 64], lhsT=MTt[d][sl][:, hh * 128:(hh + 1) * 128],
                                                      rhs=xs_tok[:, tg, hh * 64:(hh + 1) * 64], start=True, stop=True),
                     r=[MTK[d][sl], ("xs_tok", tg)], w=[("ps", py)])
            S.pe(lambda e, py=py: e.matmul(PS[py][:, 256:512], lhsT=postC[:, cc:cc + 128], rhs=SB2[d][islot], start=True, stop=True),
                 r=RK("postC") + [SB2K[d][islot]], w=[("ps", py)])
            S.dve(lambda e, py=py: e.tensor_tensor(out=T2[d].rearrange("p (h q) -> p h q", h=4), in0=PS[py][:, 256:512].rearrange("p (h q) -> p h q", h=4),
                                                   in1=ea[:, c, hs:hs + 4].unsqueeze(2).to_broadcast([128, 4, 64]), op=ALU.mult),
                  r=[("ps", py), "ea"], w=[T2K[d]])
            S.pool(lambda e: e.tensor_tensor(out=y_acc[:, c, :], in0=y_acc[:, c, :], in1=T2[d], op=ALU.add),
                   r=[T2K[d], ("y_acc", c)], w=[("y_acc", c)])
            S.dve(lambda e, py=py: e.tensor_tensor(out=y_acc[:, c, :], in0=PS[py][:, 0:256], in1=y_acc[:, c, :], op=ALU.add),
                  r=[("ps", py), ("y_acc", c)], w=[("y_acc", c)])

        for c in range(16):
            S.pool(lambda e, c=c: e.tensor_tensor(out=y_acc[:, c, :].rearrange("p (h q) -> p h q", h=4),
                                                  in0=xs_tok[:, c + 2, :].rearrange("p (h q) -> p h q", h=4),
                                                  in1=dsk_b[:, hd0:hd0 + 4].unsqueeze(2).to_broadcast([128, 4, 64]), op=ALU.mult),
                   r=[("xs_tok", c + 2), "dsk_b"], w=[("y_acc", c)])
        for t in (1, 0):
            state_update(1, xs_tok[:, t, :], ("xs_tok", t), B_tok[:, t, :], ("B_tok", t), t)
        plist = list(range(33, 17, -1))

        def pfront(t):
            q = t % 2
            pi = k.ps()
            for j, (pre, prek) in enumerate(((preA, "preA"), (preB, "preB"))):
                S.pe(lambda e, j=j, pre=pre: e.transpose(out=psb(pi)[:, j * 128:(j + 1) * 128], in_=pre[:, bcol(t):bcol(t) + 128],
                                                         identity=ident_b[:]), r=RK(prek) + ["ident_b"], w=[("ps", pi)])
            S.pe(lambda e: e.transpose(out=psb(pi)[:, 256:384], in_=postB[:, bcol(t):bcol(t) + 128], identity=ident_b[:]),
                 r=RK("postB") + ["ident_b"], w=[("ps", pi)])
            S.act(lambda e: e.activation(out=xsp[q], in_=psb(pi)[:, 0:256], func=AF.Copy), r=[("ps", pi)], w=[("xsp", q)])
            S.act(lambda e: e.activation(out=Bp[q], in_=psb(pi)[:, 256:384], func=AF.Copy), r=[("ps", pi)], w=[("Bp", q)])
            state_update(1, xsp[q], ("xsp", q), Bp[q], ("Bp", q), t, xslot=q, split="front")

        def pback(t, last):
            q = t % 2
            state_update(1, xsp[q], ("xsp", q), Bp[q], ("Bp", q), t, xslot=q, split="back", copy=last)

        pfront(plist[0])
        for n_ in range(len(plist)):
            if n_ + 1 < len(plist):
                pfront(plist[n_ + 1])
            pback(plist[n_], n_ == len(plist) - 1)
        for t in (0, 1):
            state_update(0, xs_tok[:, t, :], ("xs_tok", t), B_tok[:, t, :], ("B_tok", t), t)
        for i in range(17):
            if i >= 1:
                c1, c2 = i - 1, 16 - i
                j = i - 1
                if c1 < 15:
                    state_update(0, xs_tok[:, c1 + 2, :], ("xs_tok", c1 + 2), B_tok[:, c1 + 2, :], ("B_tok", c1 + 2), c1 + 2, oslot=(j + 1) % 2, split="front")
                if c2 > 0:
                    state_update(1, xs_tok[:, c2 + 2, :], ("xs_tok", c2 + 2), B_tok[:, c2 + 2, :], ("B_tok", c2 + 2), c2 + 2, oslot=(j + 1) % 2, split="front")
            if i < 16:
                front(0, i, i % 2)
                front(1, 15 - i, i % 2)
            if i >= 1:
                if c1 < 15:
                    state_update(0, xs_tok[:, c1 + 2, :], ("xs_tok", c1 + 2), B_tok[:, c1 + 2, :], ("B_tok", c1 + 2), c1 + 2, oslot=(j + 1) % 2, split="back")
                if c2 > 0:
                    state_update(1, xs_tok[:, c2 + 2, :], ("xs_tok", c2 + 2), B_tok[:, c2 + 2, :], ("B_tok", c2 + 2), c2 + 2, oslot=(j + 1) % 2, split="back")
                back(0, c1, (i - 1) % 2, islot=j % 2)
                back(1, c2, (i - 1) % 2, islot=j % 2)

    for ob in range(4 if NG >= 1 else 0):
        bi = ob + 1
        col0, ncol = blocks[bi]
        load_hb(bi % 2, col0, ncol)
        for tt in range(4):
            gating_tile(NG - 1, ob, tt, bi)

    if NG == 8:
        S.dve(lambda e: e.tensor_reduce(out=rstd_ssd, in_=ssq2, axis=AX.X, op=ALU.add), r=[("ssq2", c, g) for c in range(16) for g in range(8)], w=["rstd_ssd"])
        S.dve(lambda e: e.tensor_scalar(out=rstd_ssd, in0=rstd_ssd, scalar1=1.0 / D_SSD, scalar2=EPS, op0=ALU.mult, op1=ALU.add), r=["rstd_ssd"], w=["rstd_ssd"])
        S.pool(lambda e: e.tensor_tensor(out=rstd_ssd, in0=rstd_ssd, in1=mhalf[:, 0:1].to_broadcast([128, 16]), op=ALU.pow), r=["rstd_ssd", "mhalf"], w=["rstd_ssd"])
    if "stageP3" in dbg:
        dssq = k.dout("dbg_ssq2", [128, 128], F32)
        S.dma("sp", lambda e: e.dma_start(out=dssq[:, :], in_=ssq2.rearrange("p a b -> p (a b)")), r=[("ssq2", c, g) for c in range(16) for g in range(NG)])
        S.emit()
        k.es.close()
        return nc
    S.barrier()
    AR.off = pmark
    NKT = 20
    nabias_d = k.din("nabias", [16, 128, 3 * 5 * 128])
    qkw_d = k.din("qkw", [1, 1024])
    ynaT_d = scr("ynaT_d", [128, 8, OWN], BF16)
    wq = aalloc([3, 8, 512], BF16)
    qT = aalloc([4, OWN], BF16)
    kT = aalloc([4, NKT * 128], BF16)
    Vaug = aalloc([NKT, 8, 65], BF16)
    qkw = aalloc([1024], F32)
    Ef = aalloc([15 * 128], F32)
    Eb = aalloc([15 * 128], BF16)
    PTs = [aalloc([7 * 128], BF16) for _ in range(3)]
    NSL = 5
    sqbs = [aalloc([512], F32) for _ in range(NSL)]
    nrms = [aalloc([512], F32) for _ in range(NSL)]
    qtks = [aalloc([512], BF16) for _ in range(NSL)]
    ss8s = [aalloc([8], F32) for _ in range(NSL)]
    denall = aalloc([16, 8], F32)
    ynatok = aalloc([16, 512], BF16)
    ynst = [aalloc([4, 512], BF16) for _ in range(2)]
    S.dma("sp", lambda e: e.dma_start(out=qkw, in_=qkw_d[0:1, :].to_broadcast([128, 1024])), w=["qkw"])
    S.dve(lambda e: e.memset(Vaug, 1.0), w=["Vaug"])

    def ktile_cols(kt):
        return kt * 128 if kt < 2 else 256 + (kt - 2) * 128

    for hp in range(2):
        for j3 in range(3 if hp == 0 else 0):
            c0 = C_QKV + j3 * 1024 + hp * 512
            S.dma("pool", lambda e, j3=j3, c0=c0: e.dma_start(out=wq[:, j3, :, :], in_=w_in[:, c0:c0 + 512].rearrange("(c p) n -> p c n", p=128)), w=["wq"])
        kblocks = [(0, 256)] + [(256 + 512 * i, 512) for i in range(5)]
        items = []
        for bi, (col0, ncol) in enumerate(kblocks):
            nt_ = 2 if bi == 5 else ncol // 128
            for tt in range(nt_):
                kt = col0 // 128 + tt
                own = 2 <= kt < 18
                for j3 in ((0, 1, 2) if own else (1, 2)):
                    items.append((bi, col0, ncol, tt, kt, j3, tt == 0 and j3 == (0 if own else 1)))
        stA = {}

        def stageA(n):
            bi, col0, ncol, tt, kt, j3, first = items[n]
            if first:
                load_hb(bi % 2, col0, ncol)
            sl = n % NSL
            pi = k.ps()
            for dc in range(8):
                S.pe(lambda e, dc=dc: e.matmul(PS[pi][:, :], lhsT=hb[bi % 2][:, dc, tt * 128:(tt + 1) * 128],
                                               rhs=wq[:, j3, dc, :], start=(dc == 0), stop=(dc == 7)),
                     r=[("hb", bi % 2), "wq"], w=[("ps", pi)])
            stA[n] = pi
            if j3 == 2:
                S.act(lambda e: e.activation(out=Vaug[:, kt, :, 0:64], in_=PS[pi][:, :].rearrange("p (h d) -> p h d", h=8), func=AF.Copy),
                      r=[("ps", pi)], w=["Vaug"])
                return
            S.act(lambda e: e.activation(out=sqbs[sl], in_=PS[pi][:, :], func=AF.Square), r=[("ps", pi)], w=[("sqb", sl)])
            S.dve(lambda e: e.tensor_reduce(out=ss8s[sl], in_=sqbs[sl].rearrange("p (h d) -> p h d", h=8), axis=AX.X, op=ALU.add), r=[("sqb", sl)], w=[("ss8", sl)])
            S.dve(lambda e: e.tensor_scalar(out=ss8s[sl], in0=ss8s[sl], scalar1=1.0 / 64, scalar2=EPS, op0=ALU.mult, op1=ALU.add), r=[("ss8", sl)], w=[("ss8", sl)])
            S.pool(lambda e: e.tensor_tensor(out=ss8s[sl], in0=ss8s[sl], in1=mhalf[:, 0:1].to_broadcast([128, 8]), op=ALU.pow), r=[("ss8", sl), "mhalf"], w=[("ss8", sl)])

        def stageB(n):
            bi, col0, ncol, tt, kt, j3, first = items[n]
            if j3 == 2:
                return
            sl = n % NSL
            pi = stA[n]
            S.dve(lambda e: e.tensor_tensor(out=nrms[sl].rearrange("p (h d) -> p h d", h=8), in0=PS[pi][:, :].rearrange("p (h d) -> p h d", h=8),
                                            in1=ss8s[sl].unsqueeze(2).to_broadcast([128, 8, 64]), op=ALU.mult), r=[("ps", pi), ("ss8", sl)], w=[("nrm", sl)])
            S.dve(lambda e: e.tensor_tensor(out=qtks[sl], in0=nrms[sl], in1=qkw[:, j3 * 512:(j3 + 1) * 512], op=ALU.mult), r=[("nrm", sl), "qkw"], w=[("qtk", sl)])
            pt = k.ps()
            for pr in range(4):
                S.pe(lambda e, pr=pr: e.transpose(out=psb(pt)[:, pr * 128:(pr + 1) * 128], in_=qtks[sl][:, pr * 128:(pr + 1) * 128], identity=ident_b[:]),
                     r=[("qtk", sl), "ident_b"], w=[("ps", pt)])
            if j3 == 0:
                c = kt - 2
                S.act(lambda e: e.activation(out=qT[:, :, c * 128:(c + 1) * 128], in_=psb(pt)[:, 0:512].rearrange("p (a b) -> p a b", a=4), func=AF.Copy),
                      r=[("ps", pt)], w=[("qT", c)])
            else:
                S.act(lambda e: e.activation(out=kT[:, :, kt * 128:(kt + 1) * 128], in_=psb(pt)[:, 0:512].rearrange("p (a b) -> p a b", a=4), func=AF.Copy),
                      r=[("ps", pt)], w=[("kT", kt)])

        LEAD = NSL - 1
        for n in range(len(items) + LEAD):
            if n < len(items):
                stageA(n)
            if n >= LEAD:
                stageB(n - LEAD)
        if hp == 0:
            for j3 in range(3):
                c0 = C_QKV + j3 * 1024 + 512
                S.dma("pool", lambda e, j3=j3, c0=c0: e.dma_start(out=wq[:, j3, :, :], in_=w_in[:, c0:c0 + 512].rearrange("(c p) n -> p c n", p=128)), w=["wq"])
        for hl in range(8):
            h = hp * 8 + hl
            pr, po = hl // 2, (hl % 2) * 64
            S.dma("sp", lambda e, h=h: e.dma_start(out=Ef, in_=nabias_d[h, :, :]), w=["Ef"])
            S.act(lambda e: e.activation(out=Eb, in_=Ef, func=AF.Exp), r=["Ef"], w=["Eb"])
            def na_front(i, sl):
                cls = min(i, 2)
                kts = [2 + x for x in ([0, 1, 2, 3, 4] if i < 2 else range(i - 2, i + 3))] + [0, 1]
                pa, pb_ = k.ps(), k.ps()
                for n_, kt in enumerate(kts):
                    dstp = PS[pa][:, n_ * 128:(n_ + 1) * 128] if n_ < 4 else PS[pb_][:, (n_ - 4) * 128:(n_ - 3) * 128]
                    S.pe(lambda e, dstp=dstp, kt=kt, i=i: e.matmul(dstp, lhsT=kT[po:po + 64, pr, kt * 128:(kt + 1) * 128],
                                                                   rhs=qT[po:po + 64, pr, i * 128:(i + 1) * 128], start=True, stop=True),
                         r=[("kT", kt), ("qT", i)], w=[("ps", pa if n_ < 4 else pb_)])
                S.act(lambda e, pa=pa: e.activation(out=PTs[sl][:, 0:512], in_=PS[pa][:, :], func=AF.Exp), r=[("ps", pa)], w=[("PTa", sl)])
                S.act(lambda e, pb_=pb_: e.activation(out=PTs[sl][:, 512:896], in_=PS[pb_][:, 0:384], func=AF.Exp), r=[("ps", pb_)], w=[("PTb", sl)])
                S.dve(lambda e: e.tensor_tensor(out=PTs[sl][:, 0:512], in0=PTs[sl][:, 0:512], in1=Eb[:, cls * 640:cls * 640 + 512], op=ALU.mult),
                      r=[("PTa", sl), "Eb"], w=[("PTa", sl)])
                S.dve(lambda e: e.tensor_tensor(out=PTs[sl][:, 512:640], in0=PTs[sl][:, 512:640], in1=Eb[:, cls * 640 + 512:cls * 640 + 640], op=ALU.mult),
                      r=[("PTb", sl), "Eb"], w=[("PTb", sl)])

            def na_back(i, sl):
                kts = [2 + x for x in ([0, 1, 2, 3, 4] if i < 2 else range(i - 2, i + 3))] + [0, 1]
                po_ = k.ps()
                for n_, kt in enumerate(kts):
                    S.pe(lambda e, po_=po_, n_=n_, kt=kt: e.matmul(PS[po_][:, 0:65], lhsT=PTs[sl][:, n_ * 128:(n_ + 1) * 128],
                                                                 rhs=Vaug[:, kt, hl, :], start=(n_ == 0), stop=(n_ == 6)),
                         r=[("PTa", sl), ("PTb", sl), "Vaug"], w=[("ps", po_)])
                S.dve(lambda e, po_=po_: e.tensor_copy(out=denall[:, i, hl:hl + 1], in_=PS[po_][:, 64:65]), r=[("ps", po_)], w=[("den", i, hl)])
                S.act(lambda e, po_=po_: e.activation(out=ynatok[:, i, hl * 64:(hl + 1) * 64], in_=PS[po_][:, 0:64], func=AF.Copy),
                      r=[("ps", po_)], w=[("ynatok", i, hl)])

            for it in range(18):
                if it < 16:
                    na_front(it, it % 3)
                if it >= 2:
                    na_back(it - 2, (it - 2) % 3)
        allden = [("den", i_, h_) for i_ in range(16) for h_ in range(8)]
        S.pool(lambda e: e.tensor_tensor(out=denall.rearrange("p a b -> p (a b)"), in0=denall.rearrange("p a b -> p (a b)"),
                                         in1=onesc[:, 0:1].to_broadcast([128, 128]), op=ALU.pow), r=allden + ["onesc"], w=["rden"])
        for i in range(16):
            S.dve(lambda e, i=i: e.tensor_tensor(out=ynatok[:, i, :].rearrange("p (h d) -> p h d", h=8), in0=ynatok[:, i, :].rearrange("p (h d) -> p h d", h=8),
                                                 in1=denall[:, i, :].unsqueeze(2).to_broadcast([128, 8, 64]), op=ALU.mult),
                  r=["rden"] + [("ynatok", i, h_) for h_ in range(8)], w=[("ynatok", i)])
        for i in range(16):
            pt = k.ps()
            for pr in range(4):
                S.pe(lambda e, pt=pt, pr=pr, i=i: e.transpose(out=psb(pt)[:, pr * 128:(pr + 1) * 128], in_=ynatok[:, i, pr * 128:(pr + 1) * 128], identity=ident_b[:]),
                     r=[("ynatok", i), "ident_b"], w=[("ps", pt)])
            sq_ = (i // 4) % 2
            st = ynst[sq_]
            S.act(lambda e, pt=pt, st=st, i=i: e.activation(out=st[:, :, (i % 4) * 128:(i % 4 + 1) * 128], in_=psb(pt)[:, 0:512].rearrange("p (a b) -> p a b", a=4), func=AF.Copy),
                  r=[("ps", pt)], w=[("ynst", sq_)])
            if i % 4 == 3:
                S.dma("sp", lambda e, st=st, i=i, hp=hp: e.dma_start(out=ynaT_d[:, hp * 4:hp * 4 + 4, (i // 4) * 512:(i // 4 + 1) * 512], in_=st),
                      r=[("ynst", sq_)], w=[("ynaT_d", hp, i // 4)])
    if "stageP4" in dbg:
        S.emit()
        k.es.close()
        return nc
    S.barrier()
    AR.off = pmark
    wbrs_d = k.din("w_br_ssd", [D_SSD, D])
    wbrn_d = k.din("w_br_na", [D, D])
    wout_d = k.din("w_out", [D, D])
    wrt_d = k.din("w_rt36", [D, 36])
    brt_d = k.din("b_rt36", [1, 36])
    w1_d = k.din("w1", [NEXP, D, 512])
    w3_d = k.din("w3", [NEXP, D, 512])
    w2_d = k.din("w2", [NEXP, 512, D])
    h2T = aalloc([8, OWN], BF16)
    gates = aalloc([16, 32], F32)
    gbb = aalloc([2048], F32)
    wrt = aalloc([8, 36], F32)
    brt = aalloc([36], F32)
    x1 = aalloc([16, D], F32)
    p5mark = AR.off
    AR.off = p5mark - 16 * D * 4
    wbrs = aalloc([16, D], BF16)
    wbrn = aalloc([8, D], BF16)
    wgt = aalloc([8, 2048], BF16)
    ygt = [aalloc([16, 128], BF16) for _ in range(2)]
    ynt_ = [aalloc([8, 128], BF16) for _ in range(2)]
    sg = aalloc([512], F32)
    m1 = aalloc([D], F32)
    mrg = aalloc([D], BF16)
    S.dma("sp", lambda e: e.dma_start(out=gbb, in_=gb_d[0:1, :].to_broadcast([128, 2048])), w=["gbb"])
    S.dma("sp", lambda e: e.dma_start(out=wrt, in_=wrt_d.rearrange("(c p) n -> p c n", p=128)), w=["wrt"])
    S.dma("sp", lambda e: e.dma_start(out=brt, in_=brt_d[0:1, :].to_broadcast([128, 36])), w=["brt"])
    S.dma("pool", lambda e: e.dma_start(out=wbrs, in_=wbrs_d.rearrange("(c p) n -> p c n", p=128)), w=["wbrs"])
    S.dma("pool", lambda e: e.dma_start(out=wbrn, in_=wbrn_d.rearrange("(c p) n -> p c n", p=128)), w=["wbrn"])
    S.dma("pool", lambda e: e.dma_start(out=wgt, in_=w_in[:, C_G:C_G + 2048].rearrange("(c p) n -> p c n", p=128)), w=["wgt"])
    ssq3 = ssq
    for ob in range(4):
        bi = ob + 1
        col0, ncol = blocks[bi]
        load_hb(bi % 2, col0, ncol)
        for tt in range(4):
            c = ob * 4 + tt
            q = c % 2
            S.dma("sp", lambda e, q=q, c=c: e.dma_start(out=ygt[q], in_=ygT_d[:, :, c * 128:(c + 1) * 128]), w=[("ygt", q)])
            S.dma("sp", lambda e, q=q, c=c: e.dma_start(out=ynt_[q], in_=ynaT_d[:, :, c * 128:(c + 1) * 128]), w=[("ynt", q)])
            for nb_ in range(2):
                cs_ = slice(nb_ * 512, (nb_ + 1) * 512)
                pa = k.ps()
                for ch in range(16):
                    S.pe(lambda e, pa=pa, ch=ch, q=q: e.matmul(PS[pa][:, :], lhsT=ygt[q][:, ch, :], rhs=wbrs[:, ch, cs_], start=(ch == 0), stop=(ch == 15)),
                         r=[("ygt", q), "wbrs"], w=[("ps", pa)])
                pg = k.ps()
                for dc in range(8):
                    S.pe(lambda e, pg=pg, dc=dc: e.matmul(PS[pg][:, :], lhsT=hb[bi % 2][:, dc, tt * 128:(tt + 1) * 128], rhs=wgt[:, dc, nb_ * 512:(nb_ + 1) * 512],
                                                          start=(dc == 0), stop=(dc == 7)), r=[("hb", bi % 2), "wgt"], w=[("ps", pg)])
                S.act(lambda e, pg=pg: e.activation(out=sg, in_=PS[pg][:, :], func=AF.Sigmoid), r=[("ps", pg)], w=["sg"])
                S.dve(lambda e, pa=pa, c=c: e.scalar_tensor_tensor(out=m1[:, cs_], in0=PS[pa][:, :], scalar=rstd_ssd[:, c:c + 1], in1=sg, op0=ALU.mult, op1=ALU.mult),
                      r=[("ps", pa), "rstd_ssd", "sg"], w=[("m1", nb_)])
                pn = k.ps()
                for ch in range(8):
                    S.pe(lambda e, pn=pn, ch=ch, q=q: e.matmul(PS[pn][:, :], lhsT=ynt_[q][:, ch, :], rhs=wbrn[:, ch, cs_], start=(ch == 0), stop=(ch == 7)),
                         r=[("ynt", q), "wbrn"], w=[("ps", pn)])
                pg2 = k.ps()
                for dc in range(8):
                    S.pe(lambda e, pg2=pg2, dc=dc: e.matmul(PS[pg2][:, :], lhsT=hb[bi % 2][:, dc, tt * 128:(tt + 1) * 128], rhs=wgt[:, dc, 1024 + nb_ * 512:1024 + (nb_ + 1) * 512],
                                                            start=(dc == 0), stop=(dc == 7)), r=[("hb", bi % 2), "wgt"], w=[("ps", pg2)])
                S.act(lambda e, pg2=pg2: e.activation(out=sg, in_=PS[pg2][:, :], func=AF.Sigmoid), r=[("ps", pg2)], w=["sg"])
                S.dve(lambda e, pn=pn: e.tensor_tensor(out=sg, in0=PS[pn][:, :], in1=sg, op=ALU.mult), r=[("ps", pn), "sg"], w=["sg"])
                S.pool(lambda e: e.tensor_tensor(out=mrg[:, cs_], in0=m1[:, cs_], in1=sg, op=ALU.add), r=[("m1", nb_), "sg"], w=[("mrg", nb_)])
            pt = k.ps()
            for ch in range(8):
                S.pe(lambda e, pt=pt, ch=ch: e.transpose(out=psb(pt)[:, ch * 128:(ch + 1) * 128], in_=mrg[:, ch * 128:(ch + 1) * 128], identity=ident_b[:]),
                     r=[("mrg", 0), ("mrg", 1), "ident_b"], w=[("ps", pt)])
            S.act(lambda e, pt=pt, c=c: e.activation(out=h2T[:, :, c * 128:(c + 1) * 128], in_=psb(pt)[:, :].rearrange("p (a b) -> p a b", a=8), func=AF.Copy),
                  r=[("ps", pt)], w=[("h2T", c)])
    S.barrier()
    AR.off = p5mark
    wout = aalloc([8, D], BF16)
    sg = aalloc([512], F32)
    m1 = aalloc([D], F32)
    h2f = aalloc([8, 128], F32)
    rla = aalloc([16, 36], F32)
    rv = aalloc([7, 16], F32)
    oh4 = aalloc([16, 4], F32)
    ge4 = aalloc([16, 4], F32)
    Em = aalloc([16, 32], F32)
    eq1 = aalloc([16, 32], F32)
    eq2 = aalloc([16, 32], F32)
    p6mark = AR.off
    S.dma("pool", lambda e: e.dma_start(out=wout, in_=wout_d.rearrange("(c p) n -> p c n", p=128)), w=["wout"])
    m1b = aalloc([D], F32)
    xsc2 = aalloc([D], F32)
    M1S = [m1, m1b]
    XLD = [xin[1], xin[2]]
    XSC = [xin[0], xsc2]

    def p5b_stage1(c):
        q = c % 2
        xl, xlk = XLD[q], f"xld{q}"
        mm_, mk_ = M1S[q], f"m1s{q}"
        xs_, xsk = XSC[q], f"xsc{q}"
        S.dma("sp", lambda e: e.dma_start(out=xl[:], in_=x_ext[c * 128:(c + 1) * 128, :]), w=[xlk])
        for nb_ in range(2):
            cs_ = slice(nb_ * 512, (nb_ + 1) * 512)
            po_ = k.ps()
            for ch in range(8):
                S.pe(lambda e, ch=ch: e.matmul(PS[po_][:, :], lhsT=h2T[:, ch, c * 128:(c + 1) * 128], rhs=wout[:, ch, cs_], start=(ch == 0), stop=(ch == 7)),
                     r=[("h2T", c), "wout"], w=[("ps", po_)])
            S.dve(lambda e: e.tensor_tensor(out=mm_[:, cs_], in0=PS[po_][:, :], in1=gbb[:, cs_], op=ALU.mult), r=[("ps", po_), "gbb"], w=[(mk_, nb_)])
            S.pool(lambda e: e.tensor_tensor(out=x1[:, c, cs_], in0=mm_[:, cs_], in1=xl[:, cs_], op=ALU.add), r=[(mk_, nb_), xlk], w=[("x1", c, nb_)])
        S.act(lambda e: e.activation(out=junk[:], in_=x1[:, c, :], func=AF.Square, accum_out=ssq3[:, c:c + 1]), r=[("x1", c, 0), ("x1", c, 1)], w=["junk", ("ssq3", c)])
        S.act(lambda e: e.activation(out=rstd[:, c:c + 1], in_=ssq3[:, c:c + 1], func=AF.Sqrt, scale=1.0 / D, bias=epsb[:, 0:1]), r=[("ssq3", c)], w=[("rstd3", c)])
        S.pool(lambda e: e.tensor_tensor(out=rstd[:, c:c + 1], in0=rstd[:, c:c + 1], in1=onesc[:, 0:1], op=ALU.pow), r=[("rstd3", c)], w=[("rstd3", c)])
        S.dve(lambda e: e.tensor_scalar(out=xs_[:] if q == 0 else xs_, in0=x1[:, c, :], scalar1=rstd[:, c:c + 1], scalar2=None, op0=ALU.mult),
              r=[("x1", c, 0), ("x1", c, 1), ("rstd3", c)], w=[xsk])

    def p5b_stage2(c):
        q = c % 2
        xs_, xsk = XSC[q], f"xsc{q}"
        pf1, pf2 = k.ps(), k.ps()
        for ch in range(8):
            pp = pf1 if ch < 4 else pf2
            S.pe(lambda e, pp=pp, ch=ch: e.transpose(out=PS[pp][:, (ch % 4) * 128:(ch % 4 + 1) * 128], in_=xs_[:, ch * 128:(ch + 1) * 128], identity=ident_f[:]),
                 r=[xsk, "ident_f"], w=[("ps", pp)])
        for ch in range(8):
            pp = pf1 if ch < 4 else pf2
            S.act(lambda e, pp=pp, ch=ch: e.activation(out=h2f[:, ch, :], in_=PS[pp][:, (ch % 4) * 128:(ch % 4 + 1) * 128], func=AF.Identity,
                                                       scale=A2[:, ch:ch + 1], bias=modT2[:, 24 + ch, 0:1]), r=[("ps", pp), "A2", "modT2"], w=[("h2f", ch)])
        S.pool(lambda e: e.tensor_copy(out=h2T[:, :, c * 128:(c + 1) * 128], in_=h2f), r=[("h2f", ch) for ch in range(8)], w=[("h2T", c)])
        pr_ = k.ps()
        for ch in range(8):
            S.pe(lambda e, ch=ch: e.matmul(PS[pr_][:, 0:36], lhsT=h2f[:, ch, :], rhs=wrt[:, ch, :], start=(ch == 0), stop=(ch == 7)),
                 r=[("h2f", ch), "wrt"], w=[("ps", pr_)])
        S.dve(lambda e: e.tensor_tensor(out=rla[:, c, :], in0=PS[pr_][:, 0:36], in1=brt, op=ALU.add), r=[("ps", pr_), "brt"], w=[("rla", c)])

    p5b_stage1(0)
    for c in range(1, 16):
        p5b_stage1(c)
        p5b_stage2(c - 1)
    p5b_stage2(15)
    RLA = [("rla", c) for c in range(16)]
    Gv = rla[:, :, 0:4]
    Ev = rla[:, :, 4:36]
    bc4 = lambda v: v.unsqueeze(2).to_broadcast([128, 16, 4])
    bc32 = lambda v: v.unsqueeze(2).to_broadcast([128, 16, 32])
    S.dve(lambda e: e.tensor_reduce(out=rv[:, 0, :], in_=Gv, axis=AX.X, op=ALU.max), r=RLA, w=["rv0"])
    S.dve(lambda e: e.tensor_tensor(out=oh4, in0=Gv, in1=bc4(rv[:, 0, :]), op=ALU.is_equal), r=RLA + ["rv0"], w=["oh4"])
    S.dve(lambda e: e.tensor_tensor(out=ge4, in0=Gv, in1=bc4(rv[:, 0, :]), op=ALU.subtract), r=RLA + ["rv0"], w=["ge4"])
    S.act(lambda e: e.activation(out=ge4, in_=ge4, func=AF.Exp), r=["ge4"], w=["ge4"])
    S.dve(lambda e: e.tensor_reduce(out=rv[:, 1, :], in_=ge4, axis=AX.X, op=ALU.add), r=["ge4"], w=["rv1"])
    S.dve(lambda e: e.tensor_scalar(out=oh4, in0=oh4, scalar1=-1.0, scalar2=1e30, op0=ALU.add, op1=ALU.mult), r=["oh4"], w=["oh4"])
    S.dve(lambda e: e.tensor_tensor(out=Em.rearrange("p t (g x) -> p t g x", g=4), in0=Ev.rearrange("p t (g x) -> p t g x", g=4),
                                    in1=oh4.unsqueeze(3).to_broadcast([128, 16, 4, 8]), op=ALU.add), r=RLA + ["oh4"], w=["Em"])
    S.dve(lambda e: e.tensor_reduce(out=rv[:, 2, :], in_=Em, axis=AX.X, op=ALU.max), r=["Em"], w=["rv2"])
    S.dve(lambda e: e.tensor_tensor(out=eq1, in0=Em, in1=bc32(rv[:, 2, :]), op=ALU.is_equal), r=["Em", "rv2"], w=["eq1"])
    S.dve(lambda e: e.scalar_tensor_tensor(out=Em, in0=eq1, scalar=-1e30, in1=Em, op0=ALU.mult, op1=ALU.add), r=["eq1", "Em"], w=["Em"])
    S.dve(lambda e: e.tensor_reduce(out=rv[:, 3, :], in_=Em, axis=AX.X, op=ALU.max), r=["Em"], w=["rv3"])
    S.dve(lambda e: e.tensor_tensor(out=eq2, in0=Em, in1=bc32(rv[:, 3, :]), op=ALU.is_equal), r=["Em", "rv3"], w=["eq2"])
    S.dve(lambda e: e.tensor_tensor(out=rv[:, 4, :], in0=rv[:, 3, :], in1=rv[:, 2, :], op=ALU.subtract), r=["rv2", "rv3"], w=["rv4"])
    S.act(lambda e: e.activation(out=rv[:, 4, :], in_=rv[:, 4, :], func=AF.Exp), r=["rv4"], w=["rv4"])
    S.dve(lambda e: e.tensor_scalar(out=rv[:, 5, :], in0=rv[:, 4, :], scalar1=1.0, scalar2=None, op0=ALU.add), r=["rv4"], w=["rv5"])
    S.dve(lambda e: e.tensor_tensor(out=rv[:, 5, :], in0=rv[:, 5, :], in1=rv[:, 1, :], op=ALU.mult), r=["rv5", "rv1"], w=["rv5"])
    S.pool(lambda e: e.tensor_tensor(out=rv[:, 5, :], in0=rv[:, 5, :], in1=onesc[:, 0:1].to_broadcast([128, 16]), op=ALU.pow), r=["rv5", "onesc"], w=["rv5"])
    S.dve(lambda e: e.tensor_tensor(out=rv[:, 6, :], in0=rv[:, 5, :], in1=rv[:, 4, :], op=ALU.mult), r=["rv5", "rv4"], w=["rv6"])
    S.dve(lambda e: e.tensor_tensor(out=eq1, in0=eq1, in1=bc32(rv[:, 5, :]), op=ALU.mult), r=["eq1", "rv5"], w=["eq1"])
    S.dve(lambda e: e.tensor_tensor(out=eq2, in0=eq2, in1=bc32(rv[:, 6, :]), op=ALU.mult), r=["eq2", "rv6"], w=["eq2"])
    S.dve(lambda e: e.tensor_tensor(out=gates, in0=eq1, in1=eq2, op=ALU.add), r=["eq1", "eq2"], w=[("gates", c) for c in range(16)])

    S.barrier()
    AR.off = p5mark
    w13a = aalloc([2, 8, 512], BF16)
    w13 = [w13a, wst[0][:].bitcast(BF16).rearrange("p a b -> p (a b)").rearrange("p (j c n) -> p j c n", j=2, c=8)]
    w2b1 = aalloc([4, D], BF16)
    w2b = [w2b1, hTt[0][:].rearrange("p a b -> p (a b)").rearrange("p (c n) -> p c n", c=4)]
    m1 = aalloc([D], F32)
    aT = aalloc([4, 512], BF16)
    hs1 = aalloc([512], F32)
    NE = int(os.environ.get("NEXPERTS", "32"))
    def load_expert(ex):
        q = ex % 2
        S.dma("pool", lambda e: e.dma_start(out=w13[q][:, 0, :, :], in_=w1_d[ex].rearrange("(c p) n -> p c n", p=128)), w=[("w13", q)])
        S.dma("pool", lambda e: e.dma_start(out=w13[q][:, 1, :, :], in_=w3_d[ex].rearrange("(c p) n -> p c n", p=128)), w=[("w13", q)])
        S.dma("pool", lambda e: e.dma_start(out=w2b[q], in_=w2_d[ex].rearrange("(c p) n -> p c n", p=128)), w=[("w2b", q)])

    load_expert(0)
    for ex in range(NE):
        q = ex % 2
        if ex + 1 < NE:
            load_expert(ex + 1)
        for tb in range(4):
            for fc in range(4):
                p1_, p3_ = k.ps(), k.ps()
                for dc in range(8):
                    S.pe(lambda e, p1_=p1_, dc=dc, q=q: e.matmul(PS[p1_][:, :], lhsT=w13[q][:, 0, dc, fc * 128:(fc + 1) * 128], rhs=h2T[:, dc, tb * 512:(tb + 1) * 512],
                                                                start=(dc == 0), stop=(dc == 7)), r=[("w13", q)] + [("h2T", tb * 4 + u) for u in range(4)], w=[("ps", p1_)])
                for dc in range(8):
                    S.pe(lambda e, p3_=p3_, dc=dc, q=q: e.matmul(PS[p3_][:, :], lhsT=w13[q][:, 1, dc, fc * 128:(fc + 1) * 128], rhs=h2T[:, dc, tb * 512:(tb + 1) * 512],
                                                                start=(dc == 0), stop=(dc == 7)), r=[("w13", q)] + [("h2T", tb * 4 + u) for u in range(4)], w=[("ps", p3_)])
                S.act(lambda e, p1_=p1_: e.activation(out=hs1, in_=PS[p1_][:, :], func=AF.Silu), r=[("ps", p1_)], w=["hs1"])
                S.dve(lambda e, p3_=p3_, fc=fc: e.tensor_tensor(out=aT[:, fc, :], in0=PS[p3_][:, :], in1=hs1, op=ALU.mult), r=[("ps", p3_), "hs1"], w=[("aT", fc)])
            for tt in range(4):
                c = tb * 4 + tt
                for nb_ in range(2):
                    cs_ = slice(nb_ * 512, (nb_ + 1) * 512)
                    po_ = k.ps()
                    for fc in range(4):
                        S.pe(lambda e, po_=po_, fc=fc, q=q: e.matmul(PS[po_][:, :], lhsT=aT[:, fc, tt * 128:(tt + 1) * 128], rhs=w2b[q][:, fc, cs_],
                                                                    start=(fc == 0), stop=(fc == 3)), r=[("aT", fc) for fc in range(4)] + [("w2b", q)], w=[("ps", po_)])
                    S.dve(lambda e, po_=po_, c=c, ex=ex: e.scalar_tensor_tensor(out=m1[:, cs_], in0=PS[po_][:, :], scalar=gates[:, c, ex:ex + 1], in1=gbb[:, 1024 + nb_ * 512:1024 + (nb_ + 1) * 512],
                                                                               op0=ALU.mult, op1=ALU.mult), r=[("ps", po_), ("gates", c), "gbb"], w=[("m1", nb_)])
                    S.pool(lambda e, c=c: e.tensor_tensor(out=x1[:, c, cs_], in0=x1[:, c, cs_], in1=m1[:, cs_], op=ALU.add), r=[("m1", nb_), ("x1", c, nb_)], w=[("x1", c, nb_)])
    for c in range(16):
        S.dma("sp", lambda e, c=c: e.dma_start(out=y_out[c * 128:(c + 1) * 128, :], in_=x1[:, c, :]), r=[("x1", c, 0), ("x1", c, 1)], w=[("y", c)])
    S.emit()
    k.es.close()
    return nc


def _consts():
    c = {}
    f32 = np.float32
    c["ident"] = np.eye(128, dtype=f32)
    kk = np.arange(128)
    c["maskU"] = (kk[:, None] <= kk[None, :]).astype(f32)
    c["maskL"] = (kk[:, None] >= kk[None, :]).astype(f32)
    P = np.zeros((128, 128), f32)
    for n in range(128):
        if (n % 64) < 32:
            P[n, n + 32] = -1.0
        else:
            P[n, n - 32] = 1.0
    c["permT"] = np.ascontiguousarray(P.T)
    sel = np.zeros((128, 32, 128), f32)
    for kq in range(128):
        sel[kq, kq % 32, :] = 1.0
    c["selc"] = sel.reshape(128, 4096)
    return c


def _na_bias(rpb, hf):
    out = np.full((16, 128, 3, 5, 128), -30000.0, np.float32)
    kidx = np.arange(128)
    for cls in range(3):
        i = cls
        kts = [0, 1, 2, 3, 4] if i < 2 else list(range(i - 2, i + 3))
        qr = 2 * i + kidx // 64
        qc = kidx % 64
        gqr, gqc = (qr, qc) if hf == 0 else (63 - qr, 63 - qc)
        rs = np.clip(gqr - 4, 0, 56)
        cs = np.clip(gqc - 8, 0, 48)
        for rel, kt in enumerate(kts):
            kr = 2 * kt + kidx // 64
            kc = kidx % 64
            gkr, gkc = (kr, kc) if hf == 0 else (63 - kr, 63 - kc)
            ok = ((gkr[:, None] >= rs[None, :]) & (gkr[:, None] <= rs[None, :] + 7) &
                  (gkc[:, None] >= cs[None, :]) & (gkc[:, None] <= cs[None, :] + 15))
            dr = np.clip(gkr[:, None] - gqr[None, :] + 7, 0, 14)
            dc = np.clip(gkc[:, None] - gqc[None, :] + 15, 0, 30)
            vals = rpb[:, dr, dc]
            out[:, :, cls, rel, :] = np.where(ok[None], vals, np.float32(-30000.0))
    return np.ascontiguousarray(out.reshape(16, 128, 3 * 5 * 128))


def _rope_tables(hf):
    i = np.arange(SEQ)
    t = i if hf == 0 else (SEQ - 1 - i)
    row = (t // 64).astype(np.float32)
    col = (t % 64).astype(np.float32)
    inv = (10000.0 ** (-np.arange(0, 64, 2, dtype=np.float32) / 64.0)).astype(np.float32)
    ar = row[None, :] * inv[:, None]
    ac = col[None, :] * inv[:, None]
    ang = np.concatenate([ar, ar, ac, ac], axis=0)
    return np.cos(ang).astype(np.float32), np.sin(ang).astype(np.float32)


def prep_core_inputs(inp, b, hf, consts):
    f32 = np.float32
    m = {}
    x = inp["x"][b]
    ctx = inp["ctx"][b]
    if hf == 1:
        x = x[::-1]
        ctx = ctx[::-1]
    m["x_ext"] = np.ascontiguousarray(x, dtype=f32)
    m["ctx_l"] = np.ascontiguousarray(ctx, dtype=f32)
    m["cvT"] = np.ascontiguousarray(np.stack([inp["c"][b], inp["c_ctx"]], axis=1), dtype=f32)
    m["w_mod"] = np.ascontiguousarray(inp["w_mod"][0], dtype=f32)
    bm = inp["b_mod"][0]
    m["b_modT"] = np.ascontiguousarray(bm.reshape(48, 128).T, dtype=f32)
    m["b_mod_g"] = np.ascontiguousarray(np.concatenate([bm[2048:3072], bm[5120:6144]])[None, :], dtype=f32)
    m["n1wT"] = np.ascontiguousarray(inp["norm1_w"][0].reshape(8, 128).T, dtype=f32)
    m["n2wT"] = np.ascontiguousarray(inp["norm2_w"][0].reshape(8, 128).T, dtype=f32)
    L = 0
    w_in = np.array(inp["w_in"][L], dtype=f32)
    if hf == 1:
        w_in[:, C_DT:C_DT + 64] = np.concatenate([w_in[:, C_DT + 32:C_DT + 64], w_in[:, C_DT:C_DT + 32]], axis=1)
    m["w_in"] = w_in
    p1, p2 = ("f", "b") if hf == 0 else ("b", "f")
    m["dtb12"] = np.concatenate([inp["dt_bias_" + p1][L], inp["dt_bias_" + p2][L]])[None, :].astype(f32)
    m["alog12"] = np.concatenate([inp["a_log_" + p1][L], inp["a_log_" + p2][L]])[None, :].astype(f32)
    m["dskip"] = np.ascontiguousarray(inp["d_skip"][L][None, :], dtype=f32)
    cw = inp["conv_w"][L]
    if hf == 1:
        cw = cw[::-1]
    m["conv_wT"] = np.ascontiguousarray(cw.T.reshape(32, 128, 5).transpose(1, 0, 2), dtype=f32)
    m["conv_bT"] = np.ascontiguousarray(inp["conv_b"][L].reshape(32, 128).T, dtype=f32)
    m["ssd_nwT"] = np.ascontiguousarray(inp["ssd_norm_w"][L].reshape(16, 128).T, dtype=f32)
    m["w_br_ssd"] = np.ascontiguousarray(inp["w_br_ssd"][L], dtype=f32)
    m["w_br_na"] = np.ascontiguousarray(inp["w_br_na"][L], dtype=f32)
    m["w_out"] = np.ascontiguousarray(inp["w_out"][L], dtype=f32)
    m["w_rt36"] = np.ascontiguousarray(np.concatenate([inp["w_grp"][L], inp["w_rt"][L]], axis=1), dtype=f32)
    m["b_rt36"] = np.concatenate([inp["b_grp"][L], inp["b_rt"][L]])[None, :].astype(f32)
    m["w1"] = np.ascontiguousarray(inp["w1"][L], dtype=f32)
    m["w3"] = np.ascontiguousarray(inp["w3"][L], dtype=f32)
    m["w2"] = np.ascontiguousarray(inp["w2"][L], dtype=f32)
    m["qkw"] = np.concatenate([np.tile(inp["q_norm_w"][L], 8) * 0.125, np.tile(inp["k_norm_w"][L], 8)])[None, :].astype(f32)
    m["nabias"] = _na_bias(inp["rpb"][L], hf)
    cs, sn = _rope_tables(hf)
    m["cosT"], m["sinT"] = cs, sn
    m.update(consts)
    return m


def kernel(**inputs):
    inp = {k_: np.asarray(v) for k_, v in inputs.items()}
    nc = build("")
    consts = _consts()
    shared = {}
    maps = []
    for c in range(8):
        m = prep_core_inputs(inp, c // 2, c % 2, consts)
        maps.append(m)
    res = run_bass_kernel_spmd(nc, maps, core_ids=list(range(8)))
    out = np.zeros((4, SEQ, D), dtype=np.float32)
    for c in range(8):
        b, hf = c // 2, c % 2
        y = np.asarray(res.results[c]["y"], dtype=np.float32)
        if hf == 0:
            out[b, :OWN] = y
        else:
            out[b, OWN:] = y[::-1]
    return out
```
